# Optimizing a Trainium2 kernel written in Bass

```python
import jax, jax.numpy as jnp
from jax import lax
import numpy as np

D_MODEL = 1024
BATCH = 32
SEQ = 2048
DEPTH = 1

CHUNK = 64
MEM_LEN = 256
MIX_WIDTH = D_MODEL
CONV_WIDTH = MIX_WIDTH // 2
CONV_K = 31
RWKV_WIDTH = MIX_WIDTH - CONV_WIDTH
RWKV_HEAD = 64
RWKV_HEADS = RWKV_WIDTH // RWKV_HEAD
DECAY_LORA = 32
AAA_LORA = 32
GATE_LORA = 96
N_XHEADS = 4
XHEAD_DIM = D_MODEL // N_XHEADS
D_FF = 4 * D_MODEL
RMS_EPS = 1e-6
LN_EPS = 1e-5
GN_EPS = 1e-5 * RWKV_HEAD
RWKV_COLS = 3 * RWKV_WIDTH + DECAY_LORA + AAA_LORA + GATE_LORA
IN_COLS = 2 * CONV_WIDTH + RWKV_COLS

kernel_name = 'hybrid_conformer_rwkv7_stream_block'


def rmsnorm(x, g):
    xf = x.astype(jnp.float32)
    y = xf * lax.rsqrt(jnp.mean(xf * xf, axis=-1, keepdims=True) + RMS_EPS)
    return (y * g.astype(jnp.float32)).astype(x.dtype)


def conv_group(pa, conv_w, conv_b, ln_g, ln_b):
    u = pa[..., :CONV_WIDTH] * jax.nn.sigmoid(pa[..., CONV_WIDTH:])
    u = lax.conv_general_dilated(
        u, conv_w[:, None, :].astype(u.dtype), window_strides=(1,),
        padding=[(CONV_K - 1, 0)],
        dimension_numbers=('NWC', 'WIO', 'NWC'),
        feature_group_count=CONV_WIDTH) + conv_b
    uf = u.astype(jnp.float32)
    mu = jnp.mean(uf, axis=-1, keepdims=True)
    var = jnp.mean(jnp.square(uf - mu), axis=-1, keepdims=True)
    un = (uf - mu) * lax.rsqrt(var + LN_EPS) * ln_g + ln_b
    return jax.nn.silu(un).astype(pa.dtype)


def wkv7_scan(r, w, k, v, kk, a):
    B, S, H, N = r.shape

    def to_chunks(z):
        return z.transpose(1, 0, 2, 3).reshape(S // CHUNK, CHUNK, B, H, N)

    def step(state, inp):
        r_t, w_t, k_t, v_t, kk_t, a_t = inp
        sa = jnp.einsum('bhvk,bhk->bhv', state, -kk_t)
        state = (state * w_t[:, :, None, :]
                 + sa[..., None] * (kk_t * a_t)[:, :, None, :]
                 + v_t[..., None] * k_t[:, :, None, :])
        return state, jnp.einsum('bhvk,bhk->bhv', state, r_t)

    def chunk_step(state, inp):
        return lax.scan(step, state, inp)

    s0 = jnp.zeros((B, H, N, N), jnp.float32)
    _, y = lax.scan(chunk_step, s0, tuple(to_chunks(z) for z in (r, w, k, v, kk, a)))
    return y.reshape(S, B, H, N).transpose(1, 0, 2, 3)


def rwkv7_group(pb, mu_b, w0, w_decay2, a0, a_lora2, g_lora2, k_k, k_a, r_k, lnx_g, lnx_b):
    B, S, _ = pb.shape
    H, N, C = RWKV_HEADS, RWKV_HEAD, RWKV_WIDTH
    prev = jnp.pad(pb, ((0, 0), (1, 0), (0, 0)))[:, :-1]
    z = (pb + mu_b * (prev - pb)).astype(jnp.float32)
    r = z[..., :C]
    k = z[..., C:2 * C]
    v = z[..., 2 * C:3 * C]
    o = 3 * C
    zw = z[..., o:o + DECAY_LORA]
    za = z[..., o + DECAY_LORA:o + DECAY_LORA + AAA_LORA]
    zg = z[..., o + DECAY_LORA + AAA_LORA:]
    w_log = -jax.nn.softplus(-(w0 + jnp.tanh(zw) @ w_decay2)) - 0.5
    decay = jnp.exp(-jnp.exp(w_log))
    a = jax.nn.sigmoid(a0 + za @ a_lora2)
    g = jax.nn.sigmoid(zg) @ g_lora2

    def heads(t):
        return t.reshape(B, S, H, N)

    kk = heads(k * k_k)
    kk = kk / jnp.maximum(jnp.sqrt(jnp.sum(kk * kk, axis=-1, keepdims=True)), 1e-12)
    k = k * (1.0 + (a - 1.0) * k_a)
    rh, kh, vh = heads(r), heads(k), heads(v)
    y = wkv7_scan(rh, heads(decay), kh, vh, kk, heads(a))
    mu = jnp.mean(y, axis=-1, keepdims=True)
    var = jnp.mean(jnp.square(y - mu), axis=-1, keepdims=True)
    y = ((y - mu) * lax.rsqrt(var + GN_EPS)).reshape(B, S, C) * lnx_g + lnx_b
    bonus = (jnp.sum(rh * kh * r_k.reshape(H, N), axis=-1, keepdims=True) * vh).reshape(B, S, C)
    return ((y + bonus) * g).astype(pb.dtype)


def cross_attention(h, mem_n, wq, wk, wv, wo):
    B, S, _ = h.shape
    M = mem_n.shape[1]
    q = (h @ wq).reshape(B, S, N_XHEADS, XHEAD_DIM)
    k = (mem_n @ wk).reshape(B, M, N_XHEADS, XHEAD_DIM)
    v = (mem_n @ wv).reshape(B, M, N_XHEADS, XHEAD_DIM)
    s = jnp.einsum('bshd,bmhd->bhsm', q, k).astype(jnp.float32) * (XHEAD_DIM ** -0.5)
    p = jax.nn.softmax(s, axis=-1).astype(h.dtype)
    out = jnp.einsum('bhsm,bmhd->bshd', p, v).reshape(B, S, D_MODEL)
    return out @ wo


def setup_inputs(seed: int = 0) -> dict:
    key = jax.random.key(seed)
    ks = jax.random.split(key, 32)
    L, D = DEPTH, D_MODEL
    f32 = jnp.float32

    def nrm(k, shape, scale):
        return jax.random.normal(k, shape, f32) * scale

    def gain(k, shape):
        return 1.0 + 0.02 * jax.random.normal(k, shape, f32)

    return {
        'x': jax.random.normal(ks[0], (BATCH, SEQ, D), f32),
        'mem': jax.random.normal(ks[1], (BATCH, MEM_LEN, D), f32),
        'g_mix': gain(ks[2], (L, D)),
        'w_in': nrm(ks[3], (L, D, IN_COLS), D ** -0.5),
        'conv_w': nrm(ks[4], (L, CONV_K, CONV_WIDTH), CONV_K ** -0.5),
        'conv_b': nrm(ks[5], (L, CONV_WIDTH), 0.02),
        'conv_ln_g': gain(ks[6], (L, CONV_WIDTH)),
        'conv_ln_b': nrm(ks[7], (L, CONV_WIDTH), 0.02),
        'mu_b': jax.random.uniform(ks[8], (L, RWKV_COLS), f32, 0.0, 1.0),
        'w0': jax.random.uniform(ks[9], (L, RWKV_WIDTH), f32, -6.0, 1.0),
        'w_decay2': nrm(ks[10], (L, DECAY_LORA, RWKV_WIDTH), 0.1 * DECAY_LORA ** -0.5),
        'a0': nrm(ks[11], (L, RWKV_WIDTH), 0.1),
        'a_lora2': nrm(ks[12], (L, AAA_LORA, RWKV_WIDTH), 0.5 * AAA_LORA ** -0.5),
        'g_lora2': nrm(ks[13], (L, GATE_LORA, RWKV_WIDTH), GATE_LORA ** -0.5),
        'k_k': 0.85 + nrm(ks[14], (L, RWKV_WIDTH), 0.02),
        'k_a': 1.0 + nrm(ks[15], (L, RWKV_WIDTH), 0.02),
        'r_k': nrm(ks[16], (L, RWKV_WIDTH), 0.1),
        'lnx_g': gain(ks[17], (L, RWKV_WIDTH)),
        'lnx_b': nrm(ks[18], (L, RWKV_WIDTH), 0.02),
        'w_out': nrm(ks[19], (L, MIX_WIDTH, D), MIX_WIDTH ** -0.5),
        'g_cross': gain(ks[20], (L, D)),
        'g_mem': gain(ks[21], (L, D)),
        'wq': nrm(ks[22], (L, D, D), D ** -0.5),
        'wk': nrm(ks[23], (L, D, D), D ** -0.5),
        'wv': nrm(ks[24], (L, D, D), D ** -0.5),
        'wo': nrm(ks[25], (L, D, D), D ** -0.5),
        'g_ffn': gain(ks[26], (L, D)),
        'w_ff1': nrm(ks[27], (L, D, D_FF), D ** -0.5),
        'w_ff2': nrm(ks[28], (L, D_FF, D), D_FF ** -0.5),
        'g_final': gain(ks[29], (D,)),
    }


def reference(x, mem, g_mix, w_in, conv_w, conv_b, conv_ln_g, conv_ln_b, mu_b, w0,
              w_decay2, a0, a_lora2, g_lora2, k_k, k_a, r_k, lnx_g, lnx_b, w_out,
              g_cross, g_mem, wq, wk, wv, wo, g_ffn, w_ff1, w_ff2, g_final):
    for l in range(DEPTH):
        h = rmsnorm(x, g_mix[l])
        p = h @ w_in[l]
        y_conv = conv_group(p[..., :2 * CONV_WIDTH], conv_w[l], conv_b[l],
                            conv_ln_g[l], conv_ln_b[l])
        y_rwkv = rwkv7_group(p[..., 2 * CONV_WIDTH:], mu_b[l], w0[l], w_decay2[l], a0[l],
                             a_lora2[l], g_lora2[l], k_k[l], k_a[l], r_k[l],
                             lnx_g[l], lnx_b[l])
        x = x + jnp.concatenate([y_conv, y_rwkv], axis=-1) @ w_out[l]
        x = x + cross_attention(rmsnorm(x, g_cross[l]), rmsnorm(mem, g_mem[l]),
                                wq[l], wk[l], wv[l], wo[l])
        hf = rmsnorm(x, g_ffn[l])
        x = x + jnp.square(jax.nn.relu(hf @ w_ff1[l])) @ w_ff2[l]
    return rmsnorm(x, g_final)
```

```python
import contextlib
import numpy as np
import concourse.bass as bass
import concourse.mybir as mybir
from concourse.bass_utils import run_bass_kernel_spmd

F32 = mybir.dt.float32
BF16 = mybir.dt.bfloat16
ALU = mybir.AluOpType
AF = mybir.ActivationFunctionType

D = 1024
SEQ = 2048
BATCH = 32
NCORES = 8
TT = 512
MEM = 256
CW = 512
RW = 512
CONV_K = 31
HIST = CONV_K - 1
DFF = 4096
NBLK = 36
NHOST = 32
BLK = 4096
NSLOT = 4
DEC = 0.6065306597126334

ENGS = ("pe", "act", "dve", "pool", "sp")


class Buf:
    __slots__ = ("name", "w", "r", "excl")

    def __init__(self, name="", excl=False):
        self.name = name
        self.w = None
        self.r = []
        self.excl = excl


class Prog:
    def __init__(self, nc, stack):
        self.nc = nc
        self.stack = stack
        self.ops = {e: [] for e in ENGS}
        self.cnt = {e: 0 for e in ENGS}
        self.waited = {e: {} for e in ENGS}
        self.sems = {}
        self.semval = {}
        for e in ENGS:
            self.sems[e] = stack.enter_context(nc.semaphore("s_" + e))
        self.n_dma_sems = 0
        self.hazard = 10 ** 9

    def dma_sem(self, name=None):
        key = "dma%d" % self.n_dma_sems
        self.n_dma_sems += 1
        self.sems[key] = self.stack.enter_context(self.nc.semaphore(name or key))
        self.semval[key] = 0
        return key

    def _deps(self, eng, reads, writes):
        need = {}

        def add(dep):
            if dep is None:
                return
            k, v = dep
            if need.get(k, -1) < v:
                need[k] = v
        for b in reads:
            add(b.w)
        for b in writes:
            add(b.w)
            for d in b.r:
                add(d)
        out = []
        wd = self.waited[eng]
        for k, v in need.items():
            if k == eng:
                if eng in ("pe", "sp") or v <= self.cnt[eng] - self.hazard:
                    continue
            if wd.get(k, -1) >= v:
                continue
            wd[k] = v
            out.append((k, v))
        return out

    def _record(self, me, reads, writes):
        for b in reads:
            if len(b.r) > 24:
                best = {}
                for k, v in b.r:
                    if best.get(k, -1) < v:
                        best[k] = v
                b.r = list(best.items())
            b.r.append(me)
        for b in writes:
            b.w = me
            b.r = []

    @staticmethod
    def _split(reads, writes):
        if any(b.excl for b in reads):
            writes = list(writes) + [b for b in reads if b.excl]
            reads = [b for b in reads if not b.excl]
        return reads, writes

    def op(self, eng, fn, reads=(), writes=()):
        reads, writes = self._split(reads, writes)
        waits = self._deps(eng, reads, writes)
        self.cnt[eng] += 1
        me = (eng, self.cnt[eng])
        self.ops[eng].append((fn, waits, (eng, 1)))
        self._record(me, reads, writes)
        return me

    def dma(self, eng, fn, semkey, reads=(), writes=()):
        reads, writes = self._split(reads, writes)
        waits = self._deps(eng, reads, writes)
        self.semval[semkey] += 16
        me = (semkey, self.semval[semkey])
        self.ops[eng].append((fn, waits, (semkey, 16)))
        self._record(me, reads, writes)
        return me

    def wait_all(self, eng, bufs):
        waits = self._deps(eng, [], bufs)
        self.ops[eng].append((None, waits, None))

    def emit(self):
        nc = self.nc
        handles = {"pe": "tensor", "act": "scalar", "dve": "vector", "pool": "gpsimd", "sp": "sync"}
        with nc.Block() as block:
            for e in ENGS:
                ops = self.ops[e]
                if not ops:
                    continue

                def body(engh, ops=ops):
                    for fn, waits, inc in ops:
                        for k, v in waits:
                            engh.wait_ge(self.sems[k], v)
                        if fn is not None:
                            ins = fn(engh)
                            if inc is not None:
                                ins.then_inc(self.sems[inc[0]], inc[1])
                getattr(block, handles[e])(body)


CV = {}
_off = 0
for _n, _w in [("g_mix", 8), ("g_cross", 8), ("g_mem", 8), ("g_ffn", 8), ("g_final", 8),
               ("conv_b", 4), ("ln_g", 4), ("ln_b", 4), ("conv_w", 124), ("mu", 15),
               ("w0", 4), ("a0", 4), ("k_k", 4), ("k_a", 4), ("r_k", 4), ("lnx_g", 4), ("lnx_b", 4)]:
    CV[_n] = _off
    _off += _w
NCV = _off


def _cm(v, nch):
    return np.ascontiguousarray(np.asarray(v, np.float32).reshape(nch, 128).T)


def pack_cvec(inp):
    cv = np.zeros((128, NCV), np.float32)

    def put(name, arr):
        cv[:, CV[name]:CV[name] + arr.shape[1]] = arr
    put("g_mix", _cm(inp["g_mix"][0], 8))
    put("g_cross", _cm(inp["g_cross"][0], 8))
    put("g_mem", _cm(inp["g_mem"][0], 8))
    put("g_ffn", _cm(inp["g_ffn"][0], 8))
    put("g_final", _cm(inp["g_final"], 8))
    put("conv_b", _cm(inp["conv_b"][0], 4))
    put("ln_g", _cm(inp["conv_ln_g"][0], 4))
    put("ln_b", _cm(inp["conv_ln_b"][0], 4))
    cw = np.asarray(inp["conv_w"][0], np.float32)
    cwp = cw.reshape(CONV_K, 4, 128).transpose(2, 0, 1)
    put("conv_w", np.ascontiguousarray(cwp.reshape(128, CONV_K * 4)))
    mu = np.asarray(inp["mu_b"][0], np.float32)
    m = np.zeros((128, 15), np.float32)
    m[:, 0:4] = _cm(mu[0:512], 4)
    m[:, 4:8] = _cm(mu[512:1024], 4)
    m[:, 8:12] = _cm(mu[1024:1536], 4)
    m[0:32, 12] = mu[1536:1568]
    m[0:32, 13] = mu[1568:1600]
    m[0:96, 14] = mu[1600:1696]
    put("mu", m)
    for nm in ("w0", "a0", "k_k", "k_a", "r_k", "lnx_g", "lnx_b"):
        put(nm, _cm(inp[nm][0], 4))
    return cv


def pack_blocks(inp):
    out = np.zeros((NHOST, 128, BLK), np.float32)

    def typeA(W, cols):
        blk = np.zeros((128, 4, 8, 128), np.float32)
        Wr = W.reshape(8, 128, -1)
        for cb, (st, wd) in enumerate(cols):
            blk[:, cb, :, :wd] = Wr[:, :, st:st + wd].transpose(1, 0, 2)
        return blk.reshape(128, BLK)

    w_in = np.asarray(inp["w_in"][0], np.float32)
    out[0] = typeA(w_in, [(0, 128), (512, 128), (128, 128), (640, 128)])
    out[1] = typeA(w_in, [(256, 128), (768, 128), (384, 128), (896, 128)])
    for j, base in enumerate((1024, 1536, 2048)):
        out[2 + j] = typeA(w_in, [(base + 128 * i, 128) for i in range(4)])
    out[5] = typeA(w_in, [(2560, 32), (2592, 32), (2624, 96)])
    for j, nm in enumerate(("w_out", "wq", "wo")):
        W = np.asarray(inp[nm][0], np.float32)
        for h in range(2):
            out[6 + 2 * j + h] = typeA(W, [(512 * h + 128 * i, 128) for i in range(4)])
    W = np.asarray(inp["w_ff1"][0], np.float32)
    for b in range(8):
        out[12 + b] = typeA(W, [(512 * b + 128 * i, 128) for i in range(4)])
    W = np.asarray(inp["w_ff2"][0], np.float32).reshape(32, 128, 1024)
    for b in range(8):
        out[20 + b] = W[:, :, 128 * b:128 * (b + 1)].transpose(1, 0, 2).reshape(128, BLK)
    W = np.asarray(inp["wk"][0], np.float32)
    for h in range(2):
        out[28 + h] = typeA(W, [(512 * h + 128 * i, 128) for i in range(4)])
    W = np.asarray(inp["wv"][0], np.float32).reshape(8, 128, 1024)
    for h in range(2):
        out[30 + h] = W[:, :, 512 * h:512 * (h + 1)].transpose(1, 0, 2).reshape(128, BLK)
    return out


def pack_lora(inp):
    lw = np.zeros((128, 3, 512), np.float32)
    lw[0:32, 0] = inp["w_decay2"][0]
    lw[0:32, 1] = inp["a_lora2"][0]
    lw[0:96, 2] = inp["g_lora2"][0]
    return lw


def build(nseq, seqlen, dbg=False):
    NT = seqlen // TT
    nc = bass.Bass("TRN2", target_bir_lowering=False)
    xT = nc.dram_tensor("xT", [nseq, D, seqlen], F32, kind="ExternalInput").ap()
    memT = nc.dram_tensor("memT", [nseq, D, MEM], F32, kind="ExternalInput").ap()
    wblk = nc.dram_tensor("wblk", [NHOST, 128, BLK], F32, kind="ExternalInput").ap()
    cvd = nc.dram_tensor("cvec", [128, NCV], F32, kind="ExternalInput").ap()
    lwd = nc.dram_tensor("lora", [128, 3, 512], F32, kind="ExternalInput").ap()
    oT = nc.dram_tensor("oT", [nseq, D, seqlen], F32, kind="ExternalOutput").ap()
    scr = nc.dram_tensor("wscr", [NBLK, 128, BLK], BF16).ap()
    dbg_out = {}

    with contextlib.ExitStack() as st:
        P = Prog(nc, st)

        def sb(name, shape, dt):
            return st.enter_context(nc.sbuf_tensor(name, shape, dt))

        cv = sb("cv", [128, NCV], F32)
        cvx = sb("cvx", [128, 32], F32)
        ident = sb("ident", [128, 128], BF16)
        ident4 = sb("ident4", [128, 4, 128], BF16)
        onesb = sb("onesb", [128, 128], BF16)
        blk1 = sb("blk1", [128, 128], BF16)
        blk64 = sb("blk64", [128, 128], BF16)
        o512 = sb("o512", [128, 128], BF16)
        mSU = sb("mSU", [128, 4, 128], BF16)
        mIU = sb("mIU", [128, 4, 128], BF16)
        mSL = sb("mSL", [128, 4, 128], BF16)
        rmask = sb("rmask", [128, TT], F32)
        lorab = sb("lorab", [128, 3, 512], BF16)
        S32 = sb("S32", [128, 4, 64], F32)
        Sbf = sb("Sbf", [128, 4, 64], BF16)
        tmpS = sb("tmpS", [128, 4, 64], F32)
        carry = sb("carry", [128, 16], F32)
        KT = sb("KT", [128, 8, MEM], BF16)
        Vt = sb("Vt", [128, 2, D], BF16)
        wslot = [sb("wslot%d" % i, [128, BLK], BF16) for i in range(NSLOT)]
        xres = [sb("xres%d" % i, [128, 8, TT], F32) for i in range(2)]
        actA = sb("actA", [128, 8, TT], BF16)
        actB = sb("actB", [128, 8, TT], BF16)
        arena = sb("arena", [128, 32 * 512], BF16)
        lob = sb("lob", [128, 3, TT], BF16)
        Bbuf = [sb("Bbuf%d" % i, [128, TT + 1], F32) for i in range(2)]
        ubuf = sb("ubuf", [128, 4, HIST + TT], BF16)
        cln = sb("cln", [128, 2, TT], F32)
        cacc = sb("cacc", [128, 4, TT], F32)
        opa = sb("opa", [128, 2, 2, TT], BF16)
        opr = sb("opr", [128, 2, 2, TT], BF16)
        opb = sb("opb", [128, 2, TT], BF16)
        opk = sb("opk", [128, 2, TT], BF16)
        tok = sb("tok", [128, 3, 2, 4, 128], BF16)
        bonus = sb("bonus", [128, 2, TT], F32)
        ybuf = sb("ybuf", [128, 2, TT], F32)
        wcb = sb("wcb", [128, 2, 4], F32)
        mats = {nm: sb("m_" + nm, [128, 4, 128], BF16)
                for nm in ("X0", "X1", "XT0", "XT1", "P0", "P1", "Aak", "Arb", "Ark")}
        RHSb = sb("RHSb", [128, 2, 2, 64], BF16)
        Ub = sb("Ub", [128, 2, 2, 64], BF16)
        PT = sb("PT", [128, 2, TT], BF16)
        rsb = sb("rsb", [128, TT], F32)
        sqr = [sb("sqr%d" % i, [128, TT], BF16) for i in range(2)]
        psum = [st.enter_context(nc.psum_tensor("ps%d" % i, [128, 512], F32)) for i in range(8)]

        B_const = Buf("const")
        B_S32 = [Buf("S32_%d" % i) for i in range(4)]
        B_Sbf = [Buf("Sbf_%d" % i) for i in range(4)]
        B_tmpS = [Buf("tmpS%d" % i) for i in range(4)]
        B_carry = [Buf("carry%d" % i) for i in range(16)]
        B_KT = Buf("KT")
        B_V = Buf("V")
        B_wslot = [Buf("wslot%d" % i) for i in range(NSLOT)]
        B_x = [[Buf("x%d_%d" % (i, c)) for c in range(8)] for i in range(2)]
        B_A = [Buf("actA%d" % c) for c in range(8)]
        B_Bc = [Buf("actB%d" % c) for c in range(8)]
        B_ar = [Buf("ar%d" % i) for i in range(32)]
        B_lob = [Buf("lob%d" % i) for i in range(3)]
        B_Bbuf = [Buf("Bbuf0"), Buf("Bbuf1")]
        B_cln = [Buf("cln0"), Buf("cln1")]
        B_u = [Buf("u%d" % c) for c in range(4)]
        B_cacc = [Buf("cacc%d" % c) for c in range(4)]
        B_opa = [Buf("opa%d" % lp) for lp in range(2)]
        B_opr = [Buf("opr%d" % lp) for lp in range(2)]
        B_opb = [Buf("opb%d" % lp) for lp in range(2)]
        B_opk = [Buf("opk%d" % lp) for lp in range(2)]
        B_tok = [[Buf("tok%d_%d" % (k, lp)) for lp in range(2)] for k in range(3)]
        B_bonus = [Buf("bonus%d" % lp) for lp in range(2)]
        B_y = [Buf("y%d" % lp) for lp in range(2)]
        B_wc = [Buf("wc%d" % lp) for lp in range(2)]
        B_m = {nm: Buf("m_" + nm) for nm in mats}
        B_RHS = [Buf("RHS%d" % lp) for lp in range(2)]
        B_U = [Buf("U%d" % lp) for lp in range(2)]
        B_PT = [Buf("PT%d" % i) for i in range(2)]
        B_rs = Buf("rs")
        B_sqr = [Buf("sqr%d" % i) for i in range(2)]
        B_ps = [Buf("psb%d" % i, excl=True) for i in range(8)]
        B_scr = [Buf("scr%d" % i) for i in range(NBLK)]

        def zrkv(j):
            return arena[:, j * 512:(j + 1) * 512], [B_ar[j]]

        def Tt(i):
            o = (12 + 2 * i) * 512
            return arena[:, o:o + 1024].bitcast(F32), [B_ar[12 + 2 * i], B_ar[13 + 2 * i]]

        def fT(j):
            return arena[:, j * 512:(j + 1) * 512], [B_ar[j]]

        ps_pos = [0]

        def ps_alloc(ncols):
            p = ps_pos[0]
            ps_pos[0] = (p + 1) % 8
            return psum[p][:, 0:ncols], [B_ps[p]]

        s_const = P.dma_sem("c")
        s_w = [P.dma_sem("w%d" % i) for i in range(NSLOT)]
        s_x = [P.dma_sem("x%d" % i) for i in range(2)]
        s_o = [P.dma_sem("o%d" % i) for i in range(2)]
        s_sw = [P.dma_sem("sw%d" % i) for i in range(NSLOT)]
        s_mem = P.dma_sem("mem")

        def ACT(out, in_, func, reads, writes, bias=None, scale=None):
            kw = {}
            if bias is not None:
                kw["bias"] = bias
            if scale is not None:
                kw["scale"] = scale
            P.op("act", lambda e: e.activation(out=out, in_=in_, func=func, **kw), reads, writes)

        def TTop(eng, out, in0, in1, op, reads, writes):
            P.op(eng, lambda e: e.tensor_tensor(out=out, in0=in0, in1=in1, op=op), reads, writes)

        def TS(eng, out, in0, s1, s2, op0, op1, reads, writes):
            if s2 is None:
                P.op(eng, lambda e: e.tensor_scalar(out=out, in0=in0, scalar1=s1, scalar2=None, op0=op0), reads, writes)
            else:
                P.op(eng, lambda e: e.tensor_scalar(out=out, in0=in0, scalar1=s1, scalar2=s2, op0=op0, op1=op1), reads, writes)

        def STT(eng, out, in0, scalar, in1, op0, op1, reads, writes):
            P.op(eng, lambda e: e.scalar_tensor_tensor(out=out, in0=in0, scalar=scalar, in1=in1, op0=op0, op1=op1), reads, writes)

        def CP(eng, out, in_, reads, writes):
            if eng == "act":
                P.op("act", lambda e: e.copy(out=out, in_=in_), reads, writes)
            else:
                P.op(eng, lambda e: e.tensor_copy(out=out, in_=in_), reads, writes)

        def MM(out, lhsT, rhs, start, stop, reads, writes, tp=None):
            if tp is None:
                P.op("pe", lambda e: e.matmul(out, lhsT=lhsT, rhs=rhs, start=start, stop=stop), reads, writes)
            else:
                P.op("pe", lambda e: e.matmul(out, lhsT=lhsT, rhs=rhs, start=start, stop=stop, tile_position=tp), reads, writes)

        def MEMSET(eng, ap, val, writes):
            P.op(eng, lambda e: e.memset(ap, val), (), writes)

        def AFSEL(out, in_, pattern, cmp, base, cm, reads, writes):
            P.op("pool", lambda e: e.affine_select(out=out, in_=in_, pattern=pattern, compare_op=cmp, fill=0.0,
                                                   base=base, channel_multiplier=cm), reads, writes)

        def cvc(name, j=0, rows=slice(0, 128)):
            o = CV[name] + j
            return cv[rows, o:o + 1]

        P.dma("sp", lambda e: e.dma_start(out=cv[:], in_=cvd), s_const, writes=[B_const])
        lw32, lwB = Tt(0)
        lw32b, lwBb = Tt(1)
        lw32c, lwBc = Tt(2)
        for j, (tv, tb) in enumerate(((lw32, lwB), (lw32b, lwBb), (lw32c, lwBc))):
            P.dma("sp", lambda e, tv=tv, j=j: e.dma_start(out=tv, in_=lwd[:, j, :]), s_const, writes=tb)
        for b_ in lwB + lwBb + lwBc + [B_const]:
            b_.w = (s_const, P.semval[s_const])
        for j, (tv, tb) in enumerate(((lw32, lwB), (lw32b, lwBb), (lw32c, lwBc))):
            CP("dve", lorab[:, j, :], tv, tb, [B_const])
        TS("dve", cvx[:, 0:15], cv[:, CV["mu"]:CV["mu"] + 15], -1.0, 1.0, ALU.mult, ALU.add, [B_const], [B_const])
        TS("dve", cvx[:, 15:19], cv[:, CV["k_a"]:CV["k_a"] + 4], -1.0, 1.0, ALU.mult, ALU.add, [B_const], [B_const])
        MEMSET("pool", cvx[:, 19:20], 1e-6, [B_const])
        MEMSET("pool", cvx[:, 20:21], 1e-5, [B_const])
        MEMSET("pool", cvx[:, 21:22], 64e-5, [B_const])
        EPS_RMS, EPS_LN, EPS_GN = cvx[:, 19:20], cvx[:, 20:21], cvx[:, 21:22]
        t3, t3b = Tt(3)
        m32 = t3[:, 0:128]
        MEMSET("pool", m32, 1.0, t3b)
        AFSEL(m32, m32, [[-1, 128]], ALU.is_equal, 0, 1, t3b, t3b)
        CP("pool", ident[:], m32, t3b, [B_const])
        for h in range(4):
            CP("pool", ident4[:, h, :], m32, t3b, [B_const])
        for msk, cmp in ((mSU, ALU.is_gt), (mIU, ALU.is_ge)):
            MEMSET("pool", m32, 1.0, t3b)
            AFSEL(m32, m32, [[1, 128]], cmp, 0, -1, t3b, t3b)
            for h in range(4):
                CP("pool", msk[:, h, :], m32, t3b, [B_const])
        MEMSET("pool", m32, 1.0, t3b)
        AFSEL(m32, m32, [[-1, 128]], ALU.is_gt, 0, 1, t3b, t3b)
        for h in range(4):
            CP("pool", mSL[:, h, :], m32, t3b, [B_const])
        MEMSET("pool", opa[:], 0.0, B_opa)
        MEMSET("pool", opr[:], 0.0, B_opr)
        MEMSET("pool", onesb[:], 1.0, [B_const])
        MEMSET("pool", o512[:], 1.0 / 512.0, [B_const])
        MEMSET("pool", blk1[:], 0.0, [B_const])
        MEMSET("pool", blk1[0:64, 0:64], 1.0, [B_const])
        MEMSET("pool", blk1[64:128, 64:128], 1.0, [B_const])
        MEMSET("pool", blk64[:], 0.0, [B_const])
        MEMSET("pool", blk64[0:64, 0:64], 1.0 / 64.0, [B_const])
        MEMSET("pool", blk64[64:128, 64:128], 1.0 / 64.0, [B_const])
        MEMSET("pool", rmask[:], 1.0, [B_const])
        for c in range(TT // 128):
            MEMSET("pool", rmask[:, c * 128:c * 128 + 1], 0.0, [B_const])

        cast_engs = ("act", "dve", "pool")
        for b in range(NHOST):
            xi = b % 2
            si = b % NSLOT
            stage = xres[xi][:].rearrange("p a b -> p (a b)")
            P.dma("sp", lambda e, stage=stage, b=b: e.dma_start(out=stage, in_=wblk[b]), s_x[xi], writes=B_x[xi])
            CP(cast_engs[b % 3], wslot[si][:], stage, B_x[xi], [B_wslot[si]])
            P.dma("sp", lambda e, si=si, b=b: e.dma_start(out=scr[b], in_=wslot[si][:]), s_sw[si],
                  reads=[B_wslot[si]], writes=[B_scr[b]])

        cwo_ = CV["conv_w"]
        for c in range(4):
            b = NHOST + c
            si = b % NSLOT
            MEMSET("pool", wslot[si][:, CONV_K * 128:BLK], 0.0, [B_wslot[si]])
            for k in range(CONV_K):
                TS("dve" if k % 2 else "pool", wslot[si][:, k * 128:(k + 1) * 128], ident[:],
                   cv[:, cwo_ + k * 4 + c:cwo_ + k * 4 + c + 1], None, ALU.mult, None, [B_const], [B_wslot[si]])
            P.dma("sp", lambda e, si=si, b=b: e.dma_start(out=scr[b], in_=wslot[si][:]), s_sw[si],
                  reads=[B_wslot[si]], writes=[B_scr[b]])

        wseq = []
        for s in range(nseq):
            for ti in range(NT):
                if ti == 0:
                    wseq += [28, 29, 30, 31]
                wseq += [0, 1, 2, 3, 4, 5, 32, 33, 34, 35] + list(range(6, 28))
        wstate = {"issued": 0, "used": 0}

        def w_issue_upto(n):
            while wstate["issued"] < min(n, len(wseq)):
                i = wstate["issued"]
                si = i % NSLOT
                blk = wseq[i]
                P.dma("sp", lambda e, si=si, blk=blk: e.dma_start(out=wslot[si][:], in_=scr[blk]), s_w[si],
                      reads=[B_scr[blk]], writes=[B_wslot[si]])
                wstate["issued"] += 1

        def w_next(expect):
            i = wstate["used"]
            assert wseq[i] == expect, (i, wseq[i], expect)
            w_issue_upto(i + NSLOT)
            wstate["used"] += 1
            si = i % NSLOT
            return wslot[si], B_wslot[si]

        def wA(ws, cb, kc, m=128):
            o = (cb * 8 + kc) * 128
            return ws[:, o:o + m]

        rr = {"sq": 0, "eng": 0}

        def rmsnorm(src, src_b, gname, dst, dst_b, n, out_f32_inplace=False):
            ps, psb = ps_alloc(512)
            for c in range(8):
                q = rr["sq"] % 2
                rr["sq"] += 1
                ACT(sqr[q][:, 0:n], src(c), AF.Square, [src_b[c]], [B_sqr[q]])
                MM(ps[:, 0:n], onesb[:], sqr[q][:, 0:n], c == 0, c == 7, [B_sqr[q], B_const], psb)
            ACT(rsb[:, 0:n], ps[:, 0:n], AF.Sqrt, psb, [B_rs], bias=EPS_RMS, scale=1.0 / D)
            P.op("dve", lambda e: e.reciprocal(out=rsb[:, 0:n], in_=rsb[:, 0:n]), [B_rs], [B_rs])
            for c in range(8):
                STT("dve", dst(c), src(c), cvc(gname, c), rsb[:, 0:n], ALU.mult, ALU.mult,
                    [src_b[c], B_rs, B_const], [dst_b[c]])

        def proj_typeA(blocks, rhs, rhs_b, ncb_total, n, evac):
            cb_glob = 0
            for blk in blocks:
                ws, wsb = w_next(blk)
                for cb in range(4):
                    if cb_glob >= ncb_total:
                        break
                    ps, psb = ps_alloc(512)
                    for kc in range(8):
                        MM(ps[:, 0:n], wA(ws, cb, kc), rhs(kc), kc == 0, kc == 7, [wsb, rhs_b[kc]], psb)
                    evac(cb_glob, ps, psb)
                    cb_glob += 1

        for s in range(nseq):
            MEMSET("pool", S32[:], 0.0, B_S32)
            MEMSET("pool", Sbf[:], 0.0, B_Sbf)
            MEMSET("pool", carry[:], 0.0, B_carry)
            for c in range(4):
                MEMSET("pool", ubuf[:, c, 0:HIST], 0.0, [B_u[c]])
            memv = cacc[:].rearrange("p a b -> p (a b)")[:, 0:8 * MEM].rearrange("p (a b) -> p a b", b=MEM)
            P.dma("sp", lambda e, s=s: e.dma_start(out=memv, in_=memT[s].rearrange("(c p) m -> p c m", p=128)),
                  s_mem, writes=B_cacc)
            rmsnorm(lambda c: memv[:, c, :], [B_cacc[c // 2] for c in range(8)], "g_mem",
                    lambda c: actA[:, c, 0:MEM], B_A, MEM)

            def evK(cb, ps, psb):
                CP("act" if cb % 2 else "dve", KT[:, cb, :], ps[:, 0:MEM], psb, [B_KT])
            proj_typeA([28, 29], lambda kc: actA[:, kc, 0:MEM], B_A, 8, MEM, evK)
            for h in range(2):
                ws, wsb = w_next(30 + h)
                for mc in range(2):
                    ps, psb = ps_alloc(512)
                    for kc in range(8):
                        MM(ps[:, :], actA[:, kc, mc * 128:(mc + 1) * 128], ws[:, kc * 512:(kc + 1) * 512],
                           kc == 0, kc == 7, [wsb, B_A[kc]], psb)
                    CP("act" if mc else "dve", Vt[:, mc, h * 512:(h + 1) * 512], ps[:, :], psb, [B_V])

            for ti in range(NT):
                gi = s * NT + ti
                xi = gi % 2
                xr = xres[xi]
                xb = B_x[xi]
                t0 = ti * TT
                P.dma("sp", lambda e, s=s, t0=t0, xr=xr: e.dma_start(
                    out=xr[:], in_=xT[s].rearrange("(c p) t -> p c t", p=128)[:, :, t0:t0 + TT]), s_x[xi], writes=xb)
                rmsnorm(lambda c: xr[:, c, :], xb, "g_mix", lambda c: actA[:, c, :], B_A, TT)

                pend = {}

                def ev_conv(cb, ps, psb):
                    ch, isgate = divmod(cb, 2)
                    if not isgate:
                        pend["val"] = (ps, psb)
                        return
                    vps, vpsb = pend.pop("val")
                    tsg, tsgb = Tt(ch % 2)
                    ACT(tsg, ps[:, :], AF.Sigmoid, psb, tsgb)
                    TTop("dve", ubuf[:, ch, HIST:HIST + TT], vps[:, :], tsg, ALU.mult, vpsb + tsgb, [B_u[ch]])
                proj_typeA([0, 1], lambda kc: actA[:, kc, :], B_A, 8, TT, ev_conv)

                def ev_shift(j, m, ps, psb, dst, dst_b):
                    q = j % 2
                    Bq, Bqb = Bbuf[q], B_Bbuf[q]
                    ACT(Bq[0:m, 1:TT + 1], ps[0:m, :], AF.Identity, psb + [B_const], [Bqb], scale=cvc("mu", j, slice(0, m)))
                    CP("dve", Bq[0:m, 0:1], carry[0:m, j:j + 1], [B_carry[j]], [Bqb])
                    STT("dve", dst, ps[0:m, :], cvx[0:m, j:j + 1], Bq[0:m, 0:TT], ALU.mult, ALU.add,
                        psb + [Bqb, B_const], dst_b)
                    CP("dve", carry[0:m, j:j + 1], Bq[0:m, TT:TT + 1], [Bqb], [B_carry[j]])

                def ev_rkv(cb, ps, psb):
                    dst, dst_b = zrkv(cb)
                    ev_shift(cb, 128, ps, psb, dst, dst_b)
                proj_typeA([2, 3, 4], lambda kc: actA[:, kc, :], B_A, 12, TT, ev_rkv)

                ws, wsb = w_next(5)
                for cb, m in ((0, 32), (1, 32), (2, 96)):
                    ps, psb = ps_alloc(512)
                    for kc in range(8):
                        MM(ps[0:m, :], wA(ws, cb, kc, m), actA[:, kc, :], kc == 0, kc == 7, [wsb, B_A[kc]], psb)
                    tz, tzb = Tt(2 + cb)
                    ev_shift(12 + cb, m, ps, psb, tz[0:m, :], tzb)
                    func = (AF.Tanh, AF.Copy, AF.Sigmoid)[cb]
                    ACT(lob[0:m, cb, :], tz[0:m, :], func, tzb, [B_lob[cb]])

                for c in range(4):
                    ws, wsb = w_next(32 + c)
                    ps, psb = ps_alloc(512)
                    for k in range(CONV_K):
                        MM(ps[:, :], ws[:, k * 128:(k + 1) * 128], ubuf[:, c, k:k + TT], k == 0, k == CONV_K - 1,
                           [wsb, B_u[c]], psb)
                    ACT(cacc[:, c, :], ps[:, :], AF.Identity, psb + [B_const], [B_cacc[c]], bias=cvc("conv_b", c))
                    CP("dve", ubuf[:, c, 0:HIST], ubuf[:, c, TT:TT + HIST], [B_u[c]], [B_u[c]])

                psm, psmb = ps_alloc(512)
                pse, pseb = ps_alloc(512)
                for c in range(4):
                    q = rr["sq"] % 2
                    rr["sq"] += 1
                    CP("act", sqr[q][:], cacc[:, c, :], [B_cacc[c]], [B_sqr[q]])
                    MM(psm[:, :], o512[:], sqr[q][:], c == 0, c == 3, [B_sqr[q], B_const], psmb)
                    q = rr["sq"] % 2
                    rr["sq"] += 1
                    ACT(sqr[q][:], cacc[:, c, :], AF.Square, [B_cacc[c]], [B_sqr[q]])
                    MM(pse[:, :], o512[:], sqr[q][:], c == 0, c == 3, [B_sqr[q], B_const], pseb)
                tm2, tm2b = cln[:, 0, :], [B_cln[0]]
                ACT(tm2, psm[:, :], AF.Square, psmb, tm2b)
                TTop("dve", tm2, pse[:, :], tm2, ALU.subtract, pseb + tm2b, tm2b)
                ACT(tm2, tm2, AF.Sqrt, tm2b, tm2b, bias=EPS_LN, scale=1.0)
                P.op("dve", lambda e, tm2=tm2: e.reciprocal(out=tm2, in_=tm2), tm2b, tm2b)
                tmean, tmeanb = cln[:, 1, :], [B_cln[1]]
                CP("act", tmean, psm[:, :], psmb, tmeanb)
                for c in range(4):
                    TTop("pool", cacc[:, c, :], cacc[:, c, :], tmean, ALU.subtract, [B_cacc[c]] + tmeanb, [B_cacc[c]])
                    TTop("pool", cacc[:, c, :], cacc[:, c, :], tm2, ALU.mult, [B_cacc[c]] + tm2b, [B_cacc[c]])
                    ACT(actB[:, c, :], cacc[:, c, :], AF.Silu, [B_cacc[c], B_const], [B_Bc[c]],
                        bias=cvc("ln_b", c), scale=cvc("ln_g", c))

                for hg in range(2):
                    for lp in range(2):
                        pr = 2 * hg + lp
                        pc = slice(pr * 128, (pr + 1) * 128)
                        zr, zrb = zrkv(pr)
                        zk, zkb = zrkv(4 + pr)
                        zv, zvb = zrkv(8 + pr)
                        T = [Tt(i) for i in range(10)]
                        ps, psb = ps_alloc(512)
                        MM(ps[:, :], lorab[0:32, 0, pc], lob[0:32, 0, :], True, True, [B_const, B_lob[0]], psb)
                        sgw, sgwb = T[0]
                        ACT(sgw, ps[:, :], AF.Sigmoid, psb + [B_const], sgwb, bias=cvc("w0", pr))
                        cum, cumb = T[1]
                        P.op("dve", lambda e, cum=cum, sgw=sgw: e.tensor_tensor_scan(
                            out=cum, data0=rmask[:], data1=sgw, initial=0.0, op0=ALU.mult, op1=ALU.add),
                            sgwb + [B_const], cumb)
                        E1, E1b = T[2]
                        ACT(E1, cum, AF.Exp, cumb, E1b, scale=-DEC)
                        E2, E2b = T[3]
                        ACT(E2, cum, AF.Exp, cumb, E2b, scale=DEC)
                        E3, E3b = T[4]
                        TTop("pool", E3, cum, sgw, ALU.subtract, cumb + sgwb, E3b)
                        ACT(E3, E3, AF.Exp, E3b, E3b, scale=-DEC)
                        CP("pool", wcb[:, lp, :], E1.rearrange("p (c t) -> p c t", t=128)[:, :, 127], E1b, [B_wc[lp]])
                        ps, psb = ps_alloc(512)
                        MM(ps[:, :], lorab[0:32, 1, pc], lob[0:32, 1, :], True, True, [B_const, B_lob[1]], psb)
                        av, avb = T[5]
                        ACT(av, ps[:, :], AF.Sigmoid, psb + [B_const], avb, bias=cvc("a0", pr))
                        kk, kkb = T[6]
                        TS("dve", kk, zk, cvc("k_k", pr), None, ALU.mult, None, zkb + [B_const], kkb)
                        q = rr["sq"] % 2
                        rr["sq"] += 1
                        ACT(sqr[q][:], kk, AF.Square, kkb, [B_sqr[q]])
                        ps, psb = ps_alloc(512)
                        MM(ps[:, :], blk1[:], sqr[q][:], True, True, [B_const, B_sqr[q]], psb)
                        rn, rnb = T[7]
                        ACT(rn, ps[:, :], AF.Sqrt, psb, rnb)
                        TS("dve", rn, rn, 1e-12, None, ALU.max, None, rnb, rnb)
                        P.op("dve", lambda e, rn=rn: e.reciprocal(out=rn, in_=rn), rnb, rnb)
                        TTop("pool", kk, kk, rn, ALU.mult, kkb + rnb, kkb)
                        kp, kpb = T[8]
                        TS("dve", kp, av, cvc("k_a", pr), cvx[:, 15 + pr:16 + pr], ALU.mult, ALU.add,
                           avb + [B_const], kpb)
                        TTop("pool", kp, kp, zk, ALU.mult, kpb + zkb, kpb)
                        ob, ok = opb[:, lp, :], opk[:, lp, :]
                        for hf in range(2):
                            rows = slice(hf * 64, (hf + 1) * 64)
                            STT("dve", opa[rows, hf, lp, :], kk[rows, :], -1.0, E3[rows, :], ALU.mult, ALU.mult,
                                kkb + E3b, [B_opa[lp]])
                            TTop("pool", opr[rows, hf, lp, :], zr[rows, :], E1[rows, :], ALU.mult, zrb + E1b, [B_opr[lp]])
                        tmp, tmpb = T[9]
                        TTop("pool", tmp, kk, av, ALU.mult, kkb + avb, tmpb)
                        TTop("dve", ob, tmp, E2, ALU.mult, tmpb + E2b, [B_opb[lp]])
                        TTop("pool", ok, kp, E2, ALU.mult, kpb + E2b, [B_opk[lp]])
                        q = rr["sq"] % 2
                        rr["sq"] += 1
                        STT("dve", sqr[q][:], zr, cvc("r_k", pr), kp, ALU.mult, ALU.mult,
                            zrb + kpb + [B_const], [B_sqr[q]])
                        ps, psb = ps_alloc(512)
                        MM(ps[:, :], blk1[:], sqr[q][:], True, True, [B_const, B_sqr[q]], psb)
                        TTop("dve", bonus[:, lp, :], ps[:, :], zv, ALU.mult, psb + zvb, [B_bonus[lp]])
                        for kind, (src, srcb) in enumerate(((ob, [B_opb[lp]]), (ok, [B_opk[lp]]), (zv, zvb))):
                            ps, psb = ps_alloc(256)
                            psv = ps.bitcast(BF16)
                            for c in range(4):
                                P.op("pe", lambda e, psv=psv, src=src, c=c: e.transpose(
                                    psv[:, c * 128:(c + 1) * 128], src[:, c * 128:(c + 1) * 128], ident[:]),
                                    srcb + [B_const], psb)
                            CP("act" if kind != 1 else "dve",
                               tok[:, kind, lp, :, :].rearrange("p a b -> p (a b)"), psv, psb, [B_tok[kind][lp]])

                    for c in range(4):
                        cc = slice(c * 128, (c + 1) * 128)
                        def opnd(kind, lp, hf):
                            if kind == "a":
                                return opa[:, hf, lp, cc], B_opa[lp]
                            if kind == "r":
                                return opr[:, hf, lp, cc], B_opr[lp]
                            if kind == "b":
                                return opb[:, lp, cc], B_opb[lp]
                            return opk[:, lp, cc], B_opk[lp]
                        specs = (("X0", "b", "a", mSU), ("XT0", "a", "b", mSL), ("Aak", "k", "a", mSU),
                                 ("Arb", "b", "r", mIU), ("Ark", "k", "r", mIU))
                        for nm, kl, kr_, msk in specs:
                            ps, psb = ps_alloc(512)
                            for hh in range(4):
                                lp, hf = divmod(hh, 2)
                                lh, lhb = opnd(kl, lp, hf)
                                rh, rhb = opnd(kr_, lp, hf)
                                MM(ps[:, hh * 128:(hh + 1) * 128], lh, rh, True, True, [lhb, rhb], psb)
                            TTop("dve", mats[nm][:].rearrange("p a b -> p (a b)"), ps[:, :],
                                 msk[:].rearrange("p a b -> p (a b)"), ALU.mult, psb + [B_const], [B_m[nm]])
                        TTop("pool", mats["P0"][:], mats["X0"][:], ident4[:], ALU.add, [B_m["X0"], B_const], [B_m["P0"]])
                        cur = 0
                        for lvl in range(1, 7):
                            Xc, XTc, Pc = "X%d" % cur, "XT%d" % cur, "P%d" % cur
                            Xn, XTn, Pn = "X%d" % (1 - cur), "XT%d" % (1 - cur), "P%d" % (1 - cur)
                            ps2, ps2b = ps_alloc(512)
                            for hh in range(4):
                                MM(ps2[:, hh * 128:(hh + 1) * 128], mats[Xc][:, hh, :], mats[XTc][:, hh, :], True, True,
                                   [B_m[Xc], B_m[XTc]], ps2b)
                            if lvl < 6:
                                ps1, ps1b = ps_alloc(512)
                                for hh in range(4):
                                    MM(ps1[:, hh * 128:(hh + 1) * 128], mats[XTc][:, hh, :], mats[Xc][:, hh, :], True, True,
                                       [B_m[Xc], B_m[XTc]], ps1b)
                            CP("act", mats[XTn][:].rearrange("p a b -> p (a b)"), ps2[:, :], ps2b, [B_m[XTn]])
                            if lvl < 6:
                                CP("dve", mats[Xn][:].rearrange("p a b -> p (a b)"), ps1[:, :], ps1b, [B_m[Xn]])
                            ps3, ps3b = ps_alloc(512)
                            for hh in range(4):
                                MM(ps3[:, hh * 128:(hh + 1) * 128], ident[:], mats[Pc][:, hh, :], True, False,
                                   [B_const, B_m[Pc]], ps3b)
                                MM(ps3[:, hh * 128:(hh + 1) * 128], mats[XTn][:, hh, :], mats[Pc][:, hh, :], False, True,
                                   [B_m[XTn], B_m[Pc]], ps3b)
                            CP("act" if lvl % 2 else "dve", mats[Pn][:].rearrange("p a b -> p (a b)"), ps3[:, :], ps3b, [B_m[Pn]])
                            cur = 1 - cur
                        Pfin = "P%d" % cur
                        for lp in range(2):
                            pr = 2 * hg + lp
                            bS = B_Sbf[pr]
                            TS("pool", tmpS[:, pr, :], S32[:, pr, :], wcb[:, lp, c:c + 1], None, ALU.mult, None,
                               [B_S32[pr], B_wc[lp]], [B_tmpS[pr]])
                            psR, psRb = ps_alloc(128)
                            for hf in range(2):
                                hh = 2 * lp + hf
                                MM(psR[:, hf * 64:(hf + 1) * 64], opa[:, hf, lp, cc], Sbf[:, pr, :], True, False,
                                   [B_opa[lp], bS], psRb)
                                MM(psR[:, hf * 64:(hf + 1) * 64], mats["Aak"][:, hh, :], tok[:, 2, lp, c, hf * 64:(hf + 1) * 64],
                                   False, True, [B_m["Aak"], B_tok[2][lp]], psRb)
                            CP("act", RHSb[:, lp, :, :].rearrange("p a b -> p (a b)"), psR, psRb, [B_RHS[lp]])
                            psU, psUb = ps_alloc(128)
                            for hf in range(2):
                                hh = 2 * lp + hf
                                MM(psU[:, hf * 64:(hf + 1) * 64], mats[Pfin][:, hh, :], RHSb[:, lp, hf, :], True, True,
                                   [B_m[Pfin], B_RHS[lp]], psUb)
                            CP("dve", Ub[:, lp, :, :].rearrange("p a b -> p (a b)"), psU, psUb, [B_U[lp]])
                            psY, psYb = ps_alloc(128)
                            psS, psSb = ps_alloc(128)
                            for hf in range(2):
                                hh = 2 * lp + hf
                                rows = slice(hf * 64, (hf + 1) * 64)
                                vt = tok[:, 2, lp, c, hf * 64:(hf + 1) * 64]
                                MM(psY[rows, :], Sbf[:, pr, :], opr[:, hf, lp, cc], True, False,
                                   [bS, B_opr[lp]], psYb, tp=(0, hf * 64))
                                MM(psY[rows, :], Ub[:, lp, hf, :], mats["Arb"][:, hh, :], False, False,
                                   [B_U[lp], B_m["Arb"]], psYb, tp=(0, hf * 64))
                                MM(psY[rows, :], vt, mats["Ark"][:, hh, :], False, True,
                                   [B_tok[2][lp], B_m["Ark"]], psYb, tp=(0, hf * 64))
                                MM(psS[rows, 0:64], tok[:, 0, lp, c, hf * 64:(hf + 1) * 64], Ub[:, lp, hf, :], True, False,
                                   [B_tok[0][lp], B_U[lp]], psSb, tp=(0, hf * 64))
                                MM(psS[rows, 0:64], tok[:, 1, lp, c, hf * 64:(hf + 1) * 64], vt, False, True,
                                   [B_tok[1][lp], B_tok[2][lp]], psSb, tp=(0, hf * 64))
                            CP("act", ybuf[:, lp, cc], psY, psYb, [B_y[lp]])
                            STT("dve", S32[:, pr, :], psS[:, 0:64], wcb[:, lp, c:c + 1], tmpS[:, pr, :], ALU.mult, ALU.add,
                                psSb + [B_wc[lp], B_tmpS[pr]], [B_S32[pr]])
                            CP("pool", Sbf[:, pr, :], S32[:, pr, :], [B_S32[pr]], [bS])

                    for lp in range(2):
                        pr = 2 * hg + lp
                        pc = slice(pr * 128, (pr + 1) * 128)
                        yv = ybuf[:, lp, :]
                        q = rr["sq"] % 2
                        rr["sq"] += 1
                        CP("act", sqr[q][:], yv, [B_y[lp]], [B_sqr[q]])
                        psm, psmb = ps_alloc(512)
                        MM(psm[:, :], blk64[:], sqr[q][:], True, True, [B_const, B_sqr[q]], psmb)
                        q = rr["sq"] % 2
                        rr["sq"] += 1
                        ACT(sqr[q][:], yv, AF.Square, [B_y[lp]], [B_sqr[q]])
                        pse, pseb = ps_alloc(512)
                        MM(pse[:, :], blk64[:], sqr[q][:], True, True, [B_const, B_sqr[q]], pseb)
                        g1, g1b = Tt(0)
                        ACT(g1, psm[:, :], AF.Square, psmb, g1b)
                        TTop("dve", g1, pse[:, :], g1, ALU.subtract, pseb + g1b, g1b)
                        ACT(g1, g1, AF.Sqrt, g1b, g1b, bias=EPS_GN, scale=1.0)
                        P.op("dve", lambda e, g1=g1: e.reciprocal(out=g1, in_=g1), g1b, g1b)
                        g2, g2b = Tt(1)
                        TTop("dve", g2, yv, psm[:, :], ALU.subtract, [B_y[lp]] + psmb, g2b)
                        TTop("pool", g2, g2, g1, ALU.mult, g2b + g1b, g2b)
                        ACT(g2, g2, AF.Identity, g2b + [B_const], g2b, bias=cvc("lnx_b", pr), scale=cvc("lnx_g", pr))
                        TTop("pool", g2, g2, bonus[:, lp, :], ALU.add, g2b + [B_bonus[lp]], g2b)
                        psg, psgb = ps_alloc(512)
                        MM(psg[:, :], lorab[0:96, 2, pc], lob[0:96, 2, :], True, True, [B_const, B_lob[2]], psgb)
                        TTop("dve", actB[:, 4 + pr, :], g2, psg[:, :], ALU.mult, g2b + psgb, [B_Bc[4 + pr]])

                def ev_res(cb, ps, psb):
                    TTop("dve", xr[:, cb, :], xr[:, cb, :], ps[:, :], ALU.add, [xb[cb]] + psb, [xb[cb]])
                proj_typeA([6, 7], lambda kc: actB[:, kc, :], B_Bc, 8, TT, ev_res)

                rmsnorm(lambda c: xr[:, c, :], xb, "g_cross", lambda c: actA[:, c, :], B_A, TT)

                def ev_q(cb, ps, psb):
                    CP("act" if cb % 2 else "dve", actB[:, cb, :], ps[:, :], psb, [B_Bc[cb]])
                proj_typeA([8, 9], lambda kc: actA[:, kc, :], B_A, 8, TT, ev_q)
                for hd in range(4):
                    for mc in range(2):
                        ps, psb = ps_alloc(512)
                        for dc in range(2):
                            MM(ps[:, :], KT[:, 2 * hd + dc, mc * 128:(mc + 1) * 128], actB[:, 2 * hd + dc, :],
                               dc == 0, dc == 1, [B_KT, B_Bc[2 * hd + dc]], psb)
                        ACT(PT[:, mc, :], ps[:, :], AF.Exp, psb, [B_PT[mc]], scale=1.0 / 16.0)
                    ps, psb = ps_alloc(512)
                    for mc in range(2):
                        MM(ps[:, :], onesb[:], PT[:, mc, :], mc == 0, mc == 1, [B_const, B_PT[mc]], psb)
                    P.op("dve", lambda e, ps=ps: e.reciprocal(out=rsb[:], in_=ps[:, :]), psb, [B_rs])
                    for dvc in range(2):
                        ch = 2 * hd + dvc
                        ps, psb = ps_alloc(512)
                        for mc in range(2):
                            MM(ps[:, :], Vt[:, mc, ch * 128:(ch + 1) * 128], PT[:, mc, :], mc == 0, mc == 1,
                               [B_V, B_PT[mc]], psb)
                        TTop("dve", actA[:, ch, :], ps[:, :], rsb[:], ALU.mult, psb + [B_rs], [B_A[ch]])
                proj_typeA([10, 11], lambda kc: actA[:, kc, :], B_A, 8, TT, ev_res)

                rmsnorm(lambda c: xr[:, c, :], xb, "g_ffn", lambda c: actB[:, c, :], B_Bc, TT)

                def ev_ff1(cb, ps, psb):
                    f, fb = fT(cb)
                    ACT(f, ps[:, :], AF.Relu, psb, fb)
                    TTop("pool" if cb % 2 else "dve", f, f, f, ALU.mult, fb, fb)
                proj_typeA(list(range(12, 20)), lambda kc: actB[:, kc, :], B_Bc, 32, TT, ev_ff1)
                for cb in range(8):
                    ws, wsb = w_next(20 + cb)
                    ps, psb = ps_alloc(512)
                    for kc in range(32):
                        f, fb = fT(kc)
                        MM(ps[:, :], ws[:, kc * 128:(kc + 1) * 128], f, kc == 0, kc == 31, [wsb] + fb, psb)
                    ev_res(cb, ps, psb)

                rmsnorm(lambda c: xr[:, c, :], xb, "g_final", lambda c: xr[:, c, :], xb, TT)
                P.dma("sp", lambda e, s=s, t0=t0, xr=xr: e.dma_start(
                    out=oT[s].rearrange("(c p) t -> p c t", p=128)[:, :, t0:t0 + TT], in_=xr[:]), s_o[xi], reads=xb)

        P.wait_all("sp", B_x[0] + B_x[1])
        P.emit()
    return nc


def prep_inputs(inputs, nseq_per_core, ncores, seqlen):
    x = np.asarray(inputs["x"], np.float32)
    mem = np.asarray(inputs["mem"], np.float32)
    cv = pack_cvec(inputs)
    blocks = pack_blocks(inputs)
    lora = pack_lora(inputs)
    in_maps = []
    for c in range(ncores):
        sl = slice(c * nseq_per_core, (c + 1) * nseq_per_core)
        in_maps.append({
            "xT": np.ascontiguousarray(x[sl, :seqlen].transpose(0, 2, 1)),
            "memT": np.ascontiguousarray(mem[sl].transpose(0, 2, 1)),
            "wblk": blocks, "cvec": cv, "lora": lora,
        })
    return in_maps


def kernel(**inputs):
    nseq = BATCH // NCORES
    nc = build(nseq, SEQ)
    in_maps = prep_inputs(inputs, nseq, NCORES, SEQ)
    res = run_bass_kernel_spmd(nc, in_maps, core_ids=list(range(NCORES)))
    outs = [np.asarray(r["oT"]).transpose(0, 2, 1) for r in res.results]
    return np.ascontiguousarray(np.concatenate(outs, axis=0)).astype(np.float32)
```

```python
import contextlib
import numpy as np
import concourse.bass as bass
import concourse.mybir as mybir
from concourse.bass_utils import run_bass_kernel_spmd

F32 = mybir.dt.float32
BF16 = mybir.dt.bfloat16
ALU = mybir.AluOpType
AF = mybir.ActivationFunctionType

D = 1024
SEQ = 2048
BATCH = 32
NCORES = 8
TT = 512
MEM = 256
CW = 512
RW = 512
CONV_K = 31
HIST = CONV_K - 1
DFF = 4096
NBLK = 36
NHOST = 32
BLK = 4096
NSLOT = 3
NSET = 2
DEC = 0.6065306597126334

ENGS = ("pe", "act", "dve", "pool", "sp")


class Buf:
    __slots__ = ("name", "w", "r", "excl")

    def __init__(self, name="", excl=False):
        self.name = name
        self.w = None
        self.r = []
        self.excl = excl


class Prog:
    tile = 0

    @property
    def phase(self):
        return "t%d:%s" % (self.tile, self._phase)

    @phase.setter
    def phase(self, v):
        self._phase = v

    def __init__(self, nc, stack):
        self.nc = nc
        self.stack = stack
        self.ops = {e: [] for e in ENGS}
        self.cnt = {e: 0 for e in ENGS}
        self.waited = {e: {} for e in ENGS}
        self.sems = {}
        self.semval = {}
        for e in ENGS:
            self.sems[e] = stack.enter_context(nc.semaphore("s_" + e))
        self.n_dma_sems = 0
        self.phase = "init"
        self.annotate = False
        self.hazard = 10 ** 9

    def dma_sem(self, name=None):
        key = "dma%d" % self.n_dma_sems
        self.n_dma_sems += 1
        self.sems[key] = self.stack.enter_context(self.nc.semaphore(name or key))
        self.semval[key] = 0
        return key

    def _deps(self, eng, reads, writes):
        need = {}

        def add(dep):
            if dep is None:
                return
            k, v = dep
            if need.get(k, -1) < v:
                need[k] = v
        for b in reads:
            add(b.w)
        for b in writes:
            add(b.w)
            for d in b.r:
                add(d)
        out = []
        wd = self.waited[eng]
        for k, v in need.items():
            if k == eng:
                if eng in ("pe", "sp") or v <= self.cnt[eng] - self.hazard:
                    continue
            if wd.get(k, -1) >= v:
                continue
            wd[k] = v
            out.append((k, v))
        return out

    def _record(self, me, reads, writes):
        for b in reads:
            if len(b.r) > 24:
                best = {}
                for k, v in b.r:
                    if best.get(k, -1) < v:
                        best[k] = v
                b.r = list(best.items())
            b.r.append(me)
        for b in writes:
            b.w = me
            b.r = []

    @staticmethod
    def _split(reads, writes):
        if any(b.excl for b in reads):
            writes = list(writes) + [b for b in reads if b.excl]
            reads = [b for b in reads if not b.excl]
        return reads, writes

    def op(self, eng, fn, reads=(), writes=()):
        reads, writes = self._split(reads, writes)
        waits = self._deps(eng, reads, writes)
        self.cnt[eng] += 1
        me = (eng, self.cnt[eng])
        self.ops[eng].append((fn, waits, (eng, 1), self.phase))
        self._record(me, reads, writes)
        return me

    def dma(self, eng, fn, semkey, reads=(), writes=()):
        reads, writes = self._split(reads, writes)
        waits = self._deps(eng, reads, writes)
        self.semval[semkey] += 16
        me = (semkey, self.semval[semkey])
        self.ops[eng].append((fn, waits, (semkey, 16), self.phase))
        self._record(me, reads, writes)
        return me

    def wait_all(self, eng, bufs):
        waits = self._deps(eng, [], bufs)
        self.ops[eng].append((None, waits, None, self.phase))

    def emit(self):
        nc = self.nc
        handles = {"pe": "tensor", "act": "scalar", "dve": "vector", "pool": "gpsimd", "sp": "sync"}
        with nc.Block() as block:
            for e in ENGS:
                ops = self.ops[e]
                if not ops:
                    continue

                def body(engh, ops=ops):
                    for fn, waits, inc, phase in ops:
                        for k, v in waits:
                            engh.wait_ge(self.sems[k], v)
                        if fn is not None:
                            ins = fn(engh)
                            if inc is not None:
                                ins.then_inc(self.sems[inc[0]], inc[1])
                            if self.annotate:
                                ins.annotate(phase)
                getattr(block, handles[e])(body)


CV = {}
_off = 0
for _n, _w in [("g_mix", 8), ("g_cross", 8), ("g_mem", 8), ("g_ffn", 8), ("g_final", 8),
               ("conv_b", 4), ("ln_g", 4), ("ln_b", 4), ("conv_w", 124), ("mu", 15),
               ("w0", 4), ("a0", 4), ("k_k", 4), ("k_a", 4), ("r_k", 4), ("lnx_g", 4), ("lnx_b", 4)]:
    CV[_n] = _off
    _off += _w
NCV = _off


def _cm(v, nch):
    return np.ascontiguousarray(np.asarray(v, np.float32).reshape(nch, 128).T)


def pack_cvec(inp):
    cv = np.zeros((128, NCV), np.float32)

    def put(name, arr):
        cv[:, CV[name]:CV[name] + arr.shape[1]] = arr
    put("g_mix", _cm(inp["g_mix"][0], 8))
    put("g_cross", _cm(inp["g_cross"][0], 8))
    put("g_mem", _cm(inp["g_mem"][0], 8))
    put("g_ffn", _cm(inp["g_ffn"][0], 8))
    put("g_final", _cm(inp["g_final"], 8))
    put("conv_b", _cm(inp["conv_b"][0], 4))
    put("ln_g", _cm(inp["conv_ln_g"][0], 4))
    put("ln_b", _cm(inp["conv_ln_b"][0], 4))
    cw = np.asarray(inp["conv_w"][0], np.float32)
    cwp = cw.reshape(CONV_K, 4, 128).transpose(2, 0, 1)
    put("conv_w", np.ascontiguousarray(cwp.reshape(128, CONV_K * 4)))
    mu = np.asarray(inp["mu_b"][0], np.float32)
    m = np.zeros((128, 15), np.float32)
    m[:, 0:4] = _cm(mu[0:512], 4)
    m[:, 4:8] = _cm(mu[512:1024], 4)
    m[:, 8:12] = _cm(mu[1024:1536], 4)
    m[0:32, 12] = mu[1536:1568]
    m[0:32, 13] = mu[1568:1600]
    m[0:96, 14] = mu[1600:1696]
    put("mu", m)
    for nm in ("w0", "a0", "k_k", "k_a", "r_k", "lnx_g", "lnx_b"):
        put(nm, _cm(inp[nm][0], 4))
    return cv


def pack_blocks(inp):
    out = np.zeros((NHOST, 128, BLK), np.float32)

    def typeA(W, cols):
        blk = np.zeros((128, 4, 8, 128), np.float32)
        Wr = W.reshape(8, 128, -1)
        for cb, (st, wd) in enumerate(cols):
            blk[:, cb, :, :wd] = Wr[:, :, st:st + wd].transpose(1, 0, 2)
        return blk.reshape(128, BLK)

    w_in = np.asarray(inp["w_in"][0], np.float32)
    out[0] = typeA(w_in, [(0, 128), (512, 128), (128, 128), (640, 128)])
    out[1] = typeA(w_in, [(256, 128), (768, 128), (384, 128), (896, 128)])
    for j, base in enumerate((1024, 1536, 2048)):
        out[2 + j] = typeA(w_in, [(base + 128 * i, 128) for i in range(4)])
    out[5] = typeA(w_in, [(2560, 32), (2592, 32), (2624, 96)])
    for j, nm in enumerate(("w_out", "wq", "wo")):
        W = np.asarray(inp[nm][0], np.float32)
        for h in range(2):
            out[6 + 2 * j + h] = typeA(W, [(512 * h + 128 * i, 128) for i in range(4)])
    W = np.asarray(inp["w_ff1"][0], np.float32)
    for b in range(8):
        out[12 + b] = typeA(W, [(512 * b + 128 * i, 128) for i in range(4)])
    W = np.asarray(inp["w_ff2"][0], np.float32).reshape(32, 128, 1024)
    for b in range(8):
        out[20 + b] = W[:, :, 128 * b:128 * (b + 1)].transpose(1, 0, 2).reshape(128, BLK)
    W = np.asarray(inp["wk"][0], np.float32)
    for h in range(2):
        out[28 + h] = typeA(W, [(512 * h + 128 * i, 128) for i in range(4)])
    W = np.asarray(inp["wv"][0], np.float32).reshape(8, 128, 1024)
    for h in range(2):
        out[30 + h] = W[:, :, 512 * h:512 * (h + 1)].transpose(1, 0, 2).reshape(128, BLK)
    return out


def pack_lora(inp):
    lw = np.zeros((128, 3, 512), np.float32)
    lw[0:32, 0] = inp["w_decay2"][0]
    lw[0:32, 1] = inp["a_lora2"][0]
    lw[0:96, 2] = inp["g_lora2"][0]
    return lw


def build(nseq, seqlen, dbg=False, annotate=False):
    NT = seqlen // TT
    nc = bass.Bass("TRN2", target_bir_lowering=False)
    xT = nc.dram_tensor("xT", [nseq, D, seqlen], F32, kind="ExternalInput").ap()
    memT = nc.dram_tensor("memT", [nseq, D, MEM], F32, kind="ExternalInput").ap()
    wblk = nc.dram_tensor("wblk", [NHOST, 128, BLK], F32, kind="ExternalInput").ap()
    cvd = nc.dram_tensor("cvec", [128, NCV], F32, kind="ExternalInput").ap()
    lwd = nc.dram_tensor("lora", [128, 3, 512], F32, kind="ExternalInput").ap()
    oT = nc.dram_tensor("oT", [nseq, D, seqlen], F32, kind="ExternalOutput").ap()
    scr = nc.dram_tensor("wscr", [NBLK, 128, BLK], BF16).ap()
    dbg_out = {}

    with contextlib.ExitStack() as st:
        P = Prog(nc, st)
        P.annotate = annotate

        def sb(name, shape, dt):
            return st.enter_context(nc.sbuf_tensor(name, shape, dt))

        cv = sb("cv", [128, NCV], F32)
        cvx = sb("cvx", [128, 32], F32)
        ident = sb("ident", [128, 128], BF16)
        ident4 = sb("ident4", [128, 4, 128], BF16)
        onesb = sb("onesb", [128, 128], BF16)
        blk1 = sb("blk1", [128, 128], BF16)
        blk64 = sb("blk64", [128, 128], BF16)
        o512 = sb("o512", [128, 128], BF16)
        mSU = sb("mSU", [128, 4, 128], BF16)
        mIU = sb("mIU", [128, 4, 128], BF16)
        mSL = sb("mSL", [128, 4, 128], BF16)
        rmask = sb("rmask", [128, TT], F32)
        lorab = sb("lorab", [128, 3, 512], BF16)
        S32 = sb("S32", [128, 4, 64], F32)
        Sbf = sb("Sbf", [128, 4, 64], BF16)
        tmpS = sb("tmpS", [128, 4, 64], F32)
        carry = sb("carry", [128, 16], F32)
        KT = sb("KT", [128, 8, MEM], BF16)
        Vt = sb("Vt", [128, 2, D], BF16)
        wslot = [sb("wslot%d" % i, [128, BLK], BF16) for i in range(NSLOT)]
        xres = [sb("xres%d" % i, [128, 8, TT], F32) for i in range(2)]
        actA = sb("actA", [128, 8, TT], BF16)
        actB = sb("actB", [128, 8, TT], BF16)
        arena = sb("arena", [128, 32 * 512], BF16)
        lob = sb("lob", [128, 3, TT], BF16)
        Bbuf = [sb("Bbuf%d" % i, [128, TT + 1], F32) for i in range(2)]
        ubuf = sb("ubuf", [128, 4, HIST + TT], BF16)
        cln = sb("cln", [128, 2, TT], F32)
        cacc = sb("cacc", [128, 4, TT], F32)
        opa = sb("opa", [128, 2, 2, TT], BF16)
        opr = sb("opr", [128, 2, 2, TT], BF16)
        opb = sb("opb", [128, 2, TT], BF16)
        opk = sb("opk", [128, 2, TT], BF16)
        tok = sb("tok", [128, 3, 2, 4, 128], BF16)
        bonus = sb("bonus", [128, 2, TT], F32)
        ybuf = sb("ybuf", [128, 2, TT], F32)
        wcb = sb("wcb", [128, 2, 4], F32)
        MATN = ("X0", "X1", "XT0", "XT1", "P0", "P1", "Aak", "Arb", "Ark")
        matsets = [{nm: sb("m%d_%s" % (i, nm), [128, 4, 128], BF16) for nm in MATN} for i in range(NSET)]
        RHSb = sb("RHSb", [128, 2, 2, 64], BF16)
        Ub = sb("Ub", [128, 2, 2, 64], BF16)
        PT = sb("PT", [128, 2, TT], BF16)
        rsb = sb("rsb", [128, TT], F32)
        sqr = [sb("sqr%d" % i, [128, TT], BF16) for i in range(2)]
        psum = [st.enter_context(nc.psum_tensor("ps%d" % i, [128, 512], F32)) for i in range(8)]

        B_const = Buf("const")
        B_S32 = [Buf("S32_%d" % i) for i in range(4)]
        B_Sbf = [Buf("Sbf_%d" % i) for i in range(4)]
        B_tmpS = [Buf("tmpS%d" % i) for i in range(4)]
        B_carry = [Buf("carry%d" % i) for i in range(16)]
        B_KT = Buf("KT")
        B_V = Buf("V")
        B_wslot = [Buf("wslot%d" % i) for i in range(NSLOT)]
        B_x = [[Buf("x%d_%d" % (i, c)) for c in range(8)] for i in range(2)]
        B_A = [Buf("actA%d" % c) for c in range(8)]
        B_Bc = [Buf("actB%d" % c) for c in range(8)]
        B_ar = [Buf("ar%d" % i) for i in range(32)]
        B_lob = [Buf("lob%d" % i) for i in range(3)]
        B_Bbuf = [Buf("Bbuf0"), Buf("Bbuf1")]
        B_cln = [Buf("cln0"), Buf("cln1")]
        B_u = [Buf("u%d" % c) for c in range(4)]
        B_cacc = [Buf("cacc%d" % c) for c in range(4)]
        B_opa = [Buf("opa%d" % lp) for lp in range(2)]
        B_opr = [Buf("opr%d" % lp) for lp in range(2)]
        B_opb = [Buf("opb%d" % lp) for lp in range(2)]
        B_opk = [Buf("opk%d" % lp) for lp in range(2)]
        B_tok = [[Buf("tok%d_%d" % (k, lp)) for lp in range(2)] for k in range(3)]
        B_bonus = [Buf("bonus%d" % lp) for lp in range(2)]
        B_y = [Buf("y%d" % lp) for lp in range(2)]
        B_wc = [Buf("wc%d" % lp) for lp in range(2)]
        B_msets = [{nm: Buf("m%d_%s" % (i, nm)) for nm in MATN} for i in range(NSET)]
        B_RHS = [Buf("RHS%d" % lp) for lp in range(2)]
        B_U = [Buf("U%d" % lp) for lp in range(2)]
        B_PT = [Buf("PT%d" % i) for i in range(2)]
        B_rs = Buf("rs")
        B_sqr = [Buf("sqr%d" % i) for i in range(2)]
        B_ps = [Buf("psb%d" % i, excl=True) for i in range(8)]
        B_scr = [Buf("scr%d" % i) for i in range(NBLK)]

        def zrkv(j):
            return arena[:, j * 512:(j + 1) * 512], [B_ar[j]]

        def Tt(i):
            o = (12 + 2 * i) * 512
            return arena[:, o:o + 1024].bitcast(F32), [B_ar[12 + 2 * i], B_ar[13 + 2 * i]]

        def fT(j):
            return arena[:, j * 512:(j + 1) * 512], [B_ar[j]]

        ps_pos = [0]

        def ps_alloc(ncols):
            p = ps_pos[0]
            ps_pos[0] = (p + 1) % 8
            return psum[p][:, 0:ncols], [B_ps[p]]

        s_const = P.dma_sem("c")
        s_w = [P.dma_sem("w%d" % i) for i in range(NSLOT)]
        s_x = [P.dma_sem("x%d" % i) for i in range(2)]
        s_o = [P.dma_sem("o%d" % i) for i in range(2)]
        s_sw = [P.dma_sem("sw%d" % i) for i in range(NSLOT)]
        s_mem = P.dma_sem("mem")

        def ACT(out, in_, func, reads, writes, bias=None, scale=None):
            kw = {}
            if bias is not None:
                kw["bias"] = bias
            if scale is not None:
                kw["scale"] = scale
            P.op("act", lambda e: e.activation(out=out, in_=in_, func=func, **kw), reads, writes)

        def TTop(eng, out, in0, in1, op, reads, writes):
            P.op(eng, lambda e: e.tensor_tensor(out=out, in0=in0, in1=in1, op=op), reads, writes)

        def TS(eng, out, in0, s1, s2, op0, op1, reads, writes):
            if s2 is None:
                P.op(eng, lambda e: e.tensor_scalar(out=out, in0=in0, scalar1=s1, scalar2=None, op0=op0), reads, writes)
            else:
                P.op(eng, lambda e: e.tensor_scalar(out=out, in0=in0, scalar1=s1, scalar2=s2, op0=op0, op1=op1), reads, writes)

        def STT(eng, out, in0, scalar, in1, op0, op1, reads, writes):
            P.op(eng, lambda e: e.scalar_tensor_tensor(out=out, in0=in0, scalar=scalar, in1=in1, op0=op0, op1=op1), reads, writes)

        def CP(eng, out, in_, reads, writes):
            if eng == "act":
                P.op("act", lambda e: e.copy(out=out, in_=in_), reads, writes)
            else:
                P.op(eng, lambda e: e.tensor_copy(out=out, in_=in_), reads, writes)

        def MM(out, lhsT, rhs, start, stop, reads, writes, tp=None):
            if tp is None:
                P.op("pe", lambda e: e.matmul(out, lhsT=lhsT, rhs=rhs, start=start, stop=stop), reads, writes)
            else:
                P.op("pe", lambda e: e.matmul(out, lhsT=lhsT, rhs=rhs, start=start, stop=stop, tile_position=tp), reads, writes)

        def MEMSET(eng, ap, val, writes):
            P.op(eng, lambda e: e.memset(ap, val), (), writes)

        def AFSEL(out, in_, pattern, cmp, base, cm, reads, writes):
            P.op("pool", lambda e: e.affine_select(out=out, in_=in_, pattern=pattern, compare_op=cmp, fill=0.0,
                                                   base=base, channel_multiplier=cm), reads, writes)

        def cvc(name, j=0, rows=slice(0, 128)):
            o = CV[name] + j
            return cv[rows, o:o + 1]

        P.dma("sp", lambda e: e.dma_start(out=cv[:], in_=cvd), s_const, writes=[B_const])
        lw32, lwB = Tt(0)
        lw32b, lwBb = Tt(1)
        lw32c, lwBc = Tt(2)
        for j, (tv, tb) in enumerate(((lw32, lwB), (lw32b, lwBb), (lw32c, lwBc))):
            P.dma("sp", lambda e, tv=tv, j=j: e.dma_start(out=tv, in_=lwd[:, j, :]), s_const, writes=tb)
        for b_ in lwB + lwBb + lwBc + [B_const]:
            b_.w = (s_const, P.semval[s_const])
        for j, (tv, tb) in enumerate(((lw32, lwB), (lw32b, lwBb), (lw32c, lwBc))):
            CP("dve", lorab[:, j, :], tv, tb, [B_const])
        TS("dve", cvx[:, 0:15], cv[:, CV["mu"]:CV["mu"] + 15], -1.0, 1.0, ALU.mult, ALU.add, [B_const], [B_const])
        TS("dve", cvx[:, 15:19], cv[:, CV["k_a"]:CV["k_a"] + 4], -1.0, 1.0, ALU.mult, ALU.add, [B_const], [B_const])
        MEMSET("pool", cvx[:, 19:20], 1e-6, [B_const])
        MEMSET("pool", cvx[:, 20:21], 1e-5, [B_const])
        MEMSET("pool", cvx[:, 21:22], 64e-5, [B_const])
        EPS_RMS, EPS_LN, EPS_GN = cvx[:, 19:20], cvx[:, 20:21], cvx[:, 21:22]
        t3, t3b = Tt(3)
        m32 = t3[:, 0:128]
        MEMSET("pool", m32, 1.0, t3b)
        AFSEL(m32, m32, [[-1, 128]], ALU.is_equal, 0, 1, t3b, t3b)
        CP("pool", ident[:], m32, t3b, [B_const])
        for h in range(4):
            CP("pool", ident4[:, h, :], m32, t3b, [B_const])
        for msk, cmp in ((mSU, ALU.is_gt), (mIU, ALU.is_ge)):
            MEMSET("pool", m32, 1.0, t3b)
            AFSEL(m32, m32, [[1, 128]], cmp, 0, -1, t3b, t3b)
            for h in range(4):
                CP("pool", msk[:, h, :], m32, t3b, [B_const])
        MEMSET("pool", m32, 1.0, t3b)
        AFSEL(m32, m32, [[-1, 128]], ALU.is_gt, 0, 1, t3b, t3b)
        for h in range(4):
            CP("pool", mSL[:, h, :], m32, t3b, [B_const])
        MEMSET("pool", opa[:], 0.0, B_opa)
        MEMSET("pool", opr[:], 0.0, B_opr)
        MEMSET("pool", onesb[:], 1.0, [B_const])
        MEMSET("pool", o512[:], 1.0 / 512.0, [B_const])
        MEMSET("pool", blk1[:], 0.0, [B_const])
        MEMSET("pool", blk1[0:64, 0:64], 1.0, [B_const])
        MEMSET("pool", blk1[64:128, 64:128], 1.0, [B_const])
        MEMSET("pool", blk64[:], 0.0, [B_const])
        MEMSET("pool", blk64[0:64, 0:64], 1.0 / 64.0, [B_const])
        MEMSET("pool", blk64[64:128, 64:128], 1.0 / 64.0, [B_const])
        MEMSET("pool", rmask[:], 1.0, [B_const])
        for c in range(TT // 128):
            MEMSET("pool", rmask[:, c * 128:c * 128 + 1], 0.0, [B_const])

        P.phase = "prologue"
        cast_engs = ("act", "dve", "pool")
        for b in range(NHOST):
            xi = b % 2
            si = b % NSLOT
            stage = xres[xi][:].rearrange("p a b -> p (a b)")
            P.dma("sp", lambda e, stage=stage, b=b: e.dma_start(out=stage, in_=wblk[b]), s_x[xi], writes=B_x[xi])
            CP(cast_engs[b % 3], wslot[si][:], stage, B_x[xi], [B_wslot[si]])
            P.dma("sp", lambda e, si=si, b=b: e.dma_start(out=scr[b], in_=wslot[si][:]), s_sw[si],
                  reads=[B_wslot[si]], writes=[B_scr[b]])

        cwo_ = CV["conv_w"]
        for c in range(4):
            b = NHOST + c
            si = b % NSLOT
            MEMSET("pool", wslot[si][:, CONV_K * 128:BLK], 0.0, [B_wslot[si]])
            for k in range(CONV_K):
                TS("dve" if k % 2 else "pool", wslot[si][:, k * 128:(k + 1) * 128], ident[:],
                   cv[:, cwo_ + k * 4 + c:cwo_ + k * 4 + c + 1], None, ALU.mult, None, [B_const], [B_wslot[si]])
            P.dma("sp", lambda e, si=si, b=b: e.dma_start(out=scr[b], in_=wslot[si][:]), s_sw[si],
                  reads=[B_wslot[si]], writes=[B_scr[b]])

        wseq = []
        for s in range(nseq):
            for ti in range(NT):
                if ti == 0:
                    wseq += [28, 29, 30, 31]
                wseq += [0, 1, 2, 3, 4, 5, 32, 33, 34, 35] + list(range(6, 28))
        wstate = {"issued": 0, "used": 0}

        def w_issue_upto(n):
            while wstate["issued"] < min(n, len(wseq)):
                i = wstate["issued"]
                si = i % NSLOT
                blk = wseq[i]
                P.dma("sp", lambda e, si=si, blk=blk: e.dma_start(out=wslot[si][:], in_=scr[blk]), s_w[si],
                      reads=[B_scr[blk]], writes=[B_wslot[si]])
                wstate["issued"] += 1

        def w_next(expect):
            i = wstate["used"]
            assert wseq[i] == expect, (i, wseq[i], expect)
            w_issue_upto(i + NSLOT)
            wstate["used"] += 1
            si = i % NSLOT
            return wslot[si], B_wslot[si]

        def wA(ws, cb, kc, m=128):
            o = (cb * 8 + kc) * 128
            return ws[:, o:o + m]

        rr = {"sq": 0, "eng": 0}

        def rmsnorm(src, src_b, gname, dst, dst_b, n, out_f32_inplace=False):
            ps, psb = ps_alloc(512)
            for c in range(8):
                q = rr["sq"] % 2
                rr["sq"] += 1
                ACT(sqr[q][:, 0:n], src(c), AF.Square, [src_b[c]], [B_sqr[q]])
                MM(ps[:, 0:n], onesb[:], sqr[q][:, 0:n], c == 0, c == 7, [B_sqr[q], B_const], psb)
            ACT(rsb[:, 0:n], ps[:, 0:n], AF.Sqrt, psb, [B_rs], bias=EPS_RMS, scale=1.0 / D)
            P.op("dve", lambda e: e.reciprocal(out=rsb[:, 0:n], in_=rsb[:, 0:n]), [B_rs], [B_rs])
            for c in range(8):
                STT("dve", dst(c), src(c), cvc(gname, c), rsb[:, 0:n], ALU.mult, ALU.mult,
                    [src_b[c], B_rs, B_const], [dst_b[c]])

        def proj_typeA(blocks, rhs, rhs_b, ncb_total, n, evac):
            cb_glob = 0
            for blk in blocks:
                ws, wsb = w_next(blk)
                for cb in range(4):
                    if cb_glob >= ncb_total:
                        break
                    ps, psb = ps_alloc(512)
                    for kc in range(8):
                        MM(ps[:, 0:n], wA(ws, cb, kc), rhs(kc), kc == 0, kc == 7, [wsb, rhs_b[kc]], psb)
                    evac(cb_glob, ps, psb)
                    cb_glob += 1

        for s in range(nseq):
            MEMSET("pool", S32[:], 0.0, B_S32)
            MEMSET("pool", Sbf[:], 0.0, B_Sbf)
            MEMSET("pool", carry[:], 0.0, B_carry)
            for c in range(4):
                MEMSET("pool", ubuf[:, c, 0:HIST], 0.0, [B_u[c]])
            P.phase = "memkv"
            memv = cacc[:].rearrange("p a b -> p (a b)")[:, 0:8 * MEM].rearrange("p (a b) -> p a b", b=MEM)
            P.dma("sp", lambda e, s=s: e.dma_start(out=memv, in_=memT[s].rearrange("(c p) m -> p c m", p=128)),
                  s_mem, writes=B_cacc)
            rmsnorm(lambda c: memv[:, c, :], [B_cacc[c // 2] for c in range(8)], "g_mem",
                    lambda c: actA[:, c, 0:MEM], B_A, MEM)

            def evK(cb, ps, psb):
                CP("act" if cb % 2 else "dve", KT[:, cb, :], ps[:, 0:MEM], psb, [B_KT])
            proj_typeA([28, 29], lambda kc: actA[:, kc, 0:MEM], B_A, 8, MEM, evK)
            for h in range(2):
                ws, wsb = w_next(30 + h)
                for mc in range(2):
                    ps, psb = ps_alloc(512)
                    for kc in range(8):
                        MM(ps[:, :], actA[:, kc, mc * 128:(mc + 1) * 128], ws[:, kc * 512:(kc + 1) * 512],
                           kc == 0, kc == 7, [wsb, B_A[kc]], psb)
                    CP("act" if mc else "dve", Vt[:, mc, h * 512:(h + 1) * 512], ps[:, :], psb, [B_V])

            for ti in range(NT):
                gi = s * NT + ti
                P.tile = gi
                xi = gi % 2
                xr = xres[xi]
                xb = B_x[xi]
                t0 = ti * TT
                P.phase = "norm1"
                P.dma("sp", lambda e, s=s, t0=t0, xr=xr: e.dma_start(
                    out=xr[:], in_=xT[s].rearrange("(c p) t -> p c t", p=128)[:, :, t0:t0 + TT]), s_x[xi], writes=xb)
                rmsnorm(lambda c: xr[:, c, :], xb, "g_mix", lambda c: actA[:, c, :], B_A, TT)

                P.phase = "w_in"
                pend = {}

                def ev_conv(cb, ps, psb):
                    ch, isgate = divmod(cb, 2)
                    if not isgate:
                        pend["val"] = (ps, psb)
                        return
                    vps, vpsb = pend.pop("val")
                    tsg, tsgb = Tt(ch % 2)
                    ACT(tsg, ps[:, :], AF.Sigmoid, psb, tsgb)
                    TTop("dve", ubuf[:, ch, HIST:HIST + TT], vps[:, :], tsg, ALU.mult, vpsb + tsgb, [B_u[ch]])
                proj_typeA([0, 1], lambda kc: actA[:, kc, :], B_A, 8, TT, ev_conv)

                def ev_shift(j, m, ps, psb, dst, dst_b):
                    q = j % 2
                    Bq, Bqb = Bbuf[q], B_Bbuf[q]
                    ACT(Bq[0:m, 1:TT + 1], ps[0:m, :], AF.Identity, psb + [B_const], [Bqb], scale=cvc("mu", j, slice(0, m)))
                    CP("dve", Bq[0:m, 0:1], carry[0:m, j:j + 1], [B_carry[j]], [Bqb])
                    STT("dve", dst, ps[0:m, :], cvx[0:m, j:j + 1], Bq[0:m, 0:TT], ALU.mult, ALU.add,
                        psb + [Bqb, B_const], dst_b)
                    CP("dve", carry[0:m, j:j + 1], Bq[0:m, TT:TT + 1], [Bqb], [B_carry[j]])

                def ev_rkv(cb, ps, psb):
                    dst, dst_b = zrkv(cb)
                    ev_shift(cb, 128, ps, psb, dst, dst_b)
                proj_typeA([2, 3, 4], lambda kc: actA[:, kc, :], B_A, 12, TT, ev_rkv)

                ws, wsb = w_next(5)
                for cb, m in ((0, 32), (1, 32), (2, 96)):
                    ps, psb = ps_alloc(512)
                    for kc in range(8):
                        MM(ps[0:m, :], wA(ws, cb, kc, m), actA[:, kc, :], kc == 0, kc == 7, [wsb, B_A[kc]], psb)
                    tz, tzb = Tt(2 + cb)
                    ev_shift(12 + cb, m, ps, psb, tz[0:m, :], tzb)
                    func = (AF.Tanh, AF.Copy, AF.Sigmoid)[cb]
                    ACT(lob[0:m, cb, :], tz[0:m, :], func, tzb, [B_lob[cb]])

                P.phase = "conv"
                for c in range(4):
                    ws, wsb = w_next(32 + c)
                    ps, psb = ps_alloc(512)
                    for k in range(CONV_K):
                        MM(ps[:, :], ws[:, k * 128:(k + 1) * 128], ubuf[:, c, k:k + TT], k == 0, k == CONV_K - 1,
                           [wsb, B_u[c]], psb)
                    ACT(cacc[:, c, :], ps[:, :], AF.Identity, psb + [B_const], [B_cacc[c]], bias=cvc("conv_b", c))
                    CP("dve", ubuf[:, c, 0:HIST], ubuf[:, c, TT:TT + HIST], [B_u[c]], [B_u[c]])

                psm, psmb = ps_alloc(512)
                pse, pseb = ps_alloc(512)
                for c in range(4):
                    q = rr["sq"] % 2
                    rr["sq"] += 1
                    CP("act", sqr[q][:], cacc[:, c, :], [B_cacc[c]], [B_sqr[q]])
                    MM(psm[:, :], o512[:], sqr[q][:], c == 0, c == 3, [B_sqr[q], B_const], psmb)
                    q = rr["sq"] % 2
                    rr["sq"] += 1
                    ACT(sqr[q][:], cacc[:, c, :], AF.Square, [B_cacc[c]], [B_sqr[q]])
                    MM(pse[:, :], o512[:], sqr[q][:], c == 0, c == 3, [B_sqr[q], B_const], pseb)
                tm2, tm2b = cln[:, 0, :], [B_cln[0]]
                ACT(tm2, psm[:, :], AF.Square, psmb, tm2b)
                TTop("dve", tm2, pse[:, :], tm2, ALU.subtract, pseb + tm2b, tm2b)
                ACT(tm2, tm2, AF.Sqrt, tm2b, tm2b, bias=EPS_LN, scale=1.0)
                P.op("dve", lambda e, tm2=tm2: e.reciprocal(out=tm2, in_=tm2), tm2b, tm2b)
                tmean, tmeanb = cln[:, 1, :], [B_cln[1]]
                CP("act", tmean, psm[:, :], psmb, tmeanb)
                for c in range(4):
                    TTop("pool", cacc[:, c, :], cacc[:, c, :], tmean, ALU.subtract, [B_cacc[c]] + tmeanb, [B_cacc[c]])
                    TTop("pool", cacc[:, c, :], cacc[:, c, :], tm2, ALU.mult, [B_cacc[c]] + tm2b, [B_cacc[c]])
                    ACT(actB[:, c, :], cacc[:, c, :], AF.Silu, [B_cacc[c], B_const], [B_Bc[c]],
                        bias=cvc("ln_b", c), scale=cvc("ln_g", c))

                P.phase = "rwkv"
                for hg in range(2):
                    P.phase = "rwkv_ew"

                    def ew_steps(lp, T):
                        pr = 2 * hg + lp
                        pc = slice(pr * 128, (pr + 1) * 128)
                        zr, zrb = zrkv(pr)
                        zk, zkb = zrkv(4 + pr)
                        zv, zvb = zrkv(8 + pr)
                        sgw, sgwb = T[0]
                        cum, cumb = T[1]
                        E1, E1b = T[2]
                        av, avb = T[3]
                        kk, kkb = T[4]
                        rn, rnb = T[5]
                        kp, kpb = T[6]
                        E3, E3b = sgw, sgwb
                        E2, E2b = cum, cumb
                        ob, ok = opb[:, lp, :], opk[:, lp, :]
                        st = []

                        def s1():
                            ps, psb = ps_alloc(512)
                            MM(ps[:, :], lorab[0:32, 0, pc], lob[0:32, 0, :], True, True, [B_const, B_lob[0]], psb)
                            ACT(sgw, ps[:, :], AF.Sigmoid, psb + [B_const], sgwb, bias=cvc("w0", pr))
                            ps, psb = ps_alloc(512)
                            MM(ps[:, :], lorab[0:32, 1, pc], lob[0:32, 1, :], True, True, [B_const, B_lob[1]], psb)
                            ACT(av, ps[:, :], AF.Sigmoid, psb + [B_const], avb, bias=cvc("a0", pr))
                        st.append(s1)

                        def s2():
                            P.op("dve", lambda e: e.tensor_tensor_scan(
                                out=cum, data0=rmask[:], data1=sgw, initial=0.0, op0=ALU.mult, op1=ALU.add),
                                sgwb + [B_const], cumb)
                            TS("pool", kk, zk, cvc("k_k", pr), None, ALU.mult, None, zkb + [B_const], kkb)
                        st.append(s2)

                        def s3():
                            ACT(E1, cum, AF.Exp, cumb, E1b, scale=-DEC)
                            TTop("dve", E3, cum, sgw, ALU.subtract, cumb + sgwb, E3b)
                            q = rr["sq"] % 2
                            rr["sq"] += 1
                            ACT(sqr[q][:], kk, AF.Square, kkb, [B_sqr[q]])
                            ps, psb = ps_alloc(512)
                            MM(ps[:, :], blk1[:], sqr[q][:], True, True, [B_const, B_sqr[q]], psb)
                            ACT(rn, ps[:, :], AF.Sqrt, psb, rnb)
                        st.append(s3)

                        def s4():
                            ACT(E3, E3, AF.Exp, E3b, E3b, scale=-DEC)
                            ACT(E2, cum, AF.Exp, cumb, E2b, scale=DEC)
                            TS("dve", rn, rn, 1e-12, None, ALU.max, None, rnb, rnb)
                            P.op("dve", lambda e: e.reciprocal(out=rn, in_=rn), rnb, rnb)
                            TS("pool", kp, av, cvc("k_a", pr), cvx[:, 15 + pr:16 + pr], ALU.mult, ALU.add,
                               avb + [B_const], kpb)
                        st.append(s4)

                        def s5():
                            CP("pool", wcb[:, lp, :], E1.rearrange("p (c t) -> p c t", t=128)[:, :, 127], E1b, [B_wc[lp]])
                            TTop("dve", kk, kk, rn, ALU.mult, kkb + rnb, kkb)
                            TTop("pool", kp, kp, zk, ALU.mult, kpb + zkb, kpb)
                        st.append(s5)

                        def s6():
                            for hf in range(2):
                                rows = slice(hf * 64, (hf + 1) * 64)
                                STT("dve", opa[rows, hf, lp, :], kk[rows, :], -1.0, E3[rows, :], ALU.mult, ALU.mult,
                                    kkb + E3b, [B_opa[lp]])
                            TTop("pool", rn, kk, av, ALU.mult, kkb + avb, rnb)
                            TTop("pool", ok, kp, E2, ALU.mult, kpb + E2b, [B_opk[lp]])
                        st.append(s6)

                        def s7():
                            TTop("dve", ob, rn, E2, ALU.mult, rnb + E2b, [B_opb[lp]])
                            for hf in range(2):
                                rows = slice(hf * 64, (hf + 1) * 64)
                                TTop("pool", opr[rows, hf, lp, :], zr[rows, :], E1[rows, :], ALU.mult, zrb + E1b, [B_opr[lp]])
                            q = rr["sq"] % 2
                            rr["sq"] += 1
                            STT("dve", sqr[q][:], zr, cvc("r_k", pr), kp, ALU.mult, ALU.mult,
                                zrb + kpb + [B_const], [B_sqr[q]])
                            ps, psb = ps_alloc(512)
                            MM(ps[:, :], blk1[:], sqr[q][:], True, True, [B_const, B_sqr[q]], psb)
                            TTop("dve", bonus[:, lp, :], ps[:, :], zv, ALU.mult, psb + zvb, [B_bonus[lp]])
                        st.append(s7)

                        def s8():
                            for kind, (src, srcb) in enumerate(((ob, [B_opb[lp]]), (ok, [B_opk[lp]]), (zv, zvb))):
                                ps, psb = ps_alloc(256)
                                psv = ps.bitcast(BF16)
                                for c in range(4):
                                    P.op("pe", lambda e, psv=psv, src=src, c=c: e.transpose(
                                        psv[:, c * 128:(c + 1) * 128], src[:, c * 128:(c + 1) * 128], ident[:]),
                                        srcb + [B_const], psb)
                                CP("act" if kind != 1 else "dve",
                                   tok[:, kind, lp, :, :].rearrange("p a b -> p (a b)"), psv, psb, [B_tok[kind][lp]])
                        st.append(s8)
                        return st

                    def actA_tmp(i):
                        return (actA[:, 2 * i:2 * i + 2, :].rearrange("p a b -> p (a b)").bitcast(F32),
                                [B_A[2 * i], B_A[2 * i + 1]])
                    Tset0 = [Tt(i) for i in range(7)]
                    Tset1 = [Tt(7), Tt(8), Tt(9)] + [actA_tmp(i) for i in range(4)]
                    sl0 = ew_steps(0, Tset0)
                    sl1 = ew_steps(1, Tset1)
                    for f0, f1 in zip(sl0, sl1):
                        f0()
                        f1()

                    P.phase = "wkv"

                    def chain_stages(c, mats, B_m):
                        cc = slice(c * 128, (c + 1) * 128)
                        fl = lambda t: t[:].rearrange("p a b -> p (a b)")

                        def opnd(kind, lp, hf):
                            if kind == "a":
                                return opa[:, hf, lp, cc], B_opa[lp]
                            if kind == "r":
                                return opr[:, hf, lp, cc], B_opr[lp]
                            if kind == "b":
                                return opb[:, lp, cc], B_opb[lp]
                            return opk[:, lp, cc], B_opk[lp]

                        def amat(nm, kl, kr_, msk, direct):
                            ps, psb = ps_alloc(512)
                            for hh in range(4):
                                lp, hf = divmod(hh, 2)
                                lh, lhb = opnd(kl, lp, hf)
                                rh, rhb = opnd(kr_, lp, hf)
                                MM(ps[:, hh * 128:(hh + 1) * 128], lh, rh, True, True, [lhb, rhb], psb)
                            if direct:
                                TTop("dve", fl(mats[nm]), ps[:, :], fl(msk), ALU.mult, psb + [B_const], [B_m[nm]])
                            else:
                                CP("act", fl(mats[nm]), ps[:, :], psb, [B_m[nm]])
                                TTop("pool", fl(mats[nm]), fl(mats[nm]), fl(msk), ALU.mult, [B_m[nm], B_const], [B_m[nm]])

                        def stA():
                            amat("X0", "b", "a", mSU, True)
                            amat("XT0", "a", "b", mSL, True)
                            TTop("pool", mats["P0"][:], mats["X0"][:], ident4[:], ALU.add, [B_m["X0"], B_const], [B_m["P0"]])
                            amat("Aak", "k", "a", mSU, False)
                            amat("Arb", "b", "r", mIU, False)
                            amat("Ark", "k", "r", mIU, False)

                        def stL(lvl):
                            cur = (lvl - 1) % 2
                            Xc, XTc, Pc = "X%d" % cur, "XT%d" % cur, "P%d" % cur
                            Xn, XTn, Pn = "X%d" % (1 - cur), "XT%d" % (1 - cur), "P%d" % (1 - cur)
                            ps2, ps2b = ps_alloc(512)
                            for hh in range(4):
                                MM(ps2[:, hh * 128:(hh + 1) * 128], mats[Xc][:, hh, :], mats[XTc][:, hh, :], True, True,
                                   [B_m[Xc], B_m[XTc]], ps2b)
                            if lvl < 6:
                                ps1, ps1b = ps_alloc(512)
                                for hh in range(4):
                                    MM(ps1[:, hh * 128:(hh + 1) * 128], mats[XTc][:, hh, :], mats[Xc][:, hh, :], True, True,
                                       [B_m[Xc], B_m[XTc]], ps1b)
                            CP("act", fl(mats[XTn]), ps2[:, :], ps2b, [B_m[XTn]])
                            if lvl < 6:
                                CP("dve", fl(mats[Xn]), ps1[:, :], ps1b, [B_m[Xn]])
                            ps3, ps3b = ps_alloc(512)
                            for hh in range(4):
                                MM(ps3[:, hh * 128:(hh + 1) * 128], ident[:], mats[Pc][:, hh, :], True, False,
                                   [B_const, B_m[Pc]], ps3b)
                                MM(ps3[:, hh * 128:(hh + 1) * 128], mats[XTn][:, hh, :], mats[Pc][:, hh, :], False, True,
                                   [B_m[XTn], B_m[Pc]], ps3b)
                            CP("act" if lvl % 2 else "dve", fl(mats[Pn]), ps3[:, :], ps3b, [B_m[Pn]])

                        def stS():
                            Pfin = "P0"
                            hold = []
                            for lp in range(2):
                                pr = 2 * hg + lp
                                bS = B_Sbf[pr]
                                ACT(tmpS[:, pr, :], S32[:, pr, :], AF.Identity, [B_S32[pr], B_wc[lp]], [B_tmpS[pr]],
                                    scale=wcb[:, lp, c:c + 1])
                                psR, psRb = ps_alloc(128)
                                for hf in range(2):
                                    hh = 2 * lp + hf
                                    MM(psR[:, hf * 64:(hf + 1) * 64], opa[:, hf, lp, cc], Sbf[:, pr, :], True, False,
                                       [B_opa[lp], bS], psRb)
                                    MM(psR[:, hf * 64:(hf + 1) * 64], mats["Aak"][:, hh, :], tok[:, 2, lp, c, hf * 64:(hf + 1) * 64],
                                       False, True, [B_m["Aak"], B_tok[2][lp]], psRb)
                                CP("act" if lp else "dve", RHSb[:, lp, :, :].rearrange("p a b -> p (a b)"), psR, psRb, [B_RHS[lp]])
                            for lp in range(2):
                                psU, psUb = ps_alloc(128)
                                for hf in range(2):
                                    hh = 2 * lp + hf
                                    MM(psU[:, hf * 64:(hf + 1) * 64], mats[Pfin][:, hh, :], RHSb[:, lp, hf, :], True, True,
                                       [B_m[Pfin], B_RHS[lp]], psUb)
                                CP("dve" if lp else "act", Ub[:, lp, :, :].rearrange("p a b -> p (a b)"), psU, psUb, [B_U[lp]])
                            for lp in range(2):
                                pr = 2 * hg + lp
                                bS = B_Sbf[pr]
                                psY, psYb = ps_alloc(128)
                                psS, psSb = ps_alloc(128)
                                for hf in range(2):
                                    hh = 2 * lp + hf
                                    rows = slice(hf * 64, (hf + 1) * 64)
                                    vt = tok[:, 2, lp, c, hf * 64:(hf + 1) * 64]
                                    MM(psY[rows, :], Sbf[:, pr, :], opr[:, hf, lp, cc], True, False,
                                       [bS, B_opr[lp]], psYb, tp=(0, hf * 64))
                                    MM(psY[rows, :], Ub[:, lp, hf, :], mats["Arb"][:, hh, :], False, False,
                                       [B_U[lp], B_m["Arb"]], psYb, tp=(0, hf * 64))
                                    MM(psY[rows, :], vt, mats["Ark"][:, hh, :], False, True,
                                       [B_tok[2][lp], B_m["Ark"]], psYb, tp=(0, hf * 64))
                                    MM(psS[rows, 0:64], tok[:, 0, lp, c, hf * 64:(hf + 1) * 64], Ub[:, lp, hf, :], True, False,
                                       [B_tok[0][lp], B_U[lp]], psSb, tp=(0, hf * 64))
                                    MM(psS[rows, 0:64], tok[:, 1, lp, c, hf * 64:(hf + 1) * 64], vt, False, True,
                                       [B_tok[1][lp], B_tok[2][lp]], psSb, tp=(0, hf * 64))
                                STT("dve", Sbf[:, pr, :], psS[:, 0:64], wcb[:, lp, c:c + 1], tmpS[:, pr, :], ALU.mult, ALU.add,
                                    psSb + [B_wc[lp], B_tmpS[pr]], [bS])
                                STT("dve", S32[:, pr, :], psS[:, 0:64], wcb[:, lp, c:c + 1], tmpS[:, pr, :], ALU.mult, ALU.add,
                                    psSb + [B_wc[lp], B_tmpS[pr]], [B_S32[pr]])
                                CP("act", ybuf[:, lp, cc], psY, psYb, [B_y[lp]])
                        return [stA] + [(lambda l=l: stL(l)) for l in range(1, 7)] + [stS]

                    chains = [chain_stages(c, matsets[(2 * hg * 0 + c) % NSET], B_msets[(c) % NSET]) for c in range(4)]
                    STAG = 8 // NSET
                    posn = [0] * 4
                    step = 0
                    while any(posn[c] < 8 for c in range(4)):
                        for c in range(4):
                            if step >= c * STAG and posn[c] < 8:
                                chains[c][posn[c]]()
                                posn[c] += 1
                        step += 1

                    P.phase = "gn"
                    for lp in range(2):
                        pr = 2 * hg + lp
                        pc = slice(pr * 128, (pr + 1) * 128)
                        yv = ybuf[:, lp, :]
                        q = rr["sq"] % 2
                        rr["sq"] += 1
                        CP("act", sqr[q][:], yv, [B_y[lp]], [B_sqr[q]])
                        psm, psmb = ps_alloc(512)
                        MM(psm[:, :], blk64[:], sqr[q][:], True, True, [B_const, B_sqr[q]], psmb)
                        q = rr["sq"] % 2
                        rr["sq"] += 1
                        ACT(sqr[q][:], yv, AF.Square, [B_y[lp]], [B_sqr[q]])
                        pse, pseb = ps_alloc(512)
                        MM(pse[:, :], blk64[:], sqr[q][:], True, True, [B_const, B_sqr[q]], pseb)
                        g1, g1b = Tt(0)
                        ACT(g1, psm[:, :], AF.Square, psmb, g1b)
                        TTop("dve", g1, pse[:, :], g1, ALU.subtract, pseb + g1b, g1b)
                        ACT(g1, g1, AF.Sqrt, g1b, g1b, bias=EPS_GN, scale=1.0)
                        P.op("dve", lambda e, g1=g1: e.reciprocal(out=g1, in_=g1), g1b, g1b)
                        g2, g2b = Tt(1)
                        TTop("dve", g2, yv, psm[:, :], ALU.subtract, [B_y[lp]] + psmb, g2b)
                        TTop("pool", g2, g2, g1, ALU.mult, g2b + g1b, g2b)
                        ACT(g2, g2, AF.Identity, g2b + [B_const], g2b, bias=cvc("lnx_b", pr), scale=cvc("lnx_g", pr))
                        TTop("pool", g2, g2, bonus[:, lp, :], ALU.add, g2b + [B_bonus[lp]], g2b)
                        psg, psgb = ps_alloc(512)
                        MM(psg[:, :], lorab[0:96, 2, pc], lob[0:96, 2, :], True, True, [B_const, B_lob[2]], psgb)
                        TTop("dve", actB[:, 4 + pr, :], g2, psg[:, :], ALU.mult, g2b + psgb, [B_Bc[4 + pr]])

                P.phase = "w_out"
                def ev_res(cb, ps, psb):
                    TTop("dve", xr[:, cb, :], xr[:, cb, :], ps[:, :], ALU.add, [xb[cb]] + psb, [xb[cb]])
                proj_typeA([6, 7], lambda kc: actB[:, kc, :], B_Bc, 8, TT, ev_res)

                P.phase = "xattn"
                rmsnorm(lambda c: xr[:, c, :], xb, "g_cross", lambda c: actA[:, c, :], B_A, TT)

                def ev_q(cb, ps, psb):
                    CP("act" if cb % 2 else "dve", actB[:, cb, :], ps[:, :], psb, [B_Bc[cb]])
                proj_typeA([8, 9], lambda kc: actA[:, kc, :], B_A, 8, TT, ev_q)
                for hd in range(4):
                    for mc in range(2):
                        ps, psb = ps_alloc(512)
                        for dc in range(2):
                            MM(ps[:, :], KT[:, 2 * hd + dc, mc * 128:(mc + 1) * 128], actB[:, 2 * hd + dc, :],
                               dc == 0, dc == 1, [B_KT, B_Bc[2 * hd + dc]], psb)
                        ACT(PT[:, mc, :], ps[:, :], AF.Exp, psb, [B_PT[mc]], scale=1.0 / 16.0)
                    ps, psb = ps_alloc(512)
                    for mc in range(2):
                        MM(ps[:, :], onesb[:], PT[:, mc, :], mc == 0, mc == 1, [B_const, B_PT[mc]], psb)
                    P.op("dve", lambda e, ps=ps: e.reciprocal(out=rsb[:], in_=ps[:, :]), psb, [B_rs])
                    for dvc in range(2):
                        ch = 2 * hd + dvc
                        ps, psb = ps_alloc(512)
                        for mc in range(2):
                            MM(ps[:, :], Vt[:, mc, ch * 128:(ch + 1) * 128], PT[:, mc, :], mc == 0, mc == 1,
                               [B_V, B_PT[mc]], psb)
                        TTop("dve", actA[:, ch, :], ps[:, :], rsb[:], ALU.mult, psb + [B_rs], [B_A[ch]])
                proj_typeA([10, 11], lambda kc: actA[:, kc, :], B_A, 8, TT, ev_res)

                P.phase = "ffn"
                rmsnorm(lambda c: xr[:, c, :], xb, "g_ffn", lambda c: actB[:, c, :], B_Bc, TT)

                def ev_ff1(cb, ps, psb):
                    f, fb = fT(cb)
                    ACT(f, ps[:, :], AF.Relu, psb, fb)
                    TTop("pool" if cb % 2 else "dve", f, f, f, ALU.mult, fb, fb)
                proj_typeA(list(range(12, 20)), lambda kc: actB[:, kc, :], B_Bc, 32, TT, ev_ff1)
                for cb in range(8):
                    ws, wsb = w_next(20 + cb)
                    ps, psb = ps_alloc(512)
                    for kc in range(32):
                        f, fb = fT(kc)
                        MM(ps[:, :], ws[:, kc * 128:(kc + 1) * 128], f, kc == 0, kc == 31, [wsb] + fb, psb)
                    ev_res(cb, ps, psb)

                P.phase = "final"
                rmsnorm(lambda c: xr[:, c, :], xb, "g_final", lambda c: xr[:, c, :], xb, TT)
                P.dma("sp", lambda e, s=s, t0=t0, xr=xr: e.dma_start(
                    out=oT[s].rearrange("(c p) t -> p c t", p=128)[:, :, t0:t0 + TT], in_=xr[:]), s_o[xi], reads=xb)

        P.wait_all("sp", B_x[0] + B_x[1])
        P.emit()
    return nc


def prep_inputs(inputs, nseq_per_core, ncores, seqlen):
    x = np.asarray(inputs["x"], np.float32)
    mem = np.asarray(inputs["mem"], np.float32)
    cv = pack_cvec(inputs)
    blocks = pack_blocks(inputs)
    lora = pack_lora(inputs)
    in_maps = []
    for c in range(ncores):
        sl = slice(c * nseq_per_core, (c + 1) * nseq_per_core)
        in_maps.append({
            "xT": np.ascontiguousarray(x[sl, :seqlen].transpose(0, 2, 1)),
            "memT": np.ascontiguousarray(mem[sl].transpose(0, 2, 1)),
            "wblk": blocks, "cvec": cv, "lora": lora,
        })
    return in_maps


def kernel(**inputs):
    nseq = BATCH // NCORES
    nc = build(nseq, SEQ)
    in_maps = prep_inputs(inputs, nseq, NCORES, SEQ)
    res = run_bass_kernel_spmd(nc, in_maps, core_ids=list(range(NCORES)))
    outs = [np.asarray(r["oT"]).transpose(0, 2, 1) for r in res.results]
    return np.ascontiguousarray(np.concatenate(outs, axis=0)).astype(np.float32)
```

```python
import contextlib
import numpy as np
import concourse.bass as bass
import concourse.mybir as mybir
from concourse.bass_utils import run_bass_kernel_spmd

F32 = mybir.dt.float32
BF16 = mybir.dt.bfloat16
ALU = mybir.AluOpType
AF = mybir.ActivationFunctionType

D = 1024
SEQ = 2048
BATCH = 32
NCORES = 8
TT = 512
MEM = 256
CW = 512
RW = 512
CONV_K = 31
HIST = CONV_K - 1
DFF = 4096
NBLK = 36
NHOST = 32
BLK = 4096
NSLOT = 3
NSET = 2
DEC = 0.6065306597126334

ENGS = ("pe", "act", "dve", "pool", "sp")


class Buf:
    __slots__ = ("name", "w", "r", "excl")

    def __init__(self, name="", excl=False):
        self.name = name
        self.w = None
        self.r = []
        self.excl = excl


class Prog:
    tile = 0

    @property
    def phase(self):
        return "t%d:%s" % (self.tile, self._phase)

    @phase.setter
    def phase(self, v):
        self._phase = v

    def __init__(self, nc, stack):
        self.nc = nc
        self.stack = stack
        self.ops = {e: [] for e in ENGS}
        self.cnt = {e: 0 for e in ENGS}
        self.waited = {e: {} for e in ENGS}
        self.sems = {}
        self.semval = {}
        for e in ENGS:
            self.sems[e] = stack.enter_context(nc.semaphore("s_" + e))
        self.n_dma_sems = 0
        self.phase = "init"
        self.annotate = False
        self.hazard = 10 ** 9

    def dma_sem(self, name=None):
        key = "dma%d" % self.n_dma_sems
        self.n_dma_sems += 1
        self.sems[key] = self.stack.enter_context(self.nc.semaphore(name or key))
        self.semval[key] = 0
        return key

    def _deps(self, eng, reads, writes):
        need = {}

        def add(dep):
            if dep is None:
                return
            k, v = dep
            if need.get(k, -1) < v:
                need[k] = v
        for b in reads:
            add(b.w)
        for b in writes:
            add(b.w)
            for d in b.r:
                add(d)
        out = []
        wd = self.waited[eng]
        for k, v in need.items():
            if k == eng:
                if eng in ("pe", "sp") or v <= self.cnt[eng] - self.hazard:
                    continue
            if wd.get(k, -1) >= v:
                continue
            wd[k] = v
            out.append((k, v))
        return out

    def _record(self, me, reads, writes):
        for b in reads:
            if len(b.r) > 24:
                best = {}
                for k, v in b.r:
                    if best.get(k, -1) < v:
                        best[k] = v
                b.r = list(best.items())
            b.r.append(me)
        for b in writes:
            b.w = me
            b.r = []

    @staticmethod
    def _split(reads, writes):
        if any(b.excl for b in reads):
            writes = list(writes) + [b for b in reads if b.excl]
            reads = [b for b in reads if not b.excl]
        return reads, writes

    def op(self, eng, fn, reads=(), writes=()):
        reads, writes = self._split(reads, writes)
        waits = self._deps(eng, reads, writes)
        self.cnt[eng] += 1
        me = (eng, self.cnt[eng])
        self.ops[eng].append((fn, waits, (eng, 1), self.phase))
        self._record(me, reads, writes)
        return me

    def dma(self, eng, fn, semkey, reads=(), writes=()):
        reads, writes = self._split(reads, writes)
        waits = self._deps(eng, reads, writes)
        self.semval[semkey] += 16
        me = (semkey, self.semval[semkey])
        self.ops[eng].append((fn, waits, (semkey, 16), self.phase))
        self._record(me, reads, writes)
        return me

    def wait_all(self, eng, bufs):
        waits = self._deps(eng, [], bufs)
        self.ops[eng].append((None, waits, None, self.phase))

    def emit(self):
        nc = self.nc
        handles = {"pe": "tensor", "act": "scalar", "dve": "vector", "pool": "gpsimd", "sp": "sync"}
        with nc.Block() as block:
            for e in ENGS:
                ops = self.ops[e]
                if not ops:
                    continue

                def body(engh, ops=ops):
                    for fn, waits, inc, phase in ops:
                        for k, v in waits:
                            engh.wait_ge(self.sems[k], v)
                        if fn is not None:
                            ins = fn(engh)
                            if inc is not None:
                                ins.then_inc(self.sems[inc[0]], inc[1])
                            if self.annotate:
                                ins.annotate(phase)
                getattr(block, handles[e])(body)


CV = {}
_off = 0
for _n, _w in [("g_mix", 8), ("g_cross", 8), ("g_mem", 8), ("g_ffn", 8), ("g_final", 8),
               ("conv_b", 4), ("ln_g", 4), ("ln_b", 4), ("conv_w", 124), ("mu", 15),
               ("w0", 4), ("a0", 4), ("k_k", 4), ("k_a", 4), ("r_k", 4), ("lnx_g", 4), ("lnx_b", 4)]:
    CV[_n] = _off
    _off += _w
NCV = _off


def _cm(v, nch):
    return np.ascontiguousarray(np.asarray(v, np.float32).reshape(nch, 128).T)


def pack_cvec(inp):
    cv = np.zeros((128, NCV), np.float32)

    def put(name, arr):
        cv[:, CV[name]:CV[name] + arr.shape[1]] = arr
    put("g_mix", _cm(inp["g_mix"][0], 8))
    put("g_cross", _cm(inp["g_cross"][0], 8))
    put("g_mem", _cm(inp["g_mem"][0], 8))
    put("g_ffn", _cm(inp["g_ffn"][0], 8))
    put("g_final", _cm(inp["g_final"], 8))
    put("conv_b", _cm(inp["conv_b"][0], 4))
    put("ln_g", _cm(inp["conv_ln_g"][0], 4))
    put("ln_b", _cm(inp["conv_ln_b"][0], 4))
    cw = np.asarray(inp["conv_w"][0], np.float32)
    cwp = cw.reshape(CONV_K, 4, 128).transpose(2, 0, 1)
    put("conv_w", np.ascontiguousarray(cwp.reshape(128, CONV_K * 4)))
    mu = np.asarray(inp["mu_b"][0], np.float32)
    m = np.zeros((128, 15), np.float32)
    m[:, 0:4] = _cm(mu[0:512], 4)
    m[:, 4:8] = _cm(mu[512:1024], 4)
    m[:, 8:12] = _cm(mu[1024:1536], 4)
    m[0:32, 12] = mu[1536:1568]
    m[0:32, 13] = mu[1568:1600]
    m[0:96, 14] = mu[1600:1696]
    put("mu", m)
    for nm in ("w0", "a0", "k_k", "k_a", "r_k", "lnx_g", "lnx_b"):
        put(nm, _cm(inp[nm][0], 4))
    return cv


def pack_blocks(inp):
    out = np.zeros((NHOST, 128, BLK), np.float32)

    def typeA(W, cols):
        blk = np.zeros((128, 4, 8, 128), np.float32)
        Wr = W.reshape(8, 128, -1)
        for cb, (st, wd) in enumerate(cols):
            blk[:, cb, :, :wd] = Wr[:, :, st:st + wd].transpose(1, 0, 2)
        return blk.reshape(128, BLK)

    w_in = np.asarray(inp["w_in"][0], np.float32)
    out[0] = typeA(w_in, [(0, 128), (512, 128), (128, 128), (640, 128)])
    out[1] = typeA(w_in, [(256, 128), (768, 128), (384, 128), (896, 128)])
    for j, base in enumerate((1024, 1536, 2048)):
        out[2 + j] = typeA(w_in, [(base + 128 * i, 128) for i in range(4)])
    out[5] = typeA(w_in, [(2560, 32), (2592, 32), (2624, 96)])
    for j, nm in enumerate(("w_out", "wq", "wo")):
        W = np.asarray(inp[nm][0], np.float32)
        for h in range(2):
            out[6 + 2 * j + h] = typeA(W, [(512 * h + 128 * i, 128) for i in range(4)])
    W = np.asarray(inp["w_ff1"][0], np.float32)
    for b in range(8):
        out[12 + b] = typeA(W, [(512 * b + 128 * i, 128) for i in range(4)])
    W = np.asarray(inp["w_ff2"][0], np.float32).reshape(32, 128, 1024)
    for b in range(8):
        out[20 + b] = W[:, :, 128 * b:128 * (b + 1)].transpose(1, 0, 2).reshape(128, BLK)
    W = np.asarray(inp["wk"][0], np.float32)
    for h in range(2):
        out[28 + h] = typeA(W, [(512 * h + 128 * i, 128) for i in range(4)])
    W = np.asarray(inp["wv"][0], np.float32).reshape(8, 128, 1024)
    for h in range(2):
        out[30 + h] = W[:, :, 512 * h:512 * (h + 1)].transpose(1, 0, 2).reshape(128, BLK)
    return out


def pack_lora(inp):
    lw = np.zeros((128, 3, 512), np.float32)
    lw[0:32, 0] = inp["w_decay2"][0]
    lw[0:32, 1] = inp["a_lora2"][0]
    lw[0:96, 2] = inp["g_lora2"][0]
    return lw


def build(nseq, seqlen, dbg=False, annotate=False):
    NT = seqlen // TT
    nc = bass.Bass("TRN2", target_bir_lowering=False)
    xT = nc.dram_tensor("xT", [nseq, D, seqlen], F32, kind="ExternalInput").ap()
    memT = nc.dram_tensor("memT", [nseq, D, MEM], F32, kind="ExternalInput").ap()
    wblk = nc.dram_tensor("wblk", [NHOST, 128, BLK], F32, kind="ExternalInput").ap()
    cvd = nc.dram_tensor("cvec", [128, NCV], F32, kind="ExternalInput").ap()
    lwd = nc.dram_tensor("lora", [128, 3, 512], F32, kind="ExternalInput").ap()
    oT = nc.dram_tensor("oT", [nseq, D, seqlen], F32, kind="ExternalOutput").ap()
    scr = nc.dram_tensor("wscr", [NBLK, 128, BLK], BF16).ap()
    dbg_out = {}

    with contextlib.ExitStack() as st:
        P = Prog(nc, st)
        P.annotate = annotate

        def sb(name, shape, dt):
            return st.enter_context(nc.sbuf_tensor(name, shape, dt))

        cv = sb("cv", [128, NCV], F32)
        cvx = sb("cvx", [128, 32], F32)
        ident = sb("ident", [128, 128], BF16)
        ident4 = sb("ident4", [128, 4, 128], BF16)
        onesb = sb("onesb", [128, 128], BF16)
        blk1 = sb("blk1", [128, 128], BF16)
        blk64 = sb("blk64", [128, 128], BF16)
        o512 = sb("o512", [128, 128], BF16)
        mSU = sb("mSU", [128, 4, 128], BF16)
        mIU = sb("mIU", [128, 4, 128], BF16)
        mSL = sb("mSL", [128, 4, 128], BF16)
        rmask = sb("rmask", [128, TT], BF16)
        lorab = sb("lorab", [128, 3, 512], BF16)
        S32 = sb("S32", [128, 4, 64], F32)
        Sbf = sb("Sbf", [128, 4, 64], BF16)
        tmpS = sb("tmpS", [128, 4, 64], F32)
        carry = sb("carry", [128, 16], F32)
        KT = sb("KT", [128, 8, MEM], BF16)
        Vt = sb("Vt", [128, 2, D], BF16)
        wslot = [sb("wslot%d" % i, [128, BLK], BF16) for i in range(NSLOT)]
        xres = [sb("xres%d" % i, [128, 8, TT], F32) for i in range(2)]
        actA = sb("actA", [128, 8, TT], BF16)
        actB = sb("actB", [128, 8, TT], BF16)
        arena = sb("arena", [128, 32 * 512], BF16)
        lob = sb("lob", [128, 3, TT], BF16)
        Bbuf = [sb("Bbuf%d" % i, [128, TT + 1], F32) for i in range(2)]
        ubuf = sb("ubuf", [128, 4, HIST + TT], BF16)
        cln = sb("cln", [128, 2, TT], F32)
        cacc = sb("cacc", [128, 4, TT], F32)
        opa_h = [sb("opa%d" % h, [128, 2, 2, TT], BF16) for h in range(2)]
        opr_h = [sb("opr%d" % h, [128, 2, 2, TT], BF16) for h in range(2)]
        opb0 = sb("opb", [128, 2, TT], BF16)
        opk0 = sb("opk", [128, 2, TT], BF16)
        tok0 = sb("tok", [128, 3, 2, 4, 128], BF16)
        bonus = sb("bonus", [128, 4, TT], BF16)
        ybuf = sb("ybuf", [128, 2, TT], F32)
        wcb = sb("wcb", [128, 4, 4], F32)
        MATN = ("X0", "X1", "XT0", "XT1", "P0", "P1", "Aak", "Arb", "Ark")
        matsets = [{nm: sb("m%d_%s" % (i, nm), [128, 4, 128], BF16) for nm in MATN} for i in range(NSET)]
        RHSb = sb("RHSb", [128, 2, 2, 64], BF16)
        Ub = sb("Ub", [128, 2, 2, 64], BF16)
        PT = sb("PT", [128, 2, TT], BF16)
        rsb = sb("rsb", [128, TT], F32)
        sqr = [sb("sqr%d" % i, [128, TT], BF16) for i in range(2)]
        psum = [st.enter_context(nc.psum_tensor("ps%d" % i, [128, 512], F32)) for i in range(8)]

        B_const = Buf("const")
        B_S32 = [Buf("S32_%d" % i) for i in range(4)]
        B_Sbf = [Buf("Sbf_%d" % i) for i in range(4)]
        B_tmpS = [Buf("tmpS%d" % i) for i in range(4)]
        B_carry = [Buf("carry%d" % i) for i in range(16)]
        B_KT = Buf("KT")
        B_V = Buf("V")
        B_wslot = [Buf("wslot%d" % i) for i in range(NSLOT)]
        B_x = [[Buf("x%d_%d" % (i, c)) for c in range(8)] for i in range(2)]
        B_A = [Buf("actA%d" % c) for c in range(8)]
        B_Bc = [Buf("actB%d" % c) for c in range(8)]
        B_ar = [Buf("ar%d" % i) for i in range(32)]
        B_lob = [Buf("lob%d" % i) for i in range(3)]
        B_Bbuf = [Buf("Bbuf0"), Buf("Bbuf1")]
        B_cln = [Buf("cln0"), Buf("cln1")]
        B_u = [Buf("u%d" % c) for c in range(4)]
        B_cacc = [Buf("cacc%d" % c) for c in range(4)]
        B_opa_h = [[Buf("opa%d_%d" % (h, lp)) for lp in range(2)] for h in range(2)]
        B_opr_h = [[Buf("opr%d_%d" % (h, lp)) for lp in range(2)] for h in range(2)]
        B_bonus = [Buf("bonus%d" % lp) for lp in range(4)]
        B_y = [Buf("y%d" % lp) for lp in range(2)]
        B_wc = [Buf("wc%d" % lp) for lp in range(4)]
        B_msets = [{nm: Buf("m%d_%s" % (i, nm)) for nm in MATN} for i in range(NSET)]
        B_RHS = [Buf("RHS%d" % lp) for lp in range(2)]
        B_U = [Buf("U%d" % lp) for lp in range(2)]
        B_PT = [Buf("PT%d" % i) for i in range(2)]
        B_rs = Buf("rs")
        B_sqr = [Buf("sqr%d" % i) for i in range(2)]
        B_ps = [Buf("psb%d" % i, excl=True) for i in range(8)]
        B_scr = [Buf("scr%d" % i) for i in range(NBLK)]

        cln_bf = cln[:].rearrange("p a b -> p (a b)").bitcast(BF16)
        cacc_bf = cacc[:].rearrange("p a b -> p (a b)").bitcast(BF16)
        opb_h = [opb0, cln_bf[:, 0:1024].rearrange("p (a b) -> p a b", b=TT)]
        opk_h = [opk0, cln_bf[:, 1024:2048].rearrange("p (a b) -> p a b", b=TT)]
        tok_h = [tok0, cacc_bf[:, 0:3072].rearrange("p (k l c n) -> p k l c n", k=3, l=2, c=4)]
        B_opb_h = [[Buf("opb0_%d" % lp) for lp in range(2)], [B_cln[0], B_cln[0]]]
        B_opk_h = [[Buf("opk0_%d" % lp) for lp in range(2)], [B_cln[1], B_cln[1]]]
        B_tok_h = [[[Buf("tok0_%d_%d" % (k, lp)) for lp in range(2)] for k in range(3)],
                   [[B_cacc[k], B_cacc[k]] for k in range(3)]]

        def zrkv(j):
            return arena[:, j * 512:(j + 1) * 512], [B_ar[j]]

        def Tt(i):
            o = (12 + 2 * i) * 512
            return arena[:, o:o + 1024].bitcast(F32), [B_ar[12 + 2 * i], B_ar[13 + 2 * i]]

        def fT(j):
            return arena[:, j * 512:(j + 1) * 512], [B_ar[j]]

        ps_pos = [0]

        def ps_alloc(ncols):
            p = ps_pos[0]
            ps_pos[0] = (p + 1) % 8
            return psum[p][:, 0:ncols], [B_ps[p]]

        s_const = P.dma_sem("c")
        s_w = [P.dma_sem("w%d" % i) for i in range(NSLOT)]
        s_x = [P.dma_sem("x%d" % i) for i in range(2)]
        s_o = [P.dma_sem("o%d" % i) for i in range(2)]
        s_sw = [P.dma_sem("sw%d" % i) for i in range(NSLOT)]
        s_mem = P.dma_sem("mem")
        s_wp = [P.dma_sem("wp%d" % i) for i in range(NSLOT)]

        def ACT(out, in_, func, reads, writes, bias=None, scale=None):
            kw = {}
            if bias is not None:
                kw["bias"] = bias
            if scale is not None:
                kw["scale"] = scale
            P.op("act", lambda e: e.activation(out=out, in_=in_, func=func, **kw), reads, writes)

        def TTop(eng, out, in0, in1, op, reads, writes):
            P.op(eng, lambda e: e.tensor_tensor(out=out, in0=in0, in1=in1, op=op), reads, writes)

        def TS(eng, out, in0, s1, s2, op0, op1, reads, writes):
            if s2 is None:
                P.op(eng, lambda e: e.tensor_scalar(out=out, in0=in0, scalar1=s1, scalar2=None, op0=op0), reads, writes)
            else:
                P.op(eng, lambda e: e.tensor_scalar(out=out, in0=in0, scalar1=s1, scalar2=s2, op0=op0, op1=op1), reads, writes)

        def STT(eng, out, in0, scalar, in1, op0, op1, reads, writes):
            P.op(eng, lambda e: e.scalar_tensor_tensor(out=out, in0=in0, scalar=scalar, in1=in1, op0=op0, op1=op1), reads, writes)

        def CP(eng, out, in_, reads, writes):
            if eng == "act":
                P.op("act", lambda e: e.copy(out=out, in_=in_), reads, writes)
            else:
                P.op(eng, lambda e: e.tensor_copy(out=out, in_=in_), reads, writes)

        def MM(out, lhsT, rhs, start, stop, reads, writes, tp=None):
            if tp is None:
                P.op("pe", lambda e: e.matmul(out, lhsT=lhsT, rhs=rhs, start=start, stop=stop), reads, writes)
            else:
                P.op("pe", lambda e: e.matmul(out, lhsT=lhsT, rhs=rhs, start=start, stop=stop, tile_position=tp), reads, writes)

        def MEMSET(eng, ap, val, writes):
            P.op(eng, lambda e: e.memset(ap, val), (), writes)

        def AFSEL(out, in_, pattern, cmp, base, cm, reads, writes):
            P.op("pool", lambda e: e.affine_select(out=out, in_=in_, pattern=pattern, compare_op=cmp, fill=0.0,
                                                   base=base, channel_multiplier=cm), reads, writes)

        def cvc(name, j=0, rows=slice(0, 128)):
            o = CV[name] + j
            return cv[rows, o:o + 1]

        P.dma("sp", lambda e: e.dma_start(out=cv[:], in_=cvd), s_const, writes=[B_const])
        lw32, lwB = Tt(0)
        lw32b, lwBb = Tt(1)
        lw32c, lwBc = Tt(2)
        for j, (tv, tb) in enumerate(((lw32, lwB), (lw32b, lwBb), (lw32c, lwBc))):
            P.dma("sp", lambda e, tv=tv, j=j: e.dma_start(out=tv, in_=lwd[:, j, :]), s_const, writes=tb)
        for b_ in lwB + lwBb + lwBc + [B_const]:
            b_.w = (s_const, P.semval[s_const])
        for j, (tv, tb) in enumerate(((lw32, lwB), (lw32b, lwBb), (lw32c, lwBc))):
            CP("dve", lorab[:, j, :], tv, tb, [B_const])
        TS("dve", cvx[:, 0:15], cv[:, CV["mu"]:CV["mu"] + 15], -1.0, 1.0, ALU.mult, ALU.add, [B_const], [B_const])
        TS("dve", cvx[:, 15:19], cv[:, CV["k_a"]:CV["k_a"] + 4], -1.0, 1.0, ALU.mult, ALU.add, [B_const], [B_const])
        MEMSET("pool", cvx[:, 19:20], 1e-6, [B_const])
        MEMSET("pool", cvx[:, 20:21], 1e-5, [B_const])
        MEMSET("pool", cvx[:, 21:22], 64e-5, [B_const])
        EPS_RMS, EPS_LN, EPS_GN = cvx[:, 19:20], cvx[:, 20:21], cvx[:, 21:22]
        t3, t3b = Tt(3)
        m32 = t3[:, 0:128]
        MEMSET("pool", m32, 1.0, t3b)
        AFSEL(m32, m32, [[-1, 128]], ALU.is_equal, 0, 1, t3b, t3b)
        CP("pool", ident[:], m32, t3b, [B_const])
        for h in range(4):
            CP("pool", ident4[:, h, :], m32, t3b, [B_const])
        for msk, cmp in ((mSU, ALU.is_gt), (mIU, ALU.is_ge)):
            MEMSET("pool", m32, 1.0, t3b)
            AFSEL(m32, m32, [[1, 128]], cmp, 0, -1, t3b, t3b)
            for h in range(4):
                CP("pool", msk[:, h, :], m32, t3b, [B_const])
        MEMSET("pool", m32, 1.0, t3b)
        AFSEL(m32, m32, [[-1, 128]], ALU.is_gt, 0, 1, t3b, t3b)
        for h in range(4):
            CP("pool", mSL[:, h, :], m32, t3b, [B_const])
        for h in range(2):
            MEMSET("pool", opa_h[h][:], 0.0, B_opa_h[h])
            MEMSET("pool", opr_h[h][:], 0.0, B_opr_h[h])
        MEMSET("pool", onesb[:], 1.0, [B_const])
        MEMSET("pool", o512[:], 1.0 / 512.0, [B_const])
        MEMSET("pool", blk1[:], 0.0, [B_const])
        MEMSET("pool", blk1[0:64, 0:64], 1.0, [B_const])
        MEMSET("pool", blk1[64:128, 64:128], 1.0, [B_const])
        MEMSET("pool", blk64[:], 0.0, [B_const])
        MEMSET("pool", blk64[0:64, 0:64], 1.0 / 64.0, [B_const])
        MEMSET("pool", blk64[64:128, 64:128], 1.0 / 64.0, [B_const])
        MEMSET("pool", rmask[:], 1.0, [B_const])
        for c in range(TT // 128):
            MEMSET("pool", rmask[:, c * 128:c * 128 + 1], 0.0, [B_const])

        wseq = []
        for s in range(nseq):
            for ti in range(NT):
                if ti == 0:
                    wseq += [28, 29, 30, 31]
                wseq += [0, 1, 2, 3, 4, 5, 32, 33, 34, 35] + list(range(6, 28))
        wstate = {"issued": 0, "used": 0}

        FIRST_PASS = 36
        cwo_ = CV["conv_w"]

        def w_issue_upto(n):
            while wstate["issued"] < min(n, len(wseq)):
                i = wstate["issued"]
                si = i % NSLOT
                blk = wseq[i]
                if i < FIRST_PASS:
                    if blk < NHOST:
                        P.dma("pool", lambda e, si=si, blk=blk: e.dma_start(out=wslot[si][:], in_=wblk[blk]), s_wp[si],
                              writes=[B_wslot[si]])
                    else:
                        c = blk - NHOST
                        MEMSET("dve", wslot[si][:, CONV_K * 128:BLK], 0.0, [B_wslot[si]])
                        for k in range(CONV_K):
                            TS("dve", wslot[si][:, k * 128:(k + 1) * 128], ident[:],
                               cv[:, cwo_ + k * 4 + c:cwo_ + k * 4 + c + 1], None, ALU.mult, None, [B_const],
                               [B_wslot[si]] if k == CONV_K - 1 else [])
                    P.dma("sp", lambda e, si=si, blk=blk: e.dma_start(out=scr[blk], in_=wslot[si][:]), s_sw[si],
                          reads=[B_wslot[si]], writes=[B_scr[blk]])
                else:
                    P.dma("sp", lambda e, si=si, blk=blk: e.dma_start(out=wslot[si][:], in_=scr[blk]), s_w[si],
                          reads=[B_scr[blk]], writes=[B_wslot[si]])
                wstate["issued"] += 1

        def w_next(expect):
            i = wstate["used"]
            assert wseq[i] == expect, (i, wseq[i], expect)
            w_issue_upto(i + NSLOT)
            wstate["used"] += 1
            si = i % NSLOT
            return wslot[si], B_wslot[si]

        def wA(ws, cb, kc, m=128):
            o = (cb * 8 + kc) * 128
            return ws[:, o:o + m]

        rr = {"sq": 0, "eng": 0}

        def rmsnorm(src, src_b, gname, dst, dst_b, n, out_f32_inplace=False):
            ps, psb = ps_alloc(512)
            for c in range(8):
                q = rr["sq"] % 2
                rr["sq"] += 1
                ACT(sqr[q][:, 0:n], src(c), AF.Square, [src_b[c]], [B_sqr[q]])
                MM(ps[:, 0:n], onesb[:], sqr[q][:, 0:n], c == 0, c == 7, [B_sqr[q], B_const], psb)
            ACT(rsb[:, 0:n], ps[:, 0:n], AF.Sqrt, psb, [B_rs], bias=EPS_RMS, scale=1.0 / D)
            P.op("dve", lambda e: e.reciprocal(out=rsb[:, 0:n], in_=rsb[:, 0:n]), [B_rs], [B_rs])
            for c in range(8):
                STT("dve", dst(c), src(c), cvc(gname, c), rsb[:, 0:n], ALU.mult, ALU.mult,
                    [src_b[c], B_rs, B_const], [dst_b[c]])

        def proj_typeA(blocks, rhs, rhs_b, ncb_total, n, evac):
            cb_glob = 0
            for blk in blocks:
                ws, wsb = w_next(blk)
                for cb in range(4):
                    if cb_glob >= ncb_total:
                        break
                    ps, psb = ps_alloc(512)
                    for kc in range(8):
                        MM(ps[:, 0:n], wA(ws, cb, kc), rhs(kc), kc == 0, kc == 7, [wsb, rhs_b[kc]], psb)
                    evac(cb_glob, ps, psb)
                    cb_glob += 1

        def seq_prologue(s):
            MEMSET("pool", S32[:], 0.0, B_S32)
            MEMSET("pool", Sbf[:], 0.0, B_Sbf)
            MEMSET("pool", carry[:], 0.0, B_carry)
            for c in range(4):
                MEMSET("pool", ubuf[:, c, 0:HIST], 0.0, [B_u[c]])
            P.phase = "memkv"
            memv = cacc[:].rearrange("p a b -> p (a b)")[:, 0:8 * MEM].rearrange("p (a b) -> p a b", b=MEM)
            P.dma("sp", lambda e, s=s: e.dma_start(out=memv, in_=memT[s].rearrange("(c p) m -> p c m", p=128)),
                  s_mem, writes=B_cacc)
            rmsnorm(lambda c: memv[:, c, :], [B_cacc[c // 2] for c in range(8)], "g_mem",
                    lambda c: actA[:, c, 0:MEM], B_A, MEM)

            def evK(cb, ps, psb):
                CP("act" if cb % 2 else "dve", KT[:, cb, :], ps[:, 0:MEM], psb, [B_KT])
            proj_typeA([28, 29], lambda kc: actA[:, kc, 0:MEM], B_A, 8, MEM, evK)
            for h in range(2):
                ws, wsb = w_next(30 + h)
                for mc in range(2):
                    ps, psb = ps_alloc(512)
                    for kc in range(8):
                        MM(ps[:, :], actA[:, kc, mc * 128:(mc + 1) * 128], ws[:, kc * 512:(kc + 1) * 512],
                           kc == 0, kc == 7, [wsb, B_A[kc]], psb)
                    CP("act" if mc else "dve", Vt[:, mc, h * 512:(h + 1) * 512], ps[:, :], psb, [B_V])


        def tile_gen(s, ti):
            gi = s * NT + ti
            P.tile = gi
            xi = gi % 2
            xr = xres[xi]
            xb = B_x[xi]
            t0 = ti * TT
            P.phase = "norm1"
            P.dma("sp", lambda e, s=s, t0=t0, xr=xr: e.dma_start(
                out=xr[:], in_=xT[s].rearrange("(c p) t -> p c t", p=128)[:, :, t0:t0 + TT]), s_x[xi], writes=xb)
            rmsnorm(lambda c: xr[:, c, :], xb, "g_mix", lambda c: actA[:, c, :], B_A, TT)

            yield "n1"
            P.tile = gi

            P.phase = "w_in"
            pend = {}

            def ev_conv(cb, ps, psb):
                ch, isgate = divmod(cb, 2)
                if not isgate:
                    pend["val"] = (ps, psb)
                    return
                vps, vpsb = pend.pop("val")
                tsg, tsgb = Tt(ch % 2)
                ACT(tsg, ps[:, :], AF.Sigmoid, psb, tsgb)
                TTop("dve", ubuf[:, ch, HIST:HIST + TT], vps[:, :], tsg, ALU.mult, vpsb + tsgb, [B_u[ch]])
            proj_typeA([0, 1], lambda kc: actA[:, kc, :], B_A, 8, TT, ev_conv)

            yield "a"
            P.tile = gi

            def ev_shift(j, m, ps, psb, dst, dst_b):
                q = j % 2
                Bq, Bqb = Bbuf[q], B_Bbuf[q]
                ACT(Bq[0:m, 1:TT + 1], ps[0:m, :], AF.Identity, psb + [B_const], [Bqb], scale=cvc("mu", j, slice(0, m)))
                CP("dve", Bq[0:m, 0:1], carry[0:m, j:j + 1], [B_carry[j]], [Bqb])
                STT("dve", dst, ps[0:m, :], cvx[0:m, j:j + 1], Bq[0:m, 0:TT], ALU.mult, ALU.add,
                    psb + [Bqb, B_const], dst_b)
                CP("dve", carry[0:m, j:j + 1], Bq[0:m, TT:TT + 1], [Bqb], [B_carry[j]])

            def ev_rkv(cb, ps, psb):
                dst, dst_b = zrkv(cb)
                ev_shift(cb, 128, ps, psb, dst, dst_b)
            proj_typeA([2, 3, 4], lambda kc: actA[:, kc, :], B_A, 12, TT, ev_rkv)

            ws, wsb = w_next(5)
            for cb, m in ((0, 32), (1, 32), (2, 96)):
                ps, psb = ps_alloc(512)
                for kc in range(8):
                    MM(ps[0:m, :], wA(ws, cb, kc, m), actA[:, kc, :], kc == 0, kc == 7, [wsb, B_A[kc]], psb)
                tz, tzb = Tt(2 + cb)
                ev_shift(12 + cb, m, ps, psb, tz[0:m, :], tzb)
                func = (AF.Tanh, AF.Copy, AF.Sigmoid)[cb]
                ACT(lob[0:m, cb, :], tz[0:m, :], func, tzb, [B_lob[cb]])

            def conv_chunk(c):
                ws, wsb = w_next(32 + c)
                ps, psb = ps_alloc(512)
                for k in range(CONV_K):
                    MM(ps[:, :], ws[:, k * 128:(k + 1) * 128], ubuf[:, c, k:k + TT], k == 0, k == CONV_K - 1,
                       [wsb, B_u[c]], psb)
                ACT(cacc[:, c, :], ps[:, :], AF.Identity, psb + [B_const], [B_cacc[c]], bias=cvc("conv_b", c))
                CP("dve", ubuf[:, c, 0:HIST], ubuf[:, c, TT:TT + HIST], [B_u[c]], [B_u[c]])

            def conv_ln():
                psm, psmb = ps_alloc(512)
                pse, pseb = ps_alloc(512)
                for c in range(4):
                    q = rr["sq"] % 2
                    rr["sq"] += 1
                    CP("act", sqr[q][:], cacc[:, c, :], [B_cacc[c]], [B_sqr[q]])
                    MM(psm[:, :], o512[:], sqr[q][:], c == 0, c == 3, [B_sqr[q], B_const], psmb)
                    q = rr["sq"] % 2
                    rr["sq"] += 1
                    ACT(sqr[q][:], cacc[:, c, :], AF.Square, [B_cacc[c]], [B_sqr[q]])
                    MM(pse[:, :], o512[:], sqr[q][:], c == 0, c == 3, [B_sqr[q], B_const], pseb)
                tm2, tm2b = cln[:, 0, :], [B_cln[0]]
                ACT(tm2, psm[:, :], AF.Square, psmb, tm2b)
                TTop("dve", tm2, pse[:, :], tm2, ALU.subtract, pseb + tm2b, tm2b)
                ACT(tm2, tm2, AF.Sqrt, tm2b, tm2b, bias=EPS_LN, scale=1.0)
                P.op("dve", lambda e, tm2=tm2: e.reciprocal(out=tm2, in_=tm2), tm2b, tm2b)
                tmean, tmeanb = cln[:, 1, :], [B_cln[1]]
                CP("act", tmean, psm[:, :], psmb, tmeanb)
                for c in range(4):
                    TTop("pool", cacc[:, c, :], cacc[:, c, :], tmean, ALU.subtract, [B_cacc[c]] + tmeanb, [B_cacc[c]])
                    TTop("pool", cacc[:, c, :], cacc[:, c, :], tm2, ALU.mult, [B_cacc[c]] + tm2b, [B_cacc[c]])
                    ACT(actB[:, c, :], cacc[:, c, :], AF.Silu, [B_cacc[c], B_const], [B_Bc[c]],
                        bias=cvc("ln_b", c), scale=cvc("ln_g", c))

            P.phase = "rwkv"
            P.phase = "rwkv_ew"

            def ew_steps(hg, lp, T):
                opa, opr, opb, opk, tok = opa_h[hg], opr_h[hg], opb_h[hg], opk_h[hg], tok_h[hg]
                B_opa, B_opr, B_opb, B_opk, B_tok = B_opa_h[hg], B_opr_h[hg], B_opb_h[hg], B_opk_h[hg], B_tok_h[hg]
                pr = 2 * hg + lp
                pc = slice(pr * 128, (pr + 1) * 128)
                zr, zrb = zrkv(pr)
                zk, zkb = zrkv(4 + pr)
                zv, zvb = zrkv(8 + pr)
                sgw, sgwb = T[0]
                cum, cumb = T[1]
                E1, E1b = T[2]
                av, avb = T[3]
                kk, kkb = T[4]
                rn, rnb = T[5]
                kp, kpb = T[6]
                E3, E3b = sgw, sgwb
                E2, E2b = cum, cumb
                ob, ok = opb[:, lp, :], opk[:, lp, :]
                st = []

                def s1():
                    ps, psb = ps_alloc(512)
                    MM(ps[:, :], lorab[0:32, 0, pc], lob[0:32, 0, :], True, True, [B_const, B_lob[0]], psb)
                    ACT(sgw, ps[:, :], AF.Sigmoid, psb + [B_const], sgwb, bias=cvc("w0", pr))
                    ps, psb = ps_alloc(512)
                    MM(ps[:, :], lorab[0:32, 1, pc], lob[0:32, 1, :], True, True, [B_const, B_lob[1]], psb)
                    ACT(av, ps[:, :], AF.Sigmoid, psb + [B_const], avb, bias=cvc("a0", pr))
                st.append(s1)

                def s2():
                    P.op("dve", lambda e: e.tensor_tensor_scan(
                        out=cum, data0=rmask[:], data1=sgw, initial=0.0, op0=ALU.mult, op1=ALU.add),
                        sgwb + [B_const], cumb)
                    TS("pool", kk, zk, cvc("k_k", pr), None, ALU.mult, None, zkb + [B_const], kkb)
                st.append(s2)

                def s3():
                    ACT(E1, cum, AF.Exp, cumb, E1b, scale=-DEC)
                    TTop("dve", E3, cum, sgw, ALU.subtract, cumb + sgwb, E3b)
                    q = rr["sq"] % 2
                    rr["sq"] += 1
                    ACT(sqr[q][:], kk, AF.Square, kkb, [B_sqr[q]])
                    ps, psb = ps_alloc(512)
                    MM(ps[:, :], blk1[:], sqr[q][:], True, True, [B_const, B_sqr[q]], psb)
                    ACT(rn, ps[:, :], AF.Sqrt, psb, rnb)
                st.append(s3)

                def s4():
                    ACT(E3, E3, AF.Exp, E3b, E3b, scale=-DEC)
                    ACT(E2, cum, AF.Exp, cumb, E2b, scale=DEC)
                    TS("dve", rn, rn, 1e-12, None, ALU.max, None, rnb, rnb)
                    P.op("dve", lambda e: e.reciprocal(out=rn, in_=rn), rnb, rnb)
                    TS("pool", kp, av, cvc("k_a", pr), cvx[:, 15 + pr:16 + pr], ALU.mult, ALU.add,
                       avb + [B_const], kpb)
                st.append(s4)

                def s5():
                    CP("pool", wcb[:, pr, :], E1.rearrange("p (c t) -> p c t", t=128)[:, :, 127], E1b, [B_wc[pr]])
                    TTop("dve", kk, kk, rn, ALU.mult, kkb + rnb, kkb)
                    TTop("pool", kp, kp, zk, ALU.mult, kpb + zkb, kpb)
                st.append(s5)

                def s6():
                    for hf in range(2):
                        rows = slice(hf * 64, (hf + 1) * 64)
                        STT("dve", opa[rows, hf, lp, :], kk[rows, :], -1.0, E3[rows, :], ALU.mult, ALU.mult,
                            kkb + E3b, [B_opa[lp]])
                    TTop("pool", rn, kk, av, ALU.mult, kkb + avb, rnb)
                    TTop("pool", ok, kp, E2, ALU.mult, kpb + E2b, [B_opk[lp]])
                st.append(s6)

                def s7():
                    TTop("dve", ob, rn, E2, ALU.mult, rnb + E2b, [B_opb[lp]])
                    for hf in range(2):
                        rows = slice(hf * 64, (hf + 1) * 64)
                        TTop("pool", opr[rows, hf, lp, :], zr[rows, :], E1[rows, :], ALU.mult, zrb + E1b, [B_opr[lp]])
                    q = rr["sq"] % 2
                    rr["sq"] += 1
                    STT("dve", sqr[q][:], zr, cvc("r_k", pr), kp, ALU.mult, ALU.mult,
                        zrb + kpb + [B_const], [B_sqr[q]])
                    ps, psb = ps_alloc(512)
                    MM(ps[:, :], blk1[:], sqr[q][:], True, True, [B_const, B_sqr[q]], psb)
                    TTop("dve", bonus[:, pr, :], ps[:, :], zv, ALU.mult, psb + zvb, [B_bonus[pr]])
                st.append(s7)

                def s8():
                    for kind, (src, srcb) in enumerate(((ob, [B_opb[lp]]), (ok, [B_opk[lp]]), (zv, zvb))):
                        ps, psb = ps_alloc(256)
                        psv = ps.bitcast(BF16)
                        for c in range(4):
                            P.op("pe", lambda e, psv=psv, src=src, c=c: e.transpose(
                                psv[:, c * 128:(c + 1) * 128], src[:, c * 128:(c + 1) * 128], ident[:]),
                                srcb + [B_const], psb)
                        CP("act" if kind != 1 else "dve",
                           tok[:, kind, lp, :, :].rearrange("p a b -> p (a b)"), psv, psb, [B_tok[kind][lp]])
                st.append(s8)
                return st

            def actA_tmp(i):
                return (actA[:, 2 * i:2 * i + 2, :].rearrange("p a b -> p (a b)").bitcast(F32),
                        [B_A[2 * i], B_A[2 * i + 1]])
            Tset0 = [Tt(i) for i in range(7)]
            Tset1 = [Tt(7), Tt(8), Tt(9)] + [actA_tmp(i) for i in range(4)]

            P.phase = "wkv"

            def chain_stages(hg, c, mats, B_m):
                opa, opr, opb, opk, tok = opa_h[hg], opr_h[hg], opb_h[hg], opk_h[hg], tok_h[hg]
                B_opa, B_opr, B_opb, B_opk, B_tok = B_opa_h[hg], B_opr_h[hg], B_opb_h[hg], B_opk_h[hg], B_tok_h[hg]
                cc = slice(c * 128, (c + 1) * 128)
                fl = lambda t: t[:].rearrange("p a b -> p (a b)")

                def opnd(kind, lp, hf):
                    if kind == "a":
                        return opa[:, hf, lp, cc], B_opa[lp]
                    if kind == "r":
                        return opr[:, hf, lp, cc], B_opr[lp]
                    if kind == "b":
                        return opb[:, lp, cc], B_opb[lp]
                    return opk[:, lp, cc], B_opk[lp]

                def amat(nm, kl, kr_, msk, direct):
                    ps, psb = ps_alloc(512)
                    for hh in range(4):
                        lp, hf = divmod(hh, 2)
                        lh, lhb = opnd(kl, lp, hf)
                        rh, rhb = opnd(kr_, lp, hf)
                        MM(ps[:, hh * 128:(hh + 1) * 128], lh, rh, True, True, [lhb, rhb], psb)
                    if direct:
                        TTop("dve", fl(mats[nm]), ps[:, :], fl(msk), ALU.mult, psb + [B_const], [B_m[nm]])
                    else:
                        CP("act", fl(mats[nm]), ps[:, :], psb, [B_m[nm]])
                        TTop("pool", fl(mats[nm]), fl(mats[nm]), fl(msk), ALU.mult, [B_m[nm], B_const], [B_m[nm]])

                def stA():
                    amat("X0", "b", "a", mSU, True)
                    amat("XT0", "a", "b", mSL, True)
                    TTop("pool", mats["P0"][:], mats["X0"][:], ident4[:], ALU.add, [B_m["X0"], B_const], [B_m["P0"]])
                    amat("Aak", "k", "a", mSU, False)
                    amat("Arb", "b", "r", mIU, False)
                    amat("Ark", "k", "r", mIU, False)

                def stL(lvl):
                    cur = (lvl - 1) % 2
                    Xc, XTc, Pc = "X%d" % cur, "XT%d" % cur, "P%d" % cur
                    Xn, XTn, Pn = "X%d" % (1 - cur), "XT%d" % (1 - cur), "P%d" % (1 - cur)
                    ps2, ps2b = ps_alloc(512)
                    for hh in range(4):
                        MM(ps2[:, hh * 128:(hh + 1) * 128], mats[Xc][:, hh, :], mats[XTc][:, hh, :], True, True,
                           [B_m[Xc], B_m[XTc]], ps2b)
                    if lvl < 6:
                        ps1, ps1b = ps_alloc(512)
                        for hh in range(4):
                            MM(ps1[:, hh * 128:(hh + 1) * 128], mats[XTc][:, hh, :], mats[Xc][:, hh, :], True, True,
                               [B_m[Xc], B_m[XTc]], ps1b)
                    CP("act", fl(mats[XTn]), ps2[:, :], ps2b, [B_m[XTn]])
                    if lvl < 6:
                        CP("dve", fl(mats[Xn]), ps1[:, :], ps1b, [B_m[Xn]])
                    ps3, ps3b = ps_alloc(512)
                    for hh in range(4):
                        MM(ps3[:, hh * 128:(hh + 1) * 128], ident[:], mats[Pc][:, hh, :], True, False,
                           [B_const, B_m[Pc]], ps3b)
                        MM(ps3[:, hh * 128:(hh + 1) * 128], mats[XTn][:, hh, :], mats[Pc][:, hh, :], False, True,
                           [B_m[XTn], B_m[Pc]], ps3b)
                    CP("act" if lvl % 2 else "dve", fl(mats[Pn]), ps3[:, :], ps3b, [B_m[Pn]])

                def stS():
                    Pfin = "P0"
                    hold = []
                    for lp in range(2):
                        pr = 2 * hg + lp
                        bS = B_Sbf[pr]
                        ACT(tmpS[:, pr, :], S32[:, pr, :], AF.Identity, [B_S32[pr], B_wc[pr]], [B_tmpS[pr]],
                            scale=wcb[:, pr, c:c + 1])
                        psR, psRb = ps_alloc(128)
                        for hf in range(2):
                            hh = 2 * lp + hf
                            MM(psR[:, hf * 64:(hf + 1) * 64], opa[:, hf, lp, cc], Sbf[:, pr, :], True, False,
                               [B_opa[lp], bS], psRb)
                            MM(psR[:, hf * 64:(hf + 1) * 64], mats["Aak"][:, hh, :], tok[:, 2, lp, c, hf * 64:(hf + 1) * 64],
                               False, True, [B_m["Aak"], B_tok[2][lp]], psRb)
                        CP("act" if lp else "dve", RHSb[:, lp, :, :].rearrange("p a b -> p (a b)"), psR, psRb, [B_RHS[lp]])
                    for lp in range(2):
                        psU, psUb = ps_alloc(128)
                        for hf in range(2):
                            hh = 2 * lp + hf
                            MM(psU[:, hf * 64:(hf + 1) * 64], mats[Pfin][:, hh, :], RHSb[:, lp, hf, :], True, True,
                               [B_m[Pfin], B_RHS[lp]], psUb)
                        CP("dve" if lp else "act", Ub[:, lp, :, :].rearrange("p a b -> p (a b)"), psU, psUb, [B_U[lp]])
                    for lp in range(2):
                        pr = 2 * hg + lp
                        bS = B_Sbf[pr]
                        psY, psYb = ps_alloc(128)
                        psS, psSb = ps_alloc(128)
                        for hf in range(2):
                            hh = 2 * lp + hf
                            rows = slice(hf * 64, (hf + 1) * 64)
                            vt = tok[:, 2, lp, c, hf * 64:(hf + 1) * 64]
                            MM(psY[rows, :], Sbf[:, pr, :], opr[:, hf, lp, cc], True, False,
                               [bS, B_opr[lp]], psYb, tp=(0, hf * 64))
                            MM(psY[rows, :], Ub[:, lp, hf, :], mats["Arb"][:, hh, :], False, False,
                               [B_U[lp], B_m["Arb"]], psYb, tp=(0, hf * 64))
                            MM(psY[rows, :], vt, mats["Ark"][:, hh, :], False, True,
                               [B_tok[2][lp], B_m["Ark"]], psYb, tp=(0, hf * 64))
                            MM(psS[rows, 0:64], tok[:, 0, lp, c, hf * 64:(hf + 1) * 64], Ub[:, lp, hf, :], True, False,
                               [B_tok[0][lp], B_U[lp]], psSb, tp=(0, hf * 64))
                            MM(psS[rows, 0:64], tok[:, 1, lp, c, hf * 64:(hf + 1) * 64], vt, False, True,
                               [B_tok[1][lp], B_tok[2][lp]], psSb, tp=(0, hf * 64))
                        STT("dve", Sbf[:, pr, :], psS[:, 0:64], wcb[:, pr, c:c + 1], tmpS[:, pr, :], ALU.mult, ALU.add,
                            psSb + [B_wc[pr], B_tmpS[pr]], [bS])
                        STT("dve", S32[:, pr, :], psS[:, 0:64], wcb[:, pr, c:c + 1], tmpS[:, pr, :], ALU.mult, ALU.add,
                            psSb + [B_wc[pr], B_tmpS[pr]], [B_S32[pr]])
                        CP("act", ybuf[:, lp, cc], psY, psYb, [B_y[lp]])
                return [stA] + [(lambda l=l: stL(l)) for l in range(1, 7)] + [stS]

            def run_wkv_all(fillers):
                P.phase = "wkv"
                fillers = list(fillers)
                chains = [chain_stages(i // 4, i % 4, matsets[i % NSET], B_msets[i % NSET]) for i in range(8)]
                STAG = 8 // NSET
                posn = [0] * 8
                step = 0
                while any(p < 8 for p in posn):
                    for i in range(8):
                        if step >= i * STAG and posn[i] < 8:
                            chains[i][posn[i]]()
                            posn[i] += 1
                    if step < len(fillers) and fillers[step] is not None:
                        fillers[step]()
                    step += 1
                for f in fillers[step:]:
                    if f is not None:
                        f()

            def gn_steps(hg):
                st = []
                g1, g1b = Tt(0)
                g2, g2b = Tt(1)
                for lp in range(2):
                    pr = 2 * hg + lp
                    pc = slice(pr * 128, (pr + 1) * 128)
                    yv = ybuf[:, lp, :]

                    def ga(lp=lp, pr=pr, pc=pc, yv=yv):
                        q = rr["sq"] % 2
                        rr["sq"] += 1
                        CP("act", sqr[q][:], yv, [B_y[lp]], [B_sqr[q]])
                        psm, psmb = ps_alloc(512)
                        MM(psm[:, :], blk64[:], sqr[q][:], True, True, [B_const, B_sqr[q]], psmb)
                        q = rr["sq"] % 2
                        rr["sq"] += 1
                        ACT(sqr[q][:], yv, AF.Square, [B_y[lp]], [B_sqr[q]])
                        pse, pseb = ps_alloc(512)
                        MM(pse[:, :], blk64[:], sqr[q][:], True, True, [B_const, B_sqr[q]], pseb)
                        ACT(g1, psm[:, :], AF.Square, psmb, g1b)
                        TTop("dve", g2, yv, psm[:, :], ALU.subtract, [B_y[lp]] + psmb, g2b)
                        TTop("dve", g1, pse[:, :], g1, ALU.subtract, pseb + g1b, g1b)
                        ACT(g1, g1, AF.Sqrt, g1b, g1b, bias=EPS_GN, scale=1.0)
                        P.op("dve", lambda e: e.reciprocal(out=g1, in_=g1), g1b, g1b)

                    def gb(lp=lp, pr=pr, pc=pc):
                        TTop("pool", g2, g2, g1, ALU.mult, g2b + g1b, g2b)
                        ACT(g2, g2, AF.Identity, g2b + [B_const], g2b, bias=cvc("lnx_b", pr), scale=cvc("lnx_g", pr))
                        TTop("pool", g2, g2, bonus[:, pr, :], ALU.add, g2b + [B_bonus[pr]], g2b)
                        psg, psgb = ps_alloc(512)
                        MM(psg[:, :], lorab[0:96, 2, pc], lob[0:96, 2, :], True, True, [B_const, B_lob[2]], psgb)
                        TTop("dve", actB[:, 4 + pr, :], g2, psg[:, :], ALU.mult, g2b + psgb, [B_Bc[4 + pr]])
                    st += [ga, gb]
                return st

            P.phase = "rwkv_ew"
            fill = [(lambda c=c: conv_chunk(c)) for c in range(4)]
            for f0, f1 in zip(ew_steps(0, 0, Tset0), ew_steps(0, 1, Tset1)):
                f0()
                f1()
                if fill:
                    fill.pop(0)()
            conv_ln()
            ew1 = [f for pair in zip(ew_steps(1, 0, Tset0), ew_steps(1, 1, Tset1)) for f in pair]
            assert len(ew1) == 16
            STAG_ = 8 // NSET
            first_hg1 = 4 * STAG_
            last_s_hg0 = 3 * STAG_ + 7
            fl = ew1 + [None] * (last_s_hg0 + 1 - len(ew1)) + gn_steps(0)
            assert len(ew1) <= first_hg1
            run_wkv_all(fl)
            P.phase = "gn"
            for f in gn_steps(1):
                f()

            P.phase = "w_out"
            def ev_res(cb, ps, psb):
                TTop("dve", xr[:, cb, :], xr[:, cb, :], ps[:, :], ALU.add, [xb[cb]] + psb, [xb[cb]])
            proj_typeA([6, 7], lambda kc: actB[:, kc, :], B_Bc, 8, TT, ev_res)

            P.phase = "xattn"
            rmsnorm(lambda c: xr[:, c, :], xb, "g_cross", lambda c: actA[:, c, :], B_A, TT)

            def ev_q(cb, ps, psb):
                CP("act" if cb % 2 else "dve", actB[:, cb, :], ps[:, :], psb, [B_Bc[cb]])
            proj_typeA([8, 9], lambda kc: actA[:, kc, :], B_A, 8, TT, ev_q)
            for hd in range(4):
                for mc in range(2):
                    ps, psb = ps_alloc(512)
                    for dc in range(2):
                        MM(ps[:, :], KT[:, 2 * hd + dc, mc * 128:(mc + 1) * 128], actB[:, 2 * hd + dc, :],
                           dc == 0, dc == 1, [B_KT, B_Bc[2 * hd + dc]], psb)
                    ACT(PT[:, mc, :], ps[:, :], AF.Exp, psb, [B_PT[mc]], scale=1.0 / 16.0)
                ps, psb = ps_alloc(512)
                for mc in range(2):
                    MM(ps[:, :], onesb[:], PT[:, mc, :], mc == 0, mc == 1, [B_const, B_PT[mc]], psb)
                P.op("dve", lambda e, ps=ps: e.reciprocal(out=rsb[:], in_=ps[:, :]), psb, [B_rs])
                for dvc in range(2):
                    ch = 2 * hd + dvc
                    ps, psb = ps_alloc(512)
                    for mc in range(2):
                        MM(ps[:, :], Vt[:, mc, ch * 128:(ch + 1) * 128], PT[:, mc, :], mc == 0, mc == 1,
                           [B_V, B_PT[mc]], psb)
                    TTop("dve", actA[:, ch, :], ps[:, :], rsb[:], ALU.mult, psb + [B_rs], [B_A[ch]])
            proj_typeA([10, 11], lambda kc: actA[:, kc, :], B_A, 8, TT, ev_res)

            P.phase = "ffn"
            rmsnorm(lambda c: xr[:, c, :], xb, "g_ffn", lambda c: actB[:, c, :], B_Bc, TT)

            def ev_ff1(cb, ps, psb):
                f, fb = fT(cb)
                ACT(f, ps[:, :], AF.Relu, psb, fb)
                TTop("pool" if cb % 2 else "dve", f, f, f, ALU.mult, fb, fb)
            proj_typeA(list(range(12, 20)), lambda kc: actB[:, kc, :], B_Bc, 32, TT, ev_ff1)

            yield "b"
            P.tile = gi
            for cb in range(8):
                ws, wsb = w_next(20 + cb)
                ps, psb = ps_alloc(512)
                for kc in range(32):
                    f, fb = fT(kc)
                    MM(ps[:, :], ws[:, kc * 128:(kc + 1) * 128], f, kc == 0, kc == 31, [wsb] + fb, psb)
                ev_res(cb, ps, psb)

            yield "c"
            P.tile = gi
            P.phase = "final"
            rmsnorm(lambda c: xr[:, c, :], xb, "g_final", lambda c: xr[:, c, :], xb, TT)
            P.dma("sp", lambda e, s=s, t0=t0, xr=xr: e.dma_start(
                out=oT[s].rearrange("(c p) t -> p c t", p=128)[:, :, t0:t0 + TT], in_=xr[:]), s_o[xi], reads=xb)
            yield "f"

        order = [(s_, t_) for s_ in range(nseq) for t_ in range(NT)]
        gens = {}

        def start(idx):
            g = tile_gen(*order[idx])
            gens[idx] = g
            next(g)
        prev_final = None
        for idx, (s_, t_) in enumerate(order):
            if t_ == 0:
                seq_prologue(s_)
                start(idx)
            g = gens.pop(idx)
            next(g)
            if prev_final is not None:
                next(prev_final)
                prev_final = None
            next(g)
            if idx + 1 < len(order) and order[idx + 1][1] != 0:
                start(idx + 1)
            next(g)
            prev_final = g
        next(prev_final)
        P.wait_all("sp", B_x[0] + B_x[1])
        P.emit()
    return nc


def prep_inputs(inputs, nseq_per_core, ncores, seqlen):
    x = np.asarray(inputs["x"], np.float32)
    mem = np.asarray(inputs["mem"], np.float32)
    cv = pack_cvec(inputs)
    blocks = pack_blocks(inputs)
    lora = pack_lora(inputs)
    in_maps = []
    for c in range(ncores):
        sl = slice(c * nseq_per_core, (c + 1) * nseq_per_core)
        in_maps.append({
            "xT": np.ascontiguousarray(x[sl, :seqlen].transpose(0, 2, 1)),
            "memT": np.ascontiguousarray(mem[sl].transpose(0, 2, 1)),
            "wblk": blocks, "cvec": cv, "lora": lora,
        })
    return in_maps


def kernel(**inputs):
    nseq = BATCH // NCORES
    nc = build(nseq, SEQ)
    in_maps = prep_inputs(inputs, nseq, NCORES, SEQ)
    res = run_bass_kernel_spmd(nc, in_maps, core_ids=list(range(NCORES)))
    outs = [np.asarray(r["oT"]).transpose(0, 2, 1) for r in res.results]
    return np.ascontiguousarray(np.concatenate(outs, axis=0)).astype(np.float32)
```

```python
import contextlib
import numpy as np
import concourse.bass as bass
import concourse.mybir as mybir
from concourse.bass_utils import run_bass_kernel_spmd

F32 = mybir.dt.float32
BF16 = mybir.dt.bfloat16
ALU = mybir.AluOpType
AF = mybir.ActivationFunctionType

D = 1024
SEQ = 2048
BATCH = 32
NCORES = 8
TT = 512
MEM = 256
CW = 512
RW = 512
CONV_K = 31
HIST = CONV_K - 1
DFF = 4096
NBLK = 36
NHOST = 32
BLK = 4096
NSLOT = 3
NSET = 2
DEC = 0.6065306597126334

ENGS = ("pe", "act", "dve", "pool", "sp")


class Buf:
    __slots__ = ("name", "w", "r", "excl")

    def __init__(self, name="", excl=False):
        self.name = name
        self.w = None
        self.r = []
        self.excl = excl


class Prog:
    tile = 0

    @property
    def phase(self):
        return "t%d:%s" % (self.tile, self._phase)

    @phase.setter
    def phase(self, v):
        self._phase = v

    def __init__(self, nc, stack):
        self.nc = nc
        self.stack = stack
        self.ops = {e: [] for e in ENGS}
        self.cnt = {e: 0 for e in ENGS}
        self.waited = {e: {} for e in ENGS}
        self.sems = {}
        self.semval = {}
        for e in ENGS:
            self.sems[e] = stack.enter_context(nc.semaphore("s_" + e))
        self.n_dma_sems = 0
        self.phase = "init"
        self.annotate = False
        self.hazard = 10 ** 9

    def dma_sem(self, name=None):
        key = "dma%d" % self.n_dma_sems
        self.n_dma_sems += 1
        self.sems[key] = self.stack.enter_context(self.nc.semaphore(name or key))
        self.semval[key] = 0
        return key

    def _deps(self, eng, reads, writes):
        need = {}

        def add(dep):
            if dep is None:
                return
            k, v = dep
            if need.get(k, -1) < v:
                need[k] = v
        for b in reads:
            add(b.w)
        for b in writes:
            add(b.w)
            for d in b.r:
                add(d)
        out = []
        wd = self.waited[eng]
        for k, v in need.items():
            if k == eng:
                if eng in ("pe", "sp") or v <= self.cnt[eng] - self.hazard:
                    continue
            if wd.get(k, -1) >= v:
                continue
            wd[k] = v
            out.append((k, v))
        return out

    def _record(self, me, reads, writes):
        for b in reads:
            if len(b.r) > 24:
                best = {}
                for k, v in b.r:
                    if best.get(k, -1) < v:
                        best[k] = v
                b.r = list(best.items())
            b.r.append(me)
        for b in writes:
            b.w = me
            b.r = []

    @staticmethod
    def _split(reads, writes):
        if any(b.excl for b in reads):
            writes = list(writes) + [b for b in reads if b.excl]
            reads = [b for b in reads if not b.excl]
        return reads, writes

    def op(self, eng, fn, reads=(), writes=()):
        reads, writes = self._split(reads, writes)
        waits = self._deps(eng, reads, writes)
        self.cnt[eng] += 1
        me = (eng, self.cnt[eng])
        self.ops[eng].append((fn, waits, (eng, 1), self.phase))
        self._record(me, reads, writes)
        return me

    def dma(self, eng, fn, semkey, reads=(), writes=()):
        reads, writes = self._split(reads, writes)
        waits = self._deps(eng, reads, writes)
        self.semval[semkey] += 16
        me = (semkey, self.semval[semkey])
        self.ops[eng].append((fn, waits, (semkey, 16), self.phase))
        self._record(me, reads, writes)
        return me

    def wait_all(self, eng, bufs):
        waits = self._deps(eng, [], bufs)
        self.ops[eng].append((None, waits, None, self.phase))

    def emit(self):
        nc = self.nc
        handles = {"pe": "tensor", "act": "scalar", "dve": "vector", "pool": "gpsimd", "sp": "sync"}
        with nc.Block() as block:
            for e in ENGS:
                ops = self.ops[e]
                if not ops:
                    continue

                def body(engh, ops=ops):
                    for fn, waits, inc, phase in ops:
                        for k, v in waits:
                            engh.wait_ge(self.sems[k], v)
                        if fn is not None:
                            ins = fn(engh)
                            if inc is not None:
                                ins.then_inc(self.sems[inc[0]], inc[1])
                            if self.annotate:
                                ins.annotate(phase)
                getattr(block, handles[e])(body)


CV = {}
_off = 0
for _n, _w in [("g_mix", 8), ("g_cross", 8), ("g_mem", 8), ("g_ffn", 8), ("g_final", 8),
               ("conv_b", 4), ("ln_g", 4), ("ln_b", 4), ("conv_w", 124), ("mu", 15),
               ("w0", 4), ("a0", 4), ("k_k", 4), ("k_a", 4), ("r_k", 4), ("lnx_g", 4), ("lnx_b", 4)]:
    CV[_n] = _off
    _off += _w
NCV = _off


def _cm(v, nch):
    return np.ascontiguousarray(np.asarray(v, np.float32).reshape(nch, 128).T)


def pack_cvec(inp):
    cv = np.zeros((128, NCV), np.float32)

    def put(name, arr):
        cv[:, CV[name]:CV[name] + arr.shape[1]] = arr
    put("g_mix", _cm(inp["g_mix"][0], 8))
    put("g_cross", _cm(inp["g_cross"][0], 8))
    put("g_mem", _cm(inp["g_mem"][0], 8))
    put("g_ffn", _cm(inp["g_ffn"][0], 8))
    put("g_final", _cm(inp["g_final"], 8))
    put("conv_b", _cm(inp["conv_b"][0], 4))
    put("ln_g", _cm(inp["conv_ln_g"][0], 4))
    put("ln_b", _cm(inp["conv_ln_b"][0], 4))
    cw = np.asarray(inp["conv_w"][0], np.float32)
    cwp = cw.reshape(CONV_K, 4, 128).transpose(2, 0, 1)
    put("conv_w", np.ascontiguousarray(cwp.reshape(128, CONV_K * 4)))
    mu = np.asarray(inp["mu_b"][0], np.float32)
    m = np.zeros((128, 15), np.float32)
    m[:, 0:4] = _cm(mu[0:512], 4)
    m[:, 4:8] = _cm(mu[512:1024], 4)
    m[:, 8:12] = _cm(mu[1024:1536], 4)
    m[0:32, 12] = mu[1536:1568]
    m[0:32, 13] = mu[1568:1600]
    m[0:96, 14] = mu[1600:1696]
    put("mu", m)
    for nm in ("w0", "a0", "k_k", "k_a", "r_k", "lnx_g", "lnx_b"):
        put(nm, _cm(inp[nm][0], 4))
    return cv


def pack_blocks(inp):
    out = np.zeros((NHOST, 128, BLK), np.float32)

    def typeA(W, cols):
        blk = np.zeros((128, 4, 8, 128), np.float32)
        Wr = W.reshape(8, 128, -1)
        for cb, (st, wd) in enumerate(cols):
            blk[:, cb, :, :wd] = Wr[:, :, st:st + wd].transpose(1, 0, 2)
        return blk.reshape(128, BLK)

    w_in = np.asarray(inp["w_in"][0], np.float32)
    out[0] = typeA(w_in, [(0, 128), (512, 128), (128, 128), (640, 128)])
    out[1] = typeA(w_in, [(256, 128), (768, 128), (384, 128), (896, 128)])
    for j, base in enumerate((1024, 1536, 2048)):
        out[2 + j] = typeA(w_in, [(base + 128 * i, 128) for i in range(4)])
    out[5] = typeA(w_in, [(2560, 32), (2592, 32), (2624, 96)])
    for j, nm in enumerate(("w_out", "wq", "wo")):
        W = np.asarray(inp[nm][0], np.float32)
        for h in range(2):
            out[6 + 2 * j + h] = typeA(W, [(512 * h + 128 * i, 128) for i in range(4)])
    W = np.asarray(inp["w_ff1"][0], np.float32)
    for b in range(8):
        out[12 + b] = typeA(W, [(512 * b + 128 * i, 128) for i in range(4)])
    W = np.asarray(inp["w_ff2"][0], np.float32).reshape(32, 128, 1024)
    for b in range(8):
        out[20 + b] = W[:, :, 128 * b:128 * (b + 1)].transpose(1, 0, 2).reshape(128, BLK)
    W = np.asarray(inp["wk"][0], np.float32)
    for h in range(2):
        out[28 + h] = typeA(W, [(512 * h + 128 * i, 128) for i in range(4)])
    W = np.asarray(inp["wv"][0], np.float32).reshape(8, 128, 1024)
    for h in range(2):
        out[30 + h] = W[:, :, 512 * h:512 * (h + 1)].transpose(1, 0, 2).reshape(128, BLK)
    return out


def pack_lora(inp):
    lw = np.zeros((128, 3, 512), np.float32)
    lw[0:32, 0] = inp["w_decay2"][0]
    lw[0:32, 1] = inp["a_lora2"][0]
    lw[0:96, 2] = inp["g_lora2"][0]
    return lw


def build(nseq, seqlen, dbg=False, annotate=False):
    NT = seqlen // TT
    nc = bass.Bass("TRN2", target_bir_lowering=False)
    xT = nc.dram_tensor("xT", [nseq, D, seqlen], F32, kind="ExternalInput").ap()
    memT = nc.dram_tensor("memT", [nseq, D, MEM], F32, kind="ExternalInput").ap()
    wblk = nc.dram_tensor("wblk", [NHOST, 128, BLK], F32, kind="ExternalInput").ap()
    cvd = nc.dram_tensor("cvec", [128, NCV], F32, kind="ExternalInput").ap()
    lwd = nc.dram_tensor("lora", [128, 3, 512], F32, kind="ExternalInput").ap()
    oT = nc.dram_tensor("oT", [nseq, D, seqlen], F32, kind="ExternalOutput").ap()
    scr = nc.dram_tensor("wscr", [NBLK, 128, BLK], BF16).ap()
    dbg_out = {}

    with contextlib.ExitStack() as st:
        P = Prog(nc, st)
        P.annotate = annotate

        def sb(name, shape, dt):
            return st.enter_context(nc.sbuf_tensor(name, shape, dt))

        cv = sb("cv", [128, NCV], F32)
        cvx = sb("cvx", [128, 32], F32)
        ident = sb("ident", [128, 128], BF16)
        ident4 = sb("ident4", [128, 4, 128], BF16)
        onesb = sb("onesb", [128, 128], BF16)
        blk1 = sb("blk1", [128, 128], BF16)
        blk64 = sb("blk64", [128, 128], BF16)
        o512 = sb("o512", [128, 128], BF16)
        mSU = sb("mSU", [128, 4, 128], BF16)
        mIU = sb("mIU", [128, 4, 128], BF16)
        mSL = sb("mSL", [128, 4, 128], BF16)
        rmask = sb("rmask", [128, TT], BF16)
        lorab = sb("lorab", [128, 3, 512], BF16)
        S32 = sb("S32", [128, 4, 64], F32)
        Sbf = sb("Sbf", [128, 4, 64], BF16)
        tmpS = sb("tmpS", [128, 4, 64], F32)
        carry = sb("carry", [128, 16], F32)
        KT = sb("KT", [128, 8, MEM], BF16)
        Vt = sb("Vt", [128, 2, D], BF16)
        wslot = [sb("wslot%d" % i, [128, BLK], BF16) for i in range(NSLOT)]
        xres = [sb("xres%d" % i, [128, 8, TT], F32) for i in range(2)]
        actA = sb("actA", [128, 8, TT], BF16)
        actB = sb("actB", [128, 8, TT], BF16)
        arena = sb("arena", [128, 32 * 512], BF16)
        lob = sb("lob", [128, 3, TT], BF16)
        Bbuf = [sb("Bbuf%d" % i, [128, TT + 1], F32) for i in range(2)]
        ubuf = sb("ubuf", [128, 4, HIST + TT], BF16)
        cln = sb("cln", [128, 2, TT], F32)
        cacc = sb("cacc", [128, 4, TT], F32)
        opa_h = [sb("opa%d" % h, [128, 2, 2, TT], BF16) for h in range(2)]
        opr_h = [sb("opr%d" % h, [128, 2, 2, TT], BF16) for h in range(2)]
        opb0 = sb("opb", [128, 2, TT], BF16)
        opk0 = sb("opk", [128, 2, TT], BF16)
        tok0 = sb("tok", [128, 3, 2, 4, 128], BF16)
        bonus = sb("bonus", [128, 4, TT], BF16)
        ybuf = sb("ybuf", [128, 2, TT], F32)
        wcb = sb("wcb", [128, 4, 4], F32)
        MATN = ("X0", "X1", "XT0", "XT1", "P0", "P1", "Aak", "Arb", "Ark")
        matsets = [{nm: sb("m%d_%s" % (i, nm), [128, 4, 128], BF16) for nm in MATN} for i in range(NSET)]
        RHSb = sb("RHSb", [128, 2, 2, 64], BF16)
        Ub = sb("Ub", [128, 2, 2, 64], BF16)
        PT = sb("PT", [128, 2, TT], BF16)
        rsb = sb("rsb", [128, TT], F32)
        sqr = [sb("sqr%d" % i, [128, TT], BF16) for i in range(2)]
        psum = [st.enter_context(nc.psum_tensor("ps%d" % i, [128, 512], F32)) for i in range(8)]

        B_const = Buf("const")
        B_S32 = [Buf("S32_%d" % i) for i in range(4)]
        B_Sbf = [Buf("Sbf_%d" % i) for i in range(4)]
        B_tmpS = [Buf("tmpS%d" % i) for i in range(4)]
        B_carry = [Buf("carry%d" % i) for i in range(16)]
        B_KT = Buf("KT")
        B_V = Buf("V")
        B_wslot = [Buf("wslot%d" % i) for i in range(NSLOT)]
        B_x = [[Buf("x%d_%d" % (i, c)) for c in range(8)] for i in range(2)]
        B_A = [Buf("actA%d" % c) for c in range(8)]
        B_Bc = [Buf("actB%d" % c) for c in range(8)]
        B_ar = [Buf("ar%d" % i) for i in range(32)]
        B_lob = [Buf("lob%d" % i) for i in range(3)]
        B_Bbuf = [Buf("Bbuf0"), Buf("Bbuf1")]
        B_cln = [Buf("cln0"), Buf("cln1")]
        B_u = [Buf("u%d" % c) for c in range(4)]
        B_cacc = [Buf("cacc%d" % c) for c in range(4)]
        B_opa_h = [[Buf("opa%d_%d" % (h, lp)) for lp in range(2)] for h in range(2)]
        B_opr_h = [[Buf("opr%d_%d" % (h, lp)) for lp in range(2)] for h in range(2)]
        B_bonus = [Buf("bonus%d" % lp) for lp in range(4)]
        B_y = [Buf("y%d" % lp) for lp in range(2)]
        B_wc = [Buf("wc%d" % lp) for lp in range(4)]
        B_msets = [{nm: Buf("m%d_%s" % (i, nm)) for nm in MATN} for i in range(NSET)]
        B_RHS = [Buf("RHS%d" % lp) for lp in range(2)]
        B_U = [Buf("U%d" % lp) for lp in range(2)]
        B_PT = [Buf("PT%d" % i) for i in range(2)]
        B_rs = Buf("rs")
        B_sqr = [Buf("sqr%d" % i) for i in range(2)]
        B_ps = [Buf("psb%d" % i, excl=True) for i in range(8)]
        B_scr = [Buf("scr%d" % i) for i in range(NBLK)]

        cln_bf = cln[:].rearrange("p a b -> p (a b)").bitcast(BF16)
        cacc_bf = cacc[:].rearrange("p a b -> p (a b)").bitcast(BF16)
        opb_h = [opb0, cln_bf[:, 0:1024].rearrange("p (a b) -> p a b", b=TT)]
        opk_h = [opk0, cln_bf[:, 1024:2048].rearrange("p (a b) -> p a b", b=TT)]
        tok_h = [tok0, cacc_bf[:, 0:3072].rearrange("p (k l c n) -> p k l c n", k=3, l=2, c=4)]
        B_opb_h = [[Buf("opb0_%d" % lp) for lp in range(2)], [B_cln[0], B_cln[0]]]
        B_opk_h = [[Buf("opk0_%d" % lp) for lp in range(2)], [B_cln[1], B_cln[1]]]
        B_tok_h = [[[Buf("tok0_%d_%d" % (k, lp)) for lp in range(2)] for k in range(3)],
                   [[B_cacc[k], B_cacc[k]] for k in range(3)]]

        def zrkv(j):
            return arena[:, j * 512:(j + 1) * 512], [B_ar[j]]

        def Tt(i):
            o = (12 + 2 * i) * 512
            return arena[:, o:o + 1024].bitcast(F32), [B_ar[12 + 2 * i], B_ar[13 + 2 * i]]

        def fT(j):
            return arena[:, j * 512:(j + 1) * 512], [B_ar[j]]

        ps_pos = [0]

        def ps_alloc(ncols):
            p = ps_pos[0]
            ps_pos[0] = (p + 1) % 8
            return psum[p][:, 0:ncols], [B_ps[p]]

        s_const = P.dma_sem("c")
        s_w = [P.dma_sem("w%d" % i) for i in range(NSLOT)]
        s_x = [P.dma_sem("x%d" % i) for i in range(2)]
        s_o = [P.dma_sem("o%d" % i) for i in range(2)]
        s_sw = [P.dma_sem("sw%d" % i) for i in range(NSLOT)]
        s_mem = P.dma_sem("mem")
        s_wp = [P.dma_sem("wp%d" % i) for i in range(NSLOT)]

        def ACT(out, in_, func, reads, writes, bias=None, scale=None):
            kw = {}
            if bias is not None:
                kw["bias"] = bias
            if scale is not None:
                kw["scale"] = scale
            P.op("act", lambda e: e.activation(out=out, in_=in_, func=func, **kw), reads, writes)

        def TTop(eng, out, in0, in1, op, reads, writes):
            P.op(eng, lambda e: e.tensor_tensor(out=out, in0=in0, in1=in1, op=op), reads, writes)

        def TS(eng, out, in0, s1, s2, op0, op1, reads, writes):
            if s2 is None:
                P.op(eng, lambda e: e.tensor_scalar(out=out, in0=in0, scalar1=s1, scalar2=None, op0=op0), reads, writes)
            else:
                P.op(eng, lambda e: e.tensor_scalar(out=out, in0=in0, scalar1=s1, scalar2=s2, op0=op0, op1=op1), reads, writes)

        def STT(eng, out, in0, scalar, in1, op0, op1, reads, writes):
            P.op(eng, lambda e: e.scalar_tensor_tensor(out=out, in0=in0, scalar=scalar, in1=in1, op0=op0, op1=op1), reads, writes)

        def CP(eng, out, in_, reads, writes):
            if eng == "act":
                P.op("act", lambda e: e.copy(out=out, in_=in_), reads, writes)
            else:
                P.op(eng, lambda e: e.tensor_copy(out=out, in_=in_), reads, writes)

        def MM(out, lhsT, rhs, start, stop, reads, writes, tp=None):
            if tp is None:
                P.op("pe", lambda e: e.matmul(out, lhsT=lhsT, rhs=rhs, start=start, stop=stop), reads, writes)
            else:
                P.op("pe", lambda e: e.matmul(out, lhsT=lhsT, rhs=rhs, start=start, stop=stop, tile_position=tp), reads, writes)

        def MEMSET(eng, ap, val, writes):
            P.op(eng, lambda e: e.memset(ap, val), (), writes)

        def AFSEL(out, in_, pattern, cmp, base, cm, reads, writes):
            P.op("pool", lambda e: e.affine_select(out=out, in_=in_, pattern=pattern, compare_op=cmp, fill=0.0,
                                                   base=base, channel_multiplier=cm), reads, writes)

        def cvc(name, j=0, rows=slice(0, 128)):
            o = CV[name] + j
            return cv[rows, o:o + 1]

        P.dma("sp", lambda e: e.dma_start(out=cv[:], in_=cvd), s_const, writes=[B_const])
        lw32, lwB = Tt(0)
        lw32b, lwBb = Tt(1)
        lw32c, lwBc = Tt(2)
        for j, (tv, tb) in enumerate(((lw32, lwB), (lw32b, lwBb), (lw32c, lwBc))):
            P.dma("sp", lambda e, tv=tv, j=j: e.dma_start(out=tv, in_=lwd[:, j, :]), s_const, writes=tb)
        for b_ in lwB + lwBb + lwBc + [B_const]:
            b_.w = (s_const, P.semval[s_const])
        for j, (tv, tb) in enumerate(((lw32, lwB), (lw32b, lwBb), (lw32c, lwBc))):
            CP("dve", lorab[:, j, :], tv, tb, [B_const])
        TS("dve", cvx[:, 0:15], cv[:, CV["mu"]:CV["mu"] + 15], -1.0, 1.0, ALU.mult, ALU.add, [B_const], [B_const])
        TS("dve", cvx[:, 15:19], cv[:, CV["k_a"]:CV["k_a"] + 4], -1.0, 1.0, ALU.mult, ALU.add, [B_const], [B_const])
        MEMSET("pool", cvx[:, 19:20], 1e-6, [B_const])
        MEMSET("pool", cvx[:, 20:21], 1e-5, [B_const])
        MEMSET("pool", cvx[:, 21:22], 64e-5, [B_const])
        EPS_RMS, EPS_LN, EPS_GN = cvx[:, 19:20], cvx[:, 20:21], cvx[:, 21:22]
        t3, t3b = Tt(3)
        m32 = t3[:, 0:128]
        MEMSET("pool", m32, 1.0, t3b)
        AFSEL(m32, m32, [[-1, 128]], ALU.is_equal, 0, 1, t3b, t3b)
        CP("pool", ident[:], m32, t3b, [B_const])
        for h in range(4):
            CP("pool", ident4[:, h, :], m32, t3b, [B_const])
        for msk, cmp in ((mSU, ALU.is_gt), (mIU, ALU.is_ge)):
            MEMSET("pool", m32, 1.0, t3b)
            AFSEL(m32, m32, [[1, 128]], cmp, 0, -1, t3b, t3b)
            for h in range(4):
                CP("pool", msk[:, h, :], m32, t3b, [B_const])
        MEMSET("pool", m32, 1.0, t3b)
        AFSEL(m32, m32, [[-1, 128]], ALU.is_gt, 0, 1, t3b, t3b)
        for h in range(4):
            CP("pool", mSL[:, h, :], m32, t3b, [B_const])
        for h in range(2):
            MEMSET("pool", opa_h[h][:], 0.0, B_opa_h[h])
            MEMSET("pool", opr_h[h][:], 0.0, B_opr_h[h])
        MEMSET("pool", onesb[:], 1.0, [B_const])
        MEMSET("pool", o512[:], 1.0 / 512.0, [B_const])
        MEMSET("pool", blk1[:], 0.0, [B_const])
        MEMSET("pool", blk1[0:64, 0:64], 1.0, [B_const])
        MEMSET("pool", blk1[64:128, 64:128], 1.0, [B_const])
        MEMSET("pool", blk64[:], 0.0, [B_const])
        MEMSET("pool", blk64[0:64, 0:64], 1.0 / 64.0, [B_const])
        MEMSET("pool", blk64[64:128, 64:128], 1.0 / 64.0, [B_const])
        MEMSET("pool", rmask[:], 1.0, [B_const])
        for c in range(TT // 128):
            MEMSET("pool", rmask[:, c * 128:c * 128 + 1], 0.0, [B_const])

        wseq = []
        for s in range(nseq):
            for ti in range(NT):
                if ti == 0:
                    wseq += [28, 29, 30, 31]
                wseq += [0, 1, 2, 3, 4, 5, 32, 33, 34, 35] + list(range(6, 28))
        wstate = {"issued": 0, "used": 0}

        FIRST_PASS = 36
        cwo_ = CV["conv_w"]

        def w_issue_upto(n):
            while wstate["issued"] < min(n, len(wseq)):
                i = wstate["issued"]
                si = i % NSLOT
                blk = wseq[i]
                if i < FIRST_PASS:
                    if blk < NHOST:
                        P.dma("pool", lambda e, si=si, blk=blk: e.dma_start(out=wslot[si][:], in_=wblk[blk]), s_wp[si],
                              writes=[B_wslot[si]])
                    else:
                        c = blk - NHOST
                        MEMSET("dve", wslot[si][:, CONV_K * 128:BLK], 0.0, [B_wslot[si]])
                        for k in range(CONV_K):
                            TS("dve", wslot[si][:, k * 128:(k + 1) * 128], ident[:],
                               cv[:, cwo_ + k * 4 + c:cwo_ + k * 4 + c + 1], None, ALU.mult, None, [B_const],
                               [B_wslot[si]] if k == CONV_K - 1 else [])
                    P.dma("sp", lambda e, si=si, blk=blk: e.dma_start(out=scr[blk], in_=wslot[si][:]), s_sw[si],
                          reads=[B_wslot[si]], writes=[B_scr[blk]])
                else:
                    P.dma("sp", lambda e, si=si, blk=blk: e.dma_start(out=wslot[si][:], in_=scr[blk]), s_w[si],
                          reads=[B_scr[blk]], writes=[B_wslot[si]])
                wstate["issued"] += 1

        def w_next(expect):
            i = wstate["used"]
            assert wseq[i] == expect, (i, wseq[i], expect)
            w_issue_upto(i + NSLOT)
            wstate["used"] += 1
            si = i % NSLOT
            return wslot[si], B_wslot[si]

        def wA(ws, cb, kc, m=128):
            o = (cb * 8 + kc) * 128
            return ws[:, o:o + m]

        rr = {"sq": 0, "eng": 0}

        def rmsnorm(src, src_b, gname, dst, dst_b, n, out_f32_inplace=False):
            ps, psb = ps_alloc(512)
            for c in range(8):
                q = rr["sq"] % 2
                rr["sq"] += 1
                ACT(sqr[q][:, 0:n], src(c), AF.Square, [src_b[c]], [B_sqr[q]])
                MM(ps[:, 0:n], onesb[:], sqr[q][:, 0:n], c == 0, c == 7, [B_sqr[q], B_const], psb)
            ACT(rsb[:, 0:n], ps[:, 0:n], AF.Ln, psb + [B_const], [B_rs], bias=EPS_RMS, scale=1.0 / D)
            ACT(rsb[:, 0:n], rsb[:, 0:n], AF.Exp, [B_rs], [B_rs], scale=-0.5)
            for c in range(8):
                STT("dve", dst(c), src(c), cvc(gname, c), rsb[:, 0:n], ALU.mult, ALU.mult,
                    [src_b[c], B_rs, B_const], [dst_b[c]])

        def proj_typeA(blocks, rhs, rhs_b, ncb_total, n, evac):
            cb_glob = 0
            for blk in blocks:
                ws, wsb = w_next(blk)
                for cb in range(4):
                    if cb_glob >= ncb_total:
                        break
                    ps, psb = ps_alloc(512)
                    for kc in range(8):
                        MM(ps[:, 0:n], wA(ws, cb, kc), rhs(kc), kc == 0, kc == 7, [wsb, rhs_b[kc]], psb)
                    evac(cb_glob, ps, psb)
                    cb_glob += 1

        def seq_prologue(s):
            MEMSET("pool", S32[:], 0.0, B_S32)
            MEMSET("pool", Sbf[:], 0.0, B_Sbf)
            MEMSET("pool", carry[:], 0.0, B_carry)
            for c in range(4):
                MEMSET("pool", ubuf[:, c, 0:HIST], 0.0, [B_u[c]])
            P.phase = "memkv"
            memv = cacc[:].rearrange("p a b -> p (a b)")[:, 0:8 * MEM].rearrange("p (a b) -> p a b", b=MEM)
            P.dma("sp", lambda e, s=s: e.dma_start(out=memv, in_=memT[s].rearrange("(c p) m -> p c m", p=128)),
                  s_mem, writes=B_cacc)
            rmsnorm(lambda c: memv[:, c, :], [B_cacc[c // 2] for c in range(8)], "g_mem",
                    lambda c: actA[:, c, 0:MEM], B_A, MEM)

            def evK(cb, ps, psb):
                CP("act" if cb % 2 else "dve", KT[:, cb, :], ps[:, 0:MEM], psb, [B_KT])
            proj_typeA([28, 29], lambda kc: actA[:, kc, 0:MEM], B_A, 8, MEM, evK)
            for h in range(2):
                ws, wsb = w_next(30 + h)
                for mc in range(2):
                    ps, psb = ps_alloc(512)
                    for kc in range(8):
                        MM(ps[:, :], actA[:, kc, mc * 128:(mc + 1) * 128], ws[:, kc * 512:(kc + 1) * 512],
                           kc == 0, kc == 7, [wsb, B_A[kc]], psb)
                    CP("act" if mc else "dve", Vt[:, mc, h * 512:(h + 1) * 512], ps[:, :], psb, [B_V])


        def tile_gen(s, ti):
            gi = s * NT + ti
            P.tile = gi
            xi = gi % 2
            xr = xres[xi]
            xb = B_x[xi]
            t0 = ti * TT
            P.phase = "norm1"
            P.dma("sp", lambda e, s=s, t0=t0, xr=xr: e.dma_start(
                out=xr[:], in_=xT[s].rearrange("(c p) t -> p c t", p=128)[:, :, t0:t0 + TT]), s_x[xi], writes=xb)
            rmsnorm(lambda c: xr[:, c, :], xb, "g_mix", lambda c: actA[:, c, :], B_A, TT)

            yield "n1"
            P.tile = gi

            P.phase = "w_in"
            pend = {}

            def ev_conv(cb, ps, psb):
                ch, isgate = divmod(cb, 2)
                if not isgate:
                    pend["val"] = (ps, psb)
                    return
                vps, vpsb = pend.pop("val")
                tsg, tsgb = Tt(ch % 2)
                ACT(tsg, ps[:, :], AF.Sigmoid, psb, tsgb)
                TTop("dve", ubuf[:, ch, HIST:HIST + TT], vps[:, :], tsg, ALU.mult, vpsb + tsgb, [B_u[ch]])
            proj_typeA([0, 1], lambda kc: actA[:, kc, :], B_A, 8, TT, ev_conv)

            yield "a"
            P.tile = gi

            def ev_shift(j, m, ps, psb, dst, dst_b):
                q = j % 2
                Bq, Bqb = Bbuf[q], B_Bbuf[q]
                ACT(Bq[0:m, 1:TT + 1], ps[0:m, :], AF.Identity, psb + [B_const], [Bqb], scale=cvc("mu", j, slice(0, m)))
                CP("dve", Bq[0:m, 0:1], carry[0:m, j:j + 1], [B_carry[j]], [Bqb])
                STT("dve", dst, ps[0:m, :], cvx[0:m, j:j + 1], Bq[0:m, 0:TT], ALU.mult, ALU.add,
                    psb + [Bqb, B_const], dst_b)
                CP("dve", carry[0:m, j:j + 1], Bq[0:m, TT:TT + 1], [Bqb], [B_carry[j]])

            def ev_rkv(cb, ps, psb):
                dst, dst_b = zrkv(cb)
                ev_shift(cb, 128, ps, psb, dst, dst_b)
            proj_typeA([2, 3, 4], lambda kc: actA[:, kc, :], B_A, 12, TT, ev_rkv)

            ws, wsb = w_next(5)
            for cb, m in ((0, 32), (1, 32), (2, 96)):
                ps, psb = ps_alloc(512)
                for kc in range(8):
                    MM(ps[0:m, :], wA(ws, cb, kc, m), actA[:, kc, :], kc == 0, kc == 7, [wsb, B_A[kc]], psb)
                tz, tzb = Tt(2 + cb)
                ev_shift(12 + cb, m, ps, psb, tz[0:m, :], tzb)
                func = (AF.Tanh, AF.Copy, AF.Sigmoid)[cb]
                ACT(lob[0:m, cb, :], tz[0:m, :], func, tzb, [B_lob[cb]])

            def conv_chunk(c):
                ws, wsb = w_next(32 + c)
                ps, psb = ps_alloc(512)
                for k in range(CONV_K):
                    MM(ps[:, :], ws[:, k * 128:(k + 1) * 128], ubuf[:, c, k:k + TT], k == 0, k == CONV_K - 1,
                       [wsb, B_u[c]], psb)
                ACT(cacc[:, c, :], ps[:, :], AF.Identity, psb + [B_const], [B_cacc[c]], bias=cvc("conv_b", c))
                CP("dve", ubuf[:, c, 0:HIST], ubuf[:, c, TT:TT + HIST], [B_u[c]], [B_u[c]])

            def conv_ln():
                psm, psmb = ps_alloc(512)
                pse, pseb = ps_alloc(512)
                for c in range(4):
                    q = rr["sq"] % 2
                    rr["sq"] += 1
                    CP("act", sqr[q][:], cacc[:, c, :], [B_cacc[c]], [B_sqr[q]])
                    MM(psm[:, :], o512[:], sqr[q][:], c == 0, c == 3, [B_sqr[q], B_const], psmb)
                    q = rr["sq"] % 2
                    rr["sq"] += 1
                    ACT(sqr[q][:], cacc[:, c, :], AF.Square, [B_cacc[c]], [B_sqr[q]])
                    MM(pse[:, :], o512[:], sqr[q][:], c == 0, c == 3, [B_sqr[q], B_const], pseb)
                tm2, tm2b = cln[:, 0, :], [B_cln[0]]
                ACT(tm2, psm[:, :], AF.Square, psmb, tm2b)
                TTop("dve", tm2, pse[:, :], tm2, ALU.subtract, pseb + tm2b, tm2b)
                ACT(tm2, tm2, AF.Ln, tm2b + [B_const], tm2b, bias=EPS_LN, scale=1.0)
                ACT(tm2, tm2, AF.Exp, tm2b, tm2b, scale=-0.5)
                tmean, tmeanb = cln[:, 1, :], [B_cln[1]]
                CP("act", tmean, psm[:, :], psmb, tmeanb)
                for c in range(4):
                    TTop("pool", cacc[:, c, :], cacc[:, c, :], tmean, ALU.subtract, [B_cacc[c]] + tmeanb, [B_cacc[c]])
                    TTop("pool", cacc[:, c, :], cacc[:, c, :], tm2, ALU.mult, [B_cacc[c]] + tm2b, [B_cacc[c]])
                    ACT(actB[:, c, :], cacc[:, c, :], AF.Silu, [B_cacc[c], B_const], [B_Bc[c]],
                        bias=cvc("ln_b", c), scale=cvc("ln_g", c))

            P.phase = "rwkv"
            P.phase = "rwkv_ew"

            def ew_steps(hg, lp, T):
                opa, opr, opb, opk, tok = opa_h[hg], opr_h[hg], opb_h[hg], opk_h[hg], tok_h[hg]
                B_opa, B_opr, B_opb, B_opk, B_tok = B_opa_h[hg], B_opr_h[hg], B_opb_h[hg], B_opk_h[hg], B_tok_h[hg]
                pr = 2 * hg + lp
                pc = slice(pr * 128, (pr + 1) * 128)
                zr, zrb = zrkv(pr)
                zk, zkb = zrkv(4 + pr)
                zv, zvb = zrkv(8 + pr)
                sgw, sgwb = T[0]
                cum, cumb = T[1]
                E1, E1b = T[2]
                av, avb = T[3]
                kk, kkb = T[4]
                rn, rnb = T[5]
                kp, kpb = T[6]
                E3, E3b = sgw, sgwb
                E2, E2b = cum, cumb
                ob, ok = opb[:, lp, :], opk[:, lp, :]
                st = []

                def s1():
                    ps, psb = ps_alloc(512)
                    MM(ps[:, :], lorab[0:32, 0, pc], lob[0:32, 0, :], True, True, [B_const, B_lob[0]], psb)
                    ACT(sgw, ps[:, :], AF.Sigmoid, psb + [B_const], sgwb, bias=cvc("w0", pr))
                    ps, psb = ps_alloc(512)
                    MM(ps[:, :], lorab[0:32, 1, pc], lob[0:32, 1, :], True, True, [B_const, B_lob[1]], psb)
                    ACT(av, ps[:, :], AF.Sigmoid, psb + [B_const], avb, bias=cvc("a0", pr))
                st.append(s1)

                def s2():
                    P.op("dve", lambda e: e.tensor_tensor_scan(
                        out=cum, data0=rmask[:], data1=sgw, initial=0.0, op0=ALU.mult, op1=ALU.add),
                        sgwb + [B_const], cumb)
                    TS("pool", kk, zk, cvc("k_k", pr), None, ALU.mult, None, zkb + [B_const], kkb)
                st.append(s2)

                def s3():
                    ACT(E1, cum, AF.Exp, cumb, E1b, scale=-DEC)
                    TTop("dve", E3, cum, sgw, ALU.subtract, cumb + sgwb, E3b)
                    q = rr["sq"] % 2
                    rr["sq"] += 1
                    ACT(sqr[q][:], kk, AF.Square, kkb, [B_sqr[q]])
                    ps, psb = ps_alloc(512)
                    MM(ps[:, :], blk1[:], sqr[q][:], True, True, [B_const, B_sqr[q]], psb)
                    TS("dve", rn, ps[:, :], 1e-24, None, ALU.max, None, psb, rnb)
                st.append(s3)

                def s4():
                    ACT(E3, E3, AF.Exp, E3b, E3b, scale=-DEC)
                    ACT(E2, cum, AF.Exp, cumb, E2b, scale=DEC)
                    ACT(rn, rn, AF.Ln, rnb, rnb)
                    ACT(rn, rn, AF.Exp, rnb, rnb, scale=-0.5)
                    TS("pool", kp, av, cvc("k_a", pr), cvx[:, 15 + pr:16 + pr], ALU.mult, ALU.add,
                       avb + [B_const], kpb)
                st.append(s4)

                def s5():
                    CP("pool", wcb[:, pr, :], E1.rearrange("p (c t) -> p c t", t=128)[:, :, 127], E1b, [B_wc[pr]])
                    TTop("dve", kk, kk, rn, ALU.mult, kkb + rnb, kkb)
                    TTop("pool", kp, kp, zk, ALU.mult, kpb + zkb, kpb)
                st.append(s5)

                def s6():
                    for hf in range(2):
                        rows = slice(hf * 64, (hf + 1) * 64)
                        STT("dve", opa[rows, hf, lp, :], kk[rows, :], -1.0, E3[rows, :], ALU.mult, ALU.mult,
                            kkb + E3b, [B_opa[lp]])
                    TTop("pool", rn, kk, av, ALU.mult, kkb + avb, rnb)
                    TTop("pool", ok, kp, E2, ALU.mult, kpb + E2b, [B_opk[lp]])
                st.append(s6)

                def s7():
                    TTop("dve", ob, rn, E2, ALU.mult, rnb + E2b, [B_opb[lp]])
                    for hf in range(2):
                        rows = slice(hf * 64, (hf + 1) * 64)
                        TTop("pool", opr[rows, hf, lp, :], zr[rows, :], E1[rows, :], ALU.mult, zrb + E1b, [B_opr[lp]])
                    q = rr["sq"] % 2
                    rr["sq"] += 1
                    STT("dve", sqr[q][:], zr, cvc("r_k", pr), kp, ALU.mult, ALU.mult,
                        zrb + kpb + [B_const], [B_sqr[q]])
                    ps, psb = ps_alloc(512)
                    MM(ps[:, :], blk1[:], sqr[q][:], True, True, [B_const, B_sqr[q]], psb)
                    TTop("dve", bonus[:, pr, :], ps[:, :], zv, ALU.mult, psb + zvb, [B_bonus[pr]])
                st.append(s7)

                def s8():
                    for kind, (src, srcb) in enumerate(((ob, [B_opb[lp]]), (ok, [B_opk[lp]]), (zv, zvb))):
                        ps, psb = ps_alloc(256)
                        psv = ps.bitcast(BF16)
                        for c in range(4):
                            P.op("pe", lambda e, psv=psv, src=src, c=c: e.transpose(
                                psv[:, c * 128:(c + 1) * 128], src[:, c * 128:(c + 1) * 128], ident[:]),
                                srcb + [B_const], psb)
                        CP("act" if kind != 1 else "dve",
                           tok[:, kind, lp, :, :].rearrange("p a b -> p (a b)"), psv, psb, [B_tok[kind][lp]])
                st.append(s8)
                return st

            def actA_tmp(i):
                return (actA[:, 2 * i:2 * i + 2, :].rearrange("p a b -> p (a b)").bitcast(F32),
                        [B_A[2 * i], B_A[2 * i + 1]])
            Tset0 = [Tt(i) for i in range(7)]
            Tset1 = [Tt(7), Tt(8), Tt(9)] + [actA_tmp(i) for i in range(4)]

            P.phase = "wkv"

            def chain_stages(hg, c, mats, B_m):
                opa, opr, opb, opk, tok = opa_h[hg], opr_h[hg], opb_h[hg], opk_h[hg], tok_h[hg]
                B_opa, B_opr, B_opb, B_opk, B_tok = B_opa_h[hg], B_opr_h[hg], B_opb_h[hg], B_opk_h[hg], B_tok_h[hg]
                cc = slice(c * 128, (c + 1) * 128)
                fl = lambda t: t[:].rearrange("p a b -> p (a b)")

                def opnd(kind, lp, hf):
                    if kind == "a":
                        return opa[:, hf, lp, cc], B_opa[lp]
                    if kind == "r":
                        return opr[:, hf, lp, cc], B_opr[lp]
                    if kind == "b":
                        return opb[:, lp, cc], B_opb[lp]
                    return opk[:, lp, cc], B_opk[lp]

                def amat(nm, kl, kr_, msk, direct):
                    ps, psb = ps_alloc(512)
                    for hh in range(4):
                        lp, hf = divmod(hh, 2)
                        lh, lhb = opnd(kl, lp, hf)
                        rh, rhb = opnd(kr_, lp, hf)
                        MM(ps[:, hh * 128:(hh + 1) * 128], lh, rh, True, True, [lhb, rhb], psb)
                    if direct:
                        TTop("dve", fl(mats[nm]), ps[:, :], fl(msk), ALU.mult, psb + [B_const], [B_m[nm]])
                    else:
                        CP("act", fl(mats[nm]), ps[:, :], psb, [B_m[nm]])
                        TTop("pool", fl(mats[nm]), fl(mats[nm]), fl(msk), ALU.mult, [B_m[nm], B_const], [B_m[nm]])

                def stA():
                    amat("X0", "b", "a", mSU, True)
                    amat("XT0", "a", "b", mSL, True)
                    TTop("pool", mats["P0"][:], mats["X0"][:], ident4[:], ALU.add, [B_m["X0"], B_const], [B_m["P0"]])
                    amat("Aak", "k", "a", mSU, False)
                    amat("Arb", "b", "r", mIU, False)
                    amat("Ark", "k", "r", mIU, False)

                def stL(lvl):
                    cur = (lvl - 1) % 2
                    Xc, XTc, Pc = "X%d" % cur, "XT%d" % cur, "P%d" % cur
                    Xn, XTn, Pn = "X%d" % (1 - cur), "XT%d" % (1 - cur), "P%d" % (1 - cur)
                    ps2, ps2b = ps_alloc(512)
                    for hh in range(4):
                        MM(ps2[:, hh * 128:(hh + 1) * 128], mats[Xc][:, hh, :], mats[XTc][:, hh, :], True, True,
                           [B_m[Xc], B_m[XTc]], ps2b)
                    if lvl < 6:
                        ps1, ps1b = ps_alloc(512)
                        for hh in range(4):
                            MM(ps1[:, hh * 128:(hh + 1) * 128], mats[XTc][:, hh, :], mats[Xc][:, hh, :], True, True,
                               [B_m[Xc], B_m[XTc]], ps1b)
                    CP("act", fl(mats[XTn]), ps2[:, :], ps2b, [B_m[XTn]])
                    if lvl < 6:
                        CP("dve", fl(mats[Xn]), ps1[:, :], ps1b, [B_m[Xn]])
                    ps3, ps3b = ps_alloc(512)
                    for hh in range(4):
                        MM(ps3[:, hh * 128:(hh + 1) * 128], ident[:], mats[Pc][:, hh, :], True, False,
                           [B_const, B_m[Pc]], ps3b)
                        MM(ps3[:, hh * 128:(hh + 1) * 128], mats[XTn][:, hh, :], mats[Pc][:, hh, :], False, True,
                           [B_m[XTn], B_m[Pc]], ps3b)
                    CP("act" if lvl % 2 else "dve", fl(mats[Pn]), ps3[:, :], ps3b, [B_m[Pn]])

                def stS():
                    Pfin = "P0"
                    hold = []
                    for lp in range(2):
                        pr = 2 * hg + lp
                        bS = B_Sbf[pr]
                        ACT(tmpS[:, pr, :], S32[:, pr, :], AF.Identity, [B_S32[pr], B_wc[pr]], [B_tmpS[pr]],
                            scale=wcb[:, pr, c:c + 1])
                        psR, psRb = ps_alloc(128)
                        for hf in range(2):
                            hh = 2 * lp + hf
                            MM(psR[:, hf * 64:(hf + 1) * 64], opa[:, hf, lp, cc], Sbf[:, pr, :], True, False,
                               [B_opa[lp], bS], psRb)
                            MM(psR[:, hf * 64:(hf + 1) * 64], mats["Aak"][:, hh, :], tok[:, 2, lp, c, hf * 64:(hf + 1) * 64],
                               False, True, [B_m["Aak"], B_tok[2][lp]], psRb)
                        CP("act" if lp else "dve", RHSb[:, lp, :, :].rearrange("p a b -> p (a b)"), psR, psRb, [B_RHS[lp]])
                    for lp in range(2):
                        psU, psUb = ps_alloc(128)
                        for hf in range(2):
                            hh = 2 * lp + hf
                            MM(psU[:, hf * 64:(hf + 1) * 64], mats[Pfin][:, hh, :], RHSb[:, lp, hf, :], True, True,
                               [B_m[Pfin], B_RHS[lp]], psUb)
                        CP("dve" if lp else "act", Ub[:, lp, :, :].rearrange("p a b -> p (a b)"), psU, psUb, [B_U[lp]])
                    for lp in range(2):
                        pr = 2 * hg + lp
                        bS = B_Sbf[pr]
                        psY, psYb = ps_alloc(128)
                        psS, psSb = ps_alloc(128)
                        for hf in range(2):
                            hh = 2 * lp + hf
                            rows = slice(hf * 64, (hf + 1) * 64)
                            vt = tok[:, 2, lp, c, hf * 64:(hf + 1) * 64]
                            MM(psY[rows, :], Sbf[:, pr, :], opr[:, hf, lp, cc], True, False,
                               [bS, B_opr[lp]], psYb, tp=(0, hf * 64))
                            MM(psY[rows, :], Ub[:, lp, hf, :], mats["Arb"][:, hh, :], False, False,
                               [B_U[lp], B_m["Arb"]], psYb, tp=(0, hf * 64))
                            MM(psY[rows, :], vt, mats["Ark"][:, hh, :], False, True,
                               [B_tok[2][lp], B_m["Ark"]], psYb, tp=(0, hf * 64))
                            MM(psS[rows, 0:64], tok[:, 0, lp, c, hf * 64:(hf + 1) * 64], Ub[:, lp, hf, :], True, False,
                               [B_tok[0][lp], B_U[lp]], psSb, tp=(0, hf * 64))
                            MM(psS[rows, 0:64], tok[:, 1, lp, c, hf * 64:(hf + 1) * 64], vt, False, True,
                               [B_tok[1][lp], B_tok[2][lp]], psSb, tp=(0, hf * 64))
                        STT("dve", Sbf[:, pr, :], psS[:, 0:64], wcb[:, pr, c:c + 1], tmpS[:, pr, :], ALU.mult, ALU.add,
                            psSb + [B_wc[pr], B_tmpS[pr]], [bS])
                        STT("dve", S32[:, pr, :], psS[:, 0:64], wcb[:, pr, c:c + 1], tmpS[:, pr, :], ALU.mult, ALU.add,
                            psSb + [B_wc[pr], B_tmpS[pr]], [B_S32[pr]])
                        CP("act", ybuf[:, lp, cc], psY, psYb, [B_y[lp]])
                return [stA] + [(lambda l=l: stL(l)) for l in range(1, 7)] + [stS]

            def run_wkv_all(fillers):
                P.phase = "wkv"
                fillers = list(fillers)
                chains = [chain_stages(i // 4, i % 4, matsets[i % NSET], B_msets[i % NSET]) for i in range(8)]
                STAG = 8 // NSET
                posn = [0] * 8
                step = 0
                while any(p < 8 for p in posn):
                    for i in range(8):
                        if step >= i * STAG and posn[i] < 8:
                            chains[i][posn[i]]()
                            posn[i] += 1
                    if step < len(fillers) and fillers[step] is not None:
                        fillers[step]()
                    step += 1
                for f in fillers[step:]:
                    if f is not None:
                        f()

            def gn_steps(hg):
                st = []
                g1, g1b = Tt(0)
                g2, g2b = Tt(1)
                for lp in range(2):
                    pr = 2 * hg + lp
                    pc = slice(pr * 128, (pr + 1) * 128)
                    yv = ybuf[:, lp, :]

                    def ga(lp=lp, pr=pr, pc=pc, yv=yv):
                        q = rr["sq"] % 2
                        rr["sq"] += 1
                        CP("act", sqr[q][:], yv, [B_y[lp]], [B_sqr[q]])
                        psm, psmb = ps_alloc(512)
                        MM(psm[:, :], blk64[:], sqr[q][:], True, True, [B_const, B_sqr[q]], psmb)
                        q = rr["sq"] % 2
                        rr["sq"] += 1
                        ACT(sqr[q][:], yv, AF.Square, [B_y[lp]], [B_sqr[q]])
                        pse, pseb = ps_alloc(512)
                        MM(pse[:, :], blk64[:], sqr[q][:], True, True, [B_const, B_sqr[q]], pseb)
                        ACT(g1, psm[:, :], AF.Square, psmb, g1b)
                        TTop("dve", g2, yv, psm[:, :], ALU.subtract, [B_y[lp]] + psmb, g2b)
                        TTop("dve", g1, pse[:, :], g1, ALU.subtract, pseb + g1b, g1b)
                        ACT(g1, g1, AF.Ln, g1b + [B_const], g1b, bias=EPS_GN, scale=1.0)
                        ACT(g1, g1, AF.Exp, g1b, g1b, scale=-0.5)

                    def gb(lp=lp, pr=pr, pc=pc):
                        TTop("pool", g2, g2, g1, ALU.mult, g2b + g1b, g2b)
                        ACT(g2, g2, AF.Identity, g2b + [B_const], g2b, bias=cvc("lnx_b", pr), scale=cvc("lnx_g", pr))
                        TTop("pool", g2, g2, bonus[:, pr, :], ALU.add, g2b + [B_bonus[pr]], g2b)
                        psg, psgb = ps_alloc(512)
                        MM(psg[:, :], lorab[0:96, 2, pc], lob[0:96, 2, :], True, True, [B_const, B_lob[2]], psgb)
                        TTop("dve", actB[:, 4 + pr, :], g2, psg[:, :], ALU.mult, g2b + psgb, [B_Bc[4 + pr]])
                    st += [ga, gb]
                return st

            P.phase = "rwkv_ew"
            fill = [(lambda c=c: conv_chunk(c)) for c in range(4)]
            for f0, f1 in zip(ew_steps(0, 0, Tset0), ew_steps(0, 1, Tset1)):
                f0()
                f1()
                if fill:
                    fill.pop(0)()
            conv_ln()
            ew1 = [f for pair in zip(ew_steps(1, 0, Tset0), ew_steps(1, 1, Tset1)) for f in pair]
            assert len(ew1) == 16
            STAG_ = 8 // NSET
            first_hg1 = 4 * STAG_
            last_s_hg0 = 3 * STAG_ + 7
            fl = ew1 + [None] * (last_s_hg0 + 1 - len(ew1)) + gn_steps(0)
            assert len(ew1) <= first_hg1
            run_wkv_all(fl)
            P.phase = "gn"
            for f in gn_steps(1):
                f()

            P.phase = "w_out"
            def ev_res(cb, ps, psb):
                TTop("dve", xr[:, cb, :], xr[:, cb, :], ps[:, :], ALU.add, [xb[cb]] + psb, [xb[cb]])
            proj_typeA([6, 7], lambda kc: actB[:, kc, :], B_Bc, 8, TT, ev_res)

            P.phase = "xattn"
            rmsnorm(lambda c: xr[:, c, :], xb, "g_cross", lambda c: actA[:, c, :], B_A, TT)

            def ev_q(cb, ps, psb):
                CP("act" if cb % 2 else "dve", actB[:, cb, :], ps[:, :], psb, [B_Bc[cb]])
            proj_typeA([8, 9], lambda kc: actA[:, kc, :], B_A, 8, TT, ev_q)
            for hd in range(4):
                for mc in range(2):
                    ps, psb = ps_alloc(512)
                    for dc in range(2):
                        MM(ps[:, :], KT[:, 2 * hd + dc, mc * 128:(mc + 1) * 128], actB[:, 2 * hd + dc, :],
                           dc == 0, dc == 1, [B_KT, B_Bc[2 * hd + dc]], psb)
                    ACT(PT[:, mc, :], ps[:, :], AF.Exp, psb, [B_PT[mc]], scale=1.0 / 16.0)
                ps, psb = ps_alloc(512)
                for mc in range(2):
                    MM(ps[:, :], onesb[:], PT[:, mc, :], mc == 0, mc == 1, [B_const, B_PT[mc]], psb)
                ACT(rsb[:], ps[:, :], AF.Ln, psb, [B_rs])
                ACT(rsb[:], rsb[:], AF.Exp, [B_rs], [B_rs], scale=-1.0)
                for dvc in range(2):
                    ch = 2 * hd + dvc
                    ps, psb = ps_alloc(512)
                    for mc in range(2):
                        MM(ps[:, :], Vt[:, mc, ch * 128:(ch + 1) * 128], PT[:, mc, :], mc == 0, mc == 1,
                           [B_V, B_PT[mc]], psb)
                    TTop("dve", actA[:, ch, :], ps[:, :], rsb[:], ALU.mult, psb + [B_rs], [B_A[ch]])
            proj_typeA([10, 11], lambda kc: actA[:, kc, :], B_A, 8, TT, ev_res)

            P.phase = "ffn"
            rmsnorm(lambda c: xr[:, c, :], xb, "g_ffn", lambda c: actB[:, c, :], B_Bc, TT)

            def ev_ff1(cb, ps, psb):
                f, fb = fT(cb)
                ACT(f, ps[:, :], AF.Relu, psb, fb)
                TTop("pool" if cb % 2 else "dve", f, f, f, ALU.mult, fb, fb)
            proj_typeA(list(range(12, 20)), lambda kc: actB[:, kc, :], B_Bc, 32, TT, ev_ff1)

            yield "b"
            P.tile = gi
            for cb in range(8):
                ws, wsb = w_next(20 + cb)
                ps, psb = ps_alloc(512)
                for kc in range(32):
                    f, fb = fT(kc)
                    MM(ps[:, :], ws[:, kc * 128:(kc + 1) * 128], f, kc == 0, kc == 31, [wsb] + fb, psb)
                ev_res(cb, ps, psb)

            yield "c"
            P.tile = gi
            P.phase = "final"
            rmsnorm(lambda c: xr[:, c, :], xb, "g_final", lambda c: xr[:, c, :], xb, TT)
            P.dma("sp", lambda e, s=s, t0=t0, xr=xr: e.dma_start(
                out=oT[s].rearrange("(c p) t -> p c t", p=128)[:, :, t0:t0 + TT], in_=xr[:]), s_o[xi], reads=xb)
            yield "f"

        order = [(s_, t_) for s_ in range(nseq) for t_ in range(NT)]
        gens = {}

        def start(idx):
            g = tile_gen(*order[idx])
            gens[idx] = g
            next(g)
        prev_final = None
        for idx, (s_, t_) in enumerate(order):
            if t_ == 0:
                seq_prologue(s_)
                start(idx)
            g = gens.pop(idx)
            next(g)
            if prev_final is not None:
                next(prev_final)
                prev_final = None
            next(g)
            if idx + 1 < len(order) and order[idx + 1][1] != 0:
                start(idx + 1)
            next(g)
            prev_final = g
        next(prev_final)
        P.wait_all("sp", B_x[0] + B_x[1])
        P.emit()
    return nc


def prep_inputs(inputs, nseq_per_core, ncores, seqlen):
    x = np.asarray(inputs["x"], np.float32)
    mem = np.asarray(inputs["mem"], np.float32)
    cv = pack_cvec(inputs)
    blocks = pack_blocks(inputs)
    lora = pack_lora(inputs)
    in_maps = []
    for c in range(ncores):
        sl = slice(c * nseq_per_core, (c + 1) * nseq_per_core)
        in_maps.append({
            "xT": np.ascontiguousarray(x[sl, :seqlen].transpose(0, 2, 1)),
            "memT": np.ascontiguousarray(mem[sl].transpose(0, 2, 1)),
            "wblk": blocks, "cvec": cv, "lora": lora,
        })
    return in_maps


def kernel(**inputs):
    nseq = BATCH // NCORES
    nc = build(nseq, SEQ)
    in_maps = prep_inputs(inputs, nseq, NCORES, SEQ)
    res = run_bass_kernel_spmd(nc, in_maps, core_ids=list(range(NCORES)))
    outs = [np.asarray(r["oT"]).transpose(0, 2, 1) for r in res.results]
    return np.ascontiguousarray(np.concatenate(outs, axis=0)).astype(np.float32)
```

```python
import contextlib
import numpy as np
import concourse.bass as bass
import concourse.mybir as mybir
from concourse.bass_utils import run_bass_kernel_spmd

F32 = mybir.dt.float32
BF16 = mybir.dt.bfloat16
ALU = mybir.AluOpType
AF = mybir.ActivationFunctionType

D = 1024
SEQ = 2048
BATCH = 32
NCORES = 8
TT = 512
MEM = 256
CW = 512
RW = 512
CONV_K = 31
HIST = CONV_K - 1
DFF = 4096
NBLK = 36
NHOST = 32
BLK = 4096
NSLOT = 3
NSET = 2
DEC = 0.6065306597126334

ENGS = ("pe", "act", "dve", "pool", "sp")


class Buf:
    __slots__ = ("name", "w", "r", "excl")

    def __init__(self, name="", excl=False):
        self.name = name
        self.w = None
        self.r = []
        self.excl = excl


class Prog:
    tile = 0

    @property
    def phase(self):
        return "t%d:%s" % (self.tile, self._phase)

    @phase.setter
    def phase(self, v):
        self._phase = v

    def __init__(self, nc, stack):
        self.nc = nc
        self.stack = stack
        self.ops = {e: [] for e in ENGS}
        self.cnt = {e: 0 for e in ENGS}
        self.waited = {e: {} for e in ENGS}
        self.sems = {}
        self.semval = {}
        for e in ENGS:
            self.sems[e] = stack.enter_context(nc.semaphore("s_" + e))
        self.n_dma_sems = 0
        self.phase = "init"
        self.annotate = False
        self.hazard = 10 ** 9

    def dma_sem(self, name=None):
        key = "dma%d" % self.n_dma_sems
        self.n_dma_sems += 1
        self.sems[key] = self.stack.enter_context(self.nc.semaphore(name or key))
        self.semval[key] = 0
        return key

    def _deps(self, eng, reads, writes):
        need = {}

        def add(dep):
            if dep is None:
                return
            k, v = dep
            if need.get(k, -1) < v:
                need[k] = v
        for b in reads:
            add(b.w)
        for b in writes:
            add(b.w)
            for d in b.r:
                add(d)
        out = []
        wd = self.waited[eng]
        for k, v in need.items():
            if k == eng:
                if eng in ("pe", "sp") or v <= self.cnt[eng] - self.hazard:
                    continue
            if wd.get(k, -1) >= v:
                continue
            wd[k] = v
            out.append((k, v))
        return out

    def _record(self, me, reads, writes):
        for b in reads:
            if len(b.r) > 24:
                best = {}
                for k, v in b.r:
                    if best.get(k, -1) < v:
                        best[k] = v
                b.r = list(best.items())
            b.r.append(me)
        for b in writes:
            b.w = me
            b.r = []

    @staticmethod
    def _split(reads, writes):
        if any(b.excl for b in reads):
            writes = list(writes) + [b for b in reads if b.excl]
            reads = [b for b in reads if not b.excl]
        return reads, writes

    def op(self, eng, fn, reads=(), writes=()):
        reads, writes = self._split(reads, writes)
        waits = self._deps(eng, reads, writes)
        self.cnt[eng] += 1
        me = (eng, self.cnt[eng])
        self.ops[eng].append((fn, waits, (eng, 1), self.phase))
        self._record(me, reads, writes)
        return me

    def dma(self, eng, fn, semkey, reads=(), writes=()):
        reads, writes = self._split(reads, writes)
        waits = self._deps(eng, reads, writes)
        self.semval[semkey] += 16
        me = (semkey, self.semval[semkey])
        self.ops[eng].append((fn, waits, (semkey, 16), self.phase))
        self._record(me, reads, writes)
        return me

    def wait_all(self, eng, bufs):
        waits = self._deps(eng, [], bufs)
        self.ops[eng].append((None, waits, None, self.phase))

    def emit(self):
        nc = self.nc
        handles = {"pe": "tensor", "act": "scalar", "dve": "vector", "pool": "gpsimd", "sp": "sync"}
        with nc.Block() as block:
            for e in ENGS:
                ops = self.ops[e]
                if not ops:
                    continue

                def body(engh, ops=ops):
                    for fn, waits, inc, phase in ops:
                        for k, v in waits:
                            engh.wait_ge(self.sems[k], v)
                        if fn is not None:
                            ins = fn(engh)
                            if inc is not None:
                                ins.then_inc(self.sems[inc[0]], inc[1])
                            if self.annotate:
                                ins.annotate(phase)
                getattr(block, handles[e])(body)


CV = {}
_off = 0
for _n, _w in [("g_mix", 8), ("g_cross", 8), ("g_mem", 8), ("g_ffn", 8), ("g_final", 8),
               ("conv_b", 4), ("ln_g", 4), ("ln_b", 4), ("conv_w", 124), ("mu", 15),
               ("w0", 4), ("a0", 4), ("k_k", 4), ("k_a", 4), ("r_k", 4), ("lnx_g", 4), ("lnx_b", 4)]:
    CV[_n] = _off
    _off += _w
NCV = _off


def _cm(v, nch):
    return np.ascontiguousarray(np.asarray(v, np.float32).reshape(nch, 128).T)


def pack_cvec(inp):
    cv = np.zeros((128, NCV), np.float32)

    def put(name, arr):
        cv[:, CV[name]:CV[name] + arr.shape[1]] = arr
    put("g_mix", _cm(inp["g_mix"][0], 8))
    put("g_cross", _cm(inp["g_cross"][0], 8))
    put("g_mem", _cm(inp["g_mem"][0], 8))
    put("g_ffn", _cm(inp["g_ffn"][0], 8))
    put("g_final", _cm(inp["g_final"], 8))
    put("conv_b", _cm(inp["conv_b"][0], 4))
    put("ln_g", _cm(inp["conv_ln_g"][0], 4))
    put("ln_b", _cm(inp["conv_ln_b"][0], 4))
    cw = np.asarray(inp["conv_w"][0], np.float32)
    cwp = cw.reshape(CONV_K, 4, 128).transpose(2, 0, 1)
    put("conv_w", np.ascontiguousarray(cwp.reshape(128, CONV_K * 4)))
    mu = np.asarray(inp["mu_b"][0], np.float32)
    m = np.zeros((128, 15), np.float32)
    m[:, 0:4] = _cm(mu[0:512], 4)
    m[:, 4:8] = _cm(mu[512:1024], 4)
    m[:, 8:12] = _cm(mu[1024:1536], 4)
    m[0:32, 12] = mu[1536:1568]
    m[0:32, 13] = mu[1568:1600]
    m[0:96, 14] = mu[1600:1696]
    put("mu", m)
    for nm in ("w0", "a0", "k_k", "k_a", "r_k", "lnx_g", "lnx_b"):
        put(nm, _cm(inp[nm][0], 4))
    return cv


def pack_blocks(inp):
    out = np.zeros((NHOST, 128, BLK), np.float32)

    def typeA(W, cols):
        blk = np.zeros((128, 4, 8, 128), np.float32)
        Wr = W.reshape(8, 128, -1)
        for cb, (st, wd) in enumerate(cols):
            blk[:, cb, :, :wd] = Wr[:, :, st:st + wd].transpose(1, 0, 2)
        return blk.reshape(128, BLK)

    w_in = np.asarray(inp["w_in"][0], np.float32)
    out[0] = typeA(w_in, [(0, 128), (512, 128), (128, 128), (640, 128)])
    out[1] = typeA(w_in, [(256, 128), (768, 128), (384, 128), (896, 128)])
    R_, K_, V_ = 1024, 1536, 2048
    col = lambda base, j: (base + 128 * j, 128)
    out[2] = typeA(w_in, [col(R_, 0), col(R_, 1), col(K_, 0), col(K_, 1)])
    out[3] = typeA(w_in, [col(V_, 0), col(V_, 1), col(R_, 2), col(R_, 3)])
    out[4] = typeA(w_in, [col(K_, 2), col(K_, 3), col(V_, 2), col(V_, 3)])
    out[5] = typeA(w_in, [(2560, 32), (2592, 32), (2624, 96)])
    for j, nm in enumerate(("w_out", "wq", "wo")):
        W = np.asarray(inp[nm][0], np.float32)
        for h in range(2):
            out[6 + 2 * j + h] = typeA(W, [(512 * h + 128 * i, 128) for i in range(4)])
    W = np.asarray(inp["w_ff1"][0], np.float32)
    for b in range(8):
        out[12 + b] = typeA(W, [(512 * b + 128 * i, 128) for i in range(4)])
    W = np.asarray(inp["w_ff2"][0], np.float32).reshape(32, 128, 1024)
    for b in range(8):
        out[20 + b] = W[:, :, 128 * b:128 * (b + 1)].transpose(1, 0, 2).reshape(128, BLK)
    W = np.asarray(inp["wk"][0], np.float32)
    for h in range(2):
        out[28 + h] = typeA(W, [(512 * h + 128 * i, 128) for i in range(4)])
    W = np.asarray(inp["wv"][0], np.float32).reshape(8, 128, 1024)
    for h in range(2):
        out[30 + h] = W[:, :, 512 * h:512 * (h + 1)].transpose(1, 0, 2).reshape(128, BLK)
    return out


def pack_lora(inp):
    lw = np.zeros((128, 3, 512), np.float32)
    lw[0:32, 0] = inp["w_decay2"][0]
    lw[0:32, 1] = inp["a_lora2"][0]
    lw[0:96, 2] = inp["g_lora2"][0]
    return lw


def build(nseq, seqlen, dbg=False, annotate=False):
    NT = seqlen // TT
    nc = bass.Bass("TRN2", target_bir_lowering=False)
    xT = nc.dram_tensor("xT", [nseq, D, seqlen], F32, kind="ExternalInput").ap()
    memT = nc.dram_tensor("memT", [nseq, D, MEM], F32, kind="ExternalInput").ap()
    wblk = nc.dram_tensor("wblk", [NHOST, 128, BLK], F32, kind="ExternalInput").ap()
    cvd = nc.dram_tensor("cvec", [128, NCV], F32, kind="ExternalInput").ap()
    lwd = nc.dram_tensor("lora", [128, 3, 512], F32, kind="ExternalInput").ap()
    oT = nc.dram_tensor("oT", [nseq, D, seqlen], F32, kind="ExternalOutput").ap()
    scr = nc.dram_tensor("wscr", [NBLK, 128, BLK], BF16).ap()
    dbg_out = {}

    with contextlib.ExitStack() as st:
        P = Prog(nc, st)
        P.annotate = annotate

        def sb(name, shape, dt):
            return st.enter_context(nc.sbuf_tensor(name, shape, dt))

        cv = sb("cv", [128, NCV], F32)
        cvx = sb("cvx", [128, 32], F32)
        ident = sb("ident", [128, 128], BF16)
        ident4 = sb("ident4", [128, 4, 128], BF16)
        onesb = sb("onesb", [128, 128], BF16)
        blk1 = sb("blk1", [128, 128], BF16)
        blk64 = sb("blk64", [128, 128], BF16)
        o512 = sb("o512", [128, 128], BF16)
        mSU = sb("mSU", [128, 4, 128], BF16)
        mIU = sb("mIU", [128, 4, 128], BF16)
        mSL = sb("mSL", [128, 4, 128], BF16)
        rmask = sb("rmask", [128, TT], BF16)
        lorab = sb("lorab", [128, 3, 512], BF16)
        S32 = sb("S32", [128, 4, 64], F32)
        Sbf = sb("Sbf", [128, 4, 64], BF16)
        tmpS = sb("tmpS", [128, 4, 64], F32)
        carry = sb("carry", [128, 16], F32)
        KT = sb("KT", [128, 8, MEM], BF16)
        Vt = sb("Vt", [128, 2, D], BF16)
        wslot = [sb("wslot%d" % i, [128, BLK], BF16) for i in range(NSLOT)]
        xres = [sb("xres%d" % i, [128, 8, TT], F32) for i in range(2)]
        actA = sb("actA", [128, 8, TT], BF16)
        actB = sb("actB", [128, 8, TT], BF16)
        arena = sb("arena", [128, 32 * 512], BF16)
        lob = sb("lob", [128, 3, TT], BF16)
        Bbuf = [sb("Bbuf%d" % i, [128, TT + 1], F32) for i in range(2)]
        ubuf = sb("ubuf", [128, 4, HIST + TT], BF16)
        cln = sb("cln", [128, 2, TT], F32)
        cacc = sb("cacc", [128, 4, TT], F32)
        opa_h = [sb("opa%d" % h, [128, 2, 2, TT], BF16) for h in range(2)]
        opr_h = [sb("opr%d" % h, [128, 2, 2, TT], BF16) for h in range(2)]
        opb0 = sb("opb", [128, 2, TT], BF16)
        opk0 = sb("opk", [128, 2, TT], BF16)
        tok0 = sb("tok", [128, 3, 2, 4, 128], BF16)
        bonus = sb("bonus", [128, 4, TT], BF16)
        ybuf = sb("ybuf", [128, 2, TT], F32)
        wcb = sb("wcb", [128, 4, 4], F32)
        MATN = ("X0", "X1", "XT0", "XT1", "P0", "P1", "Aak", "Arb", "Ark")
        matsets = [{nm: sb("m%d_%s" % (i, nm), [128, 4, 128], BF16) for nm in MATN} for i in range(NSET)]
        RHSb = sb("RHSb", [128, 2, 2, 64], BF16)
        Ub = sb("Ub", [128, 2, 2, 64], BF16)
        PT = sb("PT", [128, 2, TT], BF16)
        rsb = sb("rsb", [128, TT], F32)
        sqr = [sb("sqr%d" % i, [128, TT], BF16) for i in range(2)]
        psum = [st.enter_context(nc.psum_tensor("ps%d" % i, [128, 512], F32)) for i in range(8)]

        B_const = Buf("const")
        B_S32 = [Buf("S32_%d" % i) for i in range(4)]
        B_Sbf = [Buf("Sbf_%d" % i) for i in range(4)]
        B_tmpS = [Buf("tmpS%d" % i) for i in range(4)]
        B_carry = [Buf("carry%d" % i) for i in range(16)]
        B_KT = Buf("KT")
        B_V = Buf("V")
        B_wslot = [Buf("wslot%d" % i) for i in range(NSLOT)]
        B_x = [[Buf("x%d_%d" % (i, c)) for c in range(8)] for i in range(2)]
        B_A = [Buf("actA%d" % c) for c in range(8)]
        B_Bc = [Buf("actB%d" % c) for c in range(8)]
        B_ar = [Buf("ar%d" % i) for i in range(32)]
        B_lob = [Buf("lob%d" % i) for i in range(3)]
        B_Bbuf = [Buf("Bbuf0"), Buf("Bbuf1")]
        B_cln = [Buf("cln0"), Buf("cln1")]
        B_u = [Buf("u%d" % c) for c in range(4)]
        B_cacc = [Buf("cacc%d" % c) for c in range(4)]
        B_opa_h = [[Buf("opa%d_%d" % (h, lp)) for lp in range(2)] for h in range(2)]
        B_opr_h = [[Buf("opr%d_%d" % (h, lp)) for lp in range(2)] for h in range(2)]
        B_bonus = [Buf("bonus%d" % lp) for lp in range(4)]
        B_y = [Buf("y%d" % lp) for lp in range(2)]
        B_wc = [Buf("wc%d" % lp) for lp in range(4)]
        B_msets = [{nm: Buf("m%d_%s" % (i, nm)) for nm in MATN} for i in range(NSET)]
        B_RHS = [Buf("RHS%d" % lp) for lp in range(2)]
        B_U = [Buf("U%d" % lp) for lp in range(2)]
        B_PT = [Buf("PT%d" % i) for i in range(2)]
        B_rs = Buf("rs")
        B_sqr = [Buf("sqr%d" % i) for i in range(2)]
        B_ps = [Buf("psb%d" % i, excl=True) for i in range(8)]
        B_scr = [Buf("scr%d" % i) for i in range(NBLK)]

        cln_bf = cln[:].rearrange("p a b -> p (a b)").bitcast(BF16)
        cacc_bf = cacc[:].rearrange("p a b -> p (a b)").bitcast(BF16)
        opb_h = [opb0, cln_bf[:, 0:1024].rearrange("p (a b) -> p a b", b=TT)]
        opk_h = [opk0, cln_bf[:, 1024:2048].rearrange("p (a b) -> p a b", b=TT)]
        tok_h = [tok0, cacc_bf[:, 0:3072].rearrange("p (k l c n) -> p k l c n", k=3, l=2, c=4)]
        B_opb_h = [[Buf("opb0_%d" % lp) for lp in range(2)], [B_cln[0], B_cln[0]]]
        B_opk_h = [[Buf("opk0_%d" % lp) for lp in range(2)], [B_cln[1], B_cln[1]]]
        B_tok_h = [[[Buf("tok0_%d_%d" % (k, lp)) for lp in range(2)] for k in range(3)],
                   [[B_cacc[k], B_cacc[k]] for k in range(3)]]

        def zrkv(j):
            return arena[:, j * 512:(j + 1) * 512], [B_ar[j]]

        def Tt(i):
            o = (12 + 2 * i) * 512
            return arena[:, o:o + 1024].bitcast(F32), [B_ar[12 + 2 * i], B_ar[13 + 2 * i]]

        def fT(j):
            return arena[:, j * 512:(j + 1) * 512], [B_ar[j]]

        ps_pos = [0]

        def ps_alloc(ncols):
            p = ps_pos[0]
            ps_pos[0] = (p + 1) % 8
            return psum[p][:, 0:ncols], [B_ps[p]]

        s_const = P.dma_sem("c")
        s_w = [P.dma_sem("w%d" % i) for i in range(NSLOT)]
        s_x = [P.dma_sem("x%d" % i) for i in range(2)]
        s_o = [P.dma_sem("o%d" % i) for i in range(2)]
        s_sw = [P.dma_sem("sw%d" % i) for i in range(NSLOT)]
        s_mem = P.dma_sem("mem")
        s_wp = [P.dma_sem("wp%d" % i) for i in range(NSLOT)]

        def ACT(out, in_, func, reads, writes, bias=None, scale=None):
            kw = {}
            if bias is not None:
                kw["bias"] = bias
            if scale is not None:
                kw["scale"] = scale
            P.op("act", lambda e: e.activation(out=out, in_=in_, func=func, **kw), reads, writes)

        def TTop(eng, out, in0, in1, op, reads, writes):
            P.op(eng, lambda e: e.tensor_tensor(out=out, in0=in0, in1=in1, op=op), reads, writes)

        def TS(eng, out, in0, s1, s2, op0, op1, reads, writes):
            if s2 is None:
                P.op(eng, lambda e: e.tensor_scalar(out=out, in0=in0, scalar1=s1, scalar2=None, op0=op0), reads, writes)
            else:
                P.op(eng, lambda e: e.tensor_scalar(out=out, in0=in0, scalar1=s1, scalar2=s2, op0=op0, op1=op1), reads, writes)

        def STT(eng, out, in0, scalar, in1, op0, op1, reads, writes):
            P.op(eng, lambda e: e.scalar_tensor_tensor(out=out, in0=in0, scalar=scalar, in1=in1, op0=op0, op1=op1), reads, writes)

        def CP(eng, out, in_, reads, writes):
            if eng == "act":
                P.op("act", lambda e: e.copy(out=out, in_=in_), reads, writes)
            else:
                P.op(eng, lambda e: e.tensor_copy(out=out, in_=in_), reads, writes)

        def MM(out, lhsT, rhs, start, stop, reads, writes, tp=None):
            if tp is None:
                P.op("pe", lambda e: e.matmul(out, lhsT=lhsT, rhs=rhs, start=start, stop=stop), reads, writes)
            else:
                P.op("pe", lambda e: e.matmul(out, lhsT=lhsT, rhs=rhs, start=start, stop=stop, tile_position=tp), reads, writes)

        def MEMSET(eng, ap, val, writes):
            P.op(eng, lambda e: e.memset(ap, val), (), writes)

        def AFSEL(out, in_, pattern, cmp, base, cm, reads, writes):
            P.op("pool", lambda e: e.affine_select(out=out, in_=in_, pattern=pattern, compare_op=cmp, fill=0.0,
                                                   base=base, channel_multiplier=cm), reads, writes)

        def cvc(name, j=0, rows=slice(0, 128)):
            o = CV[name] + j
            return cv[rows, o:o + 1]

        P.dma("sp", lambda e: e.dma_start(out=cv[:], in_=cvd), s_const, writes=[B_const])
        lw32, lwB = Tt(0)
        lw32b, lwBb = Tt(1)
        lw32c, lwBc = Tt(2)
        for j, (tv, tb) in enumerate(((lw32, lwB), (lw32b, lwBb), (lw32c, lwBc))):
            P.dma("sp", lambda e, tv=tv, j=j: e.dma_start(out=tv, in_=lwd[:, j, :]), s_const, writes=tb)
        for b_ in lwB + lwBb + lwBc + [B_const]:
            b_.w = (s_const, P.semval[s_const])
        for j, (tv, tb) in enumerate(((lw32, lwB), (lw32b, lwBb), (lw32c, lwBc))):
            CP("dve", lorab[:, j, :], tv, tb, [B_const])
        TS("dve", cvx[:, 0:15], cv[:, CV["mu"]:CV["mu"] + 15], -1.0, 1.0, ALU.mult, ALU.add, [B_const], [B_const])
        TS("dve", cvx[:, 15:19], cv[:, CV["k_a"]:CV["k_a"] + 4], -1.0, 1.0, ALU.mult, ALU.add, [B_const], [B_const])
        MEMSET("pool", cvx[:, 19:20], 1e-6, [B_const])
        MEMSET("pool", cvx[:, 20:21], 1e-5, [B_const])
        MEMSET("pool", cvx[:, 21:22], 64e-5, [B_const])
        EPS_RMS, EPS_LN, EPS_GN = cvx[:, 19:20], cvx[:, 20:21], cvx[:, 21:22]
        t3, t3b = Tt(3)
        m32 = t3[:, 0:128]
        MEMSET("pool", m32, 1.0, t3b)
        AFSEL(m32, m32, [[-1, 128]], ALU.is_equal, 0, 1, t3b, t3b)
        CP("pool", ident[:], m32, t3b, [B_const])
        for h in range(4):
            CP("pool", ident4[:, h, :], m32, t3b, [B_const])
        for msk, cmp in ((mSU, ALU.is_gt), (mIU, ALU.is_ge)):
            MEMSET("pool", m32, 1.0, t3b)
            AFSEL(m32, m32, [[1, 128]], cmp, 0, -1, t3b, t3b)
            for h in range(4):
                CP("pool", msk[:, h, :], m32, t3b, [B_const])
        MEMSET("pool", m32, 1.0, t3b)
        AFSEL(m32, m32, [[-1, 128]], ALU.is_gt, 0, 1, t3b, t3b)
        for h in range(4):
            CP("pool", mSL[:, h, :], m32, t3b, [B_const])
        for h in range(2):
            MEMSET("pool", opa_h[h][:], 0.0, B_opa_h[h])
            MEMSET("pool", opr_h[h][:], 0.0, B_opr_h[h])
        MEMSET("pool", onesb[:], 1.0, [B_const])
        MEMSET("pool", o512[:], 1.0 / 512.0, [B_const])
        MEMSET("pool", blk1[:], 0.0, [B_const])
        MEMSET("pool", blk1[0:64, 0:64], 1.0, [B_const])
        MEMSET("pool", blk1[64:128, 64:128], 1.0, [B_const])
        MEMSET("pool", blk64[:], 0.0, [B_const])
        MEMSET("pool", blk64[0:64, 0:64], 1.0 / 64.0, [B_const])
        MEMSET("pool", blk64[64:128, 64:128], 1.0 / 64.0, [B_const])
        MEMSET("pool", rmask[:], 1.0, [B_const])
        for c in range(TT // 128):
            MEMSET("pool", rmask[:, c * 128:c * 128 + 1], 0.0, [B_const])

        wseq = []
        for s in range(nseq):
            for ti in range(NT):
                if ti == 0:
                    wseq += [28, 29, 30, 31]
                wseq += [0, 1, 5, 2, 3, 4, 32, 33, 34, 35] + list(range(6, 28))
        wstate = {"issued": 0, "used": 0}

        FIRST_PASS = 36
        cwo_ = CV["conv_w"]

        def w_issue_upto(n):
            while wstate["issued"] < min(n, len(wseq)):
                i = wstate["issued"]
                si = i % NSLOT
                blk = wseq[i]
                if i < FIRST_PASS:
                    if blk < NHOST:
                        P.dma("pool", lambda e, si=si, blk=blk: e.dma_start(out=wslot[si][:], in_=wblk[blk]), s_wp[si],
                              writes=[B_wslot[si]])
                    else:
                        c = blk - NHOST
                        MEMSET("dve", wslot[si][:, CONV_K * 128:BLK], 0.0, [B_wslot[si]])
                        for k in range(CONV_K):
                            TS("dve", wslot[si][:, k * 128:(k + 1) * 128], ident[:],
                               cv[:, cwo_ + k * 4 + c:cwo_ + k * 4 + c + 1], None, ALU.mult, None, [B_const],
                               [B_wslot[si]] if k == CONV_K - 1 else [])
                    P.dma("sp", lambda e, si=si, blk=blk: e.dma_start(out=scr[blk], in_=wslot[si][:]), s_sw[si],
                          reads=[B_wslot[si]], writes=[B_scr[blk]])
                else:
                    P.dma("sp", lambda e, si=si, blk=blk: e.dma_start(out=wslot[si][:], in_=scr[blk]), s_w[si],
                          reads=[B_scr[blk]], writes=[B_wslot[si]])
                wstate["issued"] += 1

        def w_next(expect):
            i = wstate["used"]
            assert wseq[i] == expect, (i, wseq[i], expect)
            w_issue_upto(i + NSLOT)
            wstate["used"] += 1
            si = i % NSLOT
            return wslot[si], B_wslot[si]

        def wA(ws, cb, kc, m=128):
            o = (cb * 8 + kc) * 128
            return ws[:, o:o + m]

        rr = {"sq": 0, "eng": 0}

        def rmsnorm(src, src_b, gname, dst, dst_b, n, out_f32_inplace=False):
            ps, psb = ps_alloc(512)
            for c in range(8):
                q = rr["sq"] % 2
                rr["sq"] += 1
                ACT(sqr[q][:, 0:n], src(c), AF.Square, [src_b[c]], [B_sqr[q]])
                MM(ps[:, 0:n], onesb[:], sqr[q][:, 0:n], c == 0, c == 7, [B_sqr[q], B_const], psb)
            ACT(rsb[:, 0:n], ps[:, 0:n], AF.Ln, psb + [B_const], [B_rs], bias=EPS_RMS, scale=1.0 / D)
            ACT(rsb[:, 0:n], rsb[:, 0:n], AF.Exp, [B_rs], [B_rs], scale=-0.5)
            for c in range(8):
                STT("dve", dst(c), src(c), cvc(gname, c), rsb[:, 0:n], ALU.mult, ALU.mult,
                    [src_b[c], B_rs, B_const], [dst_b[c]])

        def proj_typeA(blocks, rhs, rhs_b, ncb_total, n, evac):
            cb_glob = 0
            for blk in blocks:
                ws, wsb = w_next(blk)
                for cb in range(4):
                    if cb_glob >= ncb_total:
                        break
                    ps, psb = ps_alloc(512)
                    for kc in range(8):
                        MM(ps[:, 0:n], wA(ws, cb, kc), rhs(kc), kc == 0, kc == 7, [wsb, rhs_b[kc]], psb)
                    evac(cb_glob, ps, psb)
                    cb_glob += 1

        def seq_prologue(s):
            MEMSET("pool", S32[:], 0.0, B_S32)
            MEMSET("pool", Sbf[:], 0.0, B_Sbf)
            MEMSET("pool", carry[:], 0.0, B_carry)
            for c in range(4):
                MEMSET("pool", ubuf[:, c, 0:HIST], 0.0, [B_u[c]])
            P.phase = "memkv"
            memv = cacc[:].rearrange("p a b -> p (a b)")[:, 0:8 * MEM].rearrange("p (a b) -> p a b", b=MEM)
            P.dma("sp", lambda e, s=s: e.dma_start(out=memv, in_=memT[s].rearrange("(c p) m -> p c m", p=128)),
                  s_mem, writes=B_cacc)
            rmsnorm(lambda c: memv[:, c, :], [B_cacc[c // 2] for c in range(8)], "g_mem",
                    lambda c: actA[:, c, 0:MEM], B_A, MEM)

            def evK(cb, ps, psb):
                CP("act" if cb % 2 else "dve", KT[:, cb, :], ps[:, 0:MEM], psb, [B_KT])
            proj_typeA([28, 29], lambda kc: actA[:, kc, 0:MEM], B_A, 8, MEM, evK)
            for h in range(2):
                ws, wsb = w_next(30 + h)
                for mc in range(2):
                    ps, psb = ps_alloc(512)
                    for kc in range(8):
                        MM(ps[:, :], actA[:, kc, mc * 128:(mc + 1) * 128], ws[:, kc * 512:(kc + 1) * 512],
                           kc == 0, kc == 7, [wsb, B_A[kc]], psb)
                    CP("act" if mc else "dve", Vt[:, mc, h * 512:(h + 1) * 512], ps[:, :], psb, [B_V])


        def tile_gen(s, ti):
            gi = s * NT + ti
            P.tile = gi
            xi = gi % 2
            xr = xres[xi]
            xb = B_x[xi]
            t0 = ti * TT
            P.phase = "norm1"
            P.dma("sp", lambda e, s=s, t0=t0, xr=xr: e.dma_start(
                out=xr[:], in_=xT[s].rearrange("(c p) t -> p c t", p=128)[:, :, t0:t0 + TT]), s_x[xi], writes=xb)
            rmsnorm(lambda c: xr[:, c, :], xb, "g_mix", lambda c: actA[:, c, :], B_A, TT)

            yield "n1"
            P.tile = gi

            P.phase = "w_in"
            pend = {}

            def ev_conv(cb, ps, psb):
                ch, isgate = divmod(cb, 2)
                if not isgate:
                    pend["val"] = (ps, psb)
                    return
                vps, vpsb = pend.pop("val")
                tsg, tsgb = Tt(ch % 2)
                ACT(tsg, ps[:, :], AF.Sigmoid, psb, tsgb)
                TTop("dve", ubuf[:, ch, HIST:HIST + TT], vps[:, :], tsg, ALU.mult, vpsb + tsgb, [B_u[ch]])
            proj_typeA([0, 1], lambda kc: actA[:, kc, :], B_A, 8, TT, ev_conv)

            yield "a"
            P.tile = gi

            def ev_shift(j, m, ps, psb, dst, dst_b):
                q = j % 2
                Bq, Bqb = Bbuf[q], B_Bbuf[q]
                ACT(Bq[0:m, 1:TT + 1], ps[0:m, :], AF.Identity, psb + [B_const], [Bqb], scale=cvc("mu", j, slice(0, m)))
                CP("dve", Bq[0:m, 0:1], carry[0:m, j:j + 1], [B_carry[j]], [Bqb])
                STT("dve", dst, ps[0:m, :], cvx[0:m, j:j + 1], Bq[0:m, 0:TT], ALU.mult, ALU.add,
                    psb + [Bqb, B_const], dst_b)
                CP("dve", carry[0:m, j:j + 1], Bq[0:m, TT:TT + 1], [Bqb], [B_carry[j]])

            ws, wsb = w_next(5)
            for cb, m in ((0, 32), (1, 32), (2, 96)):
                ps, psb = ps_alloc(512)
                for kc in range(8):
                    MM(ps[0:m, :], wA(ws, cb, kc, m), actA[:, kc, :], kc == 0, kc == 7, [wsb, B_A[kc]], psb)
                tz, tzb = Tt(2 + cb)
                ev_shift(12 + cb, m, ps, psb, tz[0:m, :], tzb)
                func = (AF.Tanh, AF.Copy, AF.Sigmoid)[cb]
                ACT(lob[0:m, cb, :], tz[0:m, :], func, tzb, [B_lob[cb]])

            RKV_MAP = [0, 1, 4, 5, 8, 9, 2, 3, 6, 7, 10, 11]

            def ev_rkv(cb, ps, psb):
                idx = RKV_MAP[cb]
                dst, dst_b = zrkv(idx)
                ev_shift(idx, 128, ps, psb, dst, dst_b)
            proj_typeA([2, 3], lambda kc: actA[:, kc, :], B_A, 8, TT, ev_rkv)

            def rkv_tail():
                ws4, ws4b = w_next(4)
                fs = []
                for cb in range(4):
                    def f(cb=cb):
                        ps, psb = ps_alloc(512)
                        for kc in range(8):
                            MM(ps[:, :], wA(ws4, cb, kc), actA[:, kc, :], kc == 0, kc == 7, [ws4b, B_A[kc]], psb)
                        ev_rkv(8 + cb, ps, psb)
                    fs.append(f)
                return fs

            def conv_chunk(c):
                ws, wsb = w_next(32 + c)
                ps, psb = ps_alloc(512)
                for k in range(CONV_K):
                    MM(ps[:, :], ws[:, k * 128:(k + 1) * 128], ubuf[:, c, k:k + TT], k == 0, k == CONV_K - 1,
                       [wsb, B_u[c]], psb)
                ACT(cacc[:, c, :], ps[:, :], AF.Identity, psb + [B_const], [B_cacc[c]], bias=cvc("conv_b", c))
                CP("dve", ubuf[:, c, 0:HIST], ubuf[:, c, TT:TT + HIST], [B_u[c]], [B_u[c]])

            def conv_ln():
                psm, psmb = ps_alloc(512)
                pse, pseb = ps_alloc(512)
                for c in range(4):
                    q = rr["sq"] % 2
                    rr["sq"] += 1
                    CP("act", sqr[q][:], cacc[:, c, :], [B_cacc[c]], [B_sqr[q]])
                    MM(psm[:, :], o512[:], sqr[q][:], c == 0, c == 3, [B_sqr[q], B_const], psmb)
                    q = rr["sq"] % 2
                    rr["sq"] += 1
                    ACT(sqr[q][:], cacc[:, c, :], AF.Square, [B_cacc[c]], [B_sqr[q]])
                    MM(pse[:, :], o512[:], sqr[q][:], c == 0, c == 3, [B_sqr[q], B_const], pseb)
                tm2, tm2b = cln[:, 0, :], [B_cln[0]]
                ACT(tm2, psm[:, :], AF.Square, psmb, tm2b)
                TTop("dve", tm2, pse[:, :], tm2, ALU.subtract, pseb + tm2b, tm2b)
                ACT(tm2, tm2, AF.Ln, tm2b + [B_const], tm2b, bias=EPS_LN, scale=1.0)
                ACT(tm2, tm2, AF.Exp, tm2b, tm2b, scale=-0.5)
                tmean, tmeanb = cln[:, 1, :], [B_cln[1]]
                CP("act", tmean, psm[:, :], psmb, tmeanb)
                for c in range(4):
                    TTop("pool", cacc[:, c, :], cacc[:, c, :], tmean, ALU.subtract, [B_cacc[c]] + tmeanb, [B_cacc[c]])
                    TTop("pool", cacc[:, c, :], cacc[:, c, :], tm2, ALU.mult, [B_cacc[c]] + tm2b, [B_cacc[c]])
                    ACT(actB[:, c, :], cacc[:, c, :], AF.Silu, [B_cacc[c], B_const], [B_Bc[c]],
                        bias=cvc("ln_b", c), scale=cvc("ln_g", c))

            P.phase = "rwkv"
            P.phase = "rwkv_ew"

            def ew_steps(hg, lp, T):
                opa, opr, opb, opk, tok = opa_h[hg], opr_h[hg], opb_h[hg], opk_h[hg], tok_h[hg]
                B_opa, B_opr, B_opb, B_opk, B_tok = B_opa_h[hg], B_opr_h[hg], B_opb_h[hg], B_opk_h[hg], B_tok_h[hg]
                pr = 2 * hg + lp
                pc = slice(pr * 128, (pr + 1) * 128)
                zr, zrb = zrkv(pr)
                zk, zkb = zrkv(4 + pr)
                zv, zvb = zrkv(8 + pr)
                sgw, sgwb = T[0]
                cum, cumb = T[1]
                E1, E1b = T[2]
                av, avb = T[3]
                kk, kkb = T[4]
                rn, rnb = T[5]
                kp, kpb = T[6]
                E3, E3b = sgw, sgwb
                E2, E2b = cum, cumb
                ob, ok = opb[:, lp, :], opk[:, lp, :]
                st = []

                def s1():
                    ps, psb = ps_alloc(512)
                    MM(ps[:, :], lorab[0:32, 0, pc], lob[0:32, 0, :], True, True, [B_const, B_lob[0]], psb)
                    ACT(sgw, ps[:, :], AF.Sigmoid, psb + [B_const], sgwb, bias=cvc("w0", pr))
                    ps, psb = ps_alloc(512)
                    MM(ps[:, :], lorab[0:32, 1, pc], lob[0:32, 1, :], True, True, [B_const, B_lob[1]], psb)
                    ACT(av, ps[:, :], AF.Sigmoid, psb + [B_const], avb, bias=cvc("a0", pr))
                st.append(s1)

                def s2():
                    P.op("dve", lambda e: e.tensor_tensor_scan(
                        out=cum, data0=rmask[:], data1=sgw, initial=0.0, op0=ALU.mult, op1=ALU.add),
                        sgwb + [B_const], cumb)
                    ACT(kk, zk, AF.Identity, zkb + [B_const], kkb, scale=cvc("k_k", pr))
                st.append(s2)

                def s3():
                    ACT(E1, cum, AF.Exp, cumb, E1b, scale=-DEC)
                    TTop("dve", E3, cum, sgw, ALU.subtract, cumb + sgwb, E3b)
                    q = rr["sq"] % 2
                    rr["sq"] += 1
                    ACT(sqr[q][:], kk, AF.Square, kkb, [B_sqr[q]])
                    ps, psb = ps_alloc(512)
                    MM(ps[:, :], blk1[:], sqr[q][:], True, True, [B_const, B_sqr[q]], psb)
                    TS("dve", rn, ps[:, :], 1e-24, None, ALU.max, None, psb, rnb)
                st.append(s3)

                def s4():
                    ACT(E3, E3, AF.Exp, E3b, E3b, scale=-DEC)
                    ACT(E2, cum, AF.Exp, cumb, E2b, scale=DEC)
                    ACT(rn, rn, AF.Ln, rnb, rnb)
                    ACT(rn, rn, AF.Exp, rnb, rnb, scale=-0.5)
                    ACT(kp, av, AF.Identity, avb + [B_const], kpb, bias=cvx[:, 15 + pr:16 + pr], scale=cvc("k_a", pr))
                st.append(s4)

                def s5():
                    CP("pool", wcb[:, pr, :], E1.rearrange("p (c t) -> p c t", t=128)[:, :, 127], E1b, [B_wc[pr]])
                    TTop("dve", kk, kk, rn, ALU.mult, kkb + rnb, kkb)
                    TTop("pool", kp, kp, zk, ALU.mult, kpb + zkb, kpb)
                st.append(s5)

                def s6():
                    for hf in range(2):
                        rows = slice(hf * 64, (hf + 1) * 64)
                        STT("dve", opa[rows, hf, lp, :], kk[rows, :], -1.0, E3[rows, :], ALU.mult, ALU.mult,
                            kkb + E3b, [B_opa[lp]])
                    TTop("pool", rn, kk, av, ALU.mult, kkb + avb, rnb)
                    TTop("pool", ok, kp, E2, ALU.mult, kpb + E2b, [B_opk[lp]])
                st.append(s6)

                def s7():
                    TTop("dve", ob, rn, E2, ALU.mult, rnb + E2b, [B_opb[lp]])
                    for hf in range(2):
                        rows = slice(hf * 64, (hf + 1) * 64)
                        TTop("pool", opr[rows, hf, lp, :], zr[rows, :], E1[rows, :], ALU.mult, zrb + E1b, [B_opr[lp]])
                    q = rr["sq"] % 2
                    rr["sq"] += 1
                    STT("dve", sqr[q][:], zr, cvc("r_k", pr), kp, ALU.mult, ALU.mult,
                        zrb + kpb + [B_const], [B_sqr[q]])
                    ps, psb = ps_alloc(512)
                    MM(ps[:, :], blk1[:], sqr[q][:], True, True, [B_const, B_sqr[q]], psb)
                    TTop("dve", bonus[:, pr, :], ps[:, :], zv, ALU.mult, psb + zvb, [B_bonus[pr]])
                st.append(s7)

                def s8():
                    for kind, (src, srcb) in enumerate(((ob, [B_opb[lp]]), (ok, [B_opk[lp]]), (zv, zvb))):
                        ps, psb = ps_alloc(256)
                        psv = ps.bitcast(BF16)
                        for c in range(4):
                            P.op("pe", lambda e, psv=psv, src=src, c=c: e.transpose(
                                psv[:, c * 128:(c + 1) * 128], src[:, c * 128:(c + 1) * 128], ident[:]),
                                srcb + [B_const], psb)
                        CP("act" if kind != 1 else "dve",
                           tok[:, kind, lp, :, :].rearrange("p a b -> p (a b)"), psv, psb, [B_tok[kind][lp]])
                st.append(s8)
                return st

            def actA_tmp(i):
                return (actA[:, 2 * i:2 * i + 2, :].rearrange("p a b -> p (a b)").bitcast(F32),
                        [B_A[2 * i], B_A[2 * i + 1]])
            Tset0 = [Tt(i) for i in range(7)]
            Tset1 = [Tt(7), Tt(8), Tt(9)] + [actA_tmp(i) for i in range(4)]

            P.phase = "wkv"

            def chain_stages(hg, c, mats, B_m):
                opa, opr, opb, opk, tok = opa_h[hg], opr_h[hg], opb_h[hg], opk_h[hg], tok_h[hg]
                B_opa, B_opr, B_opb, B_opk, B_tok = B_opa_h[hg], B_opr_h[hg], B_opb_h[hg], B_opk_h[hg], B_tok_h[hg]
                cc = slice(c * 128, (c + 1) * 128)
                fl = lambda t: t[:].rearrange("p a b -> p (a b)")

                def opnd(kind, lp, hf):
                    if kind == "a":
                        return opa[:, hf, lp, cc], B_opa[lp]
                    if kind == "r":
                        return opr[:, hf, lp, cc], B_opr[lp]
                    if kind == "b":
                        return opb[:, lp, cc], B_opb[lp]
                    return opk[:, lp, cc], B_opk[lp]

                def amat(nm, kl, kr_, msk, direct):
                    ps, psb = ps_alloc(512)
                    for hh in range(4):
                        lp, hf = divmod(hh, 2)
                        lh, lhb = opnd(kl, lp, hf)
                        rh, rhb = opnd(kr_, lp, hf)
                        MM(ps[:, hh * 128:(hh + 1) * 128], lh, rh, True, True, [lhb, rhb], psb)
                    if direct:
                        TTop("dve", fl(mats[nm]), ps[:, :], fl(msk), ALU.mult, psb + [B_const], [B_m[nm]])
                    else:
                        CP("act", fl(mats[nm]), ps[:, :], psb, [B_m[nm]])
                        TTop("pool", fl(mats[nm]), fl(mats[nm]), fl(msk), ALU.mult, [B_m[nm], B_const], [B_m[nm]])

                def stA():
                    amat("X0", "b", "a", mSU, True)
                    amat("XT0", "a", "b", mSL, True)
                    TTop("pool", mats["P0"][:], mats["X0"][:], ident4[:], ALU.add, [B_m["X0"], B_const], [B_m["P0"]])
                    amat("Aak", "k", "a", mSU, False)
                    amat("Arb", "b", "r", mIU, False)
                    amat("Ark", "k", "r", mIU, False)

                def stL(lvl):
                    cur = (lvl - 1) % 2
                    Xc, XTc, Pc = "X%d" % cur, "XT%d" % cur, "P%d" % cur
                    Xn, XTn, Pn = "X%d" % (1 - cur), "XT%d" % (1 - cur), "P%d" % (1 - cur)
                    ps2, ps2b = ps_alloc(512)
                    for hh in range(4):
                        MM(ps2[:, hh * 128:(hh + 1) * 128], mats[Xc][:, hh, :], mats[XTc][:, hh, :], True, True,
                           [B_m[Xc], B_m[XTc]], ps2b)
                    if lvl < 6:
                        ps1, ps1b = ps_alloc(512)
                        for hh in range(4):
                            MM(ps1[:, hh * 128:(hh + 1) * 128], mats[XTc][:, hh, :], mats[Xc][:, hh, :], True, True,
                               [B_m[Xc], B_m[XTc]], ps1b)
                    CP("act", fl(mats[XTn]), ps2[:, :], ps2b, [B_m[XTn]])
                    if lvl < 6:
                        CP("act" if lvl % 2 == 0 else "dve", fl(mats[Xn]), ps1[:, :], ps1b, [B_m[Xn]])
                    ps3, ps3b = ps_alloc(512)
                    for hh in range(4):
                        MM(ps3[:, hh * 128:(hh + 1) * 128], mats[XTn][:, hh, :], mats[Pc][:, hh, :], True, True,
                           [B_m[XTn], B_m[Pc]], ps3b)
                    TTop("dve", fl(mats[Pn]), ps3[:, :], fl(mats[Pc]), ALU.add, ps3b + [B_m[Pc]], [B_m[Pn]])

                def stS():
                    Pfin = "P0"
                    hold = []
                    for lp in range(2):
                        pr = 2 * hg + lp
                        bS = B_Sbf[pr]
                        ACT(tmpS[:, pr, :], S32[:, pr, :], AF.Identity, [B_S32[pr], B_wc[pr]], [B_tmpS[pr]],
                            scale=wcb[:, pr, c:c + 1])
                        psR, psRb = ps_alloc(128)
                        for hf in range(2):
                            hh = 2 * lp + hf
                            MM(psR[:, hf * 64:(hf + 1) * 64], opa[:, hf, lp, cc], Sbf[:, pr, :], True, False,
                               [B_opa[lp], bS], psRb)
                            MM(psR[:, hf * 64:(hf + 1) * 64], mats["Aak"][:, hh, :], tok[:, 2, lp, c, hf * 64:(hf + 1) * 64],
                               False, True, [B_m["Aak"], B_tok[2][lp]], psRb)
                        CP("act" if lp else "dve", RHSb[:, lp, :, :].rearrange("p a b -> p (a b)"), psR, psRb, [B_RHS[lp]])
                    for lp in range(2):
                        psU, psUb = ps_alloc(128)
                        for hf in range(2):
                            hh = 2 * lp + hf
                            MM(psU[:, hf * 64:(hf + 1) * 64], mats[Pfin][:, hh, :], RHSb[:, lp, hf, :], True, True,
                               [B_m[Pfin], B_RHS[lp]], psUb)
                        CP("dve" if lp else "act", Ub[:, lp, :, :].rearrange("p a b -> p (a b)"), psU, psUb, [B_U[lp]])
                    for lp in range(2):
                        pr = 2 * hg + lp
                        bS = B_Sbf[pr]
                        psY, psYb = ps_alloc(128)
                        psS, psSb = ps_alloc(128)
                        for hf in range(2):
                            hh = 2 * lp + hf
                            rows = slice(hf * 64, (hf + 1) * 64)
                            vt = tok[:, 2, lp, c, hf * 64:(hf + 1) * 64]
                            MM(psY[rows, :], Sbf[:, pr, :], opr[:, hf, lp, cc], True, False,
                               [bS, B_opr[lp]], psYb, tp=(0, hf * 64))
                            MM(psY[rows, :], Ub[:, lp, hf, :], mats["Arb"][:, hh, :], False, False,
                               [B_U[lp], B_m["Arb"]], psYb, tp=(0, hf * 64))
                            MM(psY[rows, :], vt, mats["Ark"][:, hh, :], False, True,
                               [B_tok[2][lp], B_m["Ark"]], psYb, tp=(0, hf * 64))
                            MM(psS[rows, 0:64], tok[:, 0, lp, c, hf * 64:(hf + 1) * 64], Ub[:, lp, hf, :], True, False,
                               [B_tok[0][lp], B_U[lp]], psSb, tp=(0, hf * 64))
                            MM(psS[rows, 0:64], tok[:, 1, lp, c, hf * 64:(hf + 1) * 64], vt, False, True,
                               [B_tok[1][lp], B_tok[2][lp]], psSb, tp=(0, hf * 64))
                        STT("dve", Sbf[:, pr, :], psS[:, 0:64], wcb[:, pr, c:c + 1], tmpS[:, pr, :], ALU.mult, ALU.add,
                            psSb + [B_wc[pr], B_tmpS[pr]], [bS])
                        STT("dve", S32[:, pr, :], psS[:, 0:64], wcb[:, pr, c:c + 1], tmpS[:, pr, :], ALU.mult, ALU.add,
                            psSb + [B_wc[pr], B_tmpS[pr]], [B_S32[pr]])
                        CP("act", ybuf[:, lp, cc], psY, psYb, [B_y[lp]])
                return [stA] + [(lambda l=l: stL(l)) for l in range(1, 7)] + [stS]

            def run_wkv_all(fillers):
                P.phase = "wkv"
                fillers = list(fillers)
                chains = [chain_stages(i // 4, i % 4, matsets[i % NSET], B_msets[i % NSET]) for i in range(8)]
                STAG = 8 // NSET
                posn = [0] * 8
                step = 0
                while any(p < 8 for p in posn):
                    for i in range(8):
                        if step >= i * STAG and posn[i] < 8:
                            chains[i][posn[i]]()
                            posn[i] += 1
                    if step < len(fillers) and fillers[step] is not None:
                        fillers[step]()
                    step += 1
                for f in fillers[step:]:
                    if f is not None:
                        f()

            def gn_steps(hg):
                st = []
                g1, g1b = Tt(0)
                g2, g2b = Tt(1)
                for lp in range(2):
                    pr = 2 * hg + lp
                    pc = slice(pr * 128, (pr + 1) * 128)
                    yv = ybuf[:, lp, :]

                    def ga(lp=lp, pr=pr, pc=pc, yv=yv):
                        q = rr["sq"] % 2
                        rr["sq"] += 1
                        CP("act", sqr[q][:], yv, [B_y[lp]], [B_sqr[q]])
                        psm, psmb = ps_alloc(512)
                        MM(psm[:, :], blk64[:], sqr[q][:], True, True, [B_const, B_sqr[q]], psmb)
                        q = rr["sq"] % 2
                        rr["sq"] += 1
                        ACT(sqr[q][:], yv, AF.Square, [B_y[lp]], [B_sqr[q]])
                        pse, pseb = ps_alloc(512)
                        MM(pse[:, :], blk64[:], sqr[q][:], True, True, [B_const, B_sqr[q]], pseb)
                        ACT(g1, psm[:, :], AF.Square, psmb, g1b)
                        TTop("dve", g2, yv, psm[:, :], ALU.subtract, [B_y[lp]] + psmb, g2b)
                        TTop("dve", g1, pse[:, :], g1, ALU.subtract, pseb + g1b, g1b)
                        ACT(g1, g1, AF.Ln, g1b + [B_const], g1b, bias=EPS_GN, scale=1.0)
                        ACT(g1, g1, AF.Exp, g1b, g1b, scale=-0.5)

                    def gb(lp=lp, pr=pr, pc=pc):
                        TTop("pool", g2, g2, g1, ALU.mult, g2b + g1b, g2b)
                        ACT(g2, g2, AF.Identity, g2b + [B_const], g2b, bias=cvc("lnx_b", pr), scale=cvc("lnx_g", pr))
                        TTop("pool", g2, g2, bonus[:, pr, :], ALU.add, g2b + [B_bonus[pr]], g2b)
                        psg, psgb = ps_alloc(512)
                        MM(psg[:, :], lorab[0:96, 2, pc], lob[0:96, 2, :], True, True, [B_const, B_lob[2]], psgb)
                        TTop("dve", actB[:, 4 + pr, :], g2, psg[:, :], ALU.mult, g2b + psgb, [B_Bc[4 + pr]])
                    st += [ga, gb]
                return st

            P.phase = "rwkv_ew"
            rt = rkv_tail()
            cvf = [(lambda c=c: conv_chunk(c)) for c in range(4)]
            fill = {0: rt, 2: cvf[0:2], 3: cvf[2:4]}
            Tset1a = [Tt(7), Tt(8), Tt(9), (ybuf[:, 0, :], [B_y[0]]), (ybuf[:, 1, :], [B_y[1]]),
                      (PT[:].rearrange("p a b -> p (a b)").bitcast(F32), [B_PT[0], B_PT[1]]), (rsb[:], [B_rs])]
            for k_, (f0, f1) in enumerate(zip(ew_steps(0, 0, Tset0), ew_steps(0, 1, Tset1a))):
                f0()
                f1()
                for f in fill.get(k_, ()):
                    f()
            ew1 = [f for pair in zip(ew_steps(1, 0, Tset0), ew_steps(1, 1, Tset1)) for f in pair]
            assert len(ew1) == 16
            STAG_ = 8 // NSET
            first_hg1 = 4 * STAG_
            last_s_hg0 = 3 * STAG_ + 7
            fl = [conv_ln] + ew1 + [None] * (last_s_hg0 + 1 - len(ew1) - 1) + gn_steps(0)
            assert len(ew1) + 1 <= first_hg1 + 1
            run_wkv_all(fl)
            P.phase = "gn"
            for f in gn_steps(1):
                f()

            P.phase = "w_out"
            def ev_res(cb, ps, psb):
                TTop("dve", xr[:, cb, :], xr[:, cb, :], ps[:, :], ALU.add, [xb[cb]] + psb, [xb[cb]])
            proj_typeA([6, 7], lambda kc: actB[:, kc, :], B_Bc, 8, TT, ev_res)

            P.phase = "xattn"
            rmsnorm(lambda c: xr[:, c, :], xb, "g_cross", lambda c: actA[:, c, :], B_A, TT)

            def ev_q(cb, ps, psb):
                CP("act" if cb % 2 else "dve", actB[:, cb, :], ps[:, :], psb, [B_Bc[cb]])
            proj_typeA([8, 9], lambda kc: actA[:, kc, :], B_A, 8, TT, ev_q)
            for hd in range(4):
                for mc in range(2):
                    ps, psb = ps_alloc(512)
                    for dc in range(2):
                        MM(ps[:, :], KT[:, 2 * hd + dc, mc * 128:(mc + 1) * 128], actB[:, 2 * hd + dc, :],
                           dc == 0, dc == 1, [B_KT, B_Bc[2 * hd + dc]], psb)
                    ACT(PT[:, mc, :], ps[:, :], AF.Exp, psb, [B_PT[mc]], scale=1.0 / 16.0)
                ps, psb = ps_alloc(512)
                for mc in range(2):
                    MM(ps[:, :], onesb[:], PT[:, mc, :], mc == 0, mc == 1, [B_const, B_PT[mc]], psb)
                ACT(rsb[:], ps[:, :], AF.Ln, psb, [B_rs])
                ACT(rsb[:], rsb[:], AF.Exp, [B_rs], [B_rs], scale=-1.0)
                for dvc in range(2):
                    ch = 2 * hd + dvc
                    ps, psb = ps_alloc(512)
                    for mc in range(2):
                        MM(ps[:, :], Vt[:, mc, ch * 128:(ch + 1) * 128], PT[:, mc, :], mc == 0, mc == 1,
                           [B_V, B_PT[mc]], psb)
                    TTop("dve", actA[:, ch, :], ps[:, :], rsb[:], ALU.mult, psb + [B_rs], [B_A[ch]])
            proj_typeA([10, 11], lambda kc: actA[:, kc, :], B_A, 8, TT, ev_res)

            P.phase = "ffn"
            rmsnorm(lambda c: xr[:, c, :], xb, "g_ffn", lambda c: actB[:, c, :], B_Bc, TT)

            def ev_ff1(cb, ps, psb):
                f, fb = fT(cb)
                ACT(f, ps[:, :], AF.Relu, psb, fb)
                TTop("pool" if cb % 2 else "dve", f, f, f, ALU.mult, fb, fb)
            proj_typeA(list(range(12, 20)), lambda kc: actB[:, kc, :], B_Bc, 32, TT, ev_ff1)

            yield "b"
            P.tile = gi
            for cb in range(8):
                ws, wsb = w_next(20 + cb)
                ps, psb = ps_alloc(512)
                for kc in range(32):
                    f, fb = fT(kc)
                    MM(ps[:, :], ws[:, kc * 128:(kc + 1) * 128], f, kc == 0, kc == 31, [wsb] + fb, psb)
                ev_res(cb, ps, psb)

            yield "c"
            P.tile = gi
            P.phase = "final"
            rmsnorm(lambda c: xr[:, c, :], xb, "g_final", lambda c: xr[:, c, :], xb, TT)
            P.dma("sp", lambda e, s=s, t0=t0, xr=xr: e.dma_start(
                out=oT[s].rearrange("(c p) t -> p c t", p=128)[:, :, t0:t0 + TT], in_=xr[:]), s_o[xi], reads=xb)
            yield "f"

        order = [(s_, t_) for s_ in range(nseq) for t_ in range(NT)]
        gens = {}

        def start(idx):
            g = tile_gen(*order[idx])
            gens[idx] = g
            next(g)
        prev_final = None
        for idx, (s_, t_) in enumerate(order):
            if t_ == 0:
                seq_prologue(s_)
                start(idx)
            g = gens.pop(idx)
            next(g)
            if prev_final is not None:
                next(prev_final)
                prev_final = None
            next(g)
            if idx + 1 < len(order) and order[idx + 1][1] != 0:
                start(idx + 1)
            next(g)
            prev_final = g
        next(prev_final)
        P.wait_all("sp", B_x[0] + B_x[1])
        P.emit()
    return nc


def prep_inputs(inputs, nseq_per_core, ncores, seqlen):
    x = np.asarray(inputs["x"], np.float32)
    mem = np.asarray(inputs["mem"], np.float32)
    cv = pack_cvec(inputs)
    blocks = pack_blocks(inputs)
    lora = pack_lora(inputs)
    in_maps = []
    for c in range(ncores):
        sl = slice(c * nseq_per_core, (c + 1) * nseq_per_core)
        in_maps.append({
            "xT": np.ascontiguousarray(x[sl, :seqlen].transpose(0, 2, 1)),
            "memT": np.ascontiguousarray(mem[sl].transpose(0, 2, 1)),
            "wblk": blocks, "cvec": cv, "lora": lora,
        })
    return in_maps


def kernel(**inputs):
    nseq = BATCH // NCORES
    nc = build(nseq, SEQ)
    in_maps = prep_inputs(inputs, nseq, NCORES, SEQ)
    res = run_bass_kernel_spmd(nc, in_maps, core_ids=list(range(NCORES)))
    outs = [np.asarray(r["oT"]).transpose(0, 2, 1) for r in res.results]
    return np.ascontiguousarray(np.concatenate(outs, axis=0)).astype(np.float32)
```

```python
import contextlib
import numpy as np
import concourse.bass as bass
import concourse.mybir as mybir
from concourse.bass_utils import run_bass_kernel_spmd

F32 = mybir.dt.float32
BF16 = mybir.dt.bfloat16
ALU = mybir.AluOpType
AF = mybir.ActivationFunctionType

D = 1024
SEQ = 2048
BATCH = 32
NCORES = 8
TT = 512
MEM = 256
CW = 512
RW = 512
CONV_K = 31
HIST = CONV_K - 1
DFF = 4096
NBLK = 36
NHOST = 32
BLK = 4096
NSLOT = 3
NSET = 2
NWARM = 2
DEC = 0.6065306597126334

ENGS = ("pe", "act", "dve", "pool", "sp")


class Buf:
    __slots__ = ("name", "w", "r", "excl")

    def __init__(self, name="", excl=False):
        self.name = name
        self.w = None
        self.r = []
        self.excl = excl


class Prog:
    tile = 0

    @property
    def phase(self):
        return "t%d:%s" % (self.tile, self._phase)

    @phase.setter
    def phase(self, v):
        self._phase = v

    def __init__(self, nc, stack):
        self.nc = nc
        self.stack = stack
        self.ops = {e: [] for e in ENGS}
        self.cnt = {e: 0 for e in ENGS}
        self.waited = {e: {} for e in ENGS}
        self.sems = {}
        self.semval = {}
        for e in ENGS:
            self.sems[e] = stack.enter_context(nc.semaphore("s_" + e))
        self.n_dma_sems = 0
        self.phase = "init"
        self.annotate = False
        self.hazard = 10 ** 9

    def dma_sem(self, name=None):
        key = "dma%d" % self.n_dma_sems
        self.n_dma_sems += 1
        self.sems[key] = self.stack.enter_context(self.nc.semaphore(name or key))
        self.semval[key] = 0
        return key

    def _deps(self, eng, reads, writes):
        need = {}

        def add(dep):
            if dep is None:
                return
            k, v = dep
            if need.get(k, -1) < v:
                need[k] = v
        for b in reads:
            add(b.w)
        for b in writes:
            add(b.w)
            for d in b.r:
                add(d)
        out = []
        wd = self.waited[eng]
        for k, v in need.items():
            if k == eng:
                if eng in ("pe", "sp") or v <= self.cnt[eng] - self.hazard:
                    continue
            if wd.get(k, -1) >= v:
                continue
            wd[k] = v
            out.append((k, v))
        return out

    def _record(self, me, reads, writes):
        for b in reads:
            if len(b.r) > 24:
                best = {}
                for k, v in b.r:
                    if best.get(k, -1) < v:
                        best[k] = v
                b.r = list(best.items())
            b.r.append(me)
        for b in writes:
            b.w = me
            b.r = []

    @staticmethod
    def _split(reads, writes):
        if any(b.excl for b in reads):
            writes = list(writes) + [b for b in reads if b.excl]
            reads = [b for b in reads if not b.excl]
        return reads, writes

    def op(self, eng, fn, reads=(), writes=()):
        reads, writes = self._split(reads, writes)
        waits = self._deps(eng, reads, writes)
        self.cnt[eng] += 1
        me = (eng, self.cnt[eng])
        self.ops[eng].append((fn, waits, (eng, 1), self.phase))
        self._record(me, reads, writes)
        return me

    def dma(self, eng, fn, semkey, reads=(), writes=()):
        reads, writes = self._split(reads, writes)
        waits = self._deps(eng, reads, writes)
        self.semval[semkey] += 16
        me = (semkey, self.semval[semkey])
        self.ops[eng].append((fn, waits, (semkey, 16), self.phase))
        self._record(me, reads, writes)
        return me

    def wait_all(self, eng, bufs):
        waits = self._deps(eng, [], bufs)
        self.ops[eng].append((None, waits, None, self.phase))

    def emit(self):
        nc = self.nc
        handles = {"pe": "tensor", "act": "scalar", "dve": "vector", "pool": "gpsimd", "sp": "sync"}
        with nc.Block() as block:
            for e in ENGS:
                ops = self.ops[e]
                if not ops:
                    continue

                def body(engh, ops=ops):
                    for fn, waits, inc, phase in ops:
                        for k, v in waits:
                            engh.wait_ge(self.sems[k], v)
                        if fn is not None:
                            ins = fn(engh)
                            if inc is not None:
                                ins.then_inc(self.sems[inc[0]], inc[1])
                            if self.annotate:
                                ins.annotate(phase)
                getattr(block, handles[e])(body)


CV = {}
_off = 0
for _n, _w in [("g_mix", 8), ("g_cross", 8), ("g_mem", 8), ("g_ffn", 8), ("g_final", 8),
               ("conv_b", 4), ("ln_g", 4), ("ln_b", 4), ("conv_w", 124), ("mu", 15),
               ("w0", 4), ("a0", 4), ("k_k", 4), ("k_a", 4), ("r_k", 4), ("lnx_g", 4), ("lnx_b", 4)]:
    CV[_n] = _off
    _off += _w
NCV = _off


def _cm(v, nch):
    return np.ascontiguousarray(np.asarray(v, np.float32).reshape(nch, 128).T)


def pack_cvec(inp):
    cv = np.zeros((128, NCV), np.float32)

    def put(name, arr):
        cv[:, CV[name]:CV[name] + arr.shape[1]] = arr
    put("g_mix", _cm(inp["g_mix"][0], 8))
    put("g_cross", _cm(inp["g_cross"][0], 8))
    put("g_mem", _cm(inp["g_mem"][0], 8))
    put("g_ffn", _cm(inp["g_ffn"][0], 8))
    put("g_final", _cm(inp["g_final"], 8))
    put("conv_b", _cm(inp["conv_b"][0], 4))
    put("ln_g", _cm(inp["conv_ln_g"][0], 4))
    put("ln_b", _cm(inp["conv_ln_b"][0], 4))
    cw = np.asarray(inp["conv_w"][0], np.float32)
    cwp = cw.reshape(CONV_K, 4, 128).transpose(2, 0, 1)
    put("conv_w", np.ascontiguousarray(cwp.reshape(128, CONV_K * 4)))
    mu = np.asarray(inp["mu_b"][0], np.float32)
    m = np.zeros((128, 15), np.float32)
    m[:, 0:4] = _cm(mu[0:512], 4)
    m[:, 4:8] = _cm(mu[512:1024], 4)
    m[:, 8:12] = _cm(mu[1024:1536], 4)
    m[0:32, 12] = mu[1536:1568]
    m[0:32, 13] = mu[1568:1600]
    m[0:96, 14] = mu[1600:1696]
    put("mu", m)
    for nm in ("w0", "a0", "k_k", "k_a", "r_k", "lnx_g", "lnx_b"):
        put(nm, _cm(inp[nm][0], 4))
    return cv


def pack_blocks(inp):
    out = np.zeros((NHOST, 128, BLK), np.float32)

    def typeA(W, cols):
        blk = np.zeros((128, 4, 8, 128), np.float32)
        Wr = W.reshape(8, 128, -1)
        for cb, (st, wd) in enumerate(cols):
            blk[:, cb, :, :wd] = Wr[:, :, st:st + wd].transpose(1, 0, 2)
        return blk.reshape(128, BLK)

    w_in = np.asarray(inp["w_in"][0], np.float32)
    out[0] = typeA(w_in, [(0, 128), (512, 128), (128, 128), (640, 128)])
    out[1] = typeA(w_in, [(256, 128), (768, 128), (384, 128), (896, 128)])
    R_, K_, V_ = 1024, 1536, 2048
    col = lambda base, j: (base + 128 * j, 128)
    out[2] = typeA(w_in, [col(R_, 0), col(R_, 1), col(K_, 0), col(K_, 1)])
    out[3] = typeA(w_in, [col(V_, 0), col(V_, 1), col(R_, 2), col(R_, 3)])
    out[4] = typeA(w_in, [col(K_, 2), col(K_, 3), col(V_, 2), col(V_, 3)])
    out[5] = typeA(w_in, [(2560, 32), (2592, 32), (2624, 96)])
    for j, nm in enumerate(("w_out", "wq", "wo")):
        W = np.asarray(inp[nm][0], np.float32)
        for h in range(2):
            out[6 + 2 * j + h] = typeA(W, [(512 * h + 128 * i, 128) for i in range(4)])
    W = np.asarray(inp["w_ff1"][0], np.float32)
    for b in range(8):
        out[12 + b] = typeA(W, [(512 * b + 128 * i, 128) for i in range(4)])
    W = np.asarray(inp["w_ff2"][0], np.float32).reshape(32, 128, 1024)
    for b in range(8):
        out[20 + b] = W[:, :, 128 * b:128 * (b + 1)].transpose(1, 0, 2).reshape(128, BLK)
    W = np.asarray(inp["wk"][0], np.float32)
    for h in range(2):
        out[28 + h] = typeA(W, [(512 * h + 128 * i, 128) for i in range(4)])
    W = np.asarray(inp["wv"][0], np.float32).reshape(8, 128, 1024)
    for h in range(2):
        out[30 + h] = W[:, :, 512 * h:512 * (h + 1)].transpose(1, 0, 2).reshape(128, BLK)
    return out


def pack_lora(inp):
    lw = np.zeros((128, 3, 512), np.float32)
    lw[0:32, 0] = inp["w_decay2"][0]
    lw[0:32, 1] = inp["a_lora2"][0]
    lw[0:96, 2] = inp["g_lora2"][0]
    return lw


def build(nseq, seqlen, dbg=False, annotate=False):
    NT = seqlen // TT
    nc = bass.Bass("TRN2", target_bir_lowering=False)
    xT = nc.dram_tensor("xT", [nseq, D, seqlen], F32, kind="ExternalInput").ap()
    memT = nc.dram_tensor("memT", [nseq, D, MEM], F32, kind="ExternalInput").ap()
    wblk = nc.dram_tensor("wblk", [NHOST, 128, BLK], F32, kind="ExternalInput").ap()
    cvd = nc.dram_tensor("cvec", [128, NCV], F32, kind="ExternalInput").ap()
    lwd = nc.dram_tensor("lora", [128, 3, 512], F32, kind="ExternalInput").ap()
    oT = nc.dram_tensor("oT", [nseq, D, seqlen], F32, kind="ExternalOutput").ap()
    scr = nc.dram_tensor("wscr", [NBLK, 128, BLK], BF16).ap()
    dbg_out = {}

    with contextlib.ExitStack() as st:
        P = Prog(nc, st)
        P.annotate = annotate

        def sb(name, shape, dt):
            return st.enter_context(nc.sbuf_tensor(name, shape, dt))

        cv = sb("cv", [128, NCV], F32)
        cvx = sb("cvx", [128, 32], F32)
        ident = sb("ident", [128, 128], BF16)
        ident4 = sb("ident4", [128, 4, 128], BF16)
        onesb = sb("onesb", [128, 128], BF16)
        blk1 = sb("blk1", [128, 128], BF16)
        blk64 = sb("blk64", [128, 128], BF16)
        o512 = sb("o512", [128, 128], BF16)
        mSU = sb("mSU", [128, 4, 128], BF16)
        mIU = sb("mIU", [128, 4, 128], BF16)
        mSL = sb("mSL", [128, 4, 128], BF16)
        rmask = sb("rmask", [128, TT], BF16)
        lorab = sb("lorab", [128, 3, 512], BF16)
        S32 = sb("S32", [128, 4, 64], F32)
        Sbf = sb("Sbf", [128, 4, 64], BF16)
        tmpS = sb("tmpS", [128, 4, 64], F32)
        carry = sb("carry", [128, 16], F32)
        KT = sb("KT", [128, 8, MEM], BF16)
        Vt = sb("Vt", [128, 2, D], BF16)
        wslot = [sb("wslot%d" % i, [128, BLK], BF16) for i in range(NSLOT)]
        xres = [sb("xres%d" % i, [128, 8, TT], F32) for i in range(2)]
        actA = sb("actA", [128, 8, TT], BF16)
        actB = sb("actB", [128, 8, TT], BF16)
        arena = sb("arena", [128, 32 * 512], BF16)
        lob = sb("lob", [128, 3, TT], BF16)
        Bbuf = [sb("Bbuf%d" % i, [128, TT + 1], F32) for i in range(2)]
        ubuf = sb("ubuf", [128, 4, HIST + TT], BF16)
        cln = sb("cln", [128, 2, TT], F32)
        cacc = sb("cacc", [128, 4, TT], F32)
        opa_h = [sb("opa%d" % h, [128, 2, 2, TT], BF16) for h in range(2)]
        opr_h = [sb("opr%d" % h, [128, 2, 2, TT], BF16) for h in range(2)]
        opb0 = sb("opb", [128, 2, TT], BF16)
        opk0 = sb("opk", [128, 2, TT], BF16)
        tok0 = sb("tok", [128, 3, 2, 4, 128], BF16)
        bonus = sb("bonus", [128, 4, TT], BF16)
        ybuf = sb("ybuf", [128, 2, TT], F32)
        wcb = sb("wcb", [128, 4, 4], F32)
        MATN = ("X0", "X1", "XT0", "XT1", "P0", "P1", "Aak", "Arb", "Ark")
        matsets = [{nm: sb("m%d_%s" % (i, nm), [128, 4, 128], BF16) for nm in MATN} for i in range(NSET)]
        RHSb = sb("RHSb", [128, 2, 2, 64], BF16)
        Ub = sb("Ub", [128, 2, 2, 64], BF16)
        PT = sb("PT", [128, 2, TT], BF16)
        rsb = sb("rsb", [128, TT], F32)
        sqr = [sb("sqr%d" % i, [128, TT], BF16) for i in range(2)]
        psum = [st.enter_context(nc.psum_tensor("ps%d" % i, [128, 512], F32)) for i in range(8)]

        B_const = Buf("const")
        B_S32 = [Buf("S32_%d" % i) for i in range(4)]
        B_Sbf = [Buf("Sbf_%d" % i) for i in range(4)]
        B_tmpS = [Buf("tmpS%d" % i) for i in range(4)]
        B_carry = [Buf("carry%d" % i) for i in range(16)]
        B_KT = Buf("KT")
        B_V = Buf("V")
        B_wslot = [Buf("wslot%d" % i) for i in range(NSLOT)]
        B_x = [[Buf("x%d_%d" % (i, c)) for c in range(8)] for i in range(2)]
        B_A = [Buf("actA%d" % c) for c in range(8)]
        B_Bc = [Buf("actB%d" % c) for c in range(8)]
        B_ar = [Buf("ar%d" % i) for i in range(32)]
        B_lob = [Buf("lob%d" % i) for i in range(3)]
        B_Bbuf = [Buf("Bbuf0"), Buf("Bbuf1")]
        B_cln = [Buf("cln0"), Buf("cln1")]
        B_u = [Buf("u%d" % c) for c in range(4)]
        B_cacc = [Buf("cacc%d" % c) for c in range(4)]
        B_opa_h = [[Buf("opa%d_%d" % (h, lp)) for lp in range(2)] for h in range(2)]
        B_opr_h = [[Buf("opr%d_%d" % (h, lp)) for lp in range(2)] for h in range(2)]
        B_bonus = [Buf("bonus%d" % lp) for lp in range(4)]
        B_y = [Buf("y%d" % lp) for lp in range(2)]
        B_wc = [Buf("wc%d" % lp) for lp in range(4)]
        B_msets = [{nm: Buf("m%d_%s" % (i, nm)) for nm in MATN} for i in range(NSET)]
        B_RHS = [Buf("RHS%d" % lp) for lp in range(2)]
        B_U = [Buf("U%d" % lp) for lp in range(2)]
        B_PT = [Buf("PT%d" % i) for i in range(2)]
        B_rs = Buf("rs")
        B_sqr = [Buf("sqr%d" % i) for i in range(2)]
        B_ps = [Buf("psb%d" % i, excl=True) for i in range(8)]
        B_scr = [Buf("scr%d" % i) for i in range(NBLK)]

        cln_bf = cln[:].rearrange("p a b -> p (a b)").bitcast(BF16)
        cacc_bf = cacc[:].rearrange("p a b -> p (a b)").bitcast(BF16)
        opb_h = [opb0, cln_bf[:, 0:1024].rearrange("p (a b) -> p a b", b=TT)]
        opk_h = [opk0, cln_bf[:, 1024:2048].rearrange("p (a b) -> p a b", b=TT)]
        tok_h = [tok0, cacc_bf[:, 0:3072].rearrange("p (k l c n) -> p k l c n", k=3, l=2, c=4)]
        B_opb_h = [[Buf("opb0_%d" % lp) for lp in range(2)], [B_cln[0], B_cln[0]]]
        B_opk_h = [[Buf("opk0_%d" % lp) for lp in range(2)], [B_cln[1], B_cln[1]]]
        B_tok_h = [[[Buf("tok0_%d_%d" % (k, lp)) for lp in range(2)] for k in range(3)],
                   [[B_cacc[k], B_cacc[k]] for k in range(3)]]

        def zrkv(j):
            return arena[:, j * 512:(j + 1) * 512], [B_ar[j]]

        def Tt(i):
            o = (12 + 2 * i) * 512
            return arena[:, o:o + 1024].bitcast(F32), [B_ar[12 + 2 * i], B_ar[13 + 2 * i]]

        def fT(j):
            return arena[:, j * 512:(j + 1) * 512], [B_ar[j]]

        ps_pos = [0]
        NRING = 7 if NWARM else 8

        def ps_alloc(ncols):
            p = ps_pos[0]
            ps_pos[0] = (p + 1) % NRING
            return psum[p][:, 0:ncols], [B_ps[p]]

        B_dummy = Buf("dummy")

        def pe_warm(n):
            for _ in range(n):
                MM(psum[7][:, :], onesb[:], ident4[:].rearrange("p a b -> p (a b)"), True, True, [B_const], [B_dummy])

        s_const = P.dma_sem("c")
        s_w = [P.dma_sem("w%d" % i) for i in range(NSLOT)]
        s_x = [P.dma_sem("x%d" % i) for i in range(2)]
        s_o = [P.dma_sem("o%d" % i) for i in range(2)]
        s_sw = [P.dma_sem("sw%d" % i) for i in range(NSLOT)]
        s_mem = P.dma_sem("mem")
        s_wp = [P.dma_sem("wp%d" % i) for i in range(NSLOT)]

        def ACT(out, in_, func, reads, writes, bias=None, scale=None):
            kw = {}
            if bias is not None:
                kw["bias"] = bias
            if scale is not None:
                kw["scale"] = scale
            P.op("act", lambda e: e.activation(out=out, in_=in_, func=func, **kw), reads, writes)

        def TTop(eng, out, in0, in1, op, reads, writes):
            P.op(eng, lambda e: e.tensor_tensor(out=out, in0=in0, in1=in1, op=op), reads, writes)

        def TS(eng, out, in0, s1, s2, op0, op1, reads, writes):
            if s2 is None:
                P.op(eng, lambda e: e.tensor_scalar(out=out, in0=in0, scalar1=s1, scalar2=None, op0=op0), reads, writes)
            else:
                P.op(eng, lambda e: e.tensor_scalar(out=out, in0=in0, scalar1=s1, scalar2=s2, op0=op0, op1=op1), reads, writes)

        def STT(eng, out, in0, scalar, in1, op0, op1, reads, writes):
            P.op(eng, lambda e: e.scalar_tensor_tensor(out=out, in0=in0, scalar=scalar, in1=in1, op0=op0, op1=op1), reads, writes)

        def CP(eng, out, in_, reads, writes):
            if eng == "act":
                P.op("act", lambda e: e.copy(out=out, in_=in_), reads, writes)
            else:
                P.op(eng, lambda e: e.tensor_copy(out=out, in_=in_), reads, writes)

        def MM(out, lhsT, rhs, start, stop, reads, writes, tp=None):
            if tp is None:
                P.op("pe", lambda e: e.matmul(out, lhsT=lhsT, rhs=rhs, start=start, stop=stop), reads, writes)
            else:
                P.op("pe", lambda e: e.matmul(out, lhsT=lhsT, rhs=rhs, start=start, stop=stop, tile_position=tp), reads, writes)

        def MEMSET(eng, ap, val, writes):
            P.op(eng, lambda e: e.memset(ap, val), (), writes)

        def AFSEL(out, in_, pattern, cmp, base, cm, reads, writes):
            P.op("pool", lambda e: e.affine_select(out=out, in_=in_, pattern=pattern, compare_op=cmp, fill=0.0,
                                                   base=base, channel_multiplier=cm), reads, writes)

        def cvc(name, j=0, rows=slice(0, 128)):
            o = CV[name] + j
            return cv[rows, o:o + 1]

        P.dma("sp", lambda e: e.dma_start(out=cv[:], in_=cvd), s_const, writes=[B_const])
        lw32, lwB = Tt(0)
        lw32b, lwBb = Tt(1)
        lw32c, lwBc = Tt(2)
        for j, (tv, tb) in enumerate(((lw32, lwB), (lw32b, lwBb), (lw32c, lwBc))):
            P.dma("sp", lambda e, tv=tv, j=j: e.dma_start(out=tv, in_=lwd[:, j, :]), s_const, writes=tb)
        for b_ in lwB + lwBb + lwBc + [B_const]:
            b_.w = (s_const, P.semval[s_const])
        for j, (tv, tb) in enumerate(((lw32, lwB), (lw32b, lwBb), (lw32c, lwBc))):
            CP("dve", lorab[:, j, :], tv, tb, [B_const])
        TS("dve", cvx[:, 0:15], cv[:, CV["mu"]:CV["mu"] + 15], -1.0, 1.0, ALU.mult, ALU.add, [B_const], [B_const])
        TS("dve", cvx[:, 15:19], cv[:, CV["k_a"]:CV["k_a"] + 4], -1.0, 1.0, ALU.mult, ALU.add, [B_const], [B_const])
        MEMSET("pool", cvx[:, 19:20], 1e-6, [B_const])
        MEMSET("pool", cvx[:, 20:21], 1e-5, [B_const])
        MEMSET("pool", cvx[:, 21:22], 64e-5, [B_const])
        EPS_RMS, EPS_LN, EPS_GN = cvx[:, 19:20], cvx[:, 20:21], cvx[:, 21:22]
        t3, t3b = Tt(3)
        m32 = t3[:, 0:128]
        MEMSET("pool", m32, 1.0, t3b)
        AFSEL(m32, m32, [[-1, 128]], ALU.is_equal, 0, 1, t3b, t3b)
        CP("pool", ident[:], m32, t3b, [B_const])
        for h in range(4):
            CP("pool", ident4[:, h, :], m32, t3b, [B_const])
        for msk, cmp in ((mSU, ALU.is_gt), (mIU, ALU.is_ge)):
            MEMSET("pool", m32, 1.0, t3b)
            AFSEL(m32, m32, [[1, 128]], cmp, 0, -1, t3b, t3b)
            for h in range(4):
                CP("pool", msk[:, h, :], m32, t3b, [B_const])
        MEMSET("pool", m32, 1.0, t3b)
        AFSEL(m32, m32, [[-1, 128]], ALU.is_gt, 0, 1, t3b, t3b)
        for h in range(4):
            CP("pool", mSL[:, h, :], m32, t3b, [B_const])
        for h in range(2):
            MEMSET("pool", opa_h[h][:], 0.0, B_opa_h[h])
            MEMSET("pool", opr_h[h][:], 0.0, B_opr_h[h])
        MEMSET("pool", onesb[:], 1.0, [B_const])
        MEMSET("pool", o512[:], 1.0 / 512.0, [B_const])
        MEMSET("pool", blk1[:], 0.0, [B_const])
        MEMSET("pool", blk1[0:64, 0:64], 1.0, [B_const])
        MEMSET("pool", blk1[64:128, 64:128], 1.0, [B_const])
        MEMSET("pool", blk64[:], 0.0, [B_const])
        MEMSET("pool", blk64[0:64, 0:64], 1.0 / 64.0, [B_const])
        MEMSET("pool", blk64[64:128, 64:128], 1.0 / 64.0, [B_const])
        MEMSET("pool", rmask[:], 1.0, [B_const])
        for c in range(TT // 128):
            MEMSET("pool", rmask[:, c * 128:c * 128 + 1], 0.0, [B_const])

        wseq = []
        for s in range(nseq):
            for ti in range(NT):
                if ti == 0:
                    wseq += [28, 29, 30, 31]
                wseq += [0, 1, 5, 2, 3, 4, 32, 33, 34, 35] + list(range(6, 28))
        wstate = {"issued": 0, "used": 0}

        FIRST_PASS = 36
        cwo_ = CV["conv_w"]

        def w_issue_upto(n):
            while wstate["issued"] < min(n, len(wseq)):
                i = wstate["issued"]
                si = i % NSLOT
                blk = wseq[i]
                if i < FIRST_PASS:
                    if blk < NHOST:
                        P.dma("pool", lambda e, si=si, blk=blk: e.dma_start(out=wslot[si][:], in_=wblk[blk]), s_wp[si],
                              writes=[B_wslot[si]])
                    else:
                        c = blk - NHOST
                        MEMSET("dve", wslot[si][:, CONV_K * 128:BLK], 0.0, [B_wslot[si]])
                        for k in range(CONV_K):
                            TS("dve", wslot[si][:, k * 128:(k + 1) * 128], ident[:],
                               cv[:, cwo_ + k * 4 + c:cwo_ + k * 4 + c + 1], None, ALU.mult, None, [B_const],
                               [B_wslot[si]] if k == CONV_K - 1 else [])
                    P.dma("sp", lambda e, si=si, blk=blk: e.dma_start(out=scr[blk], in_=wslot[si][:]), s_sw[si],
                          reads=[B_wslot[si]], writes=[B_scr[blk]])
                else:
                    P.dma("sp", lambda e, si=si, blk=blk: e.dma_start(out=wslot[si][:], in_=scr[blk]), s_w[si],
                          reads=[B_scr[blk]], writes=[B_wslot[si]])
                wstate["issued"] += 1

        def w_next(expect):
            i = wstate["used"]
            assert wseq[i] == expect, (i, wseq[i], expect)
            w_issue_upto(i + NSLOT)
            wstate["used"] += 1
            si = i % NSLOT
            return wslot[si], B_wslot[si]

        def wA(ws, cb, kc, m=128):
            o = (cb * 8 + kc) * 128
            return ws[:, o:o + m]

        rr = {"sq": 0, "eng": 0}

        def rmsnorm(src, src_b, gname, dst, dst_b, n, out_f32_inplace=False):
            ps, psb = ps_alloc(512)
            for c in range(8):
                q = rr["sq"] % 2
                rr["sq"] += 1
                ACT(sqr[q][:, 0:n], src(c), AF.Square, [src_b[c]], [B_sqr[q]])
                MM(ps[:, 0:n], onesb[:], sqr[q][:, 0:n], c == 0, c == 7, [B_sqr[q], B_const], psb)
            ACT(rsb[:, 0:n], ps[:, 0:n], AF.Ln, psb + [B_const], [B_rs], bias=EPS_RMS, scale=1.0 / D)
            ACT(rsb[:, 0:n], rsb[:, 0:n], AF.Exp, [B_rs], [B_rs], scale=-0.5)
            for c in range(8):
                STT("dve", dst(c), src(c), cvc(gname, c), rsb[:, 0:n], ALU.mult, ALU.mult,
                    [src_b[c], B_rs, B_const], [dst_b[c]])

        def proj_typeA(blocks, rhs, rhs_b, ncb_total, n, evac):
            cb_glob = 0
            for blk in blocks:
                ws, wsb = w_next(blk)
                for cb in range(4):
                    if cb_glob >= ncb_total:
                        break
                    ps, psb = ps_alloc(512)
                    for kc in range(8):
                        MM(ps[:, 0:n], wA(ws, cb, kc), rhs(kc), kc == 0, kc == 7, [wsb, rhs_b[kc]], psb)
                    evac(cb_glob, ps, psb)
                    cb_glob += 1

        def seq_prologue(s):
            MEMSET("pool", S32[:], 0.0, B_S32)
            MEMSET("pool", Sbf[:], 0.0, B_Sbf)
            MEMSET("pool", carry[:], 0.0, B_carry)
            for c in range(4):
                MEMSET("pool", ubuf[:, c, 0:HIST], 0.0, [B_u[c]])
            P.phase = "memkv"
            memv = cacc[:].rearrange("p a b -> p (a b)")[:, 0:8 * MEM].rearrange("p (a b) -> p a b", b=MEM)
            P.dma("sp", lambda e, s=s: e.dma_start(out=memv, in_=memT[s].rearrange("(c p) m -> p c m", p=128)),
                  s_mem, writes=B_cacc)
            rmsnorm(lambda c: memv[:, c, :], [B_cacc[c // 2] for c in range(8)], "g_mem",
                    lambda c: actA[:, c, 0:MEM], B_A, MEM)

            def evK(cb, ps, psb):
                CP("act" if cb % 2 else "dve", KT[:, cb, :], ps[:, 0:MEM], psb, [B_KT])
            proj_typeA([28, 29], lambda kc: actA[:, kc, 0:MEM], B_A, 8, MEM, evK)
            for h in range(2):
                ws, wsb = w_next(30 + h)
                for mc in range(2):
                    ps, psb = ps_alloc(512)
                    for kc in range(8):
                        MM(ps[:, :], actA[:, kc, mc * 128:(mc + 1) * 128], ws[:, kc * 512:(kc + 1) * 512],
                           kc == 0, kc == 7, [wsb, B_A[kc]], psb)
                    CP("act" if mc else "dve", Vt[:, mc, h * 512:(h + 1) * 512], ps[:, :], psb, [B_V])


        def tile_gen(s, ti):
            gi = s * NT + ti
            P.tile = gi
            xi = gi % 2
            xr = xres[xi]
            xb = B_x[xi]
            t0 = ti * TT
            P.phase = "norm1"
            P.dma("sp", lambda e, s=s, t0=t0, xr=xr: e.dma_start(
                out=xr[:], in_=xT[s].rearrange("(c p) t -> p c t", p=128)[:, :, t0:t0 + TT]), s_x[xi], writes=xb)
            rmsnorm(lambda c: xr[:, c, :], xb, "g_mix", lambda c: actA[:, c, :], B_A, TT)

            yield "n1"
            P.tile = gi

            P.phase = "w_in"
            pend = {}

            def ev_conv(cb, ps, psb):
                ch, isgate = divmod(cb, 2)
                if not isgate:
                    pend["val"] = (ps, psb)
                    return
                vps, vpsb = pend.pop("val")
                tsg, tsgb = Tt(ch % 2)
                ACT(tsg, ps[:, :], AF.Sigmoid, psb, tsgb)
                TTop("dve", ubuf[:, ch, HIST:HIST + TT], vps[:, :], tsg, ALU.mult, vpsb + tsgb, [B_u[ch]])
            proj_typeA([0, 1], lambda kc: actA[:, kc, :], B_A, 8, TT, ev_conv)

            yield "a"
            P.tile = gi

            def ev_shift(j, m, ps, psb, dst, dst_b):
                q = j % 2
                Bq, Bqb = Bbuf[q], B_Bbuf[q]
                ACT(Bq[0:m, 1:TT + 1], ps[0:m, :], AF.Identity, psb + [B_const], [Bqb], scale=cvc("mu", j, slice(0, m)))
                CP("dve", Bq[0:m, 0:1], carry[0:m, j:j + 1], [B_carry[j]], [Bqb])
                STT("dve", dst, ps[0:m, :], cvx[0:m, j:j + 1], Bq[0:m, 0:TT], ALU.mult, ALU.add,
                    psb + [Bqb, B_const], dst_b)
                CP("dve", carry[0:m, j:j + 1], Bq[0:m, TT:TT + 1], [Bqb], [B_carry[j]])

            ws, wsb = w_next(5)
            for cb, m in ((0, 32), (1, 32), (2, 96)):
                ps, psb = ps_alloc(512)
                for kc in range(8):
                    MM(ps[0:m, :], wA(ws, cb, kc, m), actA[:, kc, :], kc == 0, kc == 7, [wsb, B_A[kc]], psb)
                tz, tzb = Tt(2 + cb)
                ev_shift(12 + cb, m, ps, psb, tz[0:m, :], tzb)
                func = (AF.Tanh, AF.Copy, AF.Sigmoid)[cb]
                ACT(lob[0:m, cb, :], tz[0:m, :], func, tzb, [B_lob[cb]])

            RKV_MAP = [0, 1, 4, 5, 8, 9, 2, 3, 6, 7, 10, 11]

            def ev_rkv(cb, ps, psb):
                idx = RKV_MAP[cb]
                dst, dst_b = zrkv(idx)
                ev_shift(idx, 128, ps, psb, dst, dst_b)
            proj_typeA([2, 3], lambda kc: actA[:, kc, :], B_A, 8, TT, ev_rkv)

            def rkv_tail():
                ws4, ws4b = w_next(4)
                fs = []
                for cb in range(4):
                    def f(cb=cb):
                        ps, psb = ps_alloc(512)
                        for kc in range(8):
                            MM(ps[:, :], wA(ws4, cb, kc), actA[:, kc, :], kc == 0, kc == 7, [ws4b, B_A[kc]], psb)
                        ev_rkv(8 + cb, ps, psb)
                    fs.append(f)
                return fs

            def conv_chunk(c):
                ws, wsb = w_next(32 + c)
                ps, psb = ps_alloc(512)
                for k in range(CONV_K):
                    MM(ps[:, :], ws[:, k * 128:(k + 1) * 128], ubuf[:, c, k:k + TT], k == 0, k == CONV_K - 1,
                       [wsb, B_u[c]], psb)
                ACT(cacc[:, c, :], ps[:, :], AF.Identity, psb + [B_const], [B_cacc[c]], bias=cvc("conv_b", c))
                CP("dve", ubuf[:, c, 0:HIST], ubuf[:, c, TT:TT + HIST], [B_u[c]], [B_u[c]])

            def conv_ln():
                psm, psmb = ps_alloc(512)
                pse, pseb = ps_alloc(512)
                for c in range(4):
                    q = rr["sq"] % 2
                    rr["sq"] += 1
                    CP("act", sqr[q][:], cacc[:, c, :], [B_cacc[c]], [B_sqr[q]])
                    MM(psm[:, :], o512[:], sqr[q][:], c == 0, c == 3, [B_sqr[q], B_const], psmb)
                    q = rr["sq"] % 2
                    rr["sq"] += 1
                    ACT(sqr[q][:], cacc[:, c, :], AF.Square, [B_cacc[c]], [B_sqr[q]])
                    MM(pse[:, :], o512[:], sqr[q][:], c == 0, c == 3, [B_sqr[q], B_const], pseb)
                tm2, tm2b = cln[:, 0, :], [B_cln[0]]
                ACT(tm2, psm[:, :], AF.Square, psmb, tm2b)
                TTop("dve", tm2, pse[:, :], tm2, ALU.subtract, pseb + tm2b, tm2b)
                ACT(tm2, tm2, AF.Ln, tm2b + [B_const], tm2b, bias=EPS_LN, scale=1.0)
                ACT(tm2, tm2, AF.Exp, tm2b, tm2b, scale=-0.5)
                tmean, tmeanb = cln[:, 1, :], [B_cln[1]]
                CP("act", tmean, psm[:, :], psmb, tmeanb)
                for c in range(4):
                    TTop("pool", cacc[:, c, :], cacc[:, c, :], tmean, ALU.subtract, [B_cacc[c]] + tmeanb, [B_cacc[c]])
                    TTop("pool", cacc[:, c, :], cacc[:, c, :], tm2, ALU.mult, [B_cacc[c]] + tm2b, [B_cacc[c]])
                    ACT(actB[:, c, :], cacc[:, c, :], AF.Silu, [B_cacc[c], B_const], [B_Bc[c]],
                        bias=cvc("ln_b", c), scale=cvc("ln_g", c))

            P.phase = "rwkv"
            P.phase = "rwkv_ew"

            def ew_steps(hg, lp, T):
                opa, opr, opb, opk, tok = opa_h[hg], opr_h[hg], opb_h[hg], opk_h[hg], tok_h[hg]
                B_opa, B_opr, B_opb, B_opk, B_tok = B_opa_h[hg], B_opr_h[hg], B_opb_h[hg], B_opk_h[hg], B_tok_h[hg]
                pr = 2 * hg + lp
                pc = slice(pr * 128, (pr + 1) * 128)
                zr, zrb = zrkv(pr)
                zk, zkb = zrkv(4 + pr)
                zv, zvb = zrkv(8 + pr)
                sgw, sgwb = T[0]
                cum, cumb = T[1]
                E1, E1b = T[2]
                av, avb = T[3]
                kk, kkb = T[4]
                rn, rnb = T[5]
                kp, kpb = T[6]
                E3, E3b = sgw, sgwb
                E2, E2b = cum, cumb
                ob, ok = opb[:, lp, :], opk[:, lp, :]
                st = []

                def s1():
                    ps, psb = ps_alloc(512)
                    MM(ps[:, :], lorab[0:32, 0, pc], lob[0:32, 0, :], True, True, [B_const, B_lob[0]], psb)
                    ACT(sgw, ps[:, :], AF.Sigmoid, psb + [B_const], sgwb, bias=cvc("w0", pr))
                    ps, psb = ps_alloc(512)
                    MM(ps[:, :], lorab[0:32, 1, pc], lob[0:32, 1, :], True, True, [B_const, B_lob[1]], psb)
                    ACT(av, ps[:, :], AF.Sigmoid, psb + [B_const], avb, bias=cvc("a0", pr))
                st.append(s1)

                def s2():
                    P.op("dve", lambda e: e.tensor_tensor_scan(
                        out=cum, data0=rmask[:], data1=sgw, initial=0.0, op0=ALU.mult, op1=ALU.add),
                        sgwb + [B_const], cumb)
                    ACT(kk, zk, AF.Identity, zkb + [B_const], kkb, scale=cvc("k_k", pr))
                st.append(s2)

                def s3():
                    ACT(E1, cum, AF.Exp, cumb, E1b, scale=-DEC)
                    TTop("dve", E3, cum, sgw, ALU.subtract, cumb + sgwb, E3b)
                    q = rr["sq"] % 2
                    rr["sq"] += 1
                    ACT(sqr[q][:], kk, AF.Square, kkb, [B_sqr[q]])
                    ps, psb = ps_alloc(512)
                    MM(ps[:, :], blk1[:], sqr[q][:], True, True, [B_const, B_sqr[q]], psb)
                    TS("dve", rn, ps[:, :], 1e-24, None, ALU.max, None, psb, rnb)
                st.append(s3)

                def s4():
                    ACT(E3, E3, AF.Exp, E3b, E3b, scale=-DEC)
                    ACT(E2, cum, AF.Exp, cumb, E2b, scale=DEC)
                    ACT(rn, rn, AF.Ln, rnb, rnb)
                    ACT(rn, rn, AF.Exp, rnb, rnb, scale=-0.5)
                    ACT(kp, av, AF.Identity, avb + [B_const], kpb, bias=cvx[:, 15 + pr:16 + pr], scale=cvc("k_a", pr))
                st.append(s4)

                def s5():
                    CP("pool", wcb[:, pr, :], E1.rearrange("p (c t) -> p c t", t=128)[:, :, 127], E1b, [B_wc[pr]])
                    TTop("dve", kk, kk, rn, ALU.mult, kkb + rnb, kkb)
                    TTop("pool", kp, kp, zk, ALU.mult, kpb + zkb, kpb)
                st.append(s5)

                def s6():
                    for hf in range(2):
                        rows = slice(hf * 64, (hf + 1) * 64)
                        STT("dve", opa[rows, hf, lp, :], kk[rows, :], -1.0, E3[rows, :], ALU.mult, ALU.mult,
                            kkb + E3b, [B_opa[lp]])
                    TTop("pool", rn, kk, av, ALU.mult, kkb + avb, rnb)
                    TTop("pool", ok, kp, E2, ALU.mult, kpb + E2b, [B_opk[lp]])
                st.append(s6)

                def s7():
                    TTop("dve", ob, rn, E2, ALU.mult, rnb + E2b, [B_opb[lp]])
                    for hf in range(2):
                        rows = slice(hf * 64, (hf + 1) * 64)
                        TTop("pool", opr[rows, hf, lp, :], zr[rows, :], E1[rows, :], ALU.mult, zrb + E1b, [B_opr[lp]])
                    q = rr["sq"] % 2
                    rr["sq"] += 1
                    STT("dve", sqr[q][:], zr, cvc("r_k", pr), kp, ALU.mult, ALU.mult,
                        zrb + kpb + [B_const], [B_sqr[q]])
                    ps, psb = ps_alloc(512)
                    MM(ps[:, :], blk1[:], sqr[q][:], True, True, [B_const, B_sqr[q]], psb)
                    TTop("dve", bonus[:, pr, :], ps[:, :], zv, ALU.mult, psb + zvb, [B_bonus[pr]])
                st.append(s7)

                def s8():
                    for kind, (src, srcb) in enumerate(((ob, [B_opb[lp]]), (ok, [B_opk[lp]]), (zv, zvb))):
                        ps, psb = ps_alloc(256)
                        psv = ps.bitcast(BF16)
                        for c in range(4):
                            P.op("pe", lambda e, psv=psv, src=src, c=c: e.transpose(
                                psv[:, c * 128:(c + 1) * 128], src[:, c * 128:(c + 1) * 128], ident[:]),
                                srcb + [B_const], psb)
                        CP("act" if kind != 1 else "dve",
                           tok[:, kind, lp, :, :].rearrange("p a b -> p (a b)"), psv, psb, [B_tok[kind][lp]])
                st.append(s8)
                return st

            def actA_tmp(i):
                return (actA[:, 2 * i:2 * i + 2, :].rearrange("p a b -> p (a b)").bitcast(F32),
                        [B_A[2 * i], B_A[2 * i + 1]])
            Tset0 = [Tt(i) for i in range(7)]
            Tset1 = [Tt(7), Tt(8), Tt(9)] + [actA_tmp(i) for i in range(4)]

            P.phase = "wkv"

            def chain_stages(hg, c, mats, B_m):
                opa, opr, opb, opk, tok = opa_h[hg], opr_h[hg], opb_h[hg], opk_h[hg], tok_h[hg]
                B_opa, B_opr, B_opb, B_opk, B_tok = B_opa_h[hg], B_opr_h[hg], B_opb_h[hg], B_opk_h[hg], B_tok_h[hg]
                cc = slice(c * 128, (c + 1) * 128)
                fl = lambda t: t[:].rearrange("p a b -> p (a b)")

                def opnd(kind, lp, hf):
                    if kind == "a":
                        return opa[:, hf, lp, cc], B_opa[lp]
                    if kind == "r":
                        return opr[:, hf, lp, cc], B_opr[lp]
                    if kind == "b":
                        return opb[:, lp, cc], B_opb[lp]
                    return opk[:, lp, cc], B_opk[lp]

                def amat(nm, kl, kr_, msk, direct):
                    ps, psb = ps_alloc(512)
                    for hh in range(4):
                        lp, hf = divmod(hh, 2)
                        lh, lhb = opnd(kl, lp, hf)
                        rh, rhb = opnd(kr_, lp, hf)
                        MM(ps[:, hh * 128:(hh + 1) * 128], lh, rh, True, True, [lhb, rhb], psb)
                    if direct:
                        TTop("dve", fl(mats[nm]), ps[:, :], fl(msk), ALU.mult, psb + [B_const], [B_m[nm]])
                    else:
                        CP("act", fl(mats[nm]), ps[:, :], psb, [B_m[nm]])
                        TTop("pool", fl(mats[nm]), fl(mats[nm]), fl(msk), ALU.mult, [B_m[nm], B_const], [B_m[nm]])

                def stA():
                    amat("X0", "b", "a", mSU, True)
                    amat("XT0", "a", "b", mSL, True)
                    TTop("pool", mats["P0"][:], mats["X0"][:], ident4[:], ALU.add, [B_m["X0"], B_const], [B_m["P0"]])
                    amat("Aak", "k", "a", mSU, False)
                    amat("Arb", "b", "r", mIU, False)
                    amat("Ark", "k", "r", mIU, False)

                def stL(lvl):
                    cur = (lvl - 1) % 2
                    Xc, XTc, Pc = "X%d" % cur, "XT%d" % cur, "P%d" % cur
                    Xn, XTn, Pn = "X%d" % (1 - cur), "XT%d" % (1 - cur), "P%d" % (1 - cur)
                    ps2, ps2b = ps_alloc(512)
                    for hh in range(4):
                        MM(ps2[:, hh * 128:(hh + 1) * 128], mats[Xc][:, hh, :], mats[XTc][:, hh, :], True, True,
                           [B_m[Xc], B_m[XTc]], ps2b)
                    if lvl < 6:
                        ps1, ps1b = ps_alloc(512)
                        for hh in range(4):
                            MM(ps1[:, hh * 128:(hh + 1) * 128], mats[XTc][:, hh, :], mats[Xc][:, hh, :], True, True,
                               [B_m[Xc], B_m[XTc]], ps1b)
                    if NWARM:
                        pe_warm(NWARM)
                    CP("act", fl(mats[XTn]), ps2[:, :], ps2b, [B_m[XTn]])
                    if lvl < 6:
                        CP("act" if lvl % 2 == 0 else "dve", fl(mats[Xn]), ps1[:, :], ps1b, [B_m[Xn]])
                    ps3, ps3b = ps_alloc(512)
                    for hh in range(4):
                        MM(ps3[:, hh * 128:(hh + 1) * 128], mats[XTn][:, hh, :], mats[Pc][:, hh, :], True, True,
                           [B_m[XTn], B_m[Pc]], ps3b)
                    TTop("dve", fl(mats[Pn]), ps3[:, :], fl(mats[Pc]), ALU.add, ps3b + [B_m[Pc]], [B_m[Pn]])

                Pfin = "P0"

                def stS1():
                    for lp in range(2):
                        pr = 2 * hg + lp
                        bS = B_Sbf[pr]
                        ACT(tmpS[:, pr, :], S32[:, pr, :], AF.Identity, [B_S32[pr], B_wc[pr]], [B_tmpS[pr]],
                            scale=wcb[:, pr, c:c + 1])
                        psR, psRb = ps_alloc(128)
                        for hf in range(2):
                            hh = 2 * lp + hf
                            MM(psR[:, hf * 64:(hf + 1) * 64], opa[:, hf, lp, cc], Sbf[:, pr, :], True, False,
                               [B_opa[lp], bS], psRb)
                            MM(psR[:, hf * 64:(hf + 1) * 64], mats["Aak"][:, hh, :], tok[:, 2, lp, c, hf * 64:(hf + 1) * 64],
                               False, True, [B_m["Aak"], B_tok[2][lp]], psRb)
                        CP("act" if lp else "dve", RHSb[:, lp, :, :].rearrange("p a b -> p (a b)"), psR, psRb, [B_RHS[lp]])

                def stS2():
                    for lp in range(2):
                        psU, psUb = ps_alloc(128)
                        for hf in range(2):
                            hh = 2 * lp + hf
                            MM(psU[:, hf * 64:(hf + 1) * 64], mats[Pfin][:, hh, :], RHSb[:, lp, hf, :], True, True,
                               [B_m[Pfin], B_RHS[lp]], psUb)
                        CP("dve" if lp else "act", Ub[:, lp, :, :].rearrange("p a b -> p (a b)"), psU, psUb, [B_U[lp]])

                def stS3():
                    for lp in range(2):
                        pr = 2 * hg + lp
                        bS = B_Sbf[pr]
                        psY, psYb = ps_alloc(128)
                        psS, psSb = ps_alloc(128)
                        for hf in range(2):
                            hh = 2 * lp + hf
                            rows = slice(hf * 64, (hf + 1) * 64)
                            vt = tok[:, 2, lp, c, hf * 64:(hf + 1) * 64]
                            MM(psY[rows, :], Sbf[:, pr, :], opr[:, hf, lp, cc], True, False,
                               [bS, B_opr[lp]], psYb, tp=(0, hf * 64))
                            MM(psY[rows, :], Ub[:, lp, hf, :], mats["Arb"][:, hh, :], False, False,
                               [B_U[lp], B_m["Arb"]], psYb, tp=(0, hf * 64))
                            MM(psY[rows, :], vt, mats["Ark"][:, hh, :], False, True,
                               [B_tok[2][lp], B_m["Ark"]], psYb, tp=(0, hf * 64))
                            MM(psS[rows, 0:64], tok[:, 0, lp, c, hf * 64:(hf + 1) * 64], Ub[:, lp, hf, :], True, False,
                               [B_tok[0][lp], B_U[lp]], psSb, tp=(0, hf * 64))
                            MM(psS[rows, 0:64], tok[:, 1, lp, c, hf * 64:(hf + 1) * 64], vt, False, True,
                               [B_tok[1][lp], B_tok[2][lp]], psSb, tp=(0, hf * 64))
                        STT("dve", Sbf[:, pr, :], psS[:, 0:64], wcb[:, pr, c:c + 1], tmpS[:, pr, :], ALU.mult, ALU.add,
                            psSb + [B_wc[pr], B_tmpS[pr]], [bS])
                        STT("dve", S32[:, pr, :], psS[:, 0:64], wcb[:, pr, c:c + 1], tmpS[:, pr, :], ALU.mult, ALU.add,
                            psSb + [B_wc[pr], B_tmpS[pr]], [B_S32[pr]])
                        CP("act", ybuf[:, lp, cc], psY, psYb, [B_y[lp]])
                return [stA] + [(lambda l=l: stL(l)) for l in range(1, 7)] + [(stS1, stS2, stS3)]

            def run_wkv_all(fillers):
                P.phase = "wkv"
                fillers = list(fillers)
                chains = [chain_stages(i // 4, i % 4, matsets[i % NSET], B_msets[i % NSET]) for i in range(8)]
                STAG = 8 // NSET
                posn = [0] * 8
                step = 0
                while any(p < 8 for p in posn):
                    deferred = []
                    for i in range(8):
                        if step >= i * STAG and posn[i] < 8:
                            stg = chains[i][posn[i]]
                            if isinstance(stg, tuple):
                                stg[0]()
                                deferred = list(stg[1:])
                            else:
                                stg()
                                if deferred:
                                    deferred.pop(0)()
                            posn[i] += 1
                    for f in deferred:
                        f()
                    if step < len(fillers) and fillers[step] is not None:
                        fillers[step]()
                    step += 1
                for f in fillers[step:]:
                    if f is not None:
                        f()

            def gn_steps(hg):
                st = []
                g1, g1b = Tt(0)
                g2, g2b = Tt(1)
                for lp in range(2):
                    pr = 2 * hg + lp
                    pc = slice(pr * 128, (pr + 1) * 128)
                    yv = ybuf[:, lp, :]

                    def ga(lp=lp, pr=pr, pc=pc, yv=yv):
                        q = rr["sq"] % 2
                        rr["sq"] += 1
                        CP("act", sqr[q][:], yv, [B_y[lp]], [B_sqr[q]])
                        psm, psmb = ps_alloc(512)
                        MM(psm[:, :], blk64[:], sqr[q][:], True, True, [B_const, B_sqr[q]], psmb)
                        q = rr["sq"] % 2
                        rr["sq"] += 1
                        ACT(sqr[q][:], yv, AF.Square, [B_y[lp]], [B_sqr[q]])
                        pse, pseb = ps_alloc(512)
                        MM(pse[:, :], blk64[:], sqr[q][:], True, True, [B_const, B_sqr[q]], pseb)
                        ACT(g1, psm[:, :], AF.Square, psmb, g1b)
                        TTop("dve", g2, yv, psm[:, :], ALU.subtract, [B_y[lp]] + psmb, g2b)
                        TTop("dve", g1, pse[:, :], g1, ALU.subtract, pseb + g1b, g1b)
                        ACT(g1, g1, AF.Ln, g1b + [B_const], g1b, bias=EPS_GN, scale=1.0)
                        ACT(g1, g1, AF.Exp, g1b, g1b, scale=-0.5)

                    def gb(lp=lp, pr=pr, pc=pc):
                        TTop("pool", g2, g2, g1, ALU.mult, g2b + g1b, g2b)
                        ACT(g2, g2, AF.Identity, g2b + [B_const], g2b, bias=cvc("lnx_b", pr), scale=cvc("lnx_g", pr))
                        TTop("pool", g2, g2, bonus[:, pr, :], ALU.add, g2b + [B_bonus[pr]], g2b)
                        psg, psgb = ps_alloc(512)
                        MM(psg[:, :], lorab[0:96, 2, pc], lob[0:96, 2, :], True, True, [B_const, B_lob[2]], psgb)
                        TTop("dve", actB[:, 4 + pr, :], g2, psg[:, :], ALU.mult, g2b + psgb, [B_Bc[4 + pr]])
                    st += [ga, gb]
                return st

            P.phase = "rwkv_ew"
            rt = rkv_tail()
            cvf = [(lambda c=c: conv_chunk(c)) for c in range(4)]
            fill = {0: rt, 2: cvf[0:2], 3: cvf[2:4]}
            Tset1a = [Tt(7), Tt(8), Tt(9), (ybuf[:, 0, :], [B_y[0]]), (ybuf[:, 1, :], [B_y[1]]),
                      (PT[:].rearrange("p a b -> p (a b)").bitcast(F32), [B_PT[0], B_PT[1]]), (rsb[:], [B_rs])]
            for k_, (f0, f1) in enumerate(zip(ew_steps(0, 0, Tset0), ew_steps(0, 1, Tset1a))):
                f0()
                f1()
                for f in fill.get(k_, ()):
                    f()
            ew1 = [f for pair in zip(ew_steps(1, 0, Tset0), ew_steps(1, 1, Tset1)) for f in pair]
            assert len(ew1) == 16
            STAG_ = 8 // NSET
            first_hg1 = 4 * STAG_
            last_s_hg0 = 3 * STAG_ + 7
            fl = [conv_ln] + ew1 + [None] * (last_s_hg0 + 1 - len(ew1) - 1) + gn_steps(0)
            assert len(ew1) + 1 <= first_hg1 + 1
            run_wkv_all(fl)
            P.phase = "gn"
            for f in gn_steps(1):
                f()

            P.phase = "w_out"
            def ev_res(cb, ps, psb):
                TTop("dve", xr[:, cb, :], xr[:, cb, :], ps[:, :], ALU.add, [xb[cb]] + psb, [xb[cb]])
            proj_typeA([6, 7], lambda kc: actB[:, kc, :], B_Bc, 8, TT, ev_res)

            P.phase = "xattn"
            rmsnorm(lambda c: xr[:, c, :], xb, "g_cross", lambda c: actA[:, c, :], B_A, TT)

            def ev_q(cb, ps, psb):
                CP("act" if cb % 2 else "dve", actB[:, cb, :], ps[:, :], psb, [B_Bc[cb]])
            proj_typeA([8, 9], lambda kc: actA[:, kc, :], B_A, 8, TT, ev_q)
            for hd in range(4):
                for mc in range(2):
                    ps, psb = ps_alloc(512)
                    for dc in range(2):
                        MM(ps[:, :], KT[:, 2 * hd + dc, mc * 128:(mc + 1) * 128], actB[:, 2 * hd + dc, :],
                           dc == 0, dc == 1, [B_KT, B_Bc[2 * hd + dc]], psb)
                    ACT(PT[:, mc, :], ps[:, :], AF.Exp, psb, [B_PT[mc]], scale=1.0 / 16.0)
                ps, psb = ps_alloc(512)
                for mc in range(2):
                    MM(ps[:, :], onesb[:], PT[:, mc, :], mc == 0, mc == 1, [B_const, B_PT[mc]], psb)
                ACT(rsb[:], ps[:, :], AF.Ln, psb, [B_rs])
                ACT(rsb[:], rsb[:], AF.Exp, [B_rs], [B_rs], scale=-1.0)
                for dvc in range(2):
                    ch = 2 * hd + dvc
                    ps, psb = ps_alloc(512)
                    for mc in range(2):
                        MM(ps[:, :], Vt[:, mc, ch * 128:(ch + 1) * 128], PT[:, mc, :], mc == 0, mc == 1,
                           [B_V, B_PT[mc]], psb)
                    TTop("dve", actA[:, ch, :], ps[:, :], rsb[:], ALU.mult, psb + [B_rs], [B_A[ch]])
            proj_typeA([10, 11], lambda kc: actA[:, kc, :], B_A, 8, TT, ev_res)

            P.phase = "ffn"
            rmsnorm(lambda c: xr[:, c, :], xb, "g_ffn", lambda c: actB[:, c, :], B_Bc, TT)

            def ev_ff1(cb, ps, psb):
                f, fb = fT(cb)
                ACT(f, ps[:, :], AF.Relu, psb, fb)
                TTop("pool" if cb % 2 else "dve", f, f, f, ALU.mult, fb, fb)
            proj_typeA(list(range(12, 20)), lambda kc: actB[:, kc, :], B_Bc, 32, TT, ev_ff1)

            yield "b"
            P.tile = gi
            for cb in range(8):
                ws, wsb = w_next(20 + cb)
                ps, psb = ps_alloc(512)
                for kc in range(32):
                    f, fb = fT(kc)
                    MM(ps[:, :], ws[:, kc * 128:(kc + 1) * 128], f, kc == 0, kc == 31, [wsb] + fb, psb)
                ev_res(cb, ps, psb)

            yield "c"
            P.tile = gi
            P.phase = "final"
            rmsnorm(lambda c: xr[:, c, :], xb, "g_final", lambda c: xr[:, c, :], xb, TT)
            P.dma("sp", lambda e, s=s, t0=t0, xr=xr: e.dma_start(
                out=oT[s].rearrange("(c p) t -> p c t", p=128)[:, :, t0:t0 + TT], in_=xr[:]), s_o[xi], reads=xb)
            yield "f"

        order = [(s_, t_) for s_ in range(nseq) for t_ in range(NT)]
        gens = {}

        def start(idx):
            g = tile_gen(*order[idx])
            gens[idx] = g
            next(g)
        prev_final = None
        for idx, (s_, t_) in enumerate(order):
            if t_ == 0:
                seq_prologue(s_)
                start(idx)
            g = gens.pop(idx)
            next(g)
            if prev_final is not None:
                next(prev_final)
                prev_final = None
            next(g)
            if idx + 1 < len(order) and order[idx + 1][1] != 0:
                start(idx + 1)
            next(g)
            prev_final = g
        next(prev_final)
        P.wait_all("sp", B_x[0] + B_x[1])
        P.emit()
    return nc


def prep_inputs(inputs, nseq_per_core, ncores, seqlen):
    x = np.asarray(inputs["x"], np.float32)
    mem = np.asarray(inputs["mem"], np.float32)
    cv = pack_cvec(inputs)
    blocks = pack_blocks(inputs)
    lora = pack_lora(inputs)
    in_maps = []
    for c in range(ncores):
        sl = slice(c * nseq_per_core, (c + 1) * nseq_per_core)
        in_maps.append({
            "xT": np.ascontiguousarray(x[sl, :seqlen].transpose(0, 2, 1)),
            "memT": np.ascontiguousarray(mem[sl].transpose(0, 2, 1)),
            "wblk": blocks, "cvec": cv, "lora": lora,
        })
    return in_maps


def kernel(**inputs):
    nseq = BATCH // NCORES
    nc = build(nseq, SEQ)
    in_maps = prep_inputs(inputs, nseq, NCORES, SEQ)
    res = run_bass_kernel_spmd(nc, in_maps, core_ids=list(range(NCORES)))
    outs = [np.asarray(r["oT"]).transpose(0, 2, 1) for r in res.results]
    return np.ascontiguousarray(np.concatenate(outs, axis=0)).astype(np.float32)
```

```python
import contextlib
import numpy as np
import concourse.bass as bass
import concourse.mybir as mybir
from concourse.bass_utils import run_bass_kernel_spmd

F32 = mybir.dt.float32
BF16 = mybir.dt.bfloat16
ALU = mybir.AluOpType
AF = mybir.ActivationFunctionType

D = 1024
SEQ = 2048
BATCH = 32
NCORES = 8
TT = 512
MEM = 256
CW = 512
RW = 512
CONV_K = 31
HIST = CONV_K - 1
DFF = 4096
NBLK = 36
NHOST = 32
BLK = 4096
NSLOT = 3
NSET = 2
NWARM = 2
DEC = 0.6065306597126334

ENGS = ("pe", "act", "dve", "pool", "sp")


class Buf:
    __slots__ = ("name", "w", "r", "excl")

    def __init__(self, name="", excl=False):
        self.name = name
        self.w = None
        self.r = []
        self.excl = excl


class Prog:
    tile = 0

    @property
    def phase(self):
        return "t%d:%s" % (self.tile, self._phase)

    @phase.setter
    def phase(self, v):
        self._phase = v

    def __init__(self, nc, stack):
        self.nc = nc
        self.stack = stack
        self.ops = {e: [] for e in ENGS}
        self.cnt = {e: 0 for e in ENGS}
        self.waited = {e: {} for e in ENGS}
        self.sems = {}
        self.semval = {}
        for e in ENGS:
            self.sems[e] = stack.enter_context(nc.semaphore("s_" + e))
        self.n_dma_sems = 0
        self.phase = "init"
        self.annotate = False
        self.hazard = 10 ** 9

    def dma_sem(self, name=None):
        key = "dma%d" % self.n_dma_sems
        self.n_dma_sems += 1
        self.sems[key] = self.stack.enter_context(self.nc.semaphore(name or key))
        self.semval[key] = 0
        return key

    def _deps(self, eng, reads, writes):
        need = {}

        def add(dep):
            if dep is None:
                return
            k, v = dep
            if need.get(k, -1) < v:
                need[k] = v
        for b in reads:
            add(b.w)
        for b in writes:
            add(b.w)
            for d in b.r:
                add(d)
        out = []
        wd = self.waited[eng]
        for k, v in need.items():
            if k == eng:
                if eng in ("pe", "sp") or v <= self.cnt[eng] - self.hazard:
                    continue
            if wd.get(k, -1) >= v:
                continue
            wd[k] = v
            out.append((k, v))
        return out

    def _record(self, me, reads, writes):
        for b in reads:
            if len(b.r) > 24:
                best = {}
                for k, v in b.r:
                    if best.get(k, -1) < v:
                        best[k] = v
                b.r = list(best.items())
            b.r.append(me)
        for b in writes:
            b.w = me
            b.r = []

    @staticmethod
    def _split(reads, writes):
        if any(b.excl for b in reads):
            writes = list(writes) + [b for b in reads if b.excl]
            reads = [b for b in reads if not b.excl]
        return reads, writes

    def op(self, eng, fn, reads=(), writes=()):
        reads, writes = self._split(reads, writes)
        waits = self._deps(eng, reads, writes)
        self.cnt[eng] += 1
        me = (eng, self.cnt[eng])
        self.ops[eng].append((fn, waits, (eng, 1), self.phase))
        self._record(me, reads, writes)
        return me

    def dma(self, eng, fn, semkey, reads=(), writes=()):
        reads, writes = self._split(reads, writes)
        waits = self._deps(eng, reads, writes)
        self.semval[semkey] += 16
        me = (semkey, self.semval[semkey])
        self.ops[eng].append((fn, waits, (semkey, 16), self.phase))
        self._record(me, reads, writes)
        return me

    def wait_all(self, eng, bufs):
        waits = self._deps(eng, [], bufs)
        self.ops[eng].append((None, waits, None, self.phase))

    def emit(self):
        nc = self.nc
        handles = {"pe": "tensor", "act": "scalar", "dve": "vector", "pool": "gpsimd", "sp": "sync"}
        with nc.Block() as block:
            for e in ENGS:
                ops = self.ops[e]
                if not ops:
                    continue

                def body(engh, ops=ops):
                    for fn, waits, inc, phase in ops:
                        for k, v in waits:
                            engh.wait_ge(self.sems[k], v)
                        if fn is not None:
                            ins = fn(engh)
                            if inc is not None:
                                ins.then_inc(self.sems[inc[0]], inc[1])
                            if self.annotate:
                                ins.annotate(phase)
                getattr(block, handles[e])(body)


CV = {}
_off = 0
for _n, _w in [("g_mix", 8), ("g_cross", 8), ("g_mem", 8), ("g_ffn", 8), ("g_final", 8),
               ("conv_b", 4), ("ln_g", 4), ("ln_b", 4), ("conv_w", 124), ("mu", 15),
               ("w0", 4), ("a0", 4), ("k_k", 4), ("k_a", 4), ("r_k", 4), ("lnx_g", 4), ("lnx_b", 4)]:
    CV[_n] = _off
    _off += _w
NCV = _off


def _cm(v, nch):
    return np.ascontiguousarray(np.asarray(v, np.float32).reshape(nch, 128).T)


def pack_cvec(inp):
    cv = np.zeros((128, NCV), np.float32)

    def put(name, arr):
        cv[:, CV[name]:CV[name] + arr.shape[1]] = arr
    put("g_mix", _cm(inp["g_mix"][0], 8))
    put("g_cross", _cm(inp["g_cross"][0], 8))
    put("g_mem", _cm(inp["g_mem"][0], 8))
    put("g_ffn", _cm(inp["g_ffn"][0], 8))
    put("g_final", _cm(inp["g_final"], 8))
    put("conv_b", _cm(inp["conv_b"][0], 4))
    put("ln_g", _cm(inp["conv_ln_g"][0], 4))
    put("ln_b", _cm(inp["conv_ln_b"][0], 4))
    cw = np.asarray(inp["conv_w"][0], np.float32)
    cwp = cw.reshape(CONV_K, 4, 128).transpose(2, 0, 1)
    put("conv_w", np.ascontiguousarray(cwp.reshape(128, CONV_K * 4)))
    mu = np.asarray(inp["mu_b"][0], np.float32)
    m = np.zeros((128, 15), np.float32)
    m[:, 0:4] = _cm(mu[0:512], 4)
    m[:, 4:8] = _cm(mu[512:1024], 4)
    m[:, 8:12] = _cm(mu[1024:1536], 4)
    m[0:32, 12] = mu[1536:1568]
    m[0:32, 13] = mu[1568:1600]
    m[0:96, 14] = mu[1600:1696]
    put("mu", m)
    for nm in ("w0", "a0", "k_k", "k_a", "r_k", "lnx_g", "lnx_b"):
        put(nm, _cm(inp[nm][0], 4))
    return cv


def pack_blocks(inp):
    out = np.zeros((NHOST, 128, BLK), np.float32)

    def typeA(W, cols):
        blk = np.zeros((128, 4, 8, 128), np.float32)
        Wr = W.reshape(8, 128, -1)
        for cb, (st, wd) in enumerate(cols):
            blk[:, cb, :, :wd] = Wr[:, :, st:st + wd].transpose(1, 0, 2)
        return blk.reshape(128, BLK)

    w_in = np.asarray(inp["w_in"][0], np.float32)
    out[0] = typeA(w_in, [(0, 128), (512, 128), (128, 128), (640, 128)])
    out[1] = typeA(w_in, [(256, 128), (768, 128), (384, 128), (896, 128)])
    R_, K_, V_ = 1024, 1536, 2048
    col = lambda base, j: (base + 128 * j, 128)
    out[2] = typeA(w_in, [col(R_, 0), col(R_, 1), col(K_, 0), col(K_, 1)])
    out[3] = typeA(w_in, [col(V_, 0), col(V_, 1), col(R_, 2), col(R_, 3)])
    out[4] = typeA(w_in, [col(K_, 2), col(K_, 3), col(V_, 2), col(V_, 3)])
    out[5] = typeA(w_in, [(2560, 32), (2592, 32), (2624, 96)])
    for j, nm in enumerate(("w_out", "wq", "wo")):
        W = np.asarray(inp[nm][0], np.float32)
        for h in range(2):
            out[6 + 2 * j + h] = typeA(W, [(512 * h + 128 * i, 128) for i in range(4)])
    W = np.asarray(inp["w_ff1"][0], np.float32)
    for b in range(8):
        out[12 + b] = typeA(W, [(512 * b + 128 * i, 128) for i in range(4)])
    W = np.asarray(inp["w_ff2"][0], np.float32).reshape(32, 128, 1024)
    for b in range(8):
        out[20 + b] = W[:, :, 128 * b:128 * (b + 1)].transpose(1, 0, 2).reshape(128, BLK)
    W = np.asarray(inp["wk"][0], np.float32)
    for h in range(2):
        out[28 + h] = typeA(W, [(512 * h + 128 * i, 128) for i in range(4)])
    W = np.asarray(inp["wv"][0], np.float32).reshape(8, 128, 1024)
    for h in range(2):
        out[30 + h] = W[:, :, 512 * h:512 * (h + 1)].transpose(1, 0, 2).reshape(128, BLK)
    return out


def pack_lora(inp):
    lw = np.zeros((128, 3, 512), np.float32)
    lw[0:32, 0] = inp["w_decay2"][0]
    lw[0:32, 1] = inp["a_lora2"][0]
    lw[0:96, 2] = inp["g_lora2"][0]
    return lw


def build(nseq, seqlen, dbg=False, annotate=False):
    NT = seqlen // TT
    nc = bass.Bass("TRN2", target_bir_lowering=False)
    xT = nc.dram_tensor("xT", [nseq, D, seqlen], F32, kind="ExternalInput").ap()
    memT = nc.dram_tensor("memT", [nseq, D, MEM], F32, kind="ExternalInput").ap()
    wblk = nc.dram_tensor("wblk", [NHOST, 128, BLK], F32, kind="ExternalInput").ap()
    cvd = nc.dram_tensor("cvec", [128, NCV], F32, kind="ExternalInput").ap()
    lwd = nc.dram_tensor("lora", [128, 3, 512], F32, kind="ExternalInput").ap()
    oT = nc.dram_tensor("oT", [nseq, D, seqlen], F32, kind="ExternalOutput").ap()
    scr = nc.dram_tensor("wscr", [NBLK, 128, BLK], BF16).ap()
    dbg_out = {}

    with contextlib.ExitStack() as st:
        P = Prog(nc, st)
        P.annotate = annotate

        def sb(name, shape, dt):
            return st.enter_context(nc.sbuf_tensor(name, shape, dt))

        cv = sb("cv", [128, NCV], F32)
        cvx = sb("cvx", [128, 32], F32)
        ident = sb("ident", [128, 128], BF16)
        ident4 = sb("ident4", [128, 4, 128], BF16)
        onesb = sb("onesb", [128, 128], BF16)
        blk1 = sb("blk1", [128, 128], BF16)
        blk64 = sb("blk64", [128, 128], BF16)
        o512 = sb("o512", [128, 128], BF16)
        mSU = sb("mSU", [128, 4, 128], BF16)
        mIU = sb("mIU", [128, 4, 128], BF16)
        mSL = sb("mSL", [128, 4, 128], BF16)
        rmask = sb("rmask", [128, TT], BF16)
        lorab = sb("lorab", [128, 3, 512], BF16)
        S32 = sb("S32", [128, 4, 64], F32)
        Sbf = sb("Sbf", [128, 4, 64], BF16)
        tmpS = sb("tmpS", [128, 4, 64], F32)
        carry = sb("carry", [128, 16], F32)
        KT = sb("KT", [128, 8, MEM], BF16)
        Vt = sb("Vt", [128, 2, D], BF16)
        wslot = [sb("wslot%d" % i, [128, BLK], BF16) for i in range(NSLOT)]
        xres = [sb("xres%d" % i, [128, 8, TT], F32) for i in range(2)]
        actA = sb("actA", [128, 8, TT], BF16)
        actB = sb("actB", [128, 8, TT], BF16)
        arena = sb("arena", [128, 32 * 512], BF16)
        lob = sb("lob", [128, 3, TT], BF16)
        Bbuf = [sb("Bbuf%d" % i, [128, TT + 1], F32) for i in range(2)]
        ubuf = sb("ubuf", [128, 4, HIST + TT], BF16)
        cln = sb("cln", [128, 2, TT], F32)
        cacc = sb("cacc", [128, 4, TT], F32)
        opa_h = [sb("opa%d" % h, [128, 2, 2, TT], BF16) for h in range(2)]
        opr_h = [sb("opr%d" % h, [128, 2, 2, TT], BF16) for h in range(2)]
        opb0 = sb("opb", [128, 2, TT], BF16)
        opk0 = sb("opk", [128, 2, TT], BF16)
        tok0 = sb("tok", [128, 3, 2, 4, 128], BF16)
        bonus = sb("bonus", [128, 4, TT], BF16)
        ybuf = sb("ybuf", [128, 2, TT], F32)
        wcb = sb("wcb", [128, 4, 4], F32)
        MATN = ("X0", "X1", "XT0", "XT1", "P0", "P1", "Aak", "Arb", "Ark")
        matsets = [{nm: sb("m%d_%s" % (i, nm), [128, 4, 128], BF16) for nm in MATN} for i in range(NSET)]
        RHSb = sb("RHSb", [128, 2, 2, 64], BF16)
        Ub = sb("Ub", [128, 2, 2, 64], BF16)
        PT = sb("PT", [128, 2, TT], BF16)
        rsb = sb("rsb", [128, TT], F32)
        sqr = [sb("sqr%d" % i, [128, TT], BF16) for i in range(2)]
        psum = [st.enter_context(nc.psum_tensor("ps%d" % i, [128, 512], F32)) for i in range(8)]

        B_const = Buf("const")
        B_S32 = [Buf("S32_%d" % i) for i in range(4)]
        B_Sbf = [Buf("Sbf_%d" % i) for i in range(4)]
        B_tmpS = [Buf("tmpS%d" % i) for i in range(4)]
        B_carry = [Buf("carry%d" % i) for i in range(16)]
        B_KT = Buf("KT")
        B_V = Buf("V")
        B_wslot = [Buf("wslot%d" % i) for i in range(NSLOT)]
        B_x = [[Buf("x%d_%d" % (i, c)) for c in range(8)] for i in range(2)]
        B_A = [Buf("actA%d" % c) for c in range(8)]
        B_Bc = [Buf("actB%d" % c) for c in range(8)]
        B_ar = [Buf("ar%d" % i) for i in range(32)]
        B_lob = [Buf("lob%d" % i) for i in range(3)]
        B_Bbuf = [Buf("Bbuf0"), Buf("Bbuf1")]
        B_cln = [Buf("cln0"), Buf("cln1")]
        B_u = [Buf("u%d" % c) for c in range(4)]
        B_cacc = [Buf("cacc%d" % c) for c in range(4)]
        B_opa_h = [[Buf("opa%d_%d" % (h, lp)) for lp in range(2)] for h in range(2)]
        B_opr_h = [[Buf("opr%d_%d" % (h, lp)) for lp in range(2)] for h in range(2)]
        B_bonus = [Buf("bonus%d" % lp) for lp in range(4)]
        B_y = [Buf("y%d" % lp) for lp in range(2)]
        B_wc = [Buf("wc%d" % lp) for lp in range(4)]
        B_msets = [{nm: Buf("m%d_%s" % (i, nm)) for nm in MATN} for i in range(NSET)]
        B_RHS = [Buf("RHS%d" % lp) for lp in range(2)]
        B_U = [Buf("U%d" % lp) for lp in range(2)]
        B_PT = [Buf("PT%d" % i) for i in range(2)]
        B_rs = Buf("rs")
        B_sqr = [Buf("sqr%d" % i) for i in range(2)]
        B_ps = [Buf("psb%d" % i, excl=True) for i in range(8)]
        B_scr = [Buf("scr%d" % i) for i in range(NBLK)]

        cln_bf = cln[:].rearrange("p a b -> p (a b)").bitcast(BF16)
        cacc_bf = cacc[:].rearrange("p a b -> p (a b)").bitcast(BF16)
        opb_h = [opb0, cln_bf[:, 0:1024].rearrange("p (a b) -> p a b", b=TT)]
        opk_h = [opk0, cln_bf[:, 1024:2048].rearrange("p (a b) -> p a b", b=TT)]
        tok_h = [tok0, cacc_bf[:, 0:3072].rearrange("p (k l c n) -> p k l c n", k=3, l=2, c=4)]
        B_opb_h = [[Buf("opb0_%d" % lp) for lp in range(2)], [B_cln[0], B_cln[0]]]
        B_opk_h = [[Buf("opk0_%d" % lp) for lp in range(2)], [B_cln[1], B_cln[1]]]
        B_tok_h = [[[Buf("tok0_%d_%d" % (k, lp)) for lp in range(2)] for k in range(3)],
                   [[B_cacc[k], B_cacc[k]] for k in range(3)]]

        def zrkv(j):
            return arena[:, j * 512:(j + 1) * 512], [B_ar[j]]

        def Tt(i):
            o = (12 + 2 * i) * 512
            return arena[:, o:o + 1024].bitcast(F32), [B_ar[12 + 2 * i], B_ar[13 + 2 * i]]

        def fT(j):
            return arena[:, j * 512:(j + 1) * 512], [B_ar[j]]

        ps_pos = [0]
        NRING = 7 if NWARM else 8

        def ps_alloc(ncols):
            p = ps_pos[0]
            ps_pos[0] = (p + 1) % NRING
            return psum[p][:, 0:ncols], [B_ps[p]]

        B_dummy = Buf("dummy")

        def pe_warm(n):
            for _ in range(n):
                MM(psum[7][:, :], onesb[:], ident4[:].rearrange("p a b -> p (a b)"), True, True, [B_const], [B_dummy])

        s_const = P.dma_sem("c")
        s_w = [P.dma_sem("w%d" % i) for i in range(NSLOT)]
        s_x = [P.dma_sem("x%d" % i) for i in range(2)]
        s_o = [P.dma_sem("o%d" % i) for i in range(2)]
        s_sw = [P.dma_sem("sw%d" % i) for i in range(NSLOT)]
        s_mem = P.dma_sem("mem")
        s_wp = [P.dma_sem("wp%d" % i) for i in range(NSLOT)]

        def ACT(out, in_, func, reads, writes, bias=None, scale=None):
            kw = {}
            if bias is not None:
                kw["bias"] = bias
            if scale is not None:
                kw["scale"] = scale
            P.op("act", lambda e: e.activation(out=out, in_=in_, func=func, **kw), reads, writes)

        def TTop(eng, out, in0, in1, op, reads, writes):
            P.op(eng, lambda e: e.tensor_tensor(out=out, in0=in0, in1=in1, op=op), reads, writes)

        def TS(eng, out, in0, s1, s2, op0, op1, reads, writes):
            if s2 is None:
                P.op(eng, lambda e: e.tensor_scalar(out=out, in0=in0, scalar1=s1, scalar2=None, op0=op0), reads, writes)
            else:
                P.op(eng, lambda e: e.tensor_scalar(out=out, in0=in0, scalar1=s1, scalar2=s2, op0=op0, op1=op1), reads, writes)

        def STT(eng, out, in0, scalar, in1, op0, op1, reads, writes):
            P.op(eng, lambda e: e.scalar_tensor_tensor(out=out, in0=in0, scalar=scalar, in1=in1, op0=op0, op1=op1), reads, writes)

        def CP(eng, out, in_, reads, writes):
            if eng == "act":
                P.op("act", lambda e: e.copy(out=out, in_=in_), reads, writes)
            else:
                P.op(eng, lambda e: e.tensor_copy(out=out, in_=in_), reads, writes)

        def MM(out, lhsT, rhs, start, stop, reads, writes, tp=None):
            if tp is None:
                P.op("pe", lambda e: e.matmul(out, lhsT=lhsT, rhs=rhs, start=start, stop=stop), reads, writes)
            else:
                P.op("pe", lambda e: e.matmul(out, lhsT=lhsT, rhs=rhs, start=start, stop=stop, tile_position=tp), reads, writes)

        def MEMSET(eng, ap, val, writes):
            P.op(eng, lambda e: e.memset(ap, val), (), writes)

        def AFSEL(out, in_, pattern, cmp, base, cm, reads, writes):
            P.op("pool", lambda e: e.affine_select(out=out, in_=in_, pattern=pattern, compare_op=cmp, fill=0.0,
                                                   base=base, channel_multiplier=cm), reads, writes)

        def cvc(name, j=0, rows=slice(0, 128)):
            o = CV[name] + j
            return cv[rows, o:o + 1]

        P.dma("sp", lambda e: e.dma_start(out=cv[:], in_=cvd), s_const, writes=[B_const])
        lw32, lwB = Tt(0)
        lw32b, lwBb = Tt(1)
        lw32c, lwBc = Tt(2)
        for j, (tv, tb) in enumerate(((lw32, lwB), (lw32b, lwBb), (lw32c, lwBc))):
            P.dma("sp", lambda e, tv=tv, j=j: e.dma_start(out=tv, in_=lwd[:, j, :]), s_const, writes=tb)
        for b_ in lwB + lwBb + lwBc + [B_const]:
            b_.w = (s_const, P.semval[s_const])
        for j, (tv, tb) in enumerate(((lw32, lwB), (lw32b, lwBb), (lw32c, lwBc))):
            CP("dve", lorab[:, j, :], tv, tb, [B_const])
        TS("dve", cvx[:, 0:15], cv[:, CV["mu"]:CV["mu"] + 15], -1.0, 1.0, ALU.mult, ALU.add, [B_const], [B_const])
        TS("dve", cvx[:, 15:19], cv[:, CV["k_a"]:CV["k_a"] + 4], -1.0, 1.0, ALU.mult, ALU.add, [B_const], [B_const])
        MEMSET("pool", cvx[:, 19:20], 1e-6, [B_const])
        MEMSET("pool", cvx[:, 20:21], 1e-5, [B_const])
        MEMSET("pool", cvx[:, 21:22], 64e-5, [B_const])
        EPS_RMS, EPS_LN, EPS_GN = cvx[:, 19:20], cvx[:, 20:21], cvx[:, 21:22]
        t3, t3b = Tt(3)
        m32 = t3[:, 0:128]
        MEMSET("pool", m32, 1.0, t3b)
        AFSEL(m32, m32, [[-1, 128]], ALU.is_equal, 0, 1, t3b, t3b)
        CP("pool", ident[:], m32, t3b, [B_const])
        for h in range(4):
            CP("pool", ident4[:, h, :], m32, t3b, [B_const])
        for msk, cmp in ((mSU, ALU.is_gt), (mIU, ALU.is_ge)):
            MEMSET("pool", m32, 1.0, t3b)
            AFSEL(m32, m32, [[1, 128]], cmp, 0, -1, t3b, t3b)
            for h in range(4):
                CP("pool", msk[:, h, :], m32, t3b, [B_const])
        MEMSET("pool", m32, 1.0, t3b)
        AFSEL(m32, m32, [[-1, 128]], ALU.is_gt, 0, 1, t3b, t3b)
        for h in range(4):
            CP("pool", mSL[:, h, :], m32, t3b, [B_const])
        for h in range(2):
            MEMSET("pool", opa_h[h][:], 0.0, B_opa_h[h])
            MEMSET("pool", opr_h[h][:], 0.0, B_opr_h[h])
        MEMSET("pool", onesb[:], 1.0, [B_const])
        MEMSET("pool", o512[:], 1.0 / 512.0, [B_const])
        MEMSET("pool", blk1[:], 0.0, [B_const])
        MEMSET("pool", blk1[0:64, 0:64], 1.0, [B_const])
        MEMSET("pool", blk1[64:128, 64:128], 1.0, [B_const])
        MEMSET("pool", blk64[:], 0.0, [B_const])
        MEMSET("pool", blk64[0:64, 0:64], 1.0 / 64.0, [B_const])
        MEMSET("pool", blk64[64:128, 64:128], 1.0 / 64.0, [B_const])
        MEMSET("pool", rmask[:], 1.0, [B_const])
        for c in range(TT // 128):
            MEMSET("pool", rmask[:, c * 128:c * 128 + 1], 0.0, [B_const])

        wseq = []
        for s in range(nseq):
            for ti in range(NT):
                if ti == 0:
                    wseq += [28, 29, 30, 31]
                wseq += [0, 1, 5, 2, 3, 4, 32, 33, 34, 35] + list(range(6, 28))
        wstate = {"issued": 0, "used": 0}

        FIRST_PASS = 36
        cwo_ = CV["conv_w"]

        def w_issue_upto(n):
            while wstate["issued"] < min(n, len(wseq)):
                i = wstate["issued"]
                si = i % NSLOT
                blk = wseq[i]
                if i < FIRST_PASS:
                    if blk < NHOST:
                        P.dma("pool", lambda e, si=si, blk=blk: e.dma_start(out=wslot[si][:], in_=wblk[blk]), s_wp[si],
                              writes=[B_wslot[si]])
                    else:
                        c = blk - NHOST
                        MEMSET("dve", wslot[si][:, CONV_K * 128:BLK], 0.0, [B_wslot[si]])
                        for k in range(CONV_K):
                            TS("dve", wslot[si][:, k * 128:(k + 1) * 128], ident[:],
                               cv[:, cwo_ + k * 4 + c:cwo_ + k * 4 + c + 1], None, ALU.mult, None, [B_const],
                               [B_wslot[si]] if k == CONV_K - 1 else [])
                    P.dma("sp", lambda e, si=si, blk=blk: e.dma_start(out=scr[blk], in_=wslot[si][:]), s_sw[si],
                          reads=[B_wslot[si]], writes=[B_scr[blk]])
                else:
                    P.dma("sp", lambda e, si=si, blk=blk: e.dma_start(out=wslot[si][:], in_=scr[blk]), s_w[si],
                          reads=[B_scr[blk]], writes=[B_wslot[si]])
                wstate["issued"] += 1

        def w_next(expect):
            i = wstate["used"]
            assert wseq[i] == expect, (i, wseq[i], expect)
            w_issue_upto(i + NSLOT)
            wstate["used"] += 1
            si = i % NSLOT
            return wslot[si], B_wslot[si]

        def wA(ws, cb, kc, m=128):
            o = (cb * 8 + kc) * 128
            return ws[:, o:o + m]

        rr = {"sq": 0, "eng": 0}

        def rmsnorm(src, src_b, gname, dst, dst_b, n, out_f32_inplace=False):
            ps, psb = ps_alloc(512)
            for c in range(8):
                q = rr["sq"] % 2
                rr["sq"] += 1
                ACT(sqr[q][:, 0:n], src(c), AF.Square, [src_b[c]], [B_sqr[q]])
                MM(ps[:, 0:n], onesb[:], sqr[q][:, 0:n], c == 0, c == 7, [B_sqr[q], B_const], psb)
            ACT(rsb[:, 0:n], ps[:, 0:n], AF.Ln, psb + [B_const], [B_rs], bias=EPS_RMS, scale=1.0 / D)
            ACT(rsb[:, 0:n], rsb[:, 0:n], AF.Exp, [B_rs], [B_rs], scale=-0.5)
            for c in range(8):
                STT("dve", dst(c), src(c), cvc(gname, c), rsb[:, 0:n], ALU.mult, ALU.mult,
                    [src_b[c], B_rs, B_const], [dst_b[c]])

        def proj_typeA(blocks, rhs, rhs_b, ncb_total, n, evac):
            cb_glob = 0
            for blk in blocks:
                ws, wsb = w_next(blk)
                for cb in range(4):
                    if cb_glob >= ncb_total:
                        break
                    ps, psb = ps_alloc(512)
                    for kc in range(8):
                        MM(ps[:, 0:n], wA(ws, cb, kc), rhs(kc), kc == 0, kc == 7, [wsb, rhs_b[kc]], psb)
                    evac(cb_glob, ps, psb)
                    cb_glob += 1

        def seq_prologue(s):
            MEMSET("pool", S32[:], 0.0, B_S32)
            MEMSET("pool", Sbf[:], 0.0, B_Sbf)
            MEMSET("pool", carry[:], 0.0, B_carry)
            for c in range(4):
                MEMSET("pool", ubuf[:, c, 0:HIST], 0.0, [B_u[c]])
            P.phase = "memkv"
            memv = cacc[:].rearrange("p a b -> p (a b)")[:, 0:8 * MEM].rearrange("p (a b) -> p a b", b=MEM)
            P.dma("sp", lambda e, s=s: e.dma_start(out=memv, in_=memT[s].rearrange("(c p) m -> p c m", p=128)),
                  s_mem, writes=B_cacc)
            rmsnorm(lambda c: memv[:, c, :], [B_cacc[c // 2] for c in range(8)], "g_mem",
                    lambda c: actA[:, c, 0:MEM], B_A, MEM)

            def evK(cb, ps, psb):
                CP("act" if cb % 2 else "dve", KT[:, cb, :], ps[:, 0:MEM], psb, [B_KT])
            proj_typeA([28, 29], lambda kc: actA[:, kc, 0:MEM], B_A, 8, MEM, evK)
            for h in range(2):
                ws, wsb = w_next(30 + h)
                for mc in range(2):
                    ps, psb = ps_alloc(512)
                    for kc in range(8):
                        MM(ps[:, :], actA[:, kc, mc * 128:(mc + 1) * 128], ws[:, kc * 512:(kc + 1) * 512],
                           kc == 0, kc == 7, [wsb, B_A[kc]], psb)
                    CP("act" if mc else "dve", Vt[:, mc, h * 512:(h + 1) * 512], ps[:, :], psb, [B_V])


        def tile_gen(s, ti):
            gi = s * NT + ti
            P.tile = gi
            xi = gi % 2
            xr = xres[xi]
            xb = B_x[xi]
            t0 = ti * TT
            P.phase = "norm1"
            P.dma("sp", lambda e, s=s, t0=t0, xr=xr: e.dma_start(
                out=xr[:], in_=xT[s].rearrange("(c p) t -> p c t", p=128)[:, :, t0:t0 + TT]), s_x[xi], writes=xb)
            rmsnorm(lambda c: xr[:, c, :], xb, "g_mix", lambda c: actA[:, c, :], B_A, TT)

            yield "n1"
            P.tile = gi

            P.phase = "w_in"
            pend = {}

            def ev_conv(cb, ps, psb):
                ch, isgate = divmod(cb, 2)
                if not isgate:
                    pend["val"] = (ps, psb)
                    return
                vps, vpsb = pend.pop("val")
                tsg, tsgb = Tt(ch % 2)
                ACT(tsg, ps[:, :], AF.Sigmoid, psb, tsgb)
                TTop("dve", ubuf[:, ch, HIST:HIST + TT], vps[:, :], tsg, ALU.mult, vpsb + tsgb, [B_u[ch]])
            proj_typeA([0, 1], lambda kc: actA[:, kc, :], B_A, 8, TT, ev_conv)

            yield "a"
            P.tile = gi

            def ev_shift(j, m, ps, psb, dst, dst_b):
                q = j % 2
                Bq, Bqb = Bbuf[q], B_Bbuf[q]
                ACT(Bq[0:m, 1:TT + 1], ps[0:m, :], AF.Identity, psb + [B_const], [Bqb], scale=cvc("mu", j, slice(0, m)))
                CP("dve", Bq[0:m, 0:1], carry[0:m, j:j + 1], [B_carry[j]], [Bqb])
                STT("dve", dst, ps[0:m, :], cvx[0:m, j:j + 1], Bq[0:m, 0:TT], ALU.mult, ALU.add,
                    psb + [Bqb, B_const], dst_b)
                CP("dve", carry[0:m, j:j + 1], Bq[0:m, TT:TT + 1], [Bqb], [B_carry[j]])

            ws, wsb = w_next(5)
            for cb, m in ((0, 32), (1, 32), (2, 96)):
                ps, psb = ps_alloc(512)
                for kc in range(8):
                    MM(ps[0:m, :], wA(ws, cb, kc, m), actA[:, kc, :], kc == 0, kc == 7, [wsb, B_A[kc]], psb)
                tz, tzb = Tt(2 + cb)
                ev_shift(12 + cb, m, ps, psb, tz[0:m, :], tzb)
                func = (AF.Tanh, AF.Copy, AF.Sigmoid)[cb]
                ACT(lob[0:m, cb, :], tz[0:m, :], func, tzb, [B_lob[cb]])

            RKV_MAP = [0, 1, 4, 5, 8, 9, 2, 3, 6, 7, 10, 11]

            def ev_rkv(cb, ps, psb):
                idx = RKV_MAP[cb]
                dst, dst_b = zrkv(idx)
                ev_shift(idx, 128, ps, psb, dst, dst_b)
            proj_typeA([2, 3], lambda kc: actA[:, kc, :], B_A, 8, TT, ev_rkv)

            def rkv_tail():
                ws4, ws4b = w_next(4)
                fs = []
                for cb in range(4):
                    def f(cb=cb):
                        ps, psb = ps_alloc(512)
                        for kc in range(8):
                            MM(ps[:, :], wA(ws4, cb, kc), actA[:, kc, :], kc == 0, kc == 7, [ws4b, B_A[kc]], psb)
                        ev_rkv(8 + cb, ps, psb)
                    fs.append(f)
                return fs

            def conv_chunk(c):
                ws, wsb = w_next(32 + c)
                ps, psb = ps_alloc(512)
                for k in range(CONV_K):
                    MM(ps[:, :], ws[:, k * 128:(k + 1) * 128], ubuf[:, c, k:k + TT], k == 0, k == CONV_K - 1,
                       [wsb, B_u[c]], psb)
                ACT(cacc[:, c, :], ps[:, :], AF.Identity, psb + [B_const], [B_cacc[c]], bias=cvc("conv_b", c))
                CP("dve", ubuf[:, c, 0:HIST], ubuf[:, c, TT:TT + HIST], [B_u[c]], [B_u[c]])

            def conv_ln():
                psm, psmb = ps_alloc(512)
                pse, pseb = ps_alloc(512)
                for c in range(4):
                    q = rr["sq"] % 2
                    rr["sq"] += 1
                    CP("act", sqr[q][:], cacc[:, c, :], [B_cacc[c]], [B_sqr[q]])
                    MM(psm[:, :], o512[:], sqr[q][:], c == 0, c == 3, [B_sqr[q], B_const], psmb)
                    q = rr["sq"] % 2
                    rr["sq"] += 1
                    ACT(sqr[q][:], cacc[:, c, :], AF.Square, [B_cacc[c]], [B_sqr[q]])
                    MM(pse[:, :], o512[:], sqr[q][:], c == 0, c == 3, [B_sqr[q], B_const], pseb)
                tm2, tm2b = cln[:, 0, :], [B_cln[0]]
                ACT(tm2, psm[:, :], AF.Square, psmb, tm2b)
                TTop("dve", tm2, pse[:, :], tm2, ALU.subtract, pseb + tm2b, tm2b)
                ACT(tm2, tm2, AF.Ln, tm2b + [B_const], tm2b, bias=EPS_LN, scale=1.0)
                ACT(tm2, tm2, AF.Exp, tm2b, tm2b, scale=-0.5)
                tmean, tmeanb = cln[:, 1, :], [B_cln[1]]
                CP("act", tmean, psm[:, :], psmb, tmeanb)
                for c in range(4):
                    TTop("pool", cacc[:, c, :], cacc[:, c, :], tmean, ALU.subtract, [B_cacc[c]] + tmeanb, [B_cacc[c]])
                    TTop("pool", cacc[:, c, :], cacc[:, c, :], tm2, ALU.mult, [B_cacc[c]] + tm2b, [B_cacc[c]])
                    ACT(actB[:, c, :], cacc[:, c, :], AF.Silu, [B_cacc[c], B_const], [B_Bc[c]],
                        bias=cvc("ln_b", c), scale=cvc("ln_g", c))

            P.phase = "rwkv"
            P.phase = "rwkv_ew"

            def ew_steps(hg, lp, T):
                opa, opr, opb, opk, tok = opa_h[hg], opr_h[hg], opb_h[hg], opk_h[hg], tok_h[hg]
                B_opa, B_opr, B_opb, B_opk, B_tok = B_opa_h[hg], B_opr_h[hg], B_opb_h[hg], B_opk_h[hg], B_tok_h[hg]
                pr = 2 * hg + lp
                pc = slice(pr * 128, (pr + 1) * 128)
                zr, zrb = zrkv(pr)
                zk, zkb = zrkv(4 + pr)
                zv, zvb = zrkv(8 + pr)
                sgw, sgwb = T[0]
                cum, cumb = T[1]
                E1, E1b = T[2]
                av, avb = T[3]
                kk, kkb = T[4]
                rn, rnb = T[5]
                kp, kpb = T[6]
                E3, E3b = sgw, sgwb
                E2, E2b = cum, cumb
                ob, ok = opb[:, lp, :], opk[:, lp, :]
                st = []

                def s1():
                    ps, psb = ps_alloc(512)
                    MM(ps[:, :], lorab[0:32, 0, pc], lob[0:32, 0, :], True, True, [B_const, B_lob[0]], psb)
                    ACT(sgw, ps[:, :], AF.Sigmoid, psb + [B_const], sgwb, bias=cvc("w0", pr))
                    ps, psb = ps_alloc(512)
                    MM(ps[:, :], lorab[0:32, 1, pc], lob[0:32, 1, :], True, True, [B_const, B_lob[1]], psb)
                    ACT(av, ps[:, :], AF.Sigmoid, psb + [B_const], avb, bias=cvc("a0", pr))
                st.append(s1)

                def s2():
                    P.op("dve", lambda e: e.tensor_tensor_scan(
                        out=cum, data0=rmask[:], data1=sgw, initial=0.0, op0=ALU.mult, op1=ALU.add),
                        sgwb + [B_const], cumb)
                    ACT(kk, zk, AF.Identity, zkb + [B_const], kkb, scale=cvc("k_k", pr))
                st.append(s2)

                def s3():
                    ACT(E1, cum, AF.Exp, cumb, E1b, scale=-DEC)
                    TTop("dve", E3, cum, sgw, ALU.subtract, cumb + sgwb, E3b)
                    q = rr["sq"] % 2
                    rr["sq"] += 1
                    ACT(sqr[q][:], kk, AF.Square, kkb, [B_sqr[q]])
                    ps, psb = ps_alloc(512)
                    MM(ps[:, :], blk1[:], sqr[q][:], True, True, [B_const, B_sqr[q]], psb)
                    TS("dve", rn, ps[:, :], 1e-24, None, ALU.max, None, psb, rnb)
                st.append(s3)

                def s4():
                    ACT(E3, E3, AF.Exp, E3b, E3b, scale=-DEC)
                    ACT(E2, cum, AF.Exp, cumb, E2b, scale=DEC)
                    ACT(rn, rn, AF.Ln, rnb, rnb)
                    ACT(rn, rn, AF.Exp, rnb, rnb, scale=-0.5)
                    ACT(kp, av, AF.Identity, avb + [B_const], kpb, bias=cvx[:, 15 + pr:16 + pr], scale=cvc("k_a", pr))
                st.append(s4)

                def s5():
                    CP("pool", wcb[:, pr, :], E1.rearrange("p (c t) -> p c t", t=128)[:, :, 127], E1b, [B_wc[pr]])
                    TTop("dve", kk, kk, rn, ALU.mult, kkb + rnb, kkb)
                    TTop("pool", kp, kp, zk, ALU.mult, kpb + zkb, kpb)
                st.append(s5)

                def s6():
                    for hf in range(2):
                        rows = slice(hf * 64, (hf + 1) * 64)
                        STT("dve", opa[rows, hf, lp, :], kk[rows, :], -1.0, E3[rows, :], ALU.mult, ALU.mult,
                            kkb + E3b, [B_opa[lp]])
                    TTop("pool", rn, kk, av, ALU.mult, kkb + avb, rnb)
                    TTop("pool", ok, kp, E2, ALU.mult, kpb + E2b, [B_opk[lp]])
                st.append(s6)

                def s7():
                    TTop("dve", ob, rn, E2, ALU.mult, rnb + E2b, [B_opb[lp]])
                    for hf in range(2):
                        rows = slice(hf * 64, (hf + 1) * 64)
                        TTop("pool", opr[rows, hf, lp, :], zr[rows, :], E1[rows, :], ALU.mult, zrb + E1b, [B_opr[lp]])
                    q = rr["sq"] % 2
                    rr["sq"] += 1
                    STT("dve", sqr[q][:], zr, cvc("r_k", pr), kp, ALU.mult, ALU.mult,
                        zrb + kpb + [B_const], [B_sqr[q]])
                    ps, psb = ps_alloc(512)
                    MM(ps[:, :], blk1[:], sqr[q][:], True, True, [B_const, B_sqr[q]], psb)
                    TTop("dve", bonus[:, pr, :], ps[:, :], zv, ALU.mult, psb + zvb, [B_bonus[pr]])
                st.append(s7)

                def s8():
                    for kind, (src, srcb) in enumerate(((ob, [B_opb[lp]]), (ok, [B_opk[lp]]), (zv, zvb))):
                        ps, psb = ps_alloc(256)
                        psv = ps.bitcast(BF16)
                        for c in range(4):
                            P.op("pe", lambda e, psv=psv, src=src, c=c: e.transpose(
                                psv[:, c * 128:(c + 1) * 128], src[:, c * 128:(c + 1) * 128], ident[:]),
                                srcb + [B_const], psb)
                        CP("act" if kind != 1 else "dve",
                           tok[:, kind, lp, :, :].rearrange("p a b -> p (a b)"), psv, psb, [B_tok[kind][lp]])
                st.append(s8)
                return st

            def actA_tmp(i):
                return (actA[:, 2 * i:2 * i + 2, :].rearrange("p a b -> p (a b)").bitcast(F32),
                        [B_A[2 * i], B_A[2 * i + 1]])
            Tset0 = [Tt(i) for i in range(7)]
            Tset1 = [Tt(7), Tt(8), Tt(9)] + [actA_tmp(i) for i in range(4)]

            P.phase = "wkv"

            def chain_stages(hg, c, mats, B_m):
                opa, opr, opb, opk, tok = opa_h[hg], opr_h[hg], opb_h[hg], opk_h[hg], tok_h[hg]
                B_opa, B_opr, B_opb, B_opk, B_tok = B_opa_h[hg], B_opr_h[hg], B_opb_h[hg], B_opk_h[hg], B_tok_h[hg]
                cc = slice(c * 128, (c + 1) * 128)
                fl = lambda t: t[:].rearrange("p a b -> p (a b)")

                def opnd(kind, lp, hf):
                    if kind == "a":
                        return opa[:, hf, lp, cc], B_opa[lp]
                    if kind == "r":
                        return opr[:, hf, lp, cc], B_opr[lp]
                    if kind == "b":
                        return opb[:, lp, cc], B_opb[lp]
                    return opk[:, lp, cc], B_opk[lp]

                def amat(nm, kl, kr_, msk, direct):
                    ps, psb = ps_alloc(512)
                    for hh in range(4):
                        lp, hf = divmod(hh, 2)
                        lh, lhb = opnd(kl, lp, hf)
                        rh, rhb = opnd(kr_, lp, hf)
                        MM(ps[:, hh * 128:(hh + 1) * 128], lh, rh, True, True, [lhb, rhb], psb)
                    if direct:
                        TTop("dve", fl(mats[nm]), ps[:, :], fl(msk), ALU.mult, psb + [B_const], [B_m[nm]])
                    else:
                        CP("act", fl(mats[nm]), ps[:, :], psb, [B_m[nm]])
                        TTop("pool", fl(mats[nm]), fl(mats[nm]), fl(msk), ALU.mult, [B_m[nm], B_const], [B_m[nm]])

                def stA():
                    amat("X0", "b", "a", mSU, True)
                    amat("XT0", "a", "b", mSL, True)
                    TTop("pool", mats["P0"][:], mats["X0"][:], ident4[:], ALU.add, [B_m["X0"], B_const], [B_m["P0"]])
                    amat("Aak", "k", "a", mSU, False)
                    amat("Arb", "b", "r", mIU, False)
                    amat("Ark", "k", "r", mIU, False)

                def stL(lvl):
                    cur = (lvl - 1) % 2
                    Xc, XTc, Pc = "X%d" % cur, "XT%d" % cur, "P%d" % cur
                    Xn, XTn, Pn = "X%d" % (1 - cur), "XT%d" % (1 - cur), "P%d" % (1 - cur)
                    ps2, ps2b = ps_alloc(512)
                    for hh in range(4):
                        MM(ps2[:, hh * 128:(hh + 1) * 128], mats[Xc][:, hh, :], mats[XTc][:, hh, :], True, True,
                           [B_m[Xc], B_m[XTc]], ps2b)
                    if lvl < 6:
                        ps1, ps1b = ps_alloc(512)
                        for hh in range(4):
                            MM(ps1[:, hh * 128:(hh + 1) * 128], mats[XTc][:, hh, :], mats[Xc][:, hh, :], True, True,
                               [B_m[Xc], B_m[XTc]], ps1b)
                    if NWARM:
                        pe_warm(NWARM)
                    CP("act", fl(mats[XTn]), ps2[:, :], ps2b, [B_m[XTn]])
                    if lvl < 6:
                        CP("act" if lvl % 2 == 0 else "dve", fl(mats[Xn]), ps1[:, :], ps1b, [B_m[Xn]])

                def stLb(lvl):
                    cur = (lvl - 1) % 2
                    Pc, Pn, XTn = "P%d" % cur, "P%d" % (1 - cur), "XT%d" % (1 - cur)
                    ps3, ps3b = ps_alloc(512)
                    for hh in range(4):
                        MM(ps3[:, hh * 128:(hh + 1) * 128], mats[XTn][:, hh, :], mats[Pc][:, hh, :], True, True,
                           [B_m[XTn], B_m[Pc]], ps3b)
                    TTop("dve", fl(mats[Pn]), ps3[:, :], fl(mats[Pc]), ALU.add, ps3b + [B_m[Pc]], [B_m[Pn]])

                Pfin = "P0"

                def stS1():
                    for lp in range(2):
                        pr = 2 * hg + lp
                        bS = B_Sbf[pr]
                        ACT(tmpS[:, pr, :], S32[:, pr, :], AF.Identity, [B_S32[pr], B_wc[pr]], [B_tmpS[pr]],
                            scale=wcb[:, pr, c:c + 1])
                        psR, psRb = ps_alloc(128)
                        for hf in range(2):
                            hh = 2 * lp + hf
                            MM(psR[:, hf * 64:(hf + 1) * 64], opa[:, hf, lp, cc], Sbf[:, pr, :], True, False,
                               [B_opa[lp], bS], psRb)
                            MM(psR[:, hf * 64:(hf + 1) * 64], mats["Aak"][:, hh, :], tok[:, 2, lp, c, hf * 64:(hf + 1) * 64],
                               False, True, [B_m["Aak"], B_tok[2][lp]], psRb)
                        CP("act" if lp else "dve", RHSb[:, lp, :, :].rearrange("p a b -> p (a b)"), psR, psRb, [B_RHS[lp]])

                def stS2():
                    for lp in range(2):
                        psU, psUb = ps_alloc(128)
                        for hf in range(2):
                            hh = 2 * lp + hf
                            MM(psU[:, hf * 64:(hf + 1) * 64], mats[Pfin][:, hh, :], RHSb[:, lp, hf, :], True, True,
                               [B_m[Pfin], B_RHS[lp]], psUb)
                        CP("dve" if lp else "act", Ub[:, lp, :, :].rearrange("p a b -> p (a b)"), psU, psUb, [B_U[lp]])

                def stS3():
                    for lp in range(2):
                        pr = 2 * hg + lp
                        bS = B_Sbf[pr]
                        psY, psYb = ps_alloc(128)
                        psS, psSb = ps_alloc(128)
                        for hf in range(2):
                            hh = 2 * lp + hf
                            rows = slice(hf * 64, (hf + 1) * 64)
                            vt = tok[:, 2, lp, c, hf * 64:(hf + 1) * 64]
                            MM(psY[rows, :], Sbf[:, pr, :], opr[:, hf, lp, cc], True, False,
                               [bS, B_opr[lp]], psYb, tp=(0, hf * 64))
                            MM(psY[rows, :], Ub[:, lp, hf, :], mats["Arb"][:, hh, :], False, False,
                               [B_U[lp], B_m["Arb"]], psYb, tp=(0, hf * 64))
                            MM(psY[rows, :], vt, mats["Ark"][:, hh, :], False, True,
                               [B_tok[2][lp], B_m["Ark"]], psYb, tp=(0, hf * 64))
                            MM(psS[rows, 0:64], tok[:, 0, lp, c, hf * 64:(hf + 1) * 64], Ub[:, lp, hf, :], True, False,
                               [B_tok[0][lp], B_U[lp]], psSb, tp=(0, hf * 64))
                            MM(psS[rows, 0:64], tok[:, 1, lp, c, hf * 64:(hf + 1) * 64], vt, False, True,
                               [B_tok[1][lp], B_tok[2][lp]], psSb, tp=(0, hf * 64))
                        STT("dve", Sbf[:, pr, :], psS[:, 0:64], wcb[:, pr, c:c + 1], tmpS[:, pr, :], ALU.mult, ALU.add,
                            psSb + [B_wc[pr], B_tmpS[pr]], [bS])
                        STT("dve", S32[:, pr, :], psS[:, 0:64], wcb[:, pr, c:c + 1], tmpS[:, pr, :], ALU.mult, ALU.add,
                            psSb + [B_wc[pr], B_tmpS[pr]], [B_S32[pr]])
                        CP("act", ybuf[:, lp, cc], psY, psYb, [B_y[lp]])
                return [stA] + [[(lambda l=l: stL(l)), (lambda l=l: stLb(l))] for l in range(1, 7)] + [(stS1, stS2, stS3)]

            def run_wkv_all(fillers):
                P.phase = "wkv"
                fillers = list(fillers)
                chains = [chain_stages(i // 4, i % 4, matsets[i % NSET], B_msets[i % NSET]) for i in range(8)]
                STAG = 8 // NSET
                posn = [0] * 8
                step = 0
                while any(p < 8 for p in posn):
                    deferred = []
                    for i in range(8):
                        if step >= i * STAG and posn[i] < 8:
                            stg = chains[i][posn[i]]
                            if isinstance(stg, tuple):
                                stg[0]()
                                deferred = list(stg[1:])
                            elif isinstance(stg, list):
                                stg[0]()
                                if deferred:
                                    deferred.pop(0)()
                                stg[1]()
                                if deferred:
                                    deferred.pop(0)()
                            else:
                                stg()
                                if deferred:
                                    deferred.pop(0)()
                            posn[i] += 1
                    for f in deferred:
                        f()
                    if step < len(fillers) and fillers[step] is not None:
                        fillers[step]()
                    step += 1
                for f in fillers[step:]:
                    if f is not None:
                        f()

            def gn_steps(hg):
                st = []
                g1, g1b = Tt(0)
                g2, g2b = Tt(1)
                for lp in range(2):
                    pr = 2 * hg + lp
                    pc = slice(pr * 128, (pr + 1) * 128)
                    yv = ybuf[:, lp, :]

                    def ga(lp=lp, pr=pr, pc=pc, yv=yv):
                        q = rr["sq"] % 2
                        rr["sq"] += 1
                        CP("act", sqr[q][:], yv, [B_y[lp]], [B_sqr[q]])
                        psm, psmb = ps_alloc(512)
                        MM(psm[:, :], blk64[:], sqr[q][:], True, True, [B_const, B_sqr[q]], psmb)
                        q = rr["sq"] % 2
                        rr["sq"] += 1
                        ACT(sqr[q][:], yv, AF.Square, [B_y[lp]], [B_sqr[q]])
                        pse, pseb = ps_alloc(512)
                        MM(pse[:, :], blk64[:], sqr[q][:], True, True, [B_const, B_sqr[q]], pseb)
                        ACT(g1, psm[:, :], AF.Square, psmb, g1b)
                        TTop("dve", g2, yv, psm[:, :], ALU.subtract, [B_y[lp]] + psmb, g2b)
                        TTop("dve", g1, pse[:, :], g1, ALU.subtract, pseb + g1b, g1b)
                        ACT(g1, g1, AF.Ln, g1b + [B_const], g1b, bias=EPS_GN, scale=1.0)
                        ACT(g1, g1, AF.Exp, g1b, g1b, scale=-0.5)

                    def gb(lp=lp, pr=pr, pc=pc):
                        TTop("pool", g2, g2, g1, ALU.mult, g2b + g1b, g2b)
                        ACT(g2, g2, AF.Identity, g2b + [B_const], g2b, bias=cvc("lnx_b", pr), scale=cvc("lnx_g", pr))
                        TTop("pool", g2, g2, bonus[:, pr, :], ALU.add, g2b + [B_bonus[pr]], g2b)
                        psg, psgb = ps_alloc(512)
                        MM(psg[:, :], lorab[0:96, 2, pc], lob[0:96, 2, :], True, True, [B_const, B_lob[2]], psgb)
                        TTop("dve", actB[:, 4 + pr, :], g2, psg[:, :], ALU.mult, g2b + psgb, [B_Bc[4 + pr]])
                    st += [ga, gb]
                return st

            P.phase = "rwkv_ew"
            rt = rkv_tail()
            cvf = [(lambda c=c: conv_chunk(c)) for c in range(4)]
            fill = {0: rt, 2: cvf[0:2], 3: cvf[2:4]}
            Tset1a = [Tt(7), Tt(8), Tt(9), (ybuf[:, 0, :], [B_y[0]]), (ybuf[:, 1, :], [B_y[1]]),
                      (PT[:].rearrange("p a b -> p (a b)").bitcast(F32), [B_PT[0], B_PT[1]]), (rsb[:], [B_rs])]
            for k_, (f0, f1) in enumerate(zip(ew_steps(0, 0, Tset0), ew_steps(0, 1, Tset1a))):
                f0()
                f1()
                for f in fill.get(k_, ()):
                    f()
            ew1 = [f for pair in zip(ew_steps(1, 0, Tset0), ew_steps(1, 1, Tset1)) for f in pair]
            assert len(ew1) == 16
            STAG_ = 8 // NSET
            first_hg1 = 4 * STAG_
            last_s_hg0 = 3 * STAG_ + 7
            fl = [conv_ln] + ew1 + [None] * (last_s_hg0 + 1 - len(ew1) - 1) + gn_steps(0)
            assert len(ew1) + 1 <= first_hg1 + 1
            run_wkv_all(fl)
            P.phase = "gn"
            for f in gn_steps(1):
                f()

            P.phase = "w_out"
            def ev_res(cb, ps, psb):
                TTop("dve", xr[:, cb, :], xr[:, cb, :], ps[:, :], ALU.add, [xb[cb]] + psb, [xb[cb]])
            proj_typeA([6, 7], lambda kc: actB[:, kc, :], B_Bc, 8, TT, ev_res)

            P.phase = "xattn"
            rmsnorm(lambda c: xr[:, c, :], xb, "g_cross", lambda c: actA[:, c, :], B_A, TT)

            def ev_q(cb, ps, psb):
                CP("act" if cb % 2 else "dve", actB[:, cb, :], ps[:, :], psb, [B_Bc[cb]])
            proj_typeA([8, 9], lambda kc: actA[:, kc, :], B_A, 8, TT, ev_q)
            for hd in range(4):
                for mc in range(2):
                    ps, psb = ps_alloc(512)
                    for dc in range(2):
                        MM(ps[:, :], KT[:, 2 * hd + dc, mc * 128:(mc + 1) * 128], actB[:, 2 * hd + dc, :],
                           dc == 0, dc == 1, [B_KT, B_Bc[2 * hd + dc]], psb)
                    ACT(PT[:, mc, :], ps[:, :], AF.Exp, psb, [B_PT[mc]], scale=1.0 / 16.0)
                ps, psb = ps_alloc(512)
                for mc in range(2):
                    MM(ps[:, :], onesb[:], PT[:, mc, :], mc == 0, mc == 1, [B_const, B_PT[mc]], psb)
                ACT(rsb[:], ps[:, :], AF.Ln, psb, [B_rs])
                ACT(rsb[:], rsb[:], AF.Exp, [B_rs], [B_rs], scale=-1.0)
                for dvc in range(2):
                    ch = 2 * hd + dvc
                    ps, psb = ps_alloc(512)
                    for mc in range(2):
                        MM(ps[:, :], Vt[:, mc, ch * 128:(ch + 1) * 128], PT[:, mc, :], mc == 0, mc == 1,
                           [B_V, B_PT[mc]], psb)
                    TTop("dve", actA[:, ch, :], ps[:, :], rsb[:], ALU.mult, psb + [B_rs], [B_A[ch]])
            proj_typeA([10, 11], lambda kc: actA[:, kc, :], B_A, 8, TT, ev_res)

            P.phase = "ffn"
            rmsnorm(lambda c: xr[:, c, :], xb, "g_ffn", lambda c: actB[:, c, :], B_Bc, TT)

            def ev_ff1(cb, ps, psb):
                f, fb = fT(cb)
                ACT(f, ps[:, :], AF.Relu, psb, fb)
                TTop("pool" if cb % 2 else "dve", f, f, f, ALU.mult, fb, fb)
            proj_typeA(list(range(12, 20)), lambda kc: actB[:, kc, :], B_Bc, 32, TT, ev_ff1)

            yield "b"
            P.tile = gi
            for cb in range(8):
                ws, wsb = w_next(20 + cb)
                ps, psb = ps_alloc(512)
                for kc in range(32):
                    f, fb = fT(kc)
                    MM(ps[:, :], ws[:, kc * 128:(kc + 1) * 128], f, kc == 0, kc == 31, [wsb] + fb, psb)
                ev_res(cb, ps, psb)

            yield "c"
            P.tile = gi
            P.phase = "final"
            rmsnorm(lambda c: xr[:, c, :], xb, "g_final", lambda c: xr[:, c, :], xb, TT)
            P.dma("sp", lambda e, s=s, t0=t0, xr=xr: e.dma_start(
                out=oT[s].rearrange("(c p) t -> p c t", p=128)[:, :, t0:t0 + TT], in_=xr[:]), s_o[xi], reads=xb)
            yield "f"

        order = [(s_, t_) for s_ in range(nseq) for t_ in range(NT)]
        gens = {}

        def start(idx):
            g = tile_gen(*order[idx])
            gens[idx] = g
            next(g)
        prev_final = None
        for idx, (s_, t_) in enumerate(order):
            if t_ == 0:
                seq_prologue(s_)
                start(idx)
            g = gens.pop(idx)
            next(g)
            if prev_final is not None:
                next(prev_final)
                prev_final = None
            next(g)
            if idx + 1 < len(order) and order[idx + 1][1] != 0:
                start(idx + 1)
            next(g)
            prev_final = g
        next(prev_final)
        P.wait_all("sp", B_x[0] + B_x[1])
        P.emit()
    return nc


def prep_inputs(inputs, nseq_per_core, ncores, seqlen):
    x = np.asarray(inputs["x"], np.float32)
    mem = np.asarray(inputs["mem"], np.float32)
    cv = pack_cvec(inputs)
    blocks = pack_blocks(inputs)
    lora = pack_lora(inputs)
    in_maps = []
    for c in range(ncores):
        sl = slice(c * nseq_per_core, (c + 1) * nseq_per_core)
        in_maps.append({
            "xT": np.ascontiguousarray(x[sl, :seqlen].transpose(0, 2, 1)),
            "memT": np.ascontiguousarray(mem[sl].transpose(0, 2, 1)),
            "wblk": blocks, "cvec": cv, "lora": lora,
        })
    return in_maps


def kernel(**inputs):
    nseq = BATCH // NCORES
    nc = build(nseq, SEQ)
    in_maps = prep_inputs(inputs, nseq, NCORES, SEQ)
    res = run_bass_kernel_spmd(nc, in_maps, core_ids=list(range(NCORES)))
    outs = [np.asarray(r["oT"]).transpose(0, 2, 1) for r in res.results]
    return np.ascontiguousarray(np.concatenate(outs, axis=0)).astype(np.float32)
```
